# Optimizing a Trainium2 kernel written in Bass

```python
import math
import jax
import jax.numpy as jnp
from jax import lax
import numpy as np

D_MODEL = 2048
BATCH = 1
SEQ = 8192
DEPTH = 2

GRID_W = 64
CTX_LEN = 256
EPS = 1e-6
F32 = jnp.float32

GROUP_W = D_MODEL // 4
MIX_W = 4 * GROUP_W
RET_HEADS = 4
RET_DIM = GROUP_W // RET_HEADS
RET_CHUNK = 128
RET_COLS = 5 * GROUP_W
S5_CH = GROUP_W
S5_GROUP = 16
S5_NG = S5_CH // S5_GROUP
S5_P = 64
S5_COLS = GROUP_W
ATT_HEADS = 4
ATT_KV_HEADS = 2
ATT_DIM = GROUP_W // ATT_HEADS
ATT_COLS = (ATT_HEADS + 2 * ATT_KV_HEADS) * ATT_DIM
Q_BLOCK = 128
ROPE_THETA = 10000.0
CONV_CH = GROUP_W
CONV_W = 3
CONV_COLS = 3 * GROUP_W
IN_COLS = RET_COLS + S5_COLS + ATT_COLS + CONV_COLS
N_EXPERTS = 32
TOP_K = 4
D_EXPERT = D_MODEL // 2
SWIGLU_LIMIT = 7.0
SWIGLU_ALPHA = 1.702
MOE_BLOCK = 128

kernel_name = 'hybrid_parallel_group_flow_block'


def rms_norm(x):
    xf = x.astype(F32)
    return (xf * lax.rsqrt(jnp.mean(xf * xf, -1, keepdims=True) + EPS)).astype(x.dtype)


def modulate(x, shift, scale):
    return rms_norm(x) * (1.0 + scale) + shift


def head_rms(x, w):
    xf = x.astype(F32)
    return xf * lax.rsqrt(jnp.mean(xf * xf, -1, keepdims=True) + EPS) * w.astype(F32)


def group_norm(y):
    yc = y - jnp.mean(y, -1, keepdims=True)
    return yc * lax.rsqrt(jnp.mean(yc * yc, -1, keepdims=True) + EPS)


def rope_angles(pos, dim):
    freqs = ROPE_THETA ** (-jnp.arange(0, dim, 2, dtype=F32) / dim)
    return pos.astype(F32)[:, None] * freqs[None, :]


def apply_rope(x, ang):
    cos = jnp.cos(ang)[None, :, None, :]
    sin = jnp.sin(ang)[None, :, None, :]
    x1, x2 = jnp.split(x, 2, axis=-1)
    return jnp.concatenate([x1 * cos - x2 * sin, x1 * sin + x2 * cos], axis=-1)


def apply_axial_rope(x, rows, cols):
    half = x.shape[-1] // 2
    xr, xc = jnp.split(x, 2, axis=-1)
    return jnp.concatenate([apply_rope(xr, rope_angles(rows, half)),
                            apply_rope(xc, rope_angles(cols, half))], axis=-1)


def retention_chunkwise(q, k, v, log_g, s0, with_output):
    bsz, t, h, dk = q.shape
    dv = v.shape[-1]
    n = t // RET_CHUNK
    qc = q.reshape(bsz, n, RET_CHUNK, h, dk)
    kc = k.reshape(bsz, n, RET_CHUNK, h, dk)
    vc = v.reshape(bsz, n, RET_CHUNK, h, dv)
    idx = jnp.arange(RET_CHUNK, dtype=F32)
    zeta = jnp.exp((RET_CHUNK - 1.0 - idx)[:, None] * log_g[None, :])
    kv = jnp.einsum('bnjhd,bnjhe->bnhde', kc * zeta[None, None, :, :, None], vc)
    g_chunk = jnp.exp(RET_CHUNK * log_g)[None, :, None, None]

    def step(s, kv_i):
        return s * g_chunk + kv_i, (s if with_output else None)

    s_final, s_prev = lax.scan(step, s0, jnp.moveaxis(kv, 1, 0))
    if not with_output:
        return None, s_final
    s_prev = jnp.moveaxis(s_prev, 0, 1)
    diff = idx[:, None] - idx[None, :]
    decay = jnp.where(diff >= 0, jnp.exp(jnp.maximum(diff, 0.0)[None] * log_g[:, None, None]), 0.0)
    scores = jnp.einsum('bnihd,bnjhd->bnhij', qc, kc) * decay[None, None]
    xi = jnp.exp((idx + 1.0)[:, None] * log_g[None, :])
    y = (jnp.einsum('bnhij,bnjhe->bnihe', scores, vc)
         + jnp.einsum('bnihd,bnhde->bnihe', qc, s_prev) * xi[None, None, :, :, None])
    return y.reshape(bsz, t, h, dv), s_final


def retention_mixer(z_lat, z_ctx, pos_lat, ctx_out):
    def heads(z):
        q, k, v, gf, gb = jnp.split(z.astype(F32), 5, axis=-1)
        hd = lambda a: a.reshape(a.shape[0], a.shape[1], RET_HEADS, RET_DIM)
        return hd(q), hd(k) * RET_DIM ** -0.5, hd(v), gf, gb

    ql, kl, vl, gfl, gbl = heads(z_lat)
    qc, kc, vc, gfc, gbc = heads(z_ctx)
    ang = rope_angles(pos_lat, RET_DIM)
    ql, kl = apply_rope(ql, ang), apply_rope(kl, ang)
    log_g = jnp.log(1.0 - 2.0 ** (-5.0 - jnp.arange(RET_HEADS, dtype=F32)))
    s0 = jnp.zeros((z_lat.shape[0], RET_HEADS, RET_DIM, RET_DIM), F32)
    y_lat, y_ctx = [], []
    for dr, lg in enumerate((log_g, log_g[::-1])):
        flip = (lambda a: a[:, ::-1]) if dr == 1 else (lambda a: a)
        yc, s_ctx = retention_chunkwise(flip(qc), flip(kc), flip(vc), lg, s0, ctx_out)
        yl, _ = retention_chunkwise(flip(ql), flip(kl), flip(vl), lg, s_ctx, True)
        y_lat.append(group_norm(flip(yl)).reshape(z_lat.shape[0], z_lat.shape[1], GROUP_W))
        if ctx_out:
            y_ctx.append(group_norm(flip(yc)).reshape(z_ctx.shape[0], z_ctx.shape[1], GROUP_W))
    out_lat = jax.nn.silu(gfl) * y_lat[0] + jax.nn.silu(gbl) * y_lat[1]
    out_ctx = (jax.nn.silu(gfc) * y_ctx[0] + jax.nn.silu(gbc) * y_ctx[1]).astype(z_ctx.dtype) if ctx_out else None
    return out_lat.astype(z_lat.dtype), out_ctx


def s5_discretize(a_re, a_im, log_step):
    a_re = a_re.astype(F32)
    a_im = a_im.astype(F32)
    step = jnp.exp(log_step.astype(F32))[:, None]
    mag = jnp.exp(a_re * step)
    ab_re = mag * jnp.cos(a_im * step)
    ab_im = mag * jnp.sin(a_im * step)
    den = a_re * a_re + a_im * a_im
    num_re = ab_re - 1.0
    f_re = (num_re * a_re + ab_im * a_im) / den
    f_im = (ab_im * a_re - num_re * a_im) / den
    return ab_re, ab_im, f_re, f_im


def s5_scan(drive_re, drive_im, ab_re, ab_im, s0_re, s0_im):
    drive_re = drive_re.at[:, 0].add(ab_re * s0_re - ab_im * s0_im)
    drive_im = drive_im.at[:, 0].add(ab_re * s0_im + ab_im * s0_re)
    a_re = jnp.broadcast_to(ab_re, drive_re.shape)
    a_im = jnp.broadcast_to(ab_im, drive_im.shape)

    def combine(e1, e2):
        a1r, a1i, b1r, b1i = e1
        a2r, a2i, b2r, b2i = e2
        return (a2r * a1r - a2i * a1i, a2r * a1i + a2i * a1r,
                a2r * b1r - a2i * b1i + b2r, a2r * b1i + a2i * b1r + b2i)

    _, _, x_re, x_im = lax.associative_scan(combine, (a_re, a_im, drive_re, drive_im), axis=1)
    return x_re, x_im


def s5_mixer(u_lat, u_ctx, a_re, a_im, log_step, b_re, b_im, c_re, c_im, d_skip, w_glu, b_glu, ctx_out):
    def drive_in(u):
        uf = u.astype(F32).reshape(u.shape[0], u.shape[1], S5_NG, S5_GROUP)
        return (uf, jnp.einsum('btgc,gpc->btgp', uf, b_re.astype(F32)),
                jnp.einsum('btgc,gpc->btgp', uf, b_im.astype(F32)))

    ul, bl_re, bl_im = drive_in(u_lat)
    uc, bc_re, bc_im = drive_in(u_ctx)
    zero = jnp.zeros((u_lat.shape[0], S5_NG, S5_P), F32)
    lat_states, ctx_states = [], []
    for dr in range(2):
        ab_re, ab_im, f_re, f_im = s5_discretize(a_re[dr], a_im[dr], log_step[dr])
        flip = (lambda a: a[:, ::-1]) if dr == 1 else (lambda a: a)
        cr, ci = s5_scan(flip(f_re * bc_re - f_im * bc_im), flip(f_re * bc_im + f_im * bc_re),
                         ab_re, ab_im, zero, zero)
        lr, li = s5_scan(flip(f_re * bl_re - f_im * bl_im), flip(f_re * bl_im + f_im * bl_re),
                         ab_re, ab_im, cr[:, -1], ci[:, -1])
        lat_states.append((flip(lr), flip(li)))
        if ctx_out:
            ctx_states.append((flip(cr), flip(ci)))

    def readout(uf, states):
        bsz, t = uf.shape[:2]
        x_re = states[0][0] + states[1][0]
        x_im = states[0][1] + states[1][1]
        y = (jnp.einsum('btgp,gcp->btgc', x_re, c_re.astype(F32))
             - jnp.einsum('btgp,gcp->btgc', x_im, c_im.astype(F32))
             + uf * d_skip.astype(F32).reshape(S5_NG, S5_GROUP))
        y = jax.nn.gelu(y.reshape(bsz, t, S5_CH))
        return y * jax.nn.sigmoid(y @ w_glu.astype(F32) + b_glu.astype(F32))

    out_lat = readout(ul, lat_states).astype(u_lat.dtype)
    out_ctx = readout(uc, ctx_states).astype(u_ctx.dtype) if ctx_out else None
    return out_lat, out_ctx


def attend_blocks(q, k, v):
    bsz, t = q.shape[:2]
    grp = ATT_HEADS // ATT_KV_HEADS
    nb = t // Q_BLOCK
    qb = jnp.moveaxis(q.reshape(bsz, nb, Q_BLOCK, ATT_KV_HEADS, grp, ATT_DIM), 1, 0)
    scale = ATT_DIM ** -0.5

    def one_block(qi):
        s = jnp.einsum('bqkgd,bskd->bkgqs', qi, k) * scale
        p = jax.nn.softmax(s, axis=-1)
        return jnp.einsum('bkgqs,bskd->bqkgd', p, v)

    o = lax.map(one_block, qb)
    return jnp.moveaxis(o, 0, 1).reshape(bsz, t, ATT_HEADS * ATT_DIM)


def attention_mixer(z_lat, z_ctx, q_norm_w, k_norm_w, rows, cols, ctx_out):
    def qkv(z):
        bsz, t = z.shape[:2]
        q, k, v = jnp.split(z, [ATT_HEADS * ATT_DIM, (ATT_HEADS + ATT_KV_HEADS) * ATT_DIM], axis=-1)
        q = head_rms(q.reshape(bsz, t, ATT_HEADS, ATT_DIM), q_norm_w)
        k = head_rms(k.reshape(bsz, t, ATT_KV_HEADS, ATT_DIM), k_norm_w)
        return q, k, v.astype(F32).reshape(bsz, t, ATT_KV_HEADS, ATT_DIM)

    ql, kl, vl = qkv(z_lat)
    qc, kc, vc = qkv(z_ctx)
    ql = apply_axial_rope(ql, rows, cols)
    kl = apply_axial_rope(kl, rows, cols)
    out_lat = attend_blocks(ql, jnp.concatenate([kc, kl], axis=1), jnp.concatenate([vc, vl], axis=1))
    out_ctx = attend_blocks(qc, kc, vc).astype(z_ctx.dtype) if ctx_out else None
    return out_lat.astype(z_lat.dtype), out_ctx


def depthwise_conv(h, w):
    return lax.conv_general_dilated(h, w[:, None, :].astype(h.dtype), window_strides=(1,),
                                    padding=[(CONV_W // 2, CONV_W // 2)],
                                    dimension_numbers=('NWC', 'WIO', 'NWC'),
                                    feature_group_count=h.shape[-1])


def conv_mixer(z_lat, z_ctx, conv_w, ctx_out):
    def run(z):
        b_gate, c_gate, h = jnp.split(z, 3, axis=-1)
        return b_gate * depthwise_conv(c_gate * h, conv_w)

    return run(z_lat), (run(z_ctx) if ctx_out else None)


def moe_ffn(h, w_router, b_router, w_gate_up, b_gate_up, w_down, b_down):
    n_tok, d = h.shape
    logits = (h @ w_router + b_router).astype(F32)
    top_v, top_i = lax.top_k(logits, TOP_K)
    gates = jax.nn.softmax(top_v, axis=-1)
    flat_e = top_i.reshape(-1)
    flat_tok = jnp.repeat(jnp.arange(n_tok, dtype=jnp.int32), TOP_K)
    flat_g = gates.reshape(-1)
    order = jnp.argsort(flat_e)
    sorted_e = flat_e[order]
    counts = jnp.bincount(flat_e, length=N_EXPERTS)
    padded = (counts + MOE_BLOCK - 1) // MOE_BLOCK * MOE_BLOCK
    start = jnp.cumsum(counts) - counts
    pend = jnp.cumsum(padded)
    pstart = pend - padded
    dest = pstart[sorted_e] + jnp.arange(n_tok * TOP_K, dtype=jnp.int32) - start[sorted_e]
    n_blocks = -(-(n_tok * TOP_K + N_EXPERTS * (MOE_BLOCK - 1)) // MOE_BLOCK)
    n_slots = n_blocks * MOE_BLOCK
    slot_tok = jnp.full((n_slots,), n_tok, jnp.int32).at[dest].set(flat_tok[order])
    slot_g = jnp.zeros((n_slots,), F32).at[dest].set(flat_g[order])
    block_e = jnp.minimum(jnp.searchsorted(pend, jnp.arange(n_blocks) * MOE_BLOCK, side='right'),
                          N_EXPERTS - 1)
    h_pad = jnp.concatenate([h, jnp.zeros((1, d), h.dtype)], axis=0)
    xb = h_pad[slot_tok].reshape(n_blocks, MOE_BLOCK, d)

    def expert_block(args):
        xi, e = args
        gu = xi @ w_gate_up[e] + b_gate_up[e]
        gate = jnp.minimum(gu[:, 0::2], SWIGLU_LIMIT)
        up = jnp.clip(gu[:, 1::2], -SWIGLU_LIMIT, SWIGLU_LIMIT)
        act = (up + 1.0) * gate * jax.nn.sigmoid(gate * SWIGLU_ALPHA)
        return act @ w_down[e] + b_down[e]

    yb = lax.map(expert_block, (xb, block_e)).reshape(n_slots, d)
    y = jnp.zeros((n_tok + 1, d), F32).at[slot_tok].add(yb.astype(F32) * slot_g[:, None])
    return y[:n_tok].astype(h.dtype)


def setup_inputs(seed: int = 0) -> dict:
    key = jax.random.key(seed)
    ks = jax.random.split(key, 28)
    nrm = lambda k, shape, s: jax.random.normal(k, shape, F32) * s
    D, L = D_MODEL, DEPTH
    return {
        'x': nrm(ks[0], (BATCH, SEQ, D), 1.0),
        'c': nrm(ks[1], (BATCH, D), 1.0),
        'ctx': nrm(ks[2], (BATCH, CTX_LEN, D), 1.0),
        'c_ctx': nrm(ks[3], (D,), 1.0),
        'w_mod': nrm(ks[4], (L, D, 6 * D), 0.5 * D ** -0.5),
        'b_mod': nrm(ks[5], (L, 6 * D), 0.01),
        'w_in': nrm(ks[6], (L, D, IN_COLS), D ** -0.5),
        'w_out': nrm(ks[7], (L, MIX_W, D), MIX_W ** -0.5),
        's5_a_re': -0.5 + nrm(ks[8], (L, 2, S5_NG, S5_P), 0.01),
        's5_a_im': math.pi * jnp.arange(S5_P, dtype=F32) + nrm(ks[9], (L, 2, S5_NG, S5_P), 0.01),
        's5_log_step': jax.random.uniform(ks[10], (L, 2, S5_NG), F32, math.log(1e-3), math.log(1e-1)),
        's5_b_re': nrm(ks[11], (L, S5_NG, S5_P, S5_GROUP), (2 * S5_GROUP) ** -0.5),
        's5_b_im': nrm(ks[12], (L, S5_NG, S5_P, S5_GROUP), (2 * S5_GROUP) ** -0.5),
        's5_c_re': nrm(ks[13], (L, S5_NG, S5_GROUP, S5_P), (2 * S5_P) ** -0.5),
        's5_c_im': nrm(ks[14], (L, S5_NG, S5_GROUP, S5_P), (2 * S5_P) ** -0.5),
        's5_d': nrm(ks[15], (L, S5_CH), 1.0),
        's5_w_glu': nrm(ks[16], (L, S5_CH, S5_CH), S5_CH ** -0.5),
        's5_b_glu': nrm(ks[17], (L, S5_CH), 0.01),
        'q_norm_w': 1.0 + nrm(ks[18], (L, ATT_DIM), 0.01),
        'k_norm_w': 1.0 + nrm(ks[19], (L, ATT_DIM), 0.01),
        'conv_w': nrm(ks[20], (L, CONV_W, CONV_CH), CONV_W ** -0.5),
        'w_router': nrm(ks[21], (L, D, N_EXPERTS), D ** -0.5),
        'b_router': nrm(ks[22], (L, N_EXPERTS), 0.01),
        'w_gate_up': nrm(ks[23], (L, N_EXPERTS, D, 2 * D_EXPERT), D ** -0.5),
        'b_gate_up': nrm(ks[24], (L, N_EXPERTS, 2 * D_EXPERT), 0.01),
        'w_down': nrm(ks[25], (L, N_EXPERTS, D_EXPERT, D), D_EXPERT ** -0.5),
        'b_down': nrm(ks[26], (L, N_EXPERTS, D), 0.01),
    }


def reference(x, c, ctx, c_ctx, w_mod, b_mod, w_in, w_out, s5_a_re, s5_a_im, s5_log_step,
              s5_b_re, s5_b_im, s5_c_re, s5_c_im, s5_d, s5_w_glu, s5_b_glu, q_norm_w, k_norm_w,
              conv_w, w_router, b_router, w_gate_up, b_gate_up, w_down, b_down):
    bsz, seq_len, d = x.shape
    ctx_len = ctx.shape[1]
    n_rows = seq_len // GRID_W
    pos = jnp.arange(seq_len, dtype=jnp.int32)
    rows = jnp.repeat(jnp.arange(n_rows, dtype=jnp.int32), GRID_W, total_repeat_length=seq_len)
    cols = pos % GRID_W
    act_c = jax.nn.silu(c)[:, None, :]
    act_cc = jax.nn.silu(c_ctx)[None, None, :]
    splits = [RET_COLS, RET_COLS + S5_COLS, RET_COLS + S5_COLS + ATT_COLS]
    x_lat, x_ctx = x, ctx
    for l in range(DEPTH):
        ctx_out = l < DEPTH - 1
        sh1, sc1, g1, sh2, sc2, g2 = jnp.split(act_c @ w_mod[l] + b_mod[l], 6, axis=-1)
        csh1, csc1, cg1, csh2, csc2, cg2 = jnp.split(act_cc @ w_mod[l] + b_mod[l], 6, axis=-1)
        z_lat = modulate(x_lat, sh1, sc1) @ w_in[l]
        z_ctx = modulate(x_ctx, csh1, csc1) @ w_in[l]
        zr_l, zs_l, za_l, zc_l = jnp.split(z_lat, splits, axis=-1)
        zr_c, zs_c, za_c, zc_c = jnp.split(z_ctx, splits, axis=-1)
        r_l, r_c = retention_mixer(zr_l, zr_c, pos, ctx_out)
        s_l, s_c = s5_mixer(zs_l, zs_c, s5_a_re[l], s5_a_im[l], s5_log_step[l], s5_b_re[l], s5_b_im[l],
                            s5_c_re[l], s5_c_im[l], s5_d[l], s5_w_glu[l], s5_b_glu[l], ctx_out)
        a_l, a_c = attention_mixer(za_l, za_c, q_norm_w[l], k_norm_w[l], rows, cols, ctx_out)
        v_l, v_c = conv_mixer(zc_l, zc_c, conv_w[l], ctx_out)
        x_lat = x_lat + g1 * (jnp.concatenate([r_l, s_l, a_l, v_l], axis=-1) @ w_out[l])
        f_lat = modulate(x_lat, sh2, sc2)
        if ctx_out:
            x_ctx = x_ctx + cg1 * (jnp.concatenate([r_c, s_c, a_c, v_c], axis=-1) @ w_out[l])
            f_ctx = modulate(x_ctx, csh2, csc2)
            tok = jnp.concatenate([f_ctx, f_lat], axis=1).reshape(-1, d)
            out = moe_ffn(tok, w_router[l], b_router[l], w_gate_up[l], b_gate_up[l],
                          w_down[l], b_down[l]).reshape(bsz, ctx_len + seq_len, d)
            x_ctx = x_ctx + cg2 * out[:, :ctx_len]
            x_lat = x_lat + g2 * out[:, ctx_len:]
        else:
            out = moe_ffn(f_lat.reshape(-1, d), w_router[l], b_router[l], w_gate_up[l], b_gate_up[l],
                          w_down[l], b_down[l]).reshape(bsz, seq_len, d)
            x_lat = x_lat + g2 * out
    return x_lat
```

```python
import contextlib
import numpy as np
import ml_dtypes
import concourse.bass as bass
import concourse.mybir as mybir
from concourse.bass_utils import run_bass_kernel_spmd

F32 = mybir.dt.float32
BF16 = mybir.dt.bfloat16
I32 = mybir.dt.int32
ALU = mybir.AluOpType
AF = mybir.ActivationFunctionType
AX = mybir.AxisListType


class Buf:
    __slots__ = ("name", "w", "r")

    def __init__(self, name):
        self.name = name
        self.w = None
        self.r = []


class Prog:
    ENG = ("pe", "dve", "act", "pool", "sp")

    def __init__(self, nc):
        self.nc = nc
        self.stream = {e: [] for e in self.ENG}
        self.seen = {e: {} for e in self.ENG}
        self.needed = {e: set() for e in self.ENG}
        self.stack = contextlib.ExitStack()
        self.esem = {e: self.stack.enter_context(nc.semaphore("s_" + e))
                     for e in ("pe", "dve", "act", "pool")}
        self.dsem = {}
        self.dtoks = []
        self.nbuf = 0

    def buf(self, name=None):
        self.nbuf += 1
        return Buf(name or f"b{self.nbuf}")

    def sb(self, name, shape, dt):
        return self.stack.enter_context(self.nc.sbuf_tensor(name, list(shape), dt))

    def ps(self, name, shape, dt):
        return self.stack.enter_context(self.nc.psum_tensor(name, list(shape), dt))

    def _waits(self, eng, reads, writes):
        toks = []
        for b in reads:
            if b.w is not None:
                toks.append(b.w + (True,))
        for b in writes:
            if b.w is not None:
                toks.append(b.w + (False,))
            toks.extend(t + (False,) for t in b.r)
        need = {}
        for kind, src, val, raw in toks:
            if kind == "eng" and src == eng and (not raw or eng == "pe"):
                continue
            key = (kind, src)
            if self.seen[eng].get(key, -1) >= val:
                continue
            if need.get(key, -1) < val:
                need[key] = val
        for key, val in need.items():
            self.seen[eng][key] = val
            if key[0] == "eng":
                self.needed[key[1]].add(val)
        return list(need.items())

    def op(self, eng, fn, reads=(), writes=()):
        waits = self._waits(eng, reads, writes)
        idx = len(self.stream[eng])
        tok = ("eng", eng, idx)
        self.stream[eng].append((waits, fn, None))
        for b in reads:
            b.r.append(tok)
        for b in writes:
            b.w = tok
            b.r = []
        return tok

    def dma(self, q, semkey, fn, reads=(), writes=()):
        waits = self._waits(q, reads, writes)
        if semkey not in self.dsem:
            self.dsem[semkey] = [self.stack.enter_context(self.nc.semaphore("d_" + semkey)), 0]
        self.dsem[semkey][1] += 16
        tok = ("dma", semkey, self.dsem[semkey][1])
        self.stream[q].append((waits, fn, semkey))
        for b in reads:
            b.r.append(tok)
        for b in writes:
            b.w = tok
            b.r = []
        self.dtoks.append(tok)
        return tok

    def finish(self):
        fin = Buf("fin")
        for k, (s, v) in self.dsem.items():
            fin.r.append(("dma", k, v))
        for e in ("pe", "dve", "act", "pool"):
            if self.stream[e]:
                fin.r.append(("eng", e, len(self.stream[e]) - 1))
        waits = self._waits("sp", (), (fin,))
        self.stream["sp"].append((waits, None, None))

    def emit(self):
        nc = self.nc
        self.finish()
        val = {}
        for e in self.ENG:
            c = 0
            for idx in range(len(self.stream[e])):
                if idx in self.needed[e]:
                    c += 1
                    val[(e, idx)] = c
        with nc.Block() as block:
            decos = {"pe": block.tensor, "dve": block.vector, "act": block.scalar,
                     "pool": block.gpsimd, "sp": block.sync}
            for ename in self.ENG:
                items = self.stream[ename]

                def body(e, items=items, ename=ename):
                    for idx, (waits, fn, semkey) in enumerate(items):
                        for (kind, src), v in waits:
                            if kind == "eng":
                                e.wait_ge(self.esem[src], val[(src, v)])
                            else:
                                e.wait_ge(self.dsem[src][0], v)
                        if fn is None:
                            continue
                        ins = fn(e)
                        if semkey is not None:
                            ins.then_inc(self.dsem[semkey][0], 16)
                        elif idx in self.needed[ename]:
                            ins.then_inc(self.esem[ename], 1)

                decos[ename](body)
        self.stack.close()


D = 2048
SEQ = 8192
CTX = 256
NCORE = 8
LAT_PC = SEQ // NCORE
CTX_PC = CTX // NCORE
TOK_PC = LAT_PC + CTX_PC
T_ALL = SEQ + CTX
IN_COLS = 5632
EPS = 1e-6
TILES_PC = [(i * 128, 128) for i in range(8)] + [(1024, 32)]
NPART = 4


def _run(nc, in_maps):
    res = run_bass_kernel_spmd(nc, in_maps, core_ids=list(range(len(in_maps))))
    return res.results


def build_mod():
    nc = bass.Bass("TRN2", target_bir_lowering=False)
    cT = nc.dram_tensor("cT", [128, 16, 2], F32, kind="ExternalInput").ap()
    wm = nc.dram_tensor("wm", [2, 2048, 1536], F32, kind="ExternalInput").ap()
    bm = nc.dram_tensor("bm", [2, 2, 1536], F32, kind="ExternalInput").ap()
    out = nc.dram_tensor("mod", [2, 2, 1536], F32, kind="ExternalOutput").ap()
    P = Prog(nc)
    ct = P.sb("ct", [128, 16, 2], F32); b_ct = P.buf()
    av = P.sb("av", [128, 16, 2], F32); b_av = P.buf()
    bt = P.sb("bt", [2, 2, 1536], F32); b_bt = P.buf()
    ot = P.sb("ot", [2, 2, 1536], F32); b_ot = P.buf()
    NW = 4
    wt = [P.sb(f"wt{i}", [128, 1536], F32) for i in range(NW)]; b_wt = [P.buf() for _ in range(NW)]
    pst = [P.ps(f"ps{i}", [128, 512], F32) for i in range(3)]; b_ps = [P.buf() for _ in range(3)]
    P.dma("sp", "ct", lambda e: e.dma_start(out=ct[:], in_=cT), writes=[b_ct])
    P.dma("sp", "bt", lambda e: e.dma_start(out=bt[:], in_=bm.rearrange("l r n -> r l n")), writes=[b_bt])
    P.op("act", lambda e: e.activation(out=av[:], in_=ct[:], func=AF.Silu), reads=[b_ct], writes=[b_av])
    i = 0
    for l in range(2):
        for k in range(16):
            s = i % NW; i += 1
            P.dma("sp", f"wt{s}", lambda e, s=s, l=l, k=k: e.dma_start(out=wt[s][:], in_=wm[l, k*128:(k+1)*128, :]), writes=[b_wt[s]])
            for n in range(3):
                P.op("pe", lambda e, s=s, n=n, k=k: e.matmul(pst[n][0:2, :], av[:, k, :], wt[s][:, n*512:(n+1)*512], start=(k == 0), stop=(k == 15)),
                     reads=[b_av, b_wt[s]], writes=[b_ps[n]])
        for n in range(3):
            P.op("dve", lambda e, n=n, l=l: e.tensor_tensor(out=ot[:, l, n*512:(n+1)*512], in0=pst[n][0:2, :], in1=bt[:, l, n*512:(n+1)*512], op=ALU.add),
                 reads=[b_ps[n], b_bt], writes=[b_ot])
    P.dma("sp", "ot", lambda e: e.dma_start(out=out.rearrange("l r n -> r l n"), in_=ot[:]), reads=[b_ot])
    P.emit()
    return nc


def run_mod(c, c_ctx, w_mod, b_mod):
    cc = np.stack([c[0], c_ctx], axis=-1)
    cT = np.ascontiguousarray(cc.reshape(16, 128, 2).transpose(1, 0, 2))
    nc = build_mod()
    in_maps = []
    for i in range(NCORE):
        sl = slice(i * 1536, (i + 1) * 1536)
        in_maps.append({"cT": cT, "wm": np.ascontiguousarray(w_mod[:, :, sl]),
                        "bm": np.ascontiguousarray(np.broadcast_to(b_mod[:, None, sl], (2, 2, 1536)))})
    res = _run(nc, in_maps)
    return np.concatenate([r["mod"] for r in res], axis=-1)


def cols128(v):
    return np.ascontiguousarray(v.reshape(16, 128).T)


def build_proj(combine, project=True):
    nc = bass.Bass("TRN2", target_bir_lowering=False)
    x = nc.dram_tensor("x", [TOK_PC, D], F32, kind="ExternalInput").ap()
    ident_d = nc.dram_tensor("ident", [128, 128], F32, kind="ExternalInput").ap()
    P = Prog(nc)
    if project:
        modc = nc.dram_tensor("modc", [128, 16, 4], F32, kind="ExternalInput").ap()
        w_in = nc.dram_tensor("w_in", [D, IN_COLS], F32, kind="ExternalInput").ap()
        z = nc.dram_tensor("z", [TOK_PC, IN_COLS], F32, kind="ExternalOutput").ap()
    if combine:
        part = nc.dram_tensor("part", [NPART, TOK_PC, D], F32, kind="ExternalInput").ap()
        g2 = nc.dram_tensor("g2", [2, 128, D], F32, kind="ExternalInput").ap()
        xo = nc.dram_tensor("xo", [TOK_PC, D], F32, kind="ExternalOutput").ap()
        g2t = P.sb("g2t", [128, 2, D], F32); b_g2 = P.buf()
        P.dma("sp", "g2", lambda e: e.dma_start(out=g2t[:], in_=g2.rearrange("r p d -> p r d")), writes=[b_g2])
        pt = [P.sb(f"pt{i}", [128, D], F32) for i in range(3)]; b_pt = [P.buf() for _ in range(3)]
        acc = P.sb("acc", [128, D], F32); b_acc = P.buf()
    ident = P.sb("identt", [128, 128], F32); b_id = P.buf()
    P.dma("sp", "ident", lambda e: e.dma_start(out=ident[:], in_=ident_d), writes=[b_id])
    xt = [P.sb(f"xt{i}", [128, D], F32) for i in range(2)]; b_xt = [P.buf() for _ in range(2)]
    if project:
        mc = P.sb("mc", [128, 16, 4], F32); b_mc = P.buf()
        P.dma("sp", "mc", lambda e: e.dma_start(out=mc[:], in_=modc), writes=[b_mc])
        P.op("dve", lambda e: e.tensor_scalar(out=mc[:, :, 1], in0=mc[:, :, 1], scalar1=1.0, scalar2=None, op0=ALU.add), reads=[b_mc], writes=[b_mc])
        P.op("dve", lambda e: e.tensor_scalar(out=mc[:, :, 3], in0=mc[:, :, 3], scalar1=1.0, scalar2=None, op0=ALU.add), reads=[b_mc], writes=[b_mc])
        xn = P.sb("xn", [128, D], F32); b_xn = P.buf()
        junk = P.sb("junk", [128, D], BF16); b_junk = P.buf()
        ss = P.sb("ss", [128, 2], F32); b_ss = P.buf()
        xmT = P.sb("xmT", [128, 16, TOK_PC], BF16); b_xmT = [P.buf() for _ in TILES_PC]
        wb = [P.sb(f"wb{i}", [128, 16, 512], BF16) for i in range(2)]; b_wb = [P.buf() for _ in range(2)]
        zt = [P.sb(f"zt{i}", [128, 512], F32) for i in range(4)]; b_zt = [P.buf() for _ in range(4)]
        pst = [P.ps(f"ps{i}", [128, 512], F32) for i in range(8)]; b_ps = [P.buf() for _ in range(8)]
    psi = 0
    for ti, (r0, n) in enumerate(TILES_PC):
        s = ti % 2
        X = xt[s]; bX = b_xt[s]
        P.dma("sp", f"xt{s}", lambda e, X=X, r0=r0, n=n: e.dma_start(out=X[0:n, :], in_=x[r0:r0+n, :]), writes=[bX])
        if combine:
            isctx = 1 if r0 >= LAT_PC else 0
            for c in range(NPART):
                ps_ = c % 3
                P.dma("sp", f"pt{ps_}", lambda e, ps_=ps_, c=c, r0=r0, n=n: e.dma_start(out=pt[ps_][0:n, :], in_=part[c, r0:r0+n, :]), writes=[b_pt[ps_]])
                if c == 0:
                    P.op("pool", lambda e, ps_=ps_, n=n: e.tensor_copy(out=acc[0:n, :], in_=pt[ps_][0:n, :]), reads=[b_pt[ps_]], writes=[b_acc])
                else:
                    eng = "dve" if c % 2 else "pool"
                    P.op(eng, lambda e, ps_=ps_, n=n: e.tensor_tensor(out=acc[0:n, :], in0=acc[0:n, :], in1=pt[ps_][0:n, :], op=ALU.add), reads=[b_pt[ps_], b_acc], writes=[b_acc])
            P.op("dve", lambda e, n=n, isctx=isctx: e.tensor_tensor(out=acc[0:n, :], in0=acc[0:n, :], in1=g2t[0:n, isctx, :], op=ALU.mult), reads=[b_acc, b_g2], writes=[b_acc])
            P.op("dve", lambda e, X=X, n=n: e.tensor_tensor(out=X[0:n, :], in0=X[0:n, :], in1=acc[0:n, :], op=ALU.add), reads=[b_acc, bX], writes=[bX])
            P.dma("sp", f"xo{s}", lambda e, X=X, r0=r0, n=n: e.dma_start(out=xo[r0:r0+n, :], in_=X[0:n, :]), reads=[bX])
        if not project:
            continue
        isctx = 1 if r0 >= LAT_PC else 0
        P.op("act", lambda e, X=X, n=n: e.activation(out=junk[0:n, :], in_=X[0:n, :], func=AF.Square, accum_out=ss[0:n, 0:1]), reads=[bX], writes=[b_junk, b_ss])
        P.op("act", lambda e, n=n: e.activation(out=ss[0:n, 1:2], in_=ss[0:n, 0:1], func=AF.Sqrt, scale=1.0 / D, bias=EPS), reads=[b_ss], writes=[b_ss])
        P.op("dve", lambda e, n=n: e.reciprocal(out=ss[0:n, 1:2], in_=ss[0:n, 1:2]), reads=[b_ss], writes=[b_ss])
        P.op("dve", lambda e, X=X, n=n: e.tensor_scalar(out=xn[0:n, :], in0=X[0:n, :], scalar1=ss[0:n, 1:2], scalar2=None, op0=ALU.mult), reads=[bX, b_ss], writes=[b_xn])
        for kg in range(4):
            pb = psi % 8; psi += 1
            for kk in range(4):
                k = kg * 4 + kk
                P.op("pe", lambda e, pb=pb, kk=kk, k=k, n=n: e.transpose(pst[pb][:, kk*128:kk*128+n], xn[0:n, k*128:(k+1)*128], ident[0:n, 0:n]),
                     reads=[b_xn, b_id], writes=[b_ps[pb]])
            for kk in range(4):
                k = kg * 4 + kk
                if kk % 2 == 0:
                    P.op("dve", lambda e, pb=pb, kk=kk, k=k, n=n, r0=r0, isctx=isctx: e.tensor_scalar(
                        out=xmT[:, k, r0:r0+n], in0=pst[pb][:, kk*128:kk*128+n], scalar1=mc[:, k, 2*isctx+1:2*isctx+2], scalar2=mc[:, k, 2*isctx:2*isctx+1], op0=ALU.mult, op1=ALU.add),
                        reads=[b_ps[pb], b_mc], writes=[b_xmT[ti]])
                else:
                    P.op("act", lambda e, pb=pb, kk=kk, k=k, n=n, r0=r0, isctx=isctx: e.activation(
                        out=xmT[:, k, r0:r0+n], in_=pst[pb][:, kk*128:kk*128+n], func=AF.Identity, scale=mc[:, k, 2*isctx+1:2*isctx+2], bias=mc[:, k, 2*isctx:2*isctx+1]),
                        reads=[b_ps[pb], b_mc], writes=[b_xmT[ti]])
    if project:
        w_v = w_in.rearrange("(k p) n -> p k n", p=128)
        zi = 0
        wsi = 0
        wst = [P.sb(f"wst{i}", [128, 4, 512], F32) for i in range(3)]; b_wst = [P.buf() for _ in range(3)]
        for cb in range(globals().get("IN_COLS_RUN", IN_COLS) // 512):
            s = cb % 2
            for kq in range(4):
                ws_ = wsi % 3; wsi += 1
                P.dma("sp", f"wst{ws_}", lambda e, ws_=ws_, cb=cb, kq=kq: e.dma_start(out=wst[ws_][:], in_=w_v[:, kq*4:(kq+1)*4, cb*512:(cb+1)*512]), writes=[b_wst[ws_]])
                P.op("pool", lambda e, ws_=ws_, s=s, kq=kq: e.tensor_copy(out=wb[s][:, kq*4:(kq+1)*4, :], in_=wst[ws_][:]), reads=[b_wst[ws_]], writes=[b_wb[s]])
            for ti, (r0, n) in enumerate(TILES_PC):
                pb = psi % 8; psi += 1
                for k in range(16):
                    P.op("pe", lambda e, pb=pb, k=k, n=n, r0=r0, s=s: e.matmul(pst[pb][0:n, :], xmT[:, k, r0:r0+n], wb[s][:, k, :], start=(k == 0), stop=(k == 15)),
                         reads=[b_xmT[ti], b_wb[s]], writes=[b_ps[pb]])
                zs = zi % 4; zi += 1
                if zi % 2:
                    P.op("dve", lambda e, pb=pb, zs=zs, n=n: e.tensor_copy(out=zt[zs][0:n, :], in_=pst[pb][0:n, :]), reads=[b_ps[pb]], writes=[b_zt[zs]])
                else:
                    P.op("act", lambda e, pb=pb, zs=zs, n=n: e.copy(out=zt[zs][0:n, :], in_=pst[pb][0:n, :]), reads=[b_ps[pb]], writes=[b_zt[zs]])
                P.dma("sp", f"zt{zs}", lambda e, zs=zs, n=n, r0=r0, cb=cb: e.dma_start(out=z[r0:r0+n, cb*512:(cb+1)*512], in_=zt[zs][0:n, :]), reads=[b_zt[zs]])
    P.emit()
    return nc


def shard_tokens(lat, ctx):
    return [np.ascontiguousarray(np.concatenate([lat[i*LAT_PC:(i+1)*LAT_PC], ctx[i*CTX_PC:(i+1)*CTX_PC]], axis=0)) for i in range(NCORE)]


def unshard_tokens(per_core):
    lat = np.concatenate([p[:LAT_PC] for p in per_core], axis=0)
    ctx = np.concatenate([p[LAT_PC:] for p in per_core], axis=0)
    return lat, ctx


def run_proj(x_shards, mod_l, w_in_l, parts=None, project=True):
    combine = parts is not None
    nc = build_proj(combine, project)
    ident = np.eye(128, dtype=np.float32)
    in_maps = []
    for i in range(NCORE):
        m = {"x": x_shards[i], "ident": ident}
        if project:
            sh1, sc1 = mod_l[0, 0:D], mod_l[0, D:2*D]
            csh1, csc1 = mod_l[1, 0:D], mod_l[1, D:2*D]
            m["modc"] = np.ascontiguousarray(np.stack([cols128(sh1), cols128(sc1), cols128(csh1), cols128(csc1)], axis=-1))
            m["w_in"] = w_in_l
        if combine:
            m["part"] = parts[i]
            g2 = np.stack([np.broadcast_to(mod_l_prev_g2[0], (128, D)), np.broadcast_to(mod_l_prev_g2[1], (128, D))])
            m["g2"] = np.ascontiguousarray(g2)
        in_maps.append(m)
    res = _run(nc, in_maps)
    z = [r["z"] for r in res] if project else None
    xo = [r["xo"] for r in res] if combine else None
    return z, xo


NCH = T_ALL // 128
SEG = 384
NSEG = T_ALL // SEG
NQ = 128 + SEQ // 2
NQB = NQ // 128
ATT_SCALE = 128 ** -0.5


def build_mix():
    nc = bass.Bass("TRN2", target_bir_lowering=False)
    di = lambda name, shape, dt=F32: nc.dram_tensor(name, list(shape), dt, kind="ExternalInput").ap()
    do = lambda name, shape, dt=F32: nc.dram_tensor(name, list(shape), dt, kind="ExternalOutput").ap()
    ident_d = di("ident", [128, 128])
    maskT_d = di("maskT", [128, 128])
    r_qkv = di("r_qkv", [T_ALL, 3, 128])
    r_tab = di("r_tab", [T_ALL, 4, 128])
    r_g = di("r_g", [128, 1])
    r_out = do("r_out", [T_ALL, 128])
    s_par = di("s_par", [128, 4, 3])
    s_B = di("s_B", [128, 2, 2, 32])
    s_C = di("s_C", [128, 2, 2, 64])
    s_uT = di("s_uT", [2, 2, 32, T_ALL])
    s_iota = di("s_iota", [128, SEG])
    s_out = do("s_out", [2, T_ALL, 64])
    a_q = di("a_q", [NQ, 128]); a_k = di("a_k", [T_ALL, 128]); a_v = di("a_v", [T_ALL, 128])
    a_qtab = di("a_qtab", [NQ, 2, 128]); a_ktab = di("a_ktab", [T_ALL, 2, 128])
    a_w = di("a_w", [128, 2, 128])
    a_out = do("a_out", [NQ, 128])

    P = Prog(nc)
    NF = 6
    psf = [P.ps(f"psf{i}", [128, 512], F32) for i in range(NF)]; b_psf = [P.buf() for _ in range(NF)]
    psb = [P.ps(f"psb{i}", [128, 512], BF16) for i in range(2)]; b_psb = [P.buf() for _ in range(2)]
    cnt = {"f": 0, "b": 0}

    def nf():
        i = cnt["f"] % NF; cnt["f"] += 1
        return psf[i], b_psf[i]

    def nb():
        i = cnt["b"] % 2; cnt["b"] += 1
        return psb[i], b_psb[i]

    ident = P.sb("identt", [128, 128], F32); b_id = P.buf()
    identb = P.sb("identb", [128, 128], BF16); b_idb = P.buf()
    maskT = P.sb("maskTt", [128, 128], F32); b_mask = P.buf()
    P.dma("sp", "ident", lambda e: e.dma_start(out=ident[:], in_=ident_d), writes=[b_id])
    P.dma("sp", "maskT", lambda e: e.dma_start(out=maskT[:], in_=maskT_d), writes=[b_mask])
    P.op("dve", lambda e: e.tensor_copy(out=identb[:], in_=ident[:]), reads=[b_id], writes=[b_idb])

    def s5_unit():
        par = P.sb("s_par_t", [128, 4, 3], F32); b_par = P.buf()
        Bt = P.sb("s_B_t", [128, 2, 2, 32], F32); b_B = P.buf()
        Ct = P.sb("s_C_t", [128, 2, 2, 64], F32); b_C = P.buf()
        io = P.sb("s_iota_t", [128, SEG], F32); b_io = P.buf()
        P.dma("sp", "s_par", lambda e: e.dma_start(out=par[:], in_=s_par), writes=[b_par])
        P.dma("sp", "s_B", lambda e: e.dma_start(out=Bt[:], in_=s_B), writes=[b_B])
        P.dma("sp", "s_C", lambda e: e.dma_start(out=Ct[:], in_=s_C), writes=[b_C])
        P.dma("sp", "s_iota", lambda e: e.dma_start(out=io[:], in_=s_iota), writes=[b_io])
        P.op("dve", lambda e: e.tensor_scalar(out=Ct[:, :, 1, :], in0=Ct[:, :, 1, :], scalar1=-1.0, scalar2=None, op0=ALU.mult), reads=[b_C], writes=[b_C])
        cs = P.sb("s_cs", [128, 4, SEG], F32); sn = P.sb("s_sn", [128, 4, SEG], F32); rb = P.sb("s_rb", [128, 4, SEG], F32)
        b_tab = [P.buf() for _ in range(4)]
        col = P.sb("s_col", [128, 4, 24], F32); b_col = [P.buf() for _ in range(4)]
        ph = P.sb("s_ph", [128, SEG], F32); b_ph = P.buf()
        ph2 = P.sb("s_ph2", [128, SEG], F32); b_ph2 = P.buf()
        phi = P.sb("s_phi", [128, SEG], I32); b_phi = P.buf()
        BpT = P.sb("s_BpT", [32, 4, 2, 128], F32); b_BpT = [P.buf() for _ in range(4)]
        Bp = P.sb("s_Bp", [128, 2, 32], F32); b_Bp = P.buf()
        tmpB = P.sb("s_tmpB", [128, 32], F32); b_tmpB = P.buf()
        st = P.sb("s_st", [128, 4, 2], F32); b_st = [P.buf() for _ in range(4)]

        def frac_sin(dst, src, bsrc, bdst_list):
            P.op("dve", lambda e: e.tensor_copy(out=phi[:], in_=src), reads=[bsrc], writes=[b_phi])
            P.op("dve", lambda e: e.tensor_tensor(out=ph2[:], in0=src, in1=phi[:], op=ALU.subtract), reads=[bsrc, b_phi], writes=[b_ph2])
            P.op("dve", lambda e: e.tensor_scalar(out=phi[:], in0=ph2[:], scalar1=0.5, scalar2=None, op0=ALU.is_gt), reads=[b_ph2], writes=[b_phi])
            P.op("dve", lambda e: e.tensor_tensor(out=ph2[:], in0=ph2[:], in1=phi[:], op=ALU.subtract), reads=[b_ph2, b_phi], writes=[b_ph2])
            P.op("dve", lambda e: e.tensor_scalar(out=phi[:], in0=ph2[:], scalar1=-0.5, scalar2=None, op0=ALU.is_lt), reads=[b_ph2], writes=[b_phi])
            P.op("dve", lambda e: e.tensor_tensor(out=ph2[:], in0=ph2[:], in1=phi[:], op=ALU.add), reads=[b_ph2, b_phi], writes=[b_ph2])
            P.op("act", lambda e: e.activation(out=dst, in_=ph2[:], func=AF.Sin, scale=2.0 * 3.14159265), reads=[b_ph2], writes=bdst_list)

        for cb in range(4):
            tl = cb % 2
            c_ = lambda j, cb=cb: col[:, cb, j:j+1]
            bc = b_col[cb]
            a_re = par[:, cb, 0:1]; a_im = par[:, cb, 1:2]; lst = par[:, cb, 2:3]
            P.op("act", lambda e, c_=c_, lst=lst: e.activation(out=c_(0), in_=lst, func=AF.Exp), reads=[b_par], writes=[bc])
            P.op("dve", lambda e, c_=c_, a_re=a_re: e.tensor_tensor(out=c_(1), in0=a_re, in1=c_(0), op=ALU.mult), reads=[b_par, bc], writes=[bc])
            P.op("act", lambda e, c_=c_: e.activation(out=c_(2), in_=c_(1), func=AF.Exp), reads=[bc], writes=[bc])
            P.op("dve", lambda e, c_=c_, a_im=a_im: e.tensor_tensor(out=c_(3), in0=a_im, in1=c_(0), op=ALU.mult), reads=[b_par, bc], writes=[bc])
            P.op("dve", lambda e, c_=c_: e.tensor_scalar(out=c_(3), in0=c_(3), scalar1=1.0 / (2.0 * np.pi), scalar2=None, op0=ALU.mult), reads=[bc], writes=[bc])
            P.op("dve", lambda e, c_=c_: e.tensor_scalar(out=ph[:], in0=io[:], scalar1=c_(3), scalar2=None, op0=ALU.mult), reads=[b_io, bc], writes=[b_ph])
            frac_sin(sn[:, cb, :], ph[:], b_ph, [b_tab[cb]])
            P.op("dve", lambda e: e.tensor_scalar(out=ph[:], in0=ph[:], scalar1=0.25, scalar2=None, op0=ALU.add), reads=[b_ph], writes=[b_ph])
            frac_sin(cs[:, cb, :], ph[:], b_ph, [b_tab[cb]])
            P.op("pool", lambda e, cb=cb: e.memset(rb[:, cb, :], 1.0), writes=[b_tab[cb]])
            P.op("dve", lambda e, cb=cb, c_=c_: e.tensor_scalar(out=rb[:, cb, :], in0=rb[:, cb, :], scalar1=c_(2), scalar2=None, op0=ALU.mult), reads=[bc, b_tab[cb]], writes=[b_tab[cb]])
            tt = lambda o, a, b, op, c_=c_: P.op("dve", lambda e: e.tensor_tensor(out=o, in0=a, in1=b, op=op), reads=[bc, b_par, b_tab[cb]], writes=[bc])
            tt(c_(4), c_(2), cs[:, cb, 0:1], ALU.mult)
            tt(c_(5), c_(2), sn[:, cb, 0:1], ALU.mult)
            P.op("dve", lambda e, c_=c_: e.tensor_scalar(out=c_(6), in0=c_(4), scalar1=-1.0, scalar2=None, op0=ALU.add), reads=[bc], writes=[bc])
            tt(c_(7), c_(6), a_re, ALU.mult)
            tt(c_(8), c_(5), a_im, ALU.mult)
            tt(c_(9), c_(7), c_(8), ALU.add)
            tt(c_(10), c_(5), a_re, ALU.mult)
            tt(c_(11), c_(6), a_im, ALU.mult)
            tt(c_(12), c_(10), c_(11), ALU.subtract)
            tt(c_(13), a_re, a_re, ALU.mult)
            tt(c_(14), a_im, a_im, ALU.mult)
            tt(c_(15), c_(13), c_(14), ALU.add)
            P.op("dve", lambda e, c_=c_: e.reciprocal(out=c_(16), in_=c_(15)), reads=[bc], writes=[bc])
            tt(c_(17), c_(9), c_(16), ALU.mult)
            tt(c_(18), c_(12), c_(16), ALU.mult)
            P.op("dve", lambda e, c_=c_, tl=tl: e.tensor_scalar(out=tmpB[:], in0=Bt[:, tl, 1, :], scalar1=c_(18), scalar2=None, op0=ALU.mult), reads=[bc, b_B], writes=[b_tmpB])
            P.op("dve", lambda e, c_=c_, tl=tl: e.scalar_tensor_tensor(out=Bp[:, 0, :], in0=Bt[:, tl, 0, :], scalar=c_(17), in1=tmpB[:], op0=ALU.mult, op1=ALU.subtract), reads=[bc, b_B, b_tmpB], writes=[b_Bp])
            P.op("dve", lambda e, c_=c_, tl=tl: e.tensor_scalar(out=tmpB[:], in0=Bt[:, tl, 0, :], scalar1=c_(18), scalar2=None, op0=ALU.mult), reads=[bc, b_B], writes=[b_tmpB])
            P.op("dve", lambda e, c_=c_, tl=tl: e.scalar_tensor_tensor(out=Bp[:, 1, :], in0=Bt[:, tl, 1, :], scalar=c_(17), in1=tmpB[:], op0=ALU.mult, op1=ALU.add), reads=[bc, b_B, b_tmpB], writes=[b_Bp])
            for ri in range(2):
                pt_, bpt_ = nf()
                P.op("pe", lambda e, pt_=pt_, ri=ri: e.transpose(pt_[0:32, 0:128], Bp[:, ri, :], ident[:]), reads=[b_Bp, b_id], writes=[bpt_])
                P.op("act", lambda e, pt_=pt_, ri=ri, cb=cb: e.copy(out=BpT[:, cb, ri, :], in_=pt_[0:32, 0:128]), reads=[bpt_], writes=[b_BpT[cb]])

        NU = 3
        ut = [P.sb(f"s_ut{i}", [32, SEG], F32) for i in range(NU)]; b_ut = [P.buf() for _ in range(NU)]
        m = [P.sb(f"s_m{i}", [128, SEG], F32) for i in range(4)]; b_m = [P.buf() for _ in range(4)]
        dr = [P.sb(f"s_dr{i}", [128, SEG], F32) for i in range(2)]; b_dr = [P.buf() for _ in range(2)]
        xs_ = [P.sb(f"s_xs{i}", [128, SEG], F32) for i in range(2)]; b_xs = [P.buf() for _ in range(2)]
        xr = [[P.sb(f"s_xr{tl}{ri}", [128, SEG], F32) for ri in range(2)] for tl in range(2)]
        b_xr = [[P.buf() for _ in range(2)] for _ in range(2)]
        ysb = [P.sb(f"s_y{i}", [128, 3, 64], F32) for i in range(2)]; b_ysb = [P.buf() for _ in range(2)]
        ui = 0; yi = 0
        for d in range(2):
            for sg in range(NSEG):
                for tl in range(2):
                    cb = d * 2 + tl
                    us = ui % NU; ui += 1
                    P.dma("sp", f"s_ut{us}", lambda e, us=us, d=d, tl=tl, sg=sg: e.dma_start(out=ut[us][:], in_=s_uT[d, tl, :, sg*SEG:(sg+1)*SEG]), writes=[b_ut[us]])
                    pre, bpre = nf(); pim, bpim = nf()
                    P.op("pe", lambda e, pre=pre, us=us, cb=cb: e.matmul(pre[:, 0:SEG], BpT[:, cb, 0, :], ut[us][:], start=True, stop=True), reads=[b_BpT[cb], b_ut[us]], writes=[bpre])
                    P.op("pe", lambda e, pim=pim, us=us, cb=cb: e.matmul(pim[:, 0:SEG], BpT[:, cb, 1, :], ut[us][:], start=True, stop=True), reads=[b_BpT[cb], b_ut[us]], writes=[bpim])
                    C_ = cs[:, cb, :]; S_ = sn[:, cb, :]
                    P.op("dve", lambda e, pre=pre, C_=C_: e.tensor_tensor(out=m[0][:], in0=pre[:, 0:SEG], in1=C_, op=ALU.mult), reads=[bpre, b_tab[cb]], writes=[b_m[0]])
                    P.op("dve", lambda e, pim=pim, S_=S_: e.tensor_tensor(out=m[1][:], in0=pim[:, 0:SEG], in1=S_, op=ALU.mult), reads=[bpim, b_tab[cb]], writes=[b_m[1]])
                    P.op("dve", lambda e, pim=pim, C_=C_: e.tensor_tensor(out=m[2][:], in0=pim[:, 0:SEG], in1=C_, op=ALU.mult), reads=[bpim, b_tab[cb]], writes=[b_m[2]])
                    P.op("dve", lambda e, pre=pre, S_=S_: e.tensor_tensor(out=m[3][:], in0=pre[:, 0:SEG], in1=S_, op=ALU.mult), reads=[bpre, b_tab[cb]], writes=[b_m[3]])
                    P.op("pool", lambda e: e.tensor_tensor(out=dr[0][:], in0=m[0][:], in1=m[1][:], op=ALU.add), reads=[b_m[0], b_m[1]], writes=[b_dr[0]])
                    P.op("pool", lambda e: e.tensor_tensor(out=dr[1][:], in0=m[2][:], in1=m[3][:], op=ALU.subtract), reads=[b_m[2], b_m[3]], writes=[b_dr[1]])
                    for ri in range(2):
                        init = 0.0 if sg == 0 else st[:, cb, ri:ri+1]
                        P.op("dve", lambda e, ri=ri, init=init, cb=cb: e.tensor_tensor_scan(out=xs_[ri][:], data0=rb[:, cb, :], data1=dr[ri][:], initial=init, op0=ALU.mult, op1=ALU.add),
                             reads=[b_tab[cb], b_dr[ri], b_st[cb]], writes=[b_xs[ri]])
                    P.op("pool", lambda e, C_=C_: e.tensor_tensor(out=m[0][:], in0=xs_[0][:], in1=C_, op=ALU.mult), reads=[b_xs[0], b_tab[cb]], writes=[b_m[0]])
                    P.op("dve", lambda e, S_=S_: e.tensor_tensor(out=m[1][:], in0=xs_[1][:], in1=S_, op=ALU.mult), reads=[b_xs[1], b_tab[cb]], writes=[b_m[1]])
                    P.op("pool", lambda e, S_=S_: e.tensor_tensor(out=m[2][:], in0=xs_[0][:], in1=S_, op=ALU.mult), reads=[b_xs[0], b_tab[cb]], writes=[b_m[2]])
                    P.op("dve", lambda e, C_=C_: e.tensor_tensor(out=m[3][:], in0=xs_[1][:], in1=C_, op=ALU.mult), reads=[b_xs[1], b_tab[cb]], writes=[b_m[3]])
                    P.op("pool", lambda e, tl=tl: e.tensor_tensor(out=xr[tl][0][:], in0=m[0][:], in1=m[1][:], op=ALU.subtract), reads=[b_m[0], b_m[1]], writes=[b_xr[tl][0]])
                    P.op("pool", lambda e, tl=tl: e.tensor_tensor(out=xr[tl][1][:], in0=m[2][:], in1=m[3][:], op=ALU.add), reads=[b_m[2], b_m[3]], writes=[b_xr[tl][1]])
                    for ri in range(2):
                        P.op("act", lambda e, tl=tl, ri=ri, cb=cb: e.copy(out=st[:, cb, ri:ri+1], in_=xr[tl][ri][:, SEG-1:SEG]), reads=[b_xr[tl][ri]], writes=[b_st[cb]])
                ys = yi % 2; yi += 1
                for blk in range(3):
                    py, bpy = nf()
                    j = 0
                    for tl in range(2):
                        for ri in range(2):
                            P.op("pe", lambda e, py=py, tl=tl, ri=ri, blk=blk, j=j: e.matmul(py[:, 0:64], xr[tl][ri][:, blk*128:(blk+1)*128], Ct[:, tl, ri, :], start=(j == 0), stop=(j == 3)),
                                 reads=[b_xr[tl][ri], b_C], writes=[bpy])
                            j += 1
                    P.op("act", lambda e, py=py, ys=ys, blk=blk: e.copy(out=ysb[ys][:, blk, :], in_=py[:, 0:64]), reads=[bpy], writes=[b_ysb[ys]])
                P.dma("sp", f"s_y{ys}", lambda e, ys=ys, d=d, sg=sg: e.dma_start(out=s_out[d, sg*SEG:(sg+1)*SEG, :].rearrange("(b p) c -> p b c", p=128), in_=ysb[ys][:]), reads=[b_ysb[ys]])

    def ret_unit():
        g = P.sb("r_g_t", [128, 1], F32); b_g = P.buf()
        P.dma("sp", "r_g", lambda e: e.dma_start(out=g[:], in_=r_g), writes=[b_g])
        NB = 2
        qkv = [P.sb(f"r_qkv{i}", [128, 3, 128], F32) for i in range(NB)]; b_qkv = [P.buf() for _ in range(NB)]
        tab = [P.sb(f"r_tab{i}", [128, 4, 128], F32) for i in range(NB)]; b_tb = [P.buf() for _ in range(NB)]
        sw = P.sb("r_sw", [128, 2, 128], F32); b_sw = P.buf()
        t1 = P.sb("r_t1", [128, 2, 128], F32); b_t1 = P.buf()
        t2 = P.sb("r_t2", [128, 2, 128], F32); b_t2 = P.buf()
        qk = P.sb("r_qk", [128, 2, 128], BF16); b_qk = P.buf()
        qkT = P.sb("r_qkT", [128, 2, 128], BF16); b_qkT = P.buf()
        vb = P.sb("r_vb", [128, 128], BF16); b_vb = P.buf()
        sm = P.sb("r_sm", [128, 128], BF16); b_sm = P.buf()
        S32 = P.sb("r_S32", [128, 128], F32); b_S32 = P.buf()
        Sb = P.sb("r_Sb", [128, 128], BF16); b_Sb = P.buf()
        stt = P.sb("r_stt", [128, 6], F32); b_stt = P.buf()
        mv = P.sb("r_mv", [128, 4], F32); b_mv = P.buf()
        yo = [P.sb(f"r_yo{i}", [128, 128], F32) for i in range(2)]; b_yo = [P.buf() for _ in range(2)]
        P.op("pool", lambda e: e.memset(S32[:], 0.0), writes=[b_S32])
        P.op("pool", lambda e: e.memset(Sb[:], 0.0), writes=[b_Sb])
        for n in range(NCH):
            s = n % NB
            Q = qkv[s]; TB = tab[s]
            P.dma("sp", f"r_qkv{s}", lambda e, Q=Q, n=n: e.dma_start(out=Q[:], in_=r_qkv[n*128:(n+1)*128]), writes=[b_qkv[s]])
            P.dma("sp", f"r_tab{s}", lambda e, TB=TB, n=n: e.dma_start(out=TB[:], in_=r_tab[n*128:(n+1)*128]), writes=[b_tb[s]])
            P.op("pool", lambda e, Q=Q: e.tensor_copy(out=sw[:, :, 0:64], in_=Q[:, 0:2, 64:128]), reads=[b_qkv[s]], writes=[b_sw])
            P.op("pool", lambda e, Q=Q: e.tensor_copy(out=sw[:, :, 64:128], in_=Q[:, 0:2, 0:64]), reads=[b_qkv[s]], writes=[b_sw])
            TBv = TB[:].rearrange("p (a b) d -> p a b d", b=2)
            P.op("dve", lambda e, Q=Q, TBv=TBv: e.tensor_tensor(out=t1[:], in0=Q[:, 0:2, :], in1=TBv[:, :, 0, :], op=ALU.mult), reads=[b_qkv[s], b_tb[s]], writes=[b_t1])
            P.op("pool", lambda e, TBv=TBv: e.tensor_tensor(out=t2[:], in0=sw[:], in1=TBv[:, :, 1, :], op=ALU.mult), reads=[b_sw, b_tb[s]], writes=[b_t2])
            P.op("dve", lambda e: e.tensor_tensor(out=qk[:], in0=t1[:], in1=t2[:], op=ALU.add), reads=[b_t1, b_t2], writes=[b_qk])
            P.op("act", lambda e, Q=Q: e.copy(out=vb[:], in_=Q[:, 2, :]), reads=[b_qkv[s]], writes=[b_vb])
            pt_, bpt_ = nb()
            P.op("pe", lambda e, pt_=pt_: e.transpose(pt_[:, 0:128], qk[:, 0, :], identb[:]), reads=[b_qk, b_idb], writes=[bpt_])
            P.op("pe", lambda e, pt_=pt_: e.transpose(pt_[:, 128:256], qk[:, 1, :], identb[:]), reads=[b_qk, b_idb], writes=[bpt_])
            P.op("act", lambda e, pt_=pt_: e.copy(out=qkT[:].rearrange("p a d -> p (a d)"), in_=pt_[:, 0:256]), reads=[bpt_], writes=[b_qkT])
            ps_s, bps_s = nf()
            P.op("pe", lambda e, ps_s=ps_s: e.matmul(ps_s[:, 0:128], qkT[:, 1, :], qkT[:, 0, :], start=True, stop=True), reads=[b_qkT], writes=[bps_s])
            P.op("dve", lambda e, ps_s=ps_s: e.tensor_tensor(out=sm[:], in0=ps_s[:, 0:128], in1=maskT[:], op=ALU.mult), reads=[bps_s, b_mask], writes=[b_sm])
            ps_y, bps_y = nf()
            P.op("pe", lambda e, ps_y=ps_y: e.matmul(ps_y[:, 0:128], sm[:], vb[:], start=True, stop=False), reads=[b_sm, b_vb], writes=[bps_y])
            P.op("pe", lambda e, ps_y=ps_y: e.matmul(ps_y[:, 0:128], qkT[:, 0, :], Sb[:], start=False, stop=True), reads=[b_qkT, b_Sb], writes=[bps_y])
            ps_kv, bps_kv = nf()
            P.op("pe", lambda e, ps_kv=ps_kv: e.matmul(ps_kv[:, 0:128], qk[:, 1, :], vb[:], start=True, stop=True), reads=[b_qk, b_vb], writes=[bps_kv])
            P.op("dve", lambda e, ps_kv=ps_kv: e.tensor_tensor(out=S32[:], in0=ps_kv[:, 0:128], in1=S32[:], op=ALU.add), reads=[bps_kv, b_S32], writes=[b_S32])
            P.op("dve", lambda e: e.tensor_scalar(out=S32[:], in0=S32[:], scalar1=g[:, 0:1], scalar2=None, op0=ALU.mult), reads=[b_S32, b_g], writes=[b_S32])
            P.op("act", lambda e: e.copy(out=Sb[:], in_=S32[:]), reads=[b_S32], writes=[b_Sb])
            P.op("dve", lambda e, ps_y=ps_y: e.bn_stats(out=stt[:], in_=ps_y[:, 0:128]), reads=[bps_y], writes=[b_stt])
            P.op("dve", lambda e: e.bn_aggr(out=mv[:, 0:2], in_=stt[:]), reads=[b_stt], writes=[b_mv])
            P.op("act", lambda e: e.activation(out=mv[:, 2:3], in_=mv[:, 1:2], func=AF.Sqrt, bias=EPS, scale=1.0), reads=[b_mv], writes=[b_mv])
            P.op("dve", lambda e: e.reciprocal(out=mv[:, 3:4], in_=mv[:, 2:3]), reads=[b_mv], writes=[b_mv])
            ys = n % 2
            P.op("dve", lambda e, ps_y=ps_y, ys=ys: e.tensor_scalar(out=yo[ys][:], in0=ps_y[:, 0:128], scalar1=mv[:, 0:1], scalar2=mv[:, 3:4], op0=ALU.subtract, op1=ALU.mult), reads=[bps_y, b_mv], writes=[b_yo[ys]])
            P.dma("sp", f"r_yo{ys}", lambda e, ys=ys, n=n: e.dma_start(out=r_out[n*128:(n+1)*128, :], in_=yo[ys][:]), reads=[b_yo[ys]])

    def att_unit():
        wt = P.sb("a_w_t", [128, 2, 128], F32); b_w = P.buf()
        P.dma("sp", "a_w", lambda e: e.dma_start(out=wt[:], in_=a_w), writes=[b_w])
        kT = P.sb("a_kT", [128, T_ALL], BF16); b_kT = P.buf()
        qT = P.sb("a_qT", [128, NQ], BF16); b_qT = P.buf()
        vb = P.sb("a_vb", [128, NCH, 128], BF16); b_vb = P.buf()
        xin = [P.sb(f"a_xin{i}", [128, 128], F32) for i in range(2)]; b_xin = [P.buf() for _ in range(2)]
        tb = [P.sb(f"a_tb{i}", [128, 2, 128], F32) for i in range(2)]; b_tb = [P.buf() for _ in range(2)]
        junk = P.sb("a_junk", [128, 128], F32); b_junk = P.buf()
        col = P.sb("a_col", [128, 4], F32); b_col = P.buf()
        xn = P.sb("a_xn", [128, 128], F32); b_xn = P.buf()
        sw = P.sb("a_sw", [128, 128], F32); b_sw = P.buf()
        t1 = P.sb("a_t1", [128, 128], F32); b_t1 = P.buf()
        t2 = P.sb("a_t2", [128, 128], F32); b_t2 = P.buf()
        xr = P.sb("a_xr", [128, 128], BF16); b_xr = P.buf()
        vst = [P.sb(f"a_vst{i}", [128, 6, 128], F32) for i in range(2)]; b_vst = [P.buf() for _ in range(2)]
        a_v_v = a_v.rearrange("(b p) d -> p b d", p=128)
        for i in range(NCH // 6):
            s = i % 2
            P.dma("sp", f"a_vst{s}", lambda e, s=s, i=i: e.dma_start(out=vst[s][:], in_=a_v_v[:, i*6:(i+1)*6, :]), writes=[b_vst[s]])
            P.op("pool", lambda e, s=s, i=i: e.tensor_copy(out=vb[:, i*6:(i+1)*6, :], in_=vst[s][:]), reads=[b_vst[s]], writes=[b_vb])

        def prep(src, tabsrc, nblk, wi, dstT, b_dstT):
            for blk in range(nblk):
                s = blk % 2
                X = xin[s]; TB = tb[s]
                P.dma("sp", f"a_xin{s}", lambda e, X=X, blk=blk: e.dma_start(out=X[:], in_=src[blk*128:(blk+1)*128, :]), writes=[b_xin[s]])
                P.dma("sp", f"a_tb{s}", lambda e, TB=TB, blk=blk: e.dma_start(out=TB[:], in_=tabsrc[blk*128:(blk+1)*128]), writes=[b_tb[s]])
                P.op("act", lambda e, X=X: e.activation(out=junk[:], in_=X[:], func=AF.Square, accum_out=col[:, 0:1]), reads=[b_xin[s]], writes=[b_junk, b_col])
                P.op("act", lambda e: e.activation(out=col[:, 1:2], in_=col[:, 0:1], func=AF.Sqrt, scale=1.0 / 128, bias=EPS), reads=[b_col], writes=[b_col])
                P.op("dve", lambda e: e.reciprocal(out=col[:, 2:3], in_=col[:, 1:2]), reads=[b_col], writes=[b_col])
                P.op("dve", lambda e, X=X: e.scalar_tensor_tensor(out=xn[:], in0=X[:], scalar=col[:, 2:3], in1=wt[:, wi, :], op0=ALU.mult, op1=ALU.mult), reads=[b_xin[s], b_col, b_w], writes=[b_xn])
                xv = xn[:].rearrange("p (a b d) -> p a b d", a=2, b=2)
                sv = sw[:].rearrange("p (a b d) -> p a b d", a=2, b=2)
                P.op("pool", lambda e, xv=xv, sv=sv: e.tensor_copy(out=sv[:, :, 0, :], in_=xv[:, :, 1, :]), reads=[b_xn], writes=[b_sw])
                P.op("pool", lambda e, xv=xv, sv=sv: e.tensor_copy(out=sv[:, :, 1, :], in_=xv[:, :, 0, :]), reads=[b_xn], writes=[b_sw])
                P.op("dve", lambda e, TB=TB: e.tensor_tensor(out=t1[:], in0=xn[:], in1=TB[:, 0, :], op=ALU.mult), reads=[b_xn, b_tb[s]], writes=[b_t1])
                P.op("pool", lambda e, TB=TB: e.tensor_tensor(out=t2[:], in0=sw[:], in1=TB[:, 1, :], op=ALU.mult), reads=[b_sw, b_tb[s]], writes=[b_t2])
                P.op("dve", lambda e: e.tensor_tensor(out=xr[:], in0=t1[:], in1=t2[:], op=ALU.add), reads=[b_t1, b_t2], writes=[b_xr])
                pt_, bpt_ = nb()
                P.op("pe", lambda e, pt_=pt_: e.transpose(pt_[:, 0:128], xr[:], identb[:]), reads=[b_xr, b_idb], writes=[bpt_])
                P.op("act", lambda e, pt_=pt_, blk=blk: e.copy(out=dstT[:, blk*128:(blk+1)*128], in_=pt_[:, 0:128]), reads=[bpt_], writes=[b_dstT])

        prep(a_k, a_ktab, NCH, 1, kT, b_kT)
        prep(a_q, a_qtab, NQB, 0, qT, b_qT)

        Ssb = P.sb("a_S", [128, T_ALL], F32); b_S = P.buf()
        Pb = P.sb("a_P", [128, T_ALL], BF16); b_P = P.buf()
        PT = P.sb("a_PT", [128, NCH, 128], BF16); b_PT = P.buf()
        c2 = P.sb("a_c2", [128, 4], F32); b_c2 = P.buf()
        ob = [P.sb(f"a_o{i}", [128, 128], F32) for i in range(2)]; b_ob = [P.buf() for _ in range(2)]
        for qb in range(NQB):
            nk = CTX if qb == 0 else T_ALL
            nkt = (nk + 511) // 512
            for kt in range(nkt):
                w = min(512, nk - kt * 512)
                ps_, bps_ = nf()
                P.op("pe", lambda e, ps_=ps_, qb=qb, kt=kt, w=w: e.matmul(ps_[:, 0:w], qT[:, qb*128:(qb+1)*128], kT[:, kt*512:kt*512+w], start=True, stop=True), reads=[b_qT, b_kT], writes=[bps_])
                if kt % 2:
                    P.op("dve", lambda e, ps_=ps_, kt=kt, w=w: e.tensor_copy(out=Ssb[:, kt*512:kt*512+w], in_=ps_[:, 0:w]), reads=[bps_], writes=[b_S])
                else:
                    P.op("act", lambda e, ps_=ps_, kt=kt, w=w: e.copy(out=Ssb[:, kt*512:kt*512+w], in_=ps_[:, 0:w]), reads=[bps_], writes=[b_S])
            P.op("dve", lambda e, nk=nk: e.reduce_max(out=c2[:, 0:1], in_=Ssb[:, 0:nk], axis=AX.X), reads=[b_S], writes=[b_c2])
            P.op("dve", lambda e: e.tensor_scalar(out=c2[:, 1:2], in0=c2[:, 0:1], scalar1=-ATT_SCALE, scalar2=None, op0=ALU.mult), reads=[b_c2], writes=[b_c2])
            P.op("act", lambda e, nk=nk: e.activation(out=Pb[:, 0:nk], in_=Ssb[:, 0:nk], func=AF.Exp, scale=ATT_SCALE, bias=c2[:, 1:2], accum_out=c2[:, 2:3]), reads=[b_S, b_c2], writes=[b_P, b_c2])
            P.op("dve", lambda e: e.reciprocal(out=c2[:, 3:4], in_=c2[:, 2:3]), reads=[b_c2], writes=[b_c2])
            nkb = nk // 128
            for g0 in range(0, nkb, 4):
                gn = min(4, nkb - g0)
                pt_, bpt_ = nb()
                for j in range(gn):
                    P.op("pe", lambda e, pt_=pt_, j=j, g0=g0: e.transpose(pt_[:, j*128:(j+1)*128], Pb[:, (g0+j)*128:(g0+j+1)*128], identb[:]), reads=[b_P, b_idb], writes=[bpt_])
                if (g0 // 4) % 2:
                    P.op("dve", lambda e, pt_=pt_, g0=g0, gn=gn: e.tensor_copy(out=PT[:, g0:g0+gn, :].rearrange("p a d -> p (a d)"), in_=pt_[:, 0:gn*128]), reads=[bpt_], writes=[b_PT])
                else:
                    P.op("act", lambda e, pt_=pt_, g0=g0, gn=gn: e.copy(out=PT[:, g0:g0+gn, :].rearrange("p a d -> p (a d)"), in_=pt_[:, 0:gn*128]), reads=[bpt_], writes=[b_PT])
            po, bpo = nf()
            for kb in range(nkb):
                P.op("pe", lambda e, po=po, kb=kb, nkb=nkb: e.matmul(po[:, 0:128], PT[:, kb, :], vb[:, kb, :], start=(kb == 0), stop=(kb == nkb - 1)), reads=[b_PT, b_vb], writes=[bpo])
            os_ = qb % 2
            P.op("dve", lambda e, po=po, os_=os_: e.tensor_scalar(out=ob[os_][:], in0=po[:, 0:128], scalar1=c2[:, 3:4], scalar2=None, op0=ALU.mult), reads=[bpo, b_c2], writes=[b_ob[os_]])
            P.dma("sp", f"a_o{os_}", lambda e, os_=os_, qb=qb: e.dma_start(out=a_out[qb*128:(qb+1)*128, :], in_=ob[os_][:]), reads=[b_ob[os_]])

    units = globals().get("MIX_UNITS", "rsa")
    if "s" in units:
        s5_unit()
    if "r" in units:
        ret_unit()
    if "a" in units:
        att_unit()
    P.emit()
    return nc


def _order(d):
    if d == 0:
        return np.arange(T_ALL)
    return np.concatenate([np.arange(CTX)[::-1], CTX + np.arange(SEQ)[::-1]])


_CONST = {}


def mix_consts():
    if _CONST:
        return _CONST
    f64 = np.float64
    log_g = np.log(1.0 - 2.0 ** (-5.0 - np.arange(4, dtype=f64)))
    freqs = 10000.0 ** (-np.arange(0, 128, 2, dtype=f64) / 128)
    rt = {}
    for d in range(2):
        lg = log_g if d == 0 else log_g[::-1]
        order = _order(d)
        isl = order >= CTX
        pos = np.where(isl, order - CTX, 0).astype(f64)
        ang = pos[:, None] * freqs[None, :]
        cos = np.where(isl[:, None], np.cos(ang), 1.0)
        sin = np.where(isl[:, None], np.sin(ang), 0.0)
        cosf = np.concatenate([cos, cos], axis=1)
        sinf = np.concatenate([-sin, sin], axis=1)
        i = (np.arange(T_ALL) % 128).astype(f64)
        for h in range(4):
            gq = np.exp((i + 1.0) * lg[h])[:, None]
            gk = (128.0 ** -0.5) * np.exp(-(i + 1.0) * lg[h])[:, None]
            tab = np.stack([cosf * gq, sinf * gq, cosf * gk, sinf * gk], axis=1).astype(np.float32)
            rt[(h, d)] = (np.ascontiguousarray(tab), np.full((128, 1), np.exp(128.0 * lg[h]), np.float32))
    _CONST["ret"] = rt
    fr = 10000.0 ** (-np.arange(0, 64, 2, dtype=f64) / 64)
    pos = np.arange(SEQ)
    ar = (pos // 64).astype(f64)[:, None] * fr[None, :]
    ac = (pos % 64).astype(f64)[:, None] * fr[None, :]
    cosl = np.concatenate([np.cos(ar), np.cos(ar), np.cos(ac), np.cos(ac)], axis=1)
    sinl = np.concatenate([-np.sin(ar), np.sin(ar), -np.sin(ac), np.sin(ac)], axis=1)
    cosa = np.concatenate([np.ones((CTX, 128)), cosl], axis=0)
    sina = np.concatenate([np.zeros((CTX, 128)), sinl], axis=0)
    _CONST["att"] = np.ascontiguousarray(np.stack([cosa, sina], axis=1).astype(np.float32))
    _CONST["ident"] = np.eye(128, dtype=np.float32)
    _CONST["maskT"] = np.triu(np.ones((128, 128), np.float32))
    _CONST["iota"] = np.ascontiguousarray(np.broadcast_to(np.arange(1, SEG + 1, dtype=np.float32), (128, SEG)))
    return _CONST


def run_mix(z_all, prm):
    C = mix_consts()
    nc = build_mix()
    in_maps = []
    orders = [_order(0), _order(1)]
    for c in range(NCORE):
        m = {"ident": C["ident"], "maskT": C["maskT"]}
        h, d = c % 4, c // 4
        zo = z_all[orders[d]]
        m["r_qkv"] = np.ascontiguousarray(np.stack([zo[:, h*128:(h+1)*128], zo[:, 512+h*128:512+(h+1)*128], zo[:, 1024+h*128:1024+(h+1)*128]], axis=1))
        m["r_tab"], m["r_g"] = C["ret"][(h, d)]
        par = np.zeros((128, 4, 3), np.float32)
        sB = np.zeros((128, 2, 2, 32), np.float32)
        sC = np.zeros((128, 2, 2, 64), np.float32)
        uT = np.zeros((2, 2, 32, T_ALL), np.float32)
        for tl in range(2):
            for gi in range(2):
                g = 4 * c + 2 * tl + gi
                rows = slice(gi * 64, (gi + 1) * 64)
                for dd in range(2):
                    par[rows, dd*2+tl, 0] = prm["s5_a_re"][dd, g]
                    par[rows, dd*2+tl, 1] = prm["s5_a_im"][dd, g]
                    par[rows, dd*2+tl, 2] = prm["s5_log_step"][dd, g]
                sB[rows, tl, 0, gi*16:(gi+1)*16] = prm["s5_b_re"][g]
                sB[rows, tl, 1, gi*16:(gi+1)*16] = prm["s5_b_im"][g]
                sC[rows, tl, 0, tl*32+gi*16:tl*32+(gi+1)*16] = prm["s5_c_re"][g].T
                sC[rows, tl, 1, tl*32+gi*16:tl*32+(gi+1)*16] = prm["s5_c_im"][g].T
            for dd in range(2):
                ucols = z_all[orders[dd], 2560 + (4*c + 2*tl) * 16: 2560 + (4*c + 2*tl + 2) * 16]
                uT[dd, tl] = ucols.T
        m["s_par"], m["s_B"], m["s_C"], m["s_uT"], m["s_iota"] = par, sB, sC, uT, C["iota"]
        hq, half = c // 2, c % 2
        kvh = hq // 2
        A0 = 3072
        qsel = np.concatenate([np.arange(half*128, (half+1)*128), CTX + np.arange(half*4096, (half+1)*4096)])
        m["a_q"] = np.ascontiguousarray(z_all[qsel, A0 + hq*128: A0 + (hq+1)*128])
        m["a_k"] = np.ascontiguousarray(z_all[:, A0 + 512 + kvh*128: A0 + 512 + (kvh+1)*128])
        m["a_v"] = np.ascontiguousarray(z_all[:, A0 + 768 + kvh*128: A0 + 768 + (kvh+1)*128])
        m["a_qtab"] = np.ascontiguousarray(C["att"][qsel])
        m["a_ktab"] = C["att"]
        m["a_w"] = np.ascontiguousarray(np.stack([np.broadcast_to(prm["q_norm_w"], (128, 128)), np.broadcast_to(prm["k_norm_w"], (128, 128))], axis=1))
        in_maps.append(m)
    res = _run(nc, in_maps)
    ret = np.zeros((2, T_ALL, 512), np.float32)
    s5y = np.zeros((2, T_ALL, 512), np.float32)
    att = np.zeros((T_ALL, 512), np.float32)
    for c in range(NCORE):
        h, d = c % 4, c // 4
        if "r_out" in res[c]:
            ret[d][orders[d], h*128:(h+1)*128] = res[c]["r_out"]
        for dd in range(2):
            s5y[dd][orders[dd], c*64:(c+1)*64] = res[c]["s_out"][dd]
        hq, half = c // 2, c % 2
        qsel = np.concatenate([np.arange(half*128, (half+1)*128), CTX + np.arange(half*4096, (half+1)*4096)])
        att[qsel, hq*128:(hq+1)*128] = res[c]["a_out"]
    return ret, s5y, att


def build_out():
    nc = bass.Bass("TRN2", target_bir_lowering=False)
    di = lambda name, shape, dt=F32: nc.dram_tensor(name, list(shape), dt, kind="ExternalInput").ap()
    do = lambda name, shape, dt=F32: nc.dram_tensor(name, list(shape), dt, kind="ExternalOutput").ap()
    ident_d = di("ident", [128, 128])
    x = di("x", [TOK_PC, D])
    rg = di("rg", [TOK_PC, 4, 512])
    s5 = di("s5", [TOK_PC, 3, 512])
    att = di("att", [TOK_PC, 512])
    cv = di("cv", [TOK_PC, 7, 512])
    vecs = di("vecs", [128, 5, 512])
    w_glu = di("w_glu", [512, 512])
    w_out = di("w_out", [D, D])
    g1 = di("g1", [2, 128, D])
    modc = di("modc", [128, 16, 4])
    w_r = di("w_r", [128, 16, 32])
    b_r = di("b_r", [128, 32])
    xmid = do("xmid", [TOK_PC, D])
    fT = do("fT", [D, TOK_PC], BF16)
    gates = do("gates", [TOK_PC, 32])

    P = Prog(nc)
    NF = 5
    psf = [P.ps(f"psf{i}", [128, 512], F32) for i in range(NF)]; b_psf = [P.buf() for _ in range(NF)]
    psb = [P.ps(f"psb{i}", [128, 512], BF16) for i in range(2)]; b_psb = [P.buf() for _ in range(2)]
    cnt = {"f": 0, "b": 0}

    def nf():
        i = cnt["f"] % NF; cnt["f"] += 1
        return psf[i], b_psf[i]

    def nb():
        i = cnt["b"] % 2; cnt["b"] += 1
        return psb[i], b_psb[i]

    ident = P.sb("identt", [128, 128], F32); b_id = P.buf()
    identb = P.sb("identb", [128, 128], BF16); b_idb = P.buf()
    P.dma("sp", "ident", lambda e: e.dma_start(out=ident[:], in_=ident_d), writes=[b_id])
    P.op("dve", lambda e: e.tensor_copy(out=identb[:], in_=ident[:]), reads=[b_id], writes=[b_idb])
    vt = P.sb("vecs_t", [128, 5, 512], F32); b_vt = P.buf()
    P.dma("sp", "vecs", lambda e: e.dma_start(out=vt[:], in_=vecs), writes=[b_vt])
    g1t = P.sb("g1t", [128, 2, D], F32); b_g1 = P.buf()
    P.dma("sp", "g1", lambda e: e.dma_start(out=g1t[:], in_=g1.rearrange("r p d -> p r d")), writes=[b_g1])
    mc = P.sb("mc", [128, 16, 4], F32); b_mc = P.buf()
    P.dma("sp", "mc", lambda e: e.dma_start(out=mc[:], in_=modc), writes=[b_mc])
    P.op("dve", lambda e: e.tensor_scalar(out=mc[:, :, 1], in0=mc[:, :, 1], scalar1=1.0, scalar2=None, op0=ALU.add), reads=[b_mc], writes=[b_mc])
    P.op("dve", lambda e: e.tensor_scalar(out=mc[:, :, 3], in0=mc[:, :, 3], scalar1=1.0, scalar2=None, op0=ALU.add), reads=[b_mc], writes=[b_mc])
    wr = P.sb("wr", [128, 16, 32], F32); b_wr = P.buf()
    P.dma("sp", "wr", lambda e: e.dma_start(out=wr[:], in_=w_r), writes=[b_wr])
    brt = P.sb("brt", [128, 32], F32); b_br = P.buf()
    P.dma("sp", "brt", lambda e: e.dma_start(out=brt[:], in_=b_r), writes=[b_br])
    wst = [P.sb(f"wst{i}", [128, 4, 512], F32) for i in range(2)]; b_wst = [P.buf() for _ in range(2)]
    wg = P.sb("wg", [128, 4, 512], BF16); b_wg = P.buf()
    P.dma("sp", "wst0", lambda e: e.dma_start(out=wst[0][:], in_=w_glu.rearrange("(k p) n -> p k n", p=128)), writes=[b_wst[0]])
    P.op("pool", lambda e: e.tensor_copy(out=wg[:], in_=wst[0][:]), reads=[b_wst[0]], writes=[b_wg])

    mixT = P.sb("mixT", [128, 16, TOK_PC], BF16); b_mixT = [P.buf() for _ in TILES_PC]
    rgt = P.sb("rgt", [128, 4, 512], F32); b_rg = P.buf()
    s5t = P.sb("s5t", [128, 3, 512], F32); b_s5 = P.buf()
    att_t = P.sb("att_t", [128, 512], F32); b_att = P.buf()
    cvt = P.sb("cvt", [128, 7, 512], F32); b_cv = P.buf()
    tm = [P.sb(f"tm{i}", [128, 512], F32) for i in range(5)]; b_tm = [P.buf() for _ in range(5)]
    yb16 = P.sb("yb16", [128, 512], BF16); b_yb16 = P.buf()
    yT = P.sb("yT", [128, 4, 128], BF16); b_yT = P.buf()
    mix = P.sb("mix", [128, D], BF16); b_mix = [P.buf() for _ in range(4)]

    def tt(eng, o, a, b, op, rd, wr_):
        P.op(eng, lambda e: e.tensor_tensor(out=o, in0=a, in1=b, op=op), reads=rd, writes=wr_)

    for ti, (r0, n) in enumerate(TILES_PC):
        P.dma("sp", "rgt", lambda e, r0=r0, n=n: e.dma_start(out=rgt[0:n], in_=rg[r0:r0+n]), writes=[b_rg])
        P.dma("sp", "s5t", lambda e, r0=r0, n=n: e.dma_start(out=s5t[0:n], in_=s5[r0:r0+n]), writes=[b_s5])
        P.dma("sp", "att_t", lambda e, r0=r0, n=n: e.dma_start(out=att_t[0:n], in_=att[r0:r0+n]), writes=[b_att])
        P.dma("sp", "cvt", lambda e, r0=r0, n=n: e.dma_start(out=cvt[0:n], in_=cv[r0:r0+n]), writes=[b_cv])
        P.op("act", lambda e, n=n: e.activation(out=tm[0][0:n], in_=rgt[0:n, 2, :], func=AF.Silu), reads=[b_rg], writes=[b_tm[0]])
        P.op("act", lambda e, n=n: e.activation(out=tm[1][0:n], in_=rgt[0:n, 3, :], func=AF.Silu), reads=[b_rg], writes=[b_tm[1]])
        tt("dve", tm[0][0:n], tm[0][0:n], rgt[0:n, 0, :], ALU.mult, [b_tm[0], b_rg], [b_tm[0]])
        tt("pool", tm[1][0:n], tm[1][0:n], rgt[0:n, 1, :], ALU.mult, [b_tm[1], b_rg], [b_tm[1]])
        tt("dve", mix[0:n, 0:512], tm[0][0:n], tm[1][0:n], ALU.add, [b_tm[0], b_tm[1]], [b_mix[0]])
        tt("pool", tm[2][0:n], s5t[0:n, 0, :], s5t[0:n, 1, :], ALU.add, [b_s5], [b_tm[2]])
        tt("dve", tm[3][0:n], s5t[0:n, 2, :], vt[0:n, 0, :], ALU.mult, [b_s5, b_vt], [b_tm[3]])
        tt("pool", tm[2][0:n], tm[2][0:n], tm[3][0:n], ALU.add, [b_tm[2], b_tm[3]], [b_tm[2]])
        tt("pool", tm[3][0:n], tm[2][0:n], tm[2][0:n], ALU.mult, [b_tm[2]], [b_tm[3]])
        P.op("dve", lambda e, n=n: e.tensor_scalar(out=tm[3][0:n], in0=tm[3][0:n], scalar1=0.044715, scalar2=1.0, op0=ALU.mult, op1=ALU.add), reads=[b_tm[3]], writes=[b_tm[3]])
        tt("dve", tm[3][0:n], tm[3][0:n], tm[2][0:n], ALU.mult, [b_tm[3], b_tm[2]], [b_tm[3]])
        P.op("act", lambda e, n=n: e.activation(out=tm[3][0:n], in_=tm[3][0:n], func=AF.Sigmoid, scale=1.5957691216057308), reads=[b_tm[3]], writes=[b_tm[3]])
        tt("dve", tm[2][0:n], tm[2][0:n], tm[3][0:n], ALU.mult, [b_tm[2], b_tm[3]], [b_tm[2]])
        P.op("act", lambda e, n=n: e.copy(out=yb16[0:n], in_=tm[2][0:n]), reads=[b_tm[2]], writes=[b_yb16])
        pt_, bpt_ = nb()
        for k in range(4):
            P.op("pe", lambda e, pt_=pt_, k=k, n=n: e.transpose(pt_[:, k*128:k*128+n], yb16[0:n, k*128:(k+1)*128], identb[0:n, 0:n]), reads=[b_yb16, b_idb], writes=[bpt_])
        P.op("act", lambda e, pt_=pt_, n=n: e.copy(out=yT[:, :, 0:n], in_=pt_[:, 0:512].rearrange("p (a d) -> p a d", a=4)[:, :, 0:n]), reads=[bpt_], writes=[b_yT])
        pg, bpg = nf()
        for k in range(4):
            P.op("pe", lambda e, pg=pg, k=k, n=n: e.matmul(pg[0:n, :], yT[:, k, 0:n], wg[:, k, :], start=(k == 0), stop=(k == 3)), reads=[b_yT, b_wg], writes=[bpg])
        tt("dve", tm[3][0:n], pg[0:n, :], vt[0:n, 1, :], ALU.add, [bpg, b_vt], [b_tm[3]])
        P.op("act", lambda e, n=n: e.activation(out=tm[3][0:n], in_=tm[3][0:n], func=AF.Sigmoid), reads=[b_tm[3]], writes=[b_tm[3]])
        tt("dve", mix[0:n, 512:1024], tm[2][0:n], tm[3][0:n], ALU.mult, [b_tm[2], b_tm[3]], [b_mix[1]])
        P.op("act", lambda e, n=n: e.copy(out=mix[0:n, 1024:1536], in_=att_t[0:n]), reads=[b_att], writes=[b_mix[2]])
        tt("pool", tm[0][0:n], cvt[0:n, 1, :], cvt[0:n, 2, :], ALU.mult, [b_cv], [b_tm[0]])
        tt("dve", tm[1][0:n], cvt[0:n, 3, :], cvt[0:n, 4, :], ALU.mult, [b_cv], [b_tm[1]])
        tt("pool", tm[4][0:n], cvt[0:n, 5, :], cvt[0:n, 6, :], ALU.mult, [b_cv], [b_tm[4]])
        tt("dve", tm[0][0:n], tm[0][0:n], vt[0:n, 2, :], ALU.mult, [b_tm[0], b_vt], [b_tm[0]])
        tt("pool", tm[1][0:n], tm[1][0:n], vt[0:n, 3, :], ALU.mult, [b_tm[1], b_vt], [b_tm[1]])
        tt("dve", tm[4][0:n], tm[4][0:n], vt[0:n, 4, :], ALU.mult, [b_tm[4], b_vt], [b_tm[4]])
        tt("pool", tm[0][0:n], tm[0][0:n], tm[1][0:n], ALU.add, [b_tm[0], b_tm[1]], [b_tm[0]])
        tt("dve", tm[0][0:n], tm[0][0:n], tm[4][0:n], ALU.add, [b_tm[0], b_tm[4]], [b_tm[0]])
        tt("dve", mix[0:n, 1536:2048], tm[0][0:n], cvt[0:n, 0, :], ALU.mult, [b_tm[0], b_cv], [b_mix[3]])
        for kg in range(4):
            pt_, bpt_ = nb()
            for kk in range(4):
                k = kg * 4 + kk
                P.op("pe", lambda e, pt_=pt_, kk=kk, k=k, n=n: e.transpose(pt_[:, kk*128:kk*128+n], mix[0:n, k*128:(k+1)*128], identb[0:n, 0:n]), reads=[b_mix[kg], b_idb], writes=[bpt_])
            eng = "act" if kg % 2 else "dve"
            if eng == "act":
                P.op("act", lambda e, pt_=pt_, kg=kg, n=n, r0=r0: e.copy(out=mixT[:, kg*4:(kg+1)*4, r0:r0+n], in_=pt_[:, 0:512].rearrange("p (a d) -> p a d", a=4)[:, :, 0:n]), reads=[bpt_], writes=[b_mixT[ti]])
            else:
                P.op("dve", lambda e, pt_=pt_, kg=kg, n=n, r0=r0: e.tensor_copy(out=mixT[:, kg*4:(kg+1)*4, r0:r0+n], in_=pt_[:, 0:512].rearrange("p (a d) -> p a d", a=4)[:, :, 0:n]), reads=[bpt_], writes=[b_mixT[ti]])

    wb = [P.sb(f"wb{i}", [128, 16, 512], BF16) for i in range(2)]; b_wb = [P.buf() for _ in range(2)]
    xp = [P.sb(f"xp{i}", [128, 512], F32) for i in range(3)]; b_xp = [P.buf() for _ in range(3)]
    b_xm_dram = [P.buf() for _ in TILES_PC]
    w_v = w_out.rearrange("(k p) n -> p k n", p=128)
    wsi = 1; xi = 0
    for cb in range(4):
        s = cb % 2
        for kq in range(4):
            ws_ = wsi % 2; wsi += 1
            P.dma("sp", f"wst{ws_}", lambda e, ws_=ws_, cb=cb, kq=kq: e.dma_start(out=wst[ws_][:], in_=w_v[:, kq*4:(kq+1)*4, cb*512:(cb+1)*512]), writes=[b_wst[ws_]])
            P.op("pool", lambda e, ws_=ws_, s=s, kq=kq: e.tensor_copy(out=wb[s][:, kq*4:(kq+1)*4, :], in_=wst[ws_][:]), reads=[b_wst[ws_]], writes=[b_wb[s]])
        for ti, (r0, n) in enumerate(TILES_PC):
            isctx = 1 if r0 >= LAT_PC else 0
            xs_ = xi % 3; xi += 1
            P.dma("sp", f"xp{xs_}", lambda e, xs_=xs_, r0=r0, n=n, cb=cb: e.dma_start(out=xp[xs_][0:n], in_=x[r0:r0+n, cb*512:(cb+1)*512]), writes=[b_xp[xs_]])
            po, bpo = nf()
            for k in range(16):
                P.op("pe", lambda e, po=po, k=k, n=n, r0=r0, s=s: e.matmul(po[0:n, :], mixT[:, k, r0:r0+n], wb[s][:, k, :], start=(k == 0), stop=(k == 15)), reads=[b_mixT[ti], b_wb[s]], writes=[bpo])
            t_ = tm[xi % 2]; bt_ = b_tm[xi % 2]
            tt("dve", t_[0:n], po[0:n, :], g1t[0:n, isctx, cb*512:(cb+1)*512], ALU.mult, [bpo, b_g1], [bt_])
            tt("pool", xp[xs_][0:n], xp[xs_][0:n], t_[0:n], ALU.add, [b_xp[xs_], bt_], [b_xp[xs_]])
            P.dma("sp", f"xpo{xs_}", lambda e, xs_=xs_, r0=r0, n=n, cb=cb: e.dma_start(out=xmid[r0:r0+n, cb*512:(cb+1)*512], in_=xp[xs_][0:n]), reads=[b_xp[xs_]], writes=[b_xm_dram[ti]])

    xt = [P.sb(f"xt{i}", [128, D], F32) for i in range(1)]; b_xt = [P.buf() for _ in range(1)]
    xn = P.sb("xn", [128, D], F32); b_xn = P.buf()
    junk = mix
    ss = P.sb("ss", [128, 2], F32); b_ss = P.buf()
    f32t = P.sb("f32t", [128, 16, 128], F32); b_f32 = P.buf()
    lg = P.sb("lg", [128, 32], F32); b_lg = P.buf()
    rc = P.sb("rc", [128, 16], F32); b_rc = P.buf()
    ex = P.sb("ex", [128, 32], F32); b_ex = P.buf()
    gt = [P.sb(f"gt{i}", [128, 32], F32) for i in range(2)]; b_gt = [P.buf() for _ in range(2)]
    fT_v = fT.rearrange("(k p) t -> p k t", p=128)
    for ti, (r0, n) in enumerate(TILES_PC):
        s = ti % 2
        isctx = 1 if r0 >= LAT_PC else 0
        X = xt[0]; bX = b_xt[0]
        P.dma("sp", "xt0", lambda e, X=X, r0=r0, n=n: e.dma_start(out=X[0:n, :], in_=xmid[r0:r0+n, :]), reads=[b_xm_dram[ti]], writes=[bX])
        P.op("act", lambda e, X=X, n=n: e.activation(out=junk[0:n, :], in_=X[0:n, :], func=AF.Square, accum_out=ss[0:n, 0:1]), reads=[bX], writes=b_mix + [b_ss])
        P.op("act", lambda e, n=n: e.activation(out=ss[0:n, 1:2], in_=ss[0:n, 0:1], func=AF.Sqrt, scale=1.0 / D, bias=EPS), reads=[b_ss], writes=[b_ss])
        P.op("dve", lambda e, n=n: e.reciprocal(out=ss[0:n, 1:2], in_=ss[0:n, 1:2]), reads=[b_ss], writes=[b_ss])
        P.op("dve", lambda e, X=X, n=n: e.tensor_scalar(out=xn[0:n, :], in0=X[0:n, :], scalar1=ss[0:n, 1:2], scalar2=None, op0=ALU.mult), reads=[bX, b_ss], writes=[b_xn])
        for kg in range(4):
            pt_, bpt_ = nf()
            for kk in range(4):
                k = kg * 4 + kk
                P.op("pe", lambda e, pt_=pt_, kk=kk, k=k, n=n: e.transpose(pt_[:, kk*128:kk*128+n], xn[0:n, k*128:(k+1)*128], ident[0:n, 0:n]), reads=[b_xn, b_id], writes=[bpt_])
            for kk in range(4):
                k = kg * 4 + kk
                if kk % 2 == 0:
                    P.op("dve", lambda e, pt_=pt_, kk=kk, k=k, n=n, isctx=isctx: e.tensor_scalar(
                        out=f32t[:, k, 0:n], in0=pt_[:, kk*128:kk*128+n], scalar1=mc[:, k, 2*isctx+1:2*isctx+2], scalar2=mc[:, k, 2*isctx:2*isctx+1], op0=ALU.mult, op1=ALU.add),
                        reads=[bpt_, b_mc], writes=[b_f32])
                else:
                    P.op("act", lambda e, pt_=pt_, kk=kk, k=k, n=n, isctx=isctx: e.activation(
                        out=f32t[:, k, 0:n], in_=pt_[:, kk*128:kk*128+n], func=AF.Identity, scale=mc[:, k, 2*isctx+1:2*isctx+2], bias=mc[:, k, 2*isctx:2*isctx+1]),
                        reads=[bpt_, b_mc], writes=[b_f32])
        P.op("pool", lambda e, n=n, r0=r0: e.tensor_copy(out=mixT[:, :, r0:r0+n], in_=f32t[:, :, 0:n]), reads=[b_f32], writes=[b_mixT[ti]])
        pl, bpl = nf()
        for k in range(16):
            P.op("pe", lambda e, pl=pl, k=k, n=n: e.matmul(pl[0:n, 0:32], f32t[:, k, 0:n], wr[:, k, :], start=(k == 0), stop=(k == 15)), reads=[b_f32, b_wr], writes=[bpl])
        tt("dve", lg[0:n], pl[0:n, 0:32], brt[0:n], ALU.add, [bpl, b_br], [b_lg])
        P.op("dve", lambda e, n=n: e.max(out=rc[0:n, 0:8], in_=lg[0:n]), reads=[b_lg], writes=[b_rc])
        P.op("dve", lambda e, n=n: e.tensor_scalar(out=rc[0:n, 8:9], in0=rc[0:n, 0:1], scalar1=-1.0, scalar2=None, op0=ALU.mult), reads=[b_rc], writes=[b_rc])
        P.op("act", lambda e, n=n: e.activation(out=ex[0:n], in_=lg[0:n], func=AF.Exp, bias=rc[0:n, 8:9], scale=1.0), reads=[b_lg, b_rc], writes=[b_ex])
        P.op("dve", lambda e, n=n: e.tensor_scalar(out=lg[0:n], in0=lg[0:n], scalar1=rc[0:n, 3:4], scalar2=None, op0=ALU.is_ge), reads=[b_lg, b_rc], writes=[b_lg])
        tt("dve", ex[0:n], ex[0:n], lg[0:n], ALU.mult, [b_ex, b_lg], [b_ex])
        P.op("dve", lambda e, n=n: e.reduce_sum(out=rc[0:n, 9:10], in_=ex[0:n], axis=AX.X), reads=[b_ex], writes=[b_rc])
        P.op("dve", lambda e, n=n: e.reciprocal(out=rc[0:n, 10:11], in_=rc[0:n, 9:10]), reads=[b_rc], writes=[b_rc])
        P.op("dve", lambda e, n=n, s=s: e.tensor_scalar(out=gt[s][0:n], in0=ex[0:n], scalar1=rc[0:n, 10:11], scalar2=None, op0=ALU.mult), reads=[b_ex, b_rc], writes=[b_gt[s]])
        P.dma("sp", f"gto{s}", lambda e, s=s, n=n, r0=r0: e.dma_start(out=gates[r0:r0+n, :], in_=gt[s][0:n]), reads=[b_gt[s]])
    for k in range(16):
        P.dma("sp", "f16o", lambda e, k=k: e.dma_start(out=fT_v[:, k, :], in_=mixT[:, k, :]), reads=b_mixT)
    P.emit()
    return nc


def _shift(a, k):
    out = np.zeros_like(a)
    if k == -1:
        out[1:] = a[:-1]
    elif k == 1:
        out[:-1] = a[1:]
    return out


def run_out(x_shards, z_all, ret, s5y, att, mod_l, prm):
    C = mix_consts()
    nc = build_out()
    lat = lambda a: a[CTX:]
    ctx = lambda a: a[:CTX]
    sh = lambda a: shard_tokens(lat(a), ctx(a))
    gf = z_all[:, 1536:2048]; gb = z_all[:, 2048:2560]
    rg_s = sh(np.stack([ret[0], ret[1], gf, gb], axis=1))
    s5_s = sh(np.stack([s5y[0], s5y[1], z_all[:, 2560:3072]], axis=1))
    att_s = sh(att)
    zc = z_all[:, 4096:5632]
    bg, cg, hh = zc[:, 0:512], zc[:, 512:1024], zc[:, 1024:1536]

    def sh3(a):
        parts = []
        for k in (-1, 0, 1):
            parts.append(np.concatenate([_shift(ctx(a), k), _shift(lat(a), k)], axis=0) if k else a)
        return parts
    cs_, hs_ = sh3(cg), sh3(hh)
    cv_s = sh(np.stack([bg, cs_[0], hs_[0], cs_[1], hs_[1], cs_[2], hs_[2]], axis=1))
    rep = lambda v: np.broadcast_to(v, (128,) + v.shape)
    vecs = np.ascontiguousarray(np.stack([rep(prm["s5_d"]), rep(prm["s5_b_glu"]), rep(prm["conv_w"][0]), rep(prm["conv_w"][1]), rep(prm["conv_w"][2])], axis=1))
    g1 = np.ascontiguousarray(np.stack([rep(mod_l[0, 2*D:3*D]), rep(mod_l[1, 2*D:3*D])]))
    modc = np.ascontiguousarray(np.stack([cols128(mod_l[0, 3*D:4*D]), cols128(mod_l[0, 4*D:5*D]), cols128(mod_l[1, 3*D:4*D]), cols128(mod_l[1, 4*D:5*D])], axis=-1))
    b_r = np.ascontiguousarray(rep(prm["b_router"]))
    in_maps = []
    for c in range(NCORE):
        in_maps.append({"ident": C["ident"], "x": x_shards[c], "rg": rg_s[c], "s5": s5_s[c], "att": att_s[c], "cv": cv_s[c],
                        "vecs": vecs, "w_glu": prm["s5_w_glu"], "w_out": prm["w_out"], "g1": g1, "modc": modc,
                        "w_r": np.ascontiguousarray(prm["w_router"].reshape(16, 128, 32).transpose(1, 0, 2)), "b_r": b_r})
    res = _run(nc, in_maps)
    return [r["xmid"] for r in res], [r["fT"] for r in res], [r["gates"] for r in res]


E_PC = 8
NCORE_E = 32 // E_PC
DE = 1024
E_TILES = [(i * 512, 512) for i in range(16)] + [(8192, 256)]


def build_moe():
    nc = bass.Bass("TRN2", target_bir_lowering=False)
    di = lambda name, shape, dt=F32: nc.dram_tensor(name, list(shape), dt, kind="ExternalInput").ap()
    fT = di("fT", [D, T_ALL], BF16)
    gb = di("gb", [E_PC, 128, T_ALL])
    wg = di("wg", [E_PC, D, DE]); wu = di("wu", [E_PC, D, DE]); wd = di("wd", [E_PC, DE, D])
    bgu = di("bgu", [128, E_PC, 16]); bd = di("bd", [128, E_PC, 16])
    yT = nc.dram_tensor("yT", [D, T_ALL], F32, kind="ExternalOutput").ap()
    P = Prog(nc)
    NF = 8
    psf = [P.ps(f"psf{i}", [128, 512], F32) for i in range(NF)]; b_psf = [P.buf() for _ in range(NF)]
    cnt = {"f": 0}

    def nf():
        i = cnt["f"] % NF; cnt["f"] += 1
        return psf[i], b_psf[i]

    bgut = P.sb("bgut", [128, E_PC, 16], F32); b_bgu = P.buf()
    bdt = P.sb("bdt", [128, E_PC, 16], F32); b_bd = P.buf()
    P.dma("sp", "bgu", lambda e: e.dma_start(out=bgut[:], in_=bgu), writes=[b_bgu])
    P.dma("sp", "bd", lambda e: e.dma_start(out=bdt[:], in_=bd), writes=[b_bd])
    wgb = P.sb("wgb", [128, 16, DE], BF16); wub = P.sb("wub", [128, 16, DE], BF16); wdb = P.sb("wdb", [128, 8, D], BF16)
    b_wgb = P.buf(); b_wub = P.buf(); b_wdb = P.buf()
    wst = [P.sb(f"wst{i}", [128, 4, 512], F32) for i in range(2)]; b_wst = [P.buf() for _ in range(2)]
    ft = [P.sb(f"ft{i}", [128, 16, 512], BF16) for i in range(2)]; b_ft = [P.buf() for _ in range(2)]
    gtile = [P.sb(f"gtile{i}", [128, 512], F32) for i in range(2)]; b_gtile = [P.buf() for _ in range(2)]
    actT = P.sb("actT", [128, 8, 512], BF16); b_actT = P.buf()
    tg = [P.sb(f"tg{i}", [128, 512], F32) for i in range(2)]; b_tg = [P.buf() for _ in range(2)]
    tsg = [P.sb(f"tsg{i}", [128, 512], F32) for i in range(2)]; b_tsg = [P.buf() for _ in range(2)]
    tu = [P.sb(f"tu{i}", [128, 512], F32) for i in range(2)]; b_tu = [P.buf() for _ in range(2)]
    yp = [P.sb(f"yp{i}", [128, 512], F32) for i in range(3)]; b_yp = [P.buf() for _ in range(3)]
    yo = [P.sb(f"yo{i}", [128, 512], F32) for i in range(3)]; b_yo = [P.buf() for _ in range(3)]
    b_dram = [[P.buf() for _ in E_TILES] for _ in range(16)]
    fT_v = fT.rearrange("(k p) t -> p k t", p=128)
    yT_v = yT.rearrange("(m p) t -> p m t", p=128)
    wsi = 0; fi = 0; oi = 0; mi = 0
    ne = globals().get("MOE_NE", E_PC)
    for ex in range(ne):
        for (src, dst, bdst, nk, ncol) in ((wg, wgb, b_wgb, 16, DE), (wu, wub, b_wub, 16, DE), (wd, wdb, b_wdb, 8, D)):
            sv = src[ex].rearrange("(k p) n -> p k n", p=128)
            for kq in range(nk // 4):
                for cbk in range(ncol // 512):
                    ws_ = wsi % 2; wsi += 1
                    P.dma("sp", f"wst{ws_}_{ex % 2}", lambda e, ws_=ws_, sv=sv, kq=kq, cbk=cbk: e.dma_start(out=wst[ws_][:], in_=sv[:, kq*4:(kq+1)*4, cbk*512:(cbk+1)*512]), writes=[b_wst[ws_]])
                    eng = "pool" if wsi % 2 else "act"
                    if eng == "pool":
                        P.op("pool", lambda e, ws_=ws_, dst=dst, kq=kq, cbk=cbk: e.tensor_copy(out=dst[:, kq*4:(kq+1)*4, cbk*512:(cbk+1)*512], in_=wst[ws_][:]), reads=[b_wst[ws_]], writes=[bdst])
                    else:
                        P.op("act", lambda e, ws_=ws_, dst=dst, kq=kq, cbk=cbk: e.copy(out=dst[:, kq*4:(kq+1)*4, cbk*512:(cbk+1)*512], in_=wst[ws_][:]), reads=[b_wst[ws_]], writes=[bdst])
        for tix, (c0, w) in enumerate(E_TILES):
            fs = fi % 2; fi += 1
            for kq in range(4):
                P.dma("sp", f"ft{fs}_{ex % 2}", lambda e, fs=fs, c0=c0, w=w, kq=kq: e.dma_start(out=ft[fs][:, kq*4:(kq+1)*4, 0:w], in_=fT_v[:, kq*4:(kq+1)*4, c0:c0+w]), writes=[b_ft[fs]])
            P.dma("sp", f"gtile{fs}_{ex % 2}", lambda e, fs=fs, c0=c0, w=w, ex=ex: e.dma_start(out=gtile[fs][:, 0:w], in_=gb[ex, :, c0:c0+w]), writes=[b_gtile[fs]])
            for m in range(8):
                psg, bpsg = nf(); psu, bpsu = nf()
                for k in range(16):
                    P.op("pe", lambda e, psg=psg, k=k, m=m, fs=fs, w=w: e.matmul(psg[:, 0:w], wgb[:, k, m*128:(m+1)*128], ft[fs][:, k, 0:w], start=(k == 0), stop=(k == 15)), reads=[b_wgb, b_ft[fs]], writes=[bpsg])
                for k in range(16):
                    P.op("pe", lambda e, psu=psu, k=k, m=m, fs=fs, w=w: e.matmul(psu[:, 0:w], wub[:, k, m*128:(m+1)*128], ft[fs][:, k, 0:w], start=(k == 0), stop=(k == 15)), reads=[b_wub, b_ft[fs]], writes=[bpsu])
                s = mi % 2; mi += 1
                P.op("dve", lambda e, psg=psg, s=s, m=m, w=w, ex=ex: e.tensor_scalar(out=tg[s][:, 0:w], in0=psg[:, 0:w], scalar1=bgut[:, ex, m:m+1], scalar2=7.0, op0=ALU.add, op1=ALU.min), reads=[bpsg, b_bgu], writes=[b_tg[s]])
                P.op("act", lambda e, s=s, w=w: e.activation(out=tsg[s][:, 0:w], in_=tg[s][:, 0:w], func=AF.Sigmoid, scale=1.702), reads=[b_tg[s]], writes=[b_tsg[s]])
                P.op("dve", lambda e, psu=psu, s=s, m=m, w=w, ex=ex: e.tensor_scalar(out=tu[s][:, 0:w], in0=psu[:, 0:w], scalar1=bgut[:, ex, 8+m:9+m], scalar2=7.0, op0=ALU.add, op1=ALU.min), reads=[bpsu, b_bgu], writes=[b_tu[s]])
                P.op("dve", lambda e, s=s, w=w: e.tensor_scalar(out=tu[s][:, 0:w], in0=tu[s][:, 0:w], scalar1=-7.0, scalar2=1.0, op0=ALU.max, op1=ALU.add), reads=[b_tu[s]], writes=[b_tu[s]])
                P.op("pool", lambda e, s=s, w=w: e.tensor_tensor(out=tg[s][:, 0:w], in0=tg[s][:, 0:w], in1=tsg[s][:, 0:w], op=ALU.mult), reads=[b_tg[s], b_tsg[s]], writes=[b_tg[s]])
                P.op("dve", lambda e, s=s, m=m, w=w: e.tensor_tensor(out=actT[:, m, 0:w], in0=tg[s][:, 0:w], in1=tu[s][:, 0:w], op=ALU.mult), reads=[b_tg[s], b_tu[s]], writes=[b_actT])
            for m2 in range(16):
                psy, bpsy = nf()
                for k in range(8):
                    P.op("pe", lambda e, psy=psy, k=k, m2=m2, w=w: e.matmul(psy[:, 0:w], wdb[:, k, m2*128:(m2+1)*128], actT[:, k, 0:w], start=(k == 0), stop=(k == 7)), reads=[b_wdb, b_actT], writes=[bpsy])
                os_ = oi % 3; oi += 1
                bdr = b_dram[m2][tix]
                if ex > 0:
                    P.dma("sp", f"yp{os_}_{ex % 2}", lambda e, os_=os_, m2=m2, c0=c0, w=w: e.dma_start(out=yp[os_][:, 0:w], in_=yT_v[:, m2, c0:c0+w]), reads=[bdr], writes=[b_yp[os_]])
                P.op("dve", lambda e, psy=psy, os_=os_, m2=m2, w=w, fs=fs, ex=ex: e.scalar_tensor_tensor(out=yo[os_][:, 0:w], in0=psy[:, 0:w], scalar=bdt[:, ex, m2:m2+1], in1=gtile[fs][:, 0:w], op0=ALU.add, op1=ALU.mult),
                     reads=[bpsy, b_bd, b_gtile[fs]], writes=[b_yo[os_]])
                if ex > 0:
                    P.op("pool", lambda e, os_=os_, w=w: e.tensor_tensor(out=yo[os_][:, 0:w], in0=yo[os_][:, 0:w], in1=yp[os_][:, 0:w], op=ALU.add), reads=[b_yo[os_], b_yp[os_]], writes=[b_yo[os_]])
                P.dma("sp", f"yo{os_}_{ex % 2}", lambda e, os_=os_, m2=m2, c0=c0, w=w: e.dma_start(out=yT_v[:, m2, c0:c0+w], in_=yo[os_][:, 0:w]), reads=[b_yo[os_]], writes=[bdr])
    P.emit()
    return nc


def run_moe(fT_shards, gate_shards, prm):
    nc = build_moe()
    fT = np.ascontiguousarray(np.concatenate(fT_shards, axis=1))
    gates = np.concatenate(gate_shards, axis=0)
    in_maps = []
    for c in range(NCORE_E):
        es = slice(E_PC * c, E_PC * (c + 1))
        wgu = prm["w_gate_up"][es]
        bgu = prm["b_gate_up"][es]
        bg = bgu[:, 0::2].reshape(E_PC, 8, 128); bu = bgu[:, 1::2].reshape(E_PC, 8, 128)
        bgu_l = np.ascontiguousarray(np.concatenate([bg, bu], axis=1).transpose(2, 0, 1))
        bd_l = np.ascontiguousarray(prm["b_down"][es].reshape(E_PC, 16, 128).transpose(2, 0, 1))
        gbc = np.ascontiguousarray(np.broadcast_to(gates[:, es].T[:, None, :], (E_PC, 128, T_ALL)))
        in_maps.append({"fT": fT, "gb": gbc, "wg": np.ascontiguousarray(wgu[:, :, 0::2]), "wu": np.ascontiguousarray(wgu[:, :, 1::2]),
                        "wd": prm["w_down"][es], "bgu": bgu_l, "bd": bd_l})
    res = _run(nc, in_maps)
    parts = []
    for cp in range(NCORE):
        parts.append(np.ascontiguousarray(np.stack([res[c]["yT"][:, cp*TOK_PC:(cp+1)*TOK_PC].T for c in range(NCORE_E)], axis=0)))
    return parts


_LAYER_KEYS = ["w_in", "w_out", "s5_a_re", "s5_a_im", "s5_log_step", "s5_b_re", "s5_b_im", "s5_c_re", "s5_c_im",
               "s5_d", "s5_w_glu", "s5_b_glu", "q_norm_w", "k_norm_w", "conv_w", "w_router", "b_router",
               "w_gate_up", "b_gate_up", "w_down", "b_down"]


def _combine_maps(x_shards, parts, g2pair):
    rep = lambda v: np.broadcast_to(v, (128, D))
    g2 = np.ascontiguousarray(np.stack([rep(g2pair[0]), rep(g2pair[1])]))
    return g2


def run_proj2(x_shards, mod_l, w_in_l, parts=None, g2pair=None, project=True):
    combine = parts is not None
    nc = build_proj(combine, project)
    ident = np.eye(128, dtype=np.float32)
    in_maps = []
    for i in range(NCORE):
        m = {"x": x_shards[i], "ident": ident}
        if project:
            m["modc"] = np.ascontiguousarray(np.stack([cols128(mod_l[0, 0:D]), cols128(mod_l[0, D:2*D]), cols128(mod_l[1, 0:D]), cols128(mod_l[1, D:2*D])], axis=-1))
            m["w_in"] = w_in_l
        if combine:
            m["part"] = parts[i]
            m["g2"] = _combine_maps(x_shards, parts, g2pair)
        in_maps.append(m)
    res = _run(nc, in_maps)
    z = [r["z"] for r in res] if project else None
    xo = [r["xo"] for r in res] if combine else None
    return z, xo


def kernel(**inputs):
    inp = {k: np.asarray(v) for k, v in inputs.items()}
    mod = run_mod(inp["c"], inp["c_ctx"], inp["w_mod"], inp["b_mod"])
    x_shards = shard_tokens(inp["x"][0], inp["ctx"][0])
    parts = None
    g2pair = None
    for l in range(2):
        prm = {k: inp[k][l] for k in _LAYER_KEYS}
        z, xo = run_proj2(x_shards, mod[l], prm["w_in"], parts, g2pair)
        if xo is not None:
            x_shards = xo
        zl, zc = unshard_tokens(z)
        z_all = np.concatenate([zc, zl], axis=0)
        ret, s5y, att = run_mix(z_all, prm)
        xmid, fT, gates = run_out(x_shards, z_all, ret, s5y, att, mod[l], prm)
        parts = run_moe(fT, gates, prm)
        x_shards = xmid
        g2pair = (mod[l][0, 5*D:6*D], mod[l][1, 5*D:6*D])
    _, xo = run_proj2(x_shards, None, None, parts, g2pair, project=False)
    lat, _ = unshard_tokens(xo)
    return lat[None].astype(np.float32)
```

```python
import contextlib
import numpy as np
import ml_dtypes
import concourse.bass as bass
import concourse.mybir as mybir
from concourse.bass_utils import run_bass_kernel_spmd

F32 = mybir.dt.float32
BF16 = mybir.dt.bfloat16
I32 = mybir.dt.int32
ALU = mybir.AluOpType
AF = mybir.ActivationFunctionType
AX = mybir.AxisListType


class Buf:
    __slots__ = ("name", "w", "r")

    def __init__(self, name):
        self.name = name
        self.w = None
        self.r = []


class Prog:
    ENG = ("pe", "dve", "act", "pool", "sp")

    def __init__(self, nc):
        self.nc = nc
        self.stream = {e: [] for e in self.ENG}
        self.seen = {e: {} for e in self.ENG}
        self.needed = {e: set() for e in self.ENG}
        self.stack = contextlib.ExitStack()
        self.esem = {e: self.stack.enter_context(nc.semaphore("s_" + e))
                     for e in ("pe", "dve", "act", "pool")}
        self.dsem = {}
        self.dtoks = []
        self.nbuf = 0

    def buf(self, name=None):
        self.nbuf += 1
        return Buf(name or f"b{self.nbuf}")

    def sb(self, name, shape, dt):
        return self.stack.enter_context(self.nc.sbuf_tensor(name, list(shape), dt))

    def ps(self, name, shape, dt):
        return self.stack.enter_context(self.nc.psum_tensor(name, list(shape), dt))

    def _waits(self, eng, reads, writes):
        toks = []
        for b in reads:
            if b.w is not None:
                toks.append(b.w + (True,))
        for b in writes:
            if b.w is not None:
                toks.append(b.w + (False,))
            toks.extend(t + (False,) for t in b.r)
        need = {}
        for kind, src, val, raw in toks:
            if kind == "eng" and src == eng and (not raw or eng == "pe"):
                continue
            key = (kind, src)
            if self.seen[eng].get(key, -1) >= val:
                continue
            if need.get(key, -1) < val:
                need[key] = val
        for key, val in need.items():
            self.seen[eng][key] = val
            if key[0] == "eng":
                self.needed[key[1]].add(val)
        return list(need.items())

    def op(self, eng, fn, reads=(), writes=()):
        waits = self._waits(eng, reads, writes)
        idx = len(self.stream[eng])
        tok = ("eng", eng, idx)
        self.stream[eng].append((waits, fn, None))
        for b in reads:
            b.r.append(tok)
        for b in writes:
            b.w = tok
            b.r = []
        return tok

    def dma(self, q, semkey, fn, reads=(), writes=()):
        waits = self._waits(q, reads, writes)
        if semkey not in self.dsem:
            self.dsem[semkey] = [self.stack.enter_context(self.nc.semaphore("d_" + semkey)), 0]
        self.dsem[semkey][1] += 16
        tok = ("dma", semkey, self.dsem[semkey][1])
        self.stream[q].append((waits, fn, semkey))
        for b in reads:
            b.r.append(tok)
        for b in writes:
            b.w = tok
            b.r = []
        self.dtoks.append(tok)
        return tok

    def finish(self):
        fin = Buf("fin")
        for k, (s, v) in self.dsem.items():
            fin.r.append(("dma", k, v))
        for e in ("pe", "dve", "act", "pool"):
            if self.stream[e]:
                fin.r.append(("eng", e, len(self.stream[e]) - 1))
        waits = self._waits("sp", (), (fin,))
        self.stream["sp"].append((waits, None, None))

    def emit(self):
        nc = self.nc
        self.finish()
        val = {}
        for e in self.ENG:
            c = 0
            for idx in range(len(self.stream[e])):
                if idx in self.needed[e]:
                    c += 1
                    val[(e, idx)] = c
        with nc.Block() as block:
            decos = {"pe": block.tensor, "dve": block.vector, "act": block.scalar,
                     "pool": block.gpsimd, "sp": block.sync}
            for ename in self.ENG:
                items = self.stream[ename]

                def body(e, items=items, ename=ename):
                    for idx, (waits, fn, semkey) in enumerate(items):
                        for (kind, src), v in waits:
                            if kind == "eng":
                                e.wait_ge(self.esem[src], val[(src, v)])
                            else:
                                e.wait_ge(self.dsem[src][0], v)
                        if fn is None:
                            continue
                        ins = fn(e)
                        if semkey is not None:
                            ins.then_inc(self.dsem[semkey][0], 16)
                        elif idx in self.needed[ename]:
                            ins.then_inc(self.esem[ename], 1)

                decos[ename](body)
        self.stack.close()


D = 2048
SEQ = 8192
CTX = 256
NCORE = 8
LAT_PC = SEQ // NCORE
CTX_PC = CTX // NCORE
TOK_PC = LAT_PC + CTX_PC
T_ALL = SEQ + CTX
IN_COLS = 5632
EPS = 1e-6
TILES_PC = [(i * 128, 128) for i in range(8)] + [(1024, 32)]
NPART = 8


def _run(nc, in_maps):
    res = run_bass_kernel_spmd(nc, in_maps, core_ids=list(range(len(in_maps))))
    return res.results


def build_mod():
    nc = bass.Bass("TRN2", target_bir_lowering=False)
    cT = nc.dram_tensor("cT", [128, 16, 2], F32, kind="ExternalInput").ap()
    wm = nc.dram_tensor("wm", [2, 2048, 1536], F32, kind="ExternalInput").ap()
    bm = nc.dram_tensor("bm", [2, 2, 1536], F32, kind="ExternalInput").ap()
    out = nc.dram_tensor("mod", [2, 2, 1536], F32, kind="ExternalOutput").ap()
    P = Prog(nc)
    ct = P.sb("ct", [128, 16, 2], F32); b_ct = P.buf()
    av = P.sb("av", [128, 16, 2], F32); b_av = P.buf()
    bt = P.sb("bt", [2, 2, 1536], F32); b_bt = P.buf()
    ot = P.sb("ot", [2, 2, 1536], F32); b_ot = P.buf()
    NW = 4
    wt = [P.sb(f"wt{i}", [128, 1536], F32) for i in range(NW)]; b_wt = [P.buf() for _ in range(NW)]
    pst = [P.ps(f"ps{i}", [128, 512], F32) for i in range(3)]; b_ps = [P.buf() for _ in range(3)]
    P.dma("sp", "ct", lambda e: e.dma_start(out=ct[:], in_=cT), writes=[b_ct])
    P.dma("sp", "bt", lambda e: e.dma_start(out=bt[:], in_=bm.rearrange("l r n -> r l n")), writes=[b_bt])
    P.op("act", lambda e: e.activation(out=av[:], in_=ct[:], func=AF.Silu), reads=[b_ct], writes=[b_av])
    i = 0
    for l in range(2):
        for k in range(16):
            s = i % NW; i += 1
            P.dma("sp", f"wt{s}", lambda e, s=s, l=l, k=k: e.dma_start(out=wt[s][:], in_=wm[l, k*128:(k+1)*128, :]), writes=[b_wt[s]])
            for n in range(3):
                P.op("pe", lambda e, s=s, n=n, k=k: e.matmul(pst[n][0:2, :], av[:, k, :], wt[s][:, n*512:(n+1)*512], start=(k == 0), stop=(k == 15)),
                     reads=[b_av, b_wt[s]], writes=[b_ps[n]])
        for n in range(3):
            P.op("dve", lambda e, n=n, l=l: e.tensor_tensor(out=ot[:, l, n*512:(n+1)*512], in0=pst[n][0:2, :], in1=bt[:, l, n*512:(n+1)*512], op=ALU.add),
                 reads=[b_ps[n], b_bt], writes=[b_ot])
    P.dma("sp", "ot", lambda e: e.dma_start(out=out.rearrange("l r n -> r l n"), in_=ot[:]), reads=[b_ot])
    P.emit()
    return nc


def run_mod(c, c_ctx, w_mod, b_mod):
    cc = np.stack([c[0], c_ctx], axis=-1)
    cT = np.ascontiguousarray(cc.reshape(16, 128, 2).transpose(1, 0, 2))
    nc = build_mod()
    in_maps = []
    for i in range(NCORE):
        sl = slice(i * 1536, (i + 1) * 1536)
        in_maps.append({"cT": cT, "wm": np.ascontiguousarray(w_mod[:, :, sl]),
                        "bm": np.ascontiguousarray(np.broadcast_to(b_mod[:, None, sl], (2, 2, 1536)))})
    res = _run(nc, in_maps)
    return np.concatenate([r["mod"] for r in res], axis=-1)


def cols128(v):
    return np.ascontiguousarray(v.reshape(16, 128).T)


def build_proj(combine, project=True):
    nc = bass.Bass("TRN2", target_bir_lowering=False)
    x = nc.dram_tensor("x", [TOK_PC, D], F32, kind="ExternalInput").ap()
    ident_d = nc.dram_tensor("ident", [128, 128], F32, kind="ExternalInput").ap()
    P = Prog(nc)
    if project:
        modc = nc.dram_tensor("modc", [128, 16, 4], F32, kind="ExternalInput").ap()
        w_in = nc.dram_tensor("w_in", [D, IN_COLS], F32, kind="ExternalInput").ap()
        z = nc.dram_tensor("z", [TOK_PC, IN_COLS], F32, kind="ExternalOutput").ap()
    if combine:
        part = nc.dram_tensor("part", [NPART, TOK_PC, D], F32, kind="ExternalInput").ap()
        g2 = nc.dram_tensor("g2", [2, 128, D], F32, kind="ExternalInput").ap()
        xo = nc.dram_tensor("xo", [TOK_PC, D], F32, kind="ExternalOutput").ap()
        g2t = P.sb("g2t", [128, 2, D], F32); b_g2 = P.buf()
        P.dma("sp", "g2", lambda e: e.dma_start(out=g2t[:], in_=g2.rearrange("r p d -> p r d")), writes=[b_g2])
        pt = [P.sb(f"pt{i}", [128, D], F32) for i in range(3)]; b_pt = [P.buf() for _ in range(3)]
        acc = P.sb("acc", [128, D], F32); b_acc = P.buf()
    ident = P.sb("identt", [128, 128], F32); b_id = P.buf()
    P.dma("sp", "ident", lambda e: e.dma_start(out=ident[:], in_=ident_d), writes=[b_id])
    xt = [P.sb(f"xt{i}", [128, D], F32) for i in range(2)]; b_xt = [P.buf() for _ in range(2)]
    if project:
        mc = P.sb("mc", [128, 16, 4], F32); b_mc = P.buf()
        P.dma("sp", "mc", lambda e: e.dma_start(out=mc[:], in_=modc), writes=[b_mc])
        P.op("dve", lambda e: e.tensor_scalar(out=mc[:, :, 1], in0=mc[:, :, 1], scalar1=1.0, scalar2=None, op0=ALU.add), reads=[b_mc], writes=[b_mc])
        P.op("dve", lambda e: e.tensor_scalar(out=mc[:, :, 3], in0=mc[:, :, 3], scalar1=1.0, scalar2=None, op0=ALU.add), reads=[b_mc], writes=[b_mc])
        xn = P.sb("xn", [128, D], F32); b_xn = P.buf()
        junk = P.sb("junk", [128, D], BF16); b_junk = P.buf()
        ss = P.sb("ss", [128, 2], F32); b_ss = P.buf()
        xmT = P.sb("xmT", [128, 16, TOK_PC], BF16); b_xmT = [P.buf() for _ in TILES_PC]
        wb = [P.sb(f"wb{i}", [128, 16, 512], BF16) for i in range(2)]; b_wb = [P.buf() for _ in range(2)]
        zt = [P.sb(f"zt{i}", [128, 512], F32) for i in range(4)]; b_zt = [P.buf() for _ in range(4)]
        pst = [P.ps(f"ps{i}", [128, 512], F32) for i in range(8)]; b_ps = [P.buf() for _ in range(8)]
    psi = 0
    for ti, (r0, n) in enumerate(TILES_PC):
        s = ti % 2
        X = xt[s]; bX = b_xt[s]
        P.dma("sp", f"xt{s}", lambda e, X=X, r0=r0, n=n: e.dma_start(out=X[0:n, :], in_=x[r0:r0+n, :]), writes=[bX])
        if combine:
            isctx = 1 if r0 >= LAT_PC else 0
            for c in range(NPART):
                ps_ = c % 3
                P.dma("sp", f"pt{ps_}", lambda e, ps_=ps_, c=c, r0=r0, n=n: e.dma_start(out=pt[ps_][0:n, :], in_=part[c, r0:r0+n, :]), writes=[b_pt[ps_]])
                if c == 0:
                    P.op("pool", lambda e, ps_=ps_, n=n: e.tensor_copy(out=acc[0:n, :], in_=pt[ps_][0:n, :]), reads=[b_pt[ps_]], writes=[b_acc])
                else:
                    eng = "dve" if c % 2 else "pool"
                    P.op(eng, lambda e, ps_=ps_, n=n: e.tensor_tensor(out=acc[0:n, :], in0=acc[0:n, :], in1=pt[ps_][0:n, :], op=ALU.add), reads=[b_pt[ps_], b_acc], writes=[b_acc])
            P.op("dve", lambda e, n=n, isctx=isctx: e.tensor_tensor(out=acc[0:n, :], in0=acc[0:n, :], in1=g2t[0:n, isctx, :], op=ALU.mult), reads=[b_acc, b_g2], writes=[b_acc])
            P.op("dve", lambda e, X=X, n=n: e.tensor_tensor(out=X[0:n, :], in0=X[0:n, :], in1=acc[0:n, :], op=ALU.add), reads=[b_acc, bX], writes=[bX])
            P.dma("sp", f"xo{s}", lambda e, X=X, r0=r0, n=n: e.dma_start(out=xo[r0:r0+n, :], in_=X[0:n, :]), reads=[bX])
        if not project:
            continue
        isctx = 1 if r0 >= LAT_PC else 0
        P.op("act", lambda e, X=X, n=n: e.activation(out=junk[0:n, :], in_=X[0:n, :], func=AF.Square, accum_out=ss[0:n, 0:1]), reads=[bX], writes=[b_junk, b_ss])
        P.op("act", lambda e, n=n: e.activation(out=ss[0:n, 1:2], in_=ss[0:n, 0:1], func=AF.Sqrt, scale=1.0 / D, bias=EPS), reads=[b_ss], writes=[b_ss])
        P.op("dve", lambda e, n=n: e.reciprocal(out=ss[0:n, 1:2], in_=ss[0:n, 1:2]), reads=[b_ss], writes=[b_ss])
        P.op("dve", lambda e, X=X, n=n: e.tensor_scalar(out=xn[0:n, :], in0=X[0:n, :], scalar1=ss[0:n, 1:2], scalar2=None, op0=ALU.mult), reads=[bX, b_ss], writes=[b_xn])
        for kg in range(4):
            pb = psi % 8; psi += 1
            for kk in range(4):
                k = kg * 4 + kk
                P.op("pe", lambda e, pb=pb, kk=kk, k=k, n=n: e.transpose(pst[pb][:, kk*128:kk*128+n], xn[0:n, k*128:(k+1)*128], ident[0:n, 0:n]),
                     reads=[b_xn, b_id], writes=[b_ps[pb]])
            for kk in range(4):
                k = kg * 4 + kk
                if kk % 2 == 0:
                    P.op("dve", lambda e, pb=pb, kk=kk, k=k, n=n, r0=r0, isctx=isctx: e.tensor_scalar(
                        out=xmT[:, k, r0:r0+n], in0=pst[pb][:, kk*128:kk*128+n], scalar1=mc[:, k, 2*isctx+1:2*isctx+2], scalar2=mc[:, k, 2*isctx:2*isctx+1], op0=ALU.mult, op1=ALU.add),
                        reads=[b_ps[pb], b_mc], writes=[b_xmT[ti]])
                else:
                    P.op("act", lambda e, pb=pb, kk=kk, k=k, n=n, r0=r0, isctx=isctx: e.activation(
                        out=xmT[:, k, r0:r0+n], in_=pst[pb][:, kk*128:kk*128+n], func=AF.Identity, scale=mc[:, k, 2*isctx+1:2*isctx+2], bias=mc[:, k, 2*isctx:2*isctx+1]),
                        reads=[b_ps[pb], b_mc], writes=[b_xmT[ti]])
    if project:
        w_v = w_in.rearrange("(k p) n -> p k n", p=128)
        zi = 0
        wsi = 0
        wst = [P.sb(f"wst{i}", [128, 4, 512], F32) for i in range(3)]; b_wst = [P.buf() for _ in range(3)]
        for cb in range(globals().get("IN_COLS_RUN", IN_COLS) // 512):
            s = cb % 2
            for kq in range(4):
                ws_ = wsi % 3; wsi += 1
                P.dma("sp", f"wst{ws_}", lambda e, ws_=ws_, cb=cb, kq=kq: e.dma_start(out=wst[ws_][:], in_=w_v[:, kq*4:(kq+1)*4, cb*512:(cb+1)*512]), writes=[b_wst[ws_]])
                P.op("pool", lambda e, ws_=ws_, s=s, kq=kq: e.tensor_copy(out=wb[s][:, kq*4:(kq+1)*4, :], in_=wst[ws_][:]), reads=[b_wst[ws_]], writes=[b_wb[s]])
            for ti, (r0, n) in enumerate(TILES_PC):
                pb = psi % 8; psi += 1
                for k in range(16):
                    P.op("pe", lambda e, pb=pb, k=k, n=n, r0=r0, s=s: e.matmul(pst[pb][0:n, :], xmT[:, k, r0:r0+n], wb[s][:, k, :], start=(k == 0), stop=(k == 15)),
                         reads=[b_xmT[ti], b_wb[s]], writes=[b_ps[pb]])
                zs = zi % 4; zi += 1
                if zi % 2:
                    P.op("dve", lambda e, pb=pb, zs=zs, n=n: e.tensor_copy(out=zt[zs][0:n, :], in_=pst[pb][0:n, :]), reads=[b_ps[pb]], writes=[b_zt[zs]])
                else:
                    P.op("act", lambda e, pb=pb, zs=zs, n=n: e.copy(out=zt[zs][0:n, :], in_=pst[pb][0:n, :]), reads=[b_ps[pb]], writes=[b_zt[zs]])
                P.dma("sp", f"zt{zs}", lambda e, zs=zs, n=n, r0=r0, cb=cb: e.dma_start(out=z[r0:r0+n, cb*512:(cb+1)*512], in_=zt[zs][0:n, :]), reads=[b_zt[zs]])
    P.emit()
    return nc


def shard_tokens(lat, ctx):
    return [np.ascontiguousarray(np.concatenate([lat[i*LAT_PC:(i+1)*LAT_PC], ctx[i*CTX_PC:(i+1)*CTX_PC]], axis=0)) for i in range(NCORE)]


def unshard_tokens(per_core):
    lat = np.concatenate([p[:LAT_PC] for p in per_core], axis=0)
    ctx = np.concatenate([p[LAT_PC:] for p in per_core], axis=0)
    return lat, ctx


def run_proj(x_shards, mod_l, w_in_l, parts=None, project=True):
    combine = parts is not None
    nc = build_proj(combine, project)
    ident = np.eye(128, dtype=np.float32)
    in_maps = []
    for i in range(NCORE):
        m = {"x": x_shards[i], "ident": ident}
        if project:
            sh1, sc1 = mod_l[0, 0:D], mod_l[0, D:2*D]
            csh1, csc1 = mod_l[1, 0:D], mod_l[1, D:2*D]
            m["modc"] = np.ascontiguousarray(np.stack([cols128(sh1), cols128(sc1), cols128(csh1), cols128(csc1)], axis=-1))
            m["w_in"] = w_in_l
        if combine:
            m["part"] = parts[i]
            g2 = np.stack([np.broadcast_to(mod_l_prev_g2[0], (128, D)), np.broadcast_to(mod_l_prev_g2[1], (128, D))])
            m["g2"] = np.ascontiguousarray(g2)
        in_maps.append(m)
    res = _run(nc, in_maps)
    z = [r["z"] for r in res] if project else None
    xo = [r["xo"] for r in res] if combine else None
    return z, xo


NCH = T_ALL // 128
SEG = 384
NSEG = T_ALL // SEG
NQ = 128 + SEQ // 2
NQB = NQ // 128
ATT_SCALE = 128 ** -0.5


def build_mix():
    nc = bass.Bass("TRN2", target_bir_lowering=False)
    di = lambda name, shape, dt=F32: nc.dram_tensor(name, list(shape), dt, kind="ExternalInput").ap()
    do = lambda name, shape, dt=F32: nc.dram_tensor(name, list(shape), dt, kind="ExternalOutput").ap()
    ident_d = di("ident", [128, 128])
    maskT_d = di("maskT", [128, 128])
    r_qkv = di("r_qkv", [T_ALL, 3, 128])
    r_tab = di("r_tab", [T_ALL, 4, 128])
    r_g = di("r_g", [128, 1])
    r_out = do("r_out", [T_ALL, 128])
    s_par = di("s_par", [128, 4, 3])
    s_B = di("s_B", [128, 2, 2, 32])
    s_C = di("s_C", [128, 2, 2, 64])
    s_uT = di("s_uT", [2, 2, 32, T_ALL])
    s_iota = di("s_iota", [128, SEG])
    s_out = do("s_out", [2, T_ALL, 64])
    a_q = di("a_q", [NQ, 128]); a_k = di("a_k", [T_ALL, 128]); a_v = di("a_v", [T_ALL, 128])
    a_qtab = di("a_qtab", [NQ, 2, 128]); a_ktab = di("a_ktab", [T_ALL, 2, 128])
    a_w = di("a_w", [128, 2, 128])
    a_out = do("a_out", [NQ, 128])

    P = Prog(nc)
    NF = 6
    psf = [P.ps(f"psf{i}", [128, 512], F32) for i in range(NF)]; b_psf = [P.buf() for _ in range(NF)]
    psb = [P.ps(f"psb{i}", [128, 512], BF16) for i in range(2)]; b_psb = [P.buf() for _ in range(2)]
    cnt = {"f": 0, "b": 0}

    def nf():
        i = cnt["f"] % NF; cnt["f"] += 1
        return psf[i], b_psf[i]

    def nb():
        i = cnt["b"] % 2; cnt["b"] += 1
        return psb[i], b_psb[i]

    ident = P.sb("identt", [128, 128], F32); b_id = P.buf()
    identb = P.sb("identb", [128, 128], BF16); b_idb = P.buf()
    maskT = P.sb("maskTt", [128, 128], F32); b_mask = P.buf()
    P.dma("sp", "ident", lambda e: e.dma_start(out=ident[:], in_=ident_d), writes=[b_id])
    P.dma("sp", "maskT", lambda e: e.dma_start(out=maskT[:], in_=maskT_d), writes=[b_mask])
    P.op("dve", lambda e: e.tensor_copy(out=identb[:], in_=ident[:]), reads=[b_id], writes=[b_idb])

    def s5_unit():
        par = P.sb("s_par_t", [128, 4, 3], F32); b_par = P.buf()
        Bt = P.sb("s_B_t", [128, 2, 2, 32], F32); b_B = P.buf()
        Ct = P.sb("s_C_t", [128, 2, 2, 64], F32); b_C = P.buf()
        io = P.sb("s_iota_t", [128, SEG], F32); b_io = P.buf()
        P.dma("sp", "s_par", lambda e: e.dma_start(out=par[:], in_=s_par), writes=[b_par])
        P.dma("sp", "s_B", lambda e: e.dma_start(out=Bt[:], in_=s_B), writes=[b_B])
        P.dma("sp", "s_C", lambda e: e.dma_start(out=Ct[:], in_=s_C), writes=[b_C])
        P.dma("sp", "s_iota", lambda e: e.dma_start(out=io[:], in_=s_iota), writes=[b_io])
        P.op("dve", lambda e: e.tensor_scalar(out=Ct[:, :, 1, :], in0=Ct[:, :, 1, :], scalar1=-1.0, scalar2=None, op0=ALU.mult), reads=[b_C], writes=[b_C])
        cs = P.sb("s_cs", [128, 4, SEG], F32); sn = P.sb("s_sn", [128, 4, SEG], F32); rb = P.sb("s_rb", [128, 4, SEG], F32)
        b_tab = [P.buf() for _ in range(4)]
        col = P.sb("s_col", [128, 4, 24], F32); b_col = [P.buf() for _ in range(4)]
        ph = P.sb("s_ph", [128, SEG], F32); b_ph = P.buf()
        ph2 = P.sb("s_ph2", [128, SEG], F32); b_ph2 = P.buf()
        phi = P.sb("s_phi", [128, SEG], I32); b_phi = P.buf()
        BpT = P.sb("s_BpT", [32, 4, 2, 128], F32); b_BpT = [P.buf() for _ in range(4)]
        Bp = P.sb("s_Bp", [128, 2, 32], F32); b_Bp = P.buf()
        tmpB = P.sb("s_tmpB", [128, 32], F32); b_tmpB = P.buf()
        st = P.sb("s_st", [128, 4, 2], F32); b_st = [P.buf() for _ in range(4)]

        def frac_sin(dst, src, bsrc, bdst_list):
            P.op("dve", lambda e: e.tensor_copy(out=phi[:], in_=src), reads=[bsrc], writes=[b_phi])
            P.op("dve", lambda e: e.tensor_tensor(out=ph2[:], in0=src, in1=phi[:], op=ALU.subtract), reads=[bsrc, b_phi], writes=[b_ph2])
            P.op("dve", lambda e: e.tensor_scalar(out=phi[:], in0=ph2[:], scalar1=0.5, scalar2=None, op0=ALU.is_gt), reads=[b_ph2], writes=[b_phi])
            P.op("dve", lambda e: e.tensor_tensor(out=ph2[:], in0=ph2[:], in1=phi[:], op=ALU.subtract), reads=[b_ph2, b_phi], writes=[b_ph2])
            P.op("dve", lambda e: e.tensor_scalar(out=phi[:], in0=ph2[:], scalar1=-0.5, scalar2=None, op0=ALU.is_lt), reads=[b_ph2], writes=[b_phi])
            P.op("dve", lambda e: e.tensor_tensor(out=ph2[:], in0=ph2[:], in1=phi[:], op=ALU.add), reads=[b_ph2, b_phi], writes=[b_ph2])
            P.op("act", lambda e: e.activation(out=dst, in_=ph2[:], func=AF.Sin, scale=2.0 * 3.14159265), reads=[b_ph2], writes=bdst_list)

        for cb in range(4):
            tl = cb % 2
            c_ = lambda j, cb=cb: col[:, cb, j:j+1]
            bc = b_col[cb]
            a_re = par[:, cb, 0:1]; a_im = par[:, cb, 1:2]; lst = par[:, cb, 2:3]
            P.op("act", lambda e, c_=c_, lst=lst: e.activation(out=c_(0), in_=lst, func=AF.Exp), reads=[b_par], writes=[bc])
            P.op("dve", lambda e, c_=c_, a_re=a_re: e.tensor_tensor(out=c_(1), in0=a_re, in1=c_(0), op=ALU.mult), reads=[b_par, bc], writes=[bc])
            P.op("act", lambda e, c_=c_: e.activation(out=c_(2), in_=c_(1), func=AF.Exp), reads=[bc], writes=[bc])
            P.op("dve", lambda e, c_=c_, a_im=a_im: e.tensor_tensor(out=c_(3), in0=a_im, in1=c_(0), op=ALU.mult), reads=[b_par, bc], writes=[bc])
            P.op("dve", lambda e, c_=c_: e.tensor_scalar(out=c_(3), in0=c_(3), scalar1=1.0 / (2.0 * np.pi), scalar2=None, op0=ALU.mult), reads=[bc], writes=[bc])
            P.op("dve", lambda e, c_=c_: e.tensor_scalar(out=ph[:], in0=io[:], scalar1=c_(3), scalar2=None, op0=ALU.mult), reads=[b_io, bc], writes=[b_ph])
            frac_sin(sn[:, cb, :], ph[:], b_ph, [b_tab[cb]])
            P.op("dve", lambda e: e.tensor_scalar(out=ph[:], in0=ph[:], scalar1=0.25, scalar2=None, op0=ALU.add), reads=[b_ph], writes=[b_ph])
            frac_sin(cs[:, cb, :], ph[:], b_ph, [b_tab[cb]])
            P.op("pool", lambda e, cb=cb: e.memset(rb[:, cb, :], 1.0), writes=[b_tab[cb]])
            P.op("dve", lambda e, cb=cb, c_=c_: e.tensor_scalar(out=rb[:, cb, :], in0=rb[:, cb, :], scalar1=c_(2), scalar2=None, op0=ALU.mult), reads=[bc, b_tab[cb]], writes=[b_tab[cb]])
            tt = lambda o, a, b, op, c_=c_: P.op("dve", lambda e: e.tensor_tensor(out=o, in0=a, in1=b, op=op), reads=[bc, b_par, b_tab[cb]], writes=[bc])
            tt(c_(4), c_(2), cs[:, cb, 0:1], ALU.mult)
            tt(c_(5), c_(2), sn[:, cb, 0:1], ALU.mult)
            P.op("dve", lambda e, c_=c_: e.tensor_scalar(out=c_(6), in0=c_(4), scalar1=-1.0, scalar2=None, op0=ALU.add), reads=[bc], writes=[bc])
            tt(c_(7), c_(6), a_re, ALU.mult)
            tt(c_(8), c_(5), a_im, ALU.mult)
            tt(c_(9), c_(7), c_(8), ALU.add)
            tt(c_(10), c_(5), a_re, ALU.mult)
            tt(c_(11), c_(6), a_im, ALU.mult)
            tt(c_(12), c_(10), c_(11), ALU.subtract)
            tt(c_(13), a_re, a_re, ALU.mult)
            tt(c_(14), a_im, a_im, ALU.mult)
            tt(c_(15), c_(13), c_(14), ALU.add)
            P.op("dve", lambda e, c_=c_: e.reciprocal(out=c_(16), in_=c_(15)), reads=[bc], writes=[bc])
            tt(c_(17), c_(9), c_(16), ALU.mult)
            tt(c_(18), c_(12), c_(16), ALU.mult)
            P.op("dve", lambda e, c_=c_, tl=tl: e.tensor_scalar(out=tmpB[:], in0=Bt[:, tl, 1, :], scalar1=c_(18), scalar2=None, op0=ALU.mult), reads=[bc, b_B], writes=[b_tmpB])
            P.op("dve", lambda e, c_=c_, tl=tl: e.scalar_tensor_tensor(out=Bp[:, 0, :], in0=Bt[:, tl, 0, :], scalar=c_(17), in1=tmpB[:], op0=ALU.mult, op1=ALU.subtract), reads=[bc, b_B, b_tmpB], writes=[b_Bp])
            P.op("dve", lambda e, c_=c_, tl=tl: e.tensor_scalar(out=tmpB[:], in0=Bt[:, tl, 0, :], scalar1=c_(18), scalar2=None, op0=ALU.mult), reads=[bc, b_B], writes=[b_tmpB])
            P.op("dve", lambda e, c_=c_, tl=tl: e.scalar_tensor_tensor(out=Bp[:, 1, :], in0=Bt[:, tl, 1, :], scalar=c_(17), in1=tmpB[:], op0=ALU.mult, op1=ALU.add), reads=[bc, b_B, b_tmpB], writes=[b_Bp])
            for ri in range(2):
                pt_, bpt_ = nf()
                P.op("pe", lambda e, pt_=pt_, ri=ri: e.transpose(pt_[0:32, 0:128], Bp[:, ri, :], ident[:]), reads=[b_Bp, b_id], writes=[bpt_])
                P.op("act", lambda e, pt_=pt_, ri=ri, cb=cb: e.copy(out=BpT[:, cb, ri, :], in_=pt_[0:32, 0:128]), reads=[bpt_], writes=[b_BpT[cb]])

        NU = 3
        ut = [P.sb(f"s_ut{i}", [32, SEG], F32) for i in range(NU)]; b_ut = [P.buf() for _ in range(NU)]
        m = [P.sb(f"s_m{i}", [128, SEG], F32) for i in range(4)]; b_m = [P.buf() for _ in range(4)]
        dr = [P.sb(f"s_dr{i}", [128, SEG], F32) for i in range(2)]; b_dr = [P.buf() for _ in range(2)]
        xs_ = [P.sb(f"s_xs{i}", [128, SEG], F32) for i in range(2)]; b_xs = [P.buf() for _ in range(2)]
        xr = [[P.sb(f"s_xr{tl}{ri}", [128, SEG], F32) for ri in range(2)] for tl in range(2)]
        b_xr = [[P.buf() for _ in range(2)] for _ in range(2)]
        ysb = [P.sb(f"s_y{i}", [128, 3, 64], F32) for i in range(2)]; b_ysb = [P.buf() for _ in range(2)]
        ui = 0; yi = 0
        for d in range(2):
            for sg in range(NSEG):
                for tl in range(2):
                    cb = d * 2 + tl
                    us = ui % NU; ui += 1
                    P.dma("sp", f"s_ut{us}", lambda e, us=us, d=d, tl=tl, sg=sg: e.dma_start(out=ut[us][:], in_=s_uT[d, tl, :, sg*SEG:(sg+1)*SEG]), writes=[b_ut[us]])
                    pre, bpre = nf(); pim, bpim = nf()
                    P.op("pe", lambda e, pre=pre, us=us, cb=cb: e.matmul(pre[:, 0:SEG], BpT[:, cb, 0, :], ut[us][:], start=True, stop=True), reads=[b_BpT[cb], b_ut[us]], writes=[bpre])
                    P.op("pe", lambda e, pim=pim, us=us, cb=cb: e.matmul(pim[:, 0:SEG], BpT[:, cb, 1, :], ut[us][:], start=True, stop=True), reads=[b_BpT[cb], b_ut[us]], writes=[bpim])
                    C_ = cs[:, cb, :]; S_ = sn[:, cb, :]
                    P.op("dve", lambda e, pre=pre, C_=C_: e.tensor_tensor(out=m[0][:], in0=pre[:, 0:SEG], in1=C_, op=ALU.mult), reads=[bpre, b_tab[cb]], writes=[b_m[0]])
                    P.op("dve", lambda e, pim=pim, S_=S_: e.tensor_tensor(out=m[1][:], in0=pim[:, 0:SEG], in1=S_, op=ALU.mult), reads=[bpim, b_tab[cb]], writes=[b_m[1]])
                    P.op("dve", lambda e, pim=pim, C_=C_: e.tensor_tensor(out=m[2][:], in0=pim[:, 0:SEG], in1=C_, op=ALU.mult), reads=[bpim, b_tab[cb]], writes=[b_m[2]])
                    P.op("dve", lambda e, pre=pre, S_=S_: e.tensor_tensor(out=m[3][:], in0=pre[:, 0:SEG], in1=S_, op=ALU.mult), reads=[bpre, b_tab[cb]], writes=[b_m[3]])
                    P.op("pool", lambda e: e.tensor_tensor(out=dr[0][:], in0=m[0][:], in1=m[1][:], op=ALU.add), reads=[b_m[0], b_m[1]], writes=[b_dr[0]])
                    P.op("pool", lambda e: e.tensor_tensor(out=dr[1][:], in0=m[2][:], in1=m[3][:], op=ALU.subtract), reads=[b_m[2], b_m[3]], writes=[b_dr[1]])
                    for ri in range(2):
                        init = 0.0 if sg == 0 else st[:, cb, ri:ri+1]
                        P.op("dve", lambda e, ri=ri, init=init, cb=cb: e.tensor_tensor_scan(out=xs_[ri][:], data0=rb[:, cb, :], data1=dr[ri][:], initial=init, op0=ALU.mult, op1=ALU.add),
                             reads=[b_tab[cb], b_dr[ri], b_st[cb]], writes=[b_xs[ri]])
                    P.op("pool", lambda e, C_=C_: e.tensor_tensor(out=m[0][:], in0=xs_[0][:], in1=C_, op=ALU.mult), reads=[b_xs[0], b_tab[cb]], writes=[b_m[0]])
                    P.op("dve", lambda e, S_=S_: e.tensor_tensor(out=m[1][:], in0=xs_[1][:], in1=S_, op=ALU.mult), reads=[b_xs[1], b_tab[cb]], writes=[b_m[1]])
                    P.op("pool", lambda e, S_=S_: e.tensor_tensor(out=m[2][:], in0=xs_[0][:], in1=S_, op=ALU.mult), reads=[b_xs[0], b_tab[cb]], writes=[b_m[2]])
                    P.op("dve", lambda e, C_=C_: e.tensor_tensor(out=m[3][:], in0=xs_[1][:], in1=C_, op=ALU.mult), reads=[b_xs[1], b_tab[cb]], writes=[b_m[3]])
                    P.op("pool", lambda e, tl=tl: e.tensor_tensor(out=xr[tl][0][:], in0=m[0][:], in1=m[1][:], op=ALU.subtract), reads=[b_m[0], b_m[1]], writes=[b_xr[tl][0]])
                    P.op("pool", lambda e, tl=tl: e.tensor_tensor(out=xr[tl][1][:], in0=m[2][:], in1=m[3][:], op=ALU.add), reads=[b_m[2], b_m[3]], writes=[b_xr[tl][1]])
                    for ri in range(2):
                        P.op("act", lambda e, tl=tl, ri=ri, cb=cb: e.copy(out=st[:, cb, ri:ri+1], in_=xr[tl][ri][:, SEG-1:SEG]), reads=[b_xr[tl][ri]], writes=[b_st[cb]])
                ys = yi % 2; yi += 1
                for blk in range(3):
                    py, bpy = nf()
                    j = 0
                    for tl in range(2):
                        for ri in range(2):
                            P.op("pe", lambda e, py=py, tl=tl, ri=ri, blk=blk, j=j: e.matmul(py[:, 0:64], xr[tl][ri][:, blk*128:(blk+1)*128], Ct[:, tl, ri, :], start=(j == 0), stop=(j == 3)),
                                 reads=[b_xr[tl][ri], b_C], writes=[bpy])
                            j += 1
                    P.op("act", lambda e, py=py, ys=ys, blk=blk: e.copy(out=ysb[ys][:, blk, :], in_=py[:, 0:64]), reads=[bpy], writes=[b_ysb[ys]])
                P.dma("sp", f"s_y{ys}", lambda e, ys=ys, d=d, sg=sg: e.dma_start(out=s_out[d, sg*SEG:(sg+1)*SEG, :].rearrange("(b p) c -> p b c", p=128), in_=ysb[ys][:]), reads=[b_ysb[ys]])

    def ret_unit():
        g = P.sb("r_g_t", [128, 1], F32); b_g = P.buf()
        P.dma("sp", "r_g", lambda e: e.dma_start(out=g[:], in_=r_g), writes=[b_g])
        NB = 2
        qkv = [P.sb(f"r_qkv{i}", [128, 3, 128], F32) for i in range(NB)]; b_qkv = [P.buf() for _ in range(NB)]
        tab = [P.sb(f"r_tab{i}", [128, 4, 128], F32) for i in range(NB)]; b_tb = [P.buf() for _ in range(NB)]
        sw = P.sb("r_sw", [128, 2, 128], F32); b_sw = P.buf()
        t1 = P.sb("r_t1", [128, 2, 128], F32); b_t1 = P.buf()
        t2 = P.sb("r_t2", [128, 2, 128], F32); b_t2 = P.buf()
        qk = P.sb("r_qk", [128, 2, 128], BF16); b_qk = P.buf()
        qkT = P.sb("r_qkT", [128, 2, 128], BF16); b_qkT = P.buf()
        vb = P.sb("r_vb", [128, 128], BF16); b_vb = P.buf()
        sm = P.sb("r_sm", [128, 128], BF16); b_sm = P.buf()
        S32 = P.sb("r_S32", [128, 128], F32); b_S32 = P.buf()
        Sb = P.sb("r_Sb", [128, 128], BF16); b_Sb = P.buf()
        stt = P.sb("r_stt", [128, 6], F32); b_stt = P.buf()
        mv = P.sb("r_mv", [128, 4], F32); b_mv = P.buf()
        yo = [P.sb(f"r_yo{i}", [128, 128], F32) for i in range(2)]; b_yo = [P.buf() for _ in range(2)]
        P.op("pool", lambda e: e.memset(S32[:], 0.0), writes=[b_S32])
        P.op("pool", lambda e: e.memset(Sb[:], 0.0), writes=[b_Sb])
        for n in range(NCH):
            s = n % NB
            Q = qkv[s]; TB = tab[s]
            P.dma("sp", f"r_qkv{s}", lambda e, Q=Q, n=n: e.dma_start(out=Q[:], in_=r_qkv[n*128:(n+1)*128]), writes=[b_qkv[s]])
            P.dma("sp", f"r_tab{s}", lambda e, TB=TB, n=n: e.dma_start(out=TB[:], in_=r_tab[n*128:(n+1)*128]), writes=[b_tb[s]])
            P.op("pool", lambda e, Q=Q: e.tensor_copy(out=sw[:, :, 0:64], in_=Q[:, 0:2, 64:128]), reads=[b_qkv[s]], writes=[b_sw])
            P.op("pool", lambda e, Q=Q: e.tensor_copy(out=sw[:, :, 64:128], in_=Q[:, 0:2, 0:64]), reads=[b_qkv[s]], writes=[b_sw])
            TBv = TB[:].rearrange("p (a b) d -> p a b d", b=2)
            P.op("dve", lambda e, Q=Q, TBv=TBv: e.tensor_tensor(out=t1[:], in0=Q[:, 0:2, :], in1=TBv[:, :, 0, :], op=ALU.mult), reads=[b_qkv[s], b_tb[s]], writes=[b_t1])
            P.op("pool", lambda e, TBv=TBv: e.tensor_tensor(out=t2[:], in0=sw[:], in1=TBv[:, :, 1, :], op=ALU.mult), reads=[b_sw, b_tb[s]], writes=[b_t2])
            P.op("dve", lambda e: e.tensor_tensor(out=qk[:], in0=t1[:], in1=t2[:], op=ALU.add), reads=[b_t1, b_t2], writes=[b_qk])
            P.op("act", lambda e, Q=Q: e.copy(out=vb[:], in_=Q[:, 2, :]), reads=[b_qkv[s]], writes=[b_vb])
            pt_, bpt_ = nb()
            P.op("pe", lambda e, pt_=pt_: e.transpose(pt_[:, 0:128], qk[:, 0, :], identb[:]), reads=[b_qk, b_idb], writes=[bpt_])
            P.op("pe", lambda e, pt_=pt_: e.transpose(pt_[:, 128:256], qk[:, 1, :], identb[:]), reads=[b_qk, b_idb], writes=[bpt_])
            P.op("act", lambda e, pt_=pt_: e.copy(out=qkT[:].rearrange("p a d -> p (a d)"), in_=pt_[:, 0:256]), reads=[bpt_], writes=[b_qkT])
            ps_s, bps_s = nf()
            P.op("pe", lambda e, ps_s=ps_s: e.matmul(ps_s[:, 0:128], qkT[:, 1, :], qkT[:, 0, :], start=True, stop=True), reads=[b_qkT], writes=[bps_s])
            P.op("dve", lambda e, ps_s=ps_s: e.tensor_tensor(out=sm[:], in0=ps_s[:, 0:128], in1=maskT[:], op=ALU.mult), reads=[bps_s, b_mask], writes=[b_sm])
            ps_y, bps_y = nf()
            P.op("pe", lambda e, ps_y=ps_y: e.matmul(ps_y[:, 0:128], sm[:], vb[:], start=True, stop=False), reads=[b_sm, b_vb], writes=[bps_y])
            P.op("pe", lambda e, ps_y=ps_y: e.matmul(ps_y[:, 0:128], qkT[:, 0, :], Sb[:], start=False, stop=True), reads=[b_qkT, b_Sb], writes=[bps_y])
            ps_kv, bps_kv = nf()
            P.op("pe", lambda e, ps_kv=ps_kv: e.matmul(ps_kv[:, 0:128], qk[:, 1, :], vb[:], start=True, stop=True), reads=[b_qk, b_vb], writes=[bps_kv])
            P.op("dve", lambda e, ps_kv=ps_kv: e.tensor_tensor(out=S32[:], in0=ps_kv[:, 0:128], in1=S32[:], op=ALU.add), reads=[bps_kv, b_S32], writes=[b_S32])
            P.op("dve", lambda e: e.tensor_scalar(out=S32[:], in0=S32[:], scalar1=g[:, 0:1], scalar2=None, op0=ALU.mult), reads=[b_S32, b_g], writes=[b_S32])
            P.op("act", lambda e: e.copy(out=Sb[:], in_=S32[:]), reads=[b_S32], writes=[b_Sb])
            P.op("dve", lambda e, ps_y=ps_y: e.bn_stats(out=stt[:], in_=ps_y[:, 0:128]), reads=[bps_y], writes=[b_stt])
            P.op("dve", lambda e: e.bn_aggr(out=mv[:, 0:2], in_=stt[:]), reads=[b_stt], writes=[b_mv])
            P.op("act", lambda e: e.activation(out=mv[:, 2:3], in_=mv[:, 1:2], func=AF.Sqrt, bias=EPS, scale=1.0), reads=[b_mv], writes=[b_mv])
            P.op("dve", lambda e: e.reciprocal(out=mv[:, 3:4], in_=mv[:, 2:3]), reads=[b_mv], writes=[b_mv])
            ys = n % 2
            P.op("dve", lambda e, ps_y=ps_y, ys=ys: e.tensor_scalar(out=yo[ys][:], in0=ps_y[:, 0:128], scalar1=mv[:, 0:1], scalar2=mv[:, 3:4], op0=ALU.subtract, op1=ALU.mult), reads=[bps_y, b_mv], writes=[b_yo[ys]])
            P.dma("sp", f"r_yo{ys}", lambda e, ys=ys, n=n: e.dma_start(out=r_out[n*128:(n+1)*128, :], in_=yo[ys][:]), reads=[b_yo[ys]])

    def att_unit():
        wt = P.sb("a_w_t", [128, 2, 128], F32); b_w = P.buf()
        P.dma("sp", "a_w", lambda e: e.dma_start(out=wt[:], in_=a_w), writes=[b_w])
        kT = P.sb("a_kT", [128, T_ALL], BF16); b_kT = P.buf()
        qT = P.sb("a_qT", [128, NQ], BF16); b_qT = P.buf()
        vb = P.sb("a_vb", [128, NCH, 128], BF16); b_vb = P.buf()
        xin = [P.sb(f"a_xin{i}", [128, 128], F32) for i in range(2)]; b_xin = [P.buf() for _ in range(2)]
        tb = [P.sb(f"a_tb{i}", [128, 2, 128], F32) for i in range(2)]; b_tb = [P.buf() for _ in range(2)]
        junk = P.sb("a_junk", [128, 128], F32); b_junk = P.buf()
        col = P.sb("a_col", [128, 4], F32); b_col = P.buf()
        xn = P.sb("a_xn", [128, 128], F32); b_xn = P.buf()
        sw = P.sb("a_sw", [128, 128], F32); b_sw = P.buf()
        t1 = P.sb("a_t1", [128, 128], F32); b_t1 = P.buf()
        t2 = P.sb("a_t2", [128, 128], F32); b_t2 = P.buf()
        xr = P.sb("a_xr", [128, 128], BF16); b_xr = P.buf()
        vst = [P.sb(f"a_vst{i}", [128, 6, 128], F32) for i in range(2)]; b_vst = [P.buf() for _ in range(2)]
        a_v_v = a_v.rearrange("(b p) d -> p b d", p=128)
        for i in range(NCH // 6):
            s = i % 2
            P.dma("sp", f"a_vst{s}", lambda e, s=s, i=i: e.dma_start(out=vst[s][:], in_=a_v_v[:, i*6:(i+1)*6, :]), writes=[b_vst[s]])
            P.op("pool", lambda e, s=s, i=i: e.tensor_copy(out=vb[:, i*6:(i+1)*6, :], in_=vst[s][:]), reads=[b_vst[s]], writes=[b_vb])

        def prep(src, tabsrc, nblk, wi, dstT, b_dstT):
            for blk in range(nblk):
                s = blk % 2
                X = xin[s]; TB = tb[s]
                P.dma("sp", f"a_xin{s}", lambda e, X=X, blk=blk: e.dma_start(out=X[:], in_=src[blk*128:(blk+1)*128, :]), writes=[b_xin[s]])
                P.dma("sp", f"a_tb{s}", lambda e, TB=TB, blk=blk: e.dma_start(out=TB[:], in_=tabsrc[blk*128:(blk+1)*128]), writes=[b_tb[s]])
                P.op("act", lambda e, X=X: e.activation(out=junk[:], in_=X[:], func=AF.Square, accum_out=col[:, 0:1]), reads=[b_xin[s]], writes=[b_junk, b_col])
                P.op("act", lambda e: e.activation(out=col[:, 1:2], in_=col[:, 0:1], func=AF.Sqrt, scale=1.0 / 128, bias=EPS), reads=[b_col], writes=[b_col])
                P.op("dve", lambda e: e.reciprocal(out=col[:, 2:3], in_=col[:, 1:2]), reads=[b_col], writes=[b_col])
                P.op("dve", lambda e, X=X: e.scalar_tensor_tensor(out=xn[:], in0=X[:], scalar=col[:, 2:3], in1=wt[:, wi, :], op0=ALU.mult, op1=ALU.mult), reads=[b_xin[s], b_col, b_w], writes=[b_xn])
                xv = xn[:].rearrange("p (a b d) -> p a b d", a=2, b=2)
                sv = sw[:].rearrange("p (a b d) -> p a b d", a=2, b=2)
                P.op("pool", lambda e, xv=xv, sv=sv: e.tensor_copy(out=sv[:, :, 0, :], in_=xv[:, :, 1, :]), reads=[b_xn], writes=[b_sw])
                P.op("pool", lambda e, xv=xv, sv=sv: e.tensor_copy(out=sv[:, :, 1, :], in_=xv[:, :, 0, :]), reads=[b_xn], writes=[b_sw])
                P.op("dve", lambda e, TB=TB: e.tensor_tensor(out=t1[:], in0=xn[:], in1=TB[:, 0, :], op=ALU.mult), reads=[b_xn, b_tb[s]], writes=[b_t1])
                P.op("pool", lambda e, TB=TB: e.tensor_tensor(out=t2[:], in0=sw[:], in1=TB[:, 1, :], op=ALU.mult), reads=[b_sw, b_tb[s]], writes=[b_t2])
                P.op("dve", lambda e: e.tensor_tensor(out=xr[:], in0=t1[:], in1=t2[:], op=ALU.add), reads=[b_t1, b_t2], writes=[b_xr])
                pt_, bpt_ = nb()
                P.op("pe", lambda e, pt_=pt_: e.transpose(pt_[:, 0:128], xr[:], identb[:]), reads=[b_xr, b_idb], writes=[bpt_])
                P.op("act", lambda e, pt_=pt_, blk=blk: e.copy(out=dstT[:, blk*128:(blk+1)*128], in_=pt_[:, 0:128]), reads=[bpt_], writes=[b_dstT])

        prep(a_k, a_ktab, NCH, 1, kT, b_kT)
        prep(a_q, a_qtab, NQB, 0, qT, b_qT)

        Ssb = P.sb("a_S", [128, T_ALL], F32); b_S = P.buf()
        Pb = P.sb("a_P", [128, T_ALL], BF16); b_P = P.buf()
        PT = P.sb("a_PT", [128, NCH, 128], BF16); b_PT = P.buf()
        c2 = P.sb("a_c2", [128, 4], F32); b_c2 = P.buf()
        ob = [P.sb(f"a_o{i}", [128, 128], F32) for i in range(2)]; b_ob = [P.buf() for _ in range(2)]
        for qb in range(NQB):
            nk = CTX if qb == 0 else T_ALL
            nkt = (nk + 511) // 512
            for kt in range(nkt):
                w = min(512, nk - kt * 512)
                ps_, bps_ = nf()
                P.op("pe", lambda e, ps_=ps_, qb=qb, kt=kt, w=w: e.matmul(ps_[:, 0:w], qT[:, qb*128:(qb+1)*128], kT[:, kt*512:kt*512+w], start=True, stop=True), reads=[b_qT, b_kT], writes=[bps_])
                if kt % 2:
                    P.op("dve", lambda e, ps_=ps_, kt=kt, w=w: e.tensor_copy(out=Ssb[:, kt*512:kt*512+w], in_=ps_[:, 0:w]), reads=[bps_], writes=[b_S])
                else:
                    P.op("act", lambda e, ps_=ps_, kt=kt, w=w: e.copy(out=Ssb[:, kt*512:kt*512+w], in_=ps_[:, 0:w]), reads=[bps_], writes=[b_S])
            P.op("dve", lambda e, nk=nk: e.reduce_max(out=c2[:, 0:1], in_=Ssb[:, 0:nk], axis=AX.X), reads=[b_S], writes=[b_c2])
            P.op("dve", lambda e: e.tensor_scalar(out=c2[:, 1:2], in0=c2[:, 0:1], scalar1=-ATT_SCALE, scalar2=None, op0=ALU.mult), reads=[b_c2], writes=[b_c2])
            P.op("act", lambda e, nk=nk: e.activation(out=Pb[:, 0:nk], in_=Ssb[:, 0:nk], func=AF.Exp, scale=ATT_SCALE, bias=c2[:, 1:2], accum_out=c2[:, 2:3]), reads=[b_S, b_c2], writes=[b_P, b_c2])
            P.op("dve", lambda e: e.reciprocal(out=c2[:, 3:4], in_=c2[:, 2:3]), reads=[b_c2], writes=[b_c2])
            nkb = nk // 128
            for g0 in range(0, nkb, 4):
                gn = min(4, nkb - g0)
                pt_, bpt_ = nb()
                for j in range(gn):
                    P.op("pe", lambda e, pt_=pt_, j=j, g0=g0: e.transpose(pt_[:, j*128:(j+1)*128], Pb[:, (g0+j)*128:(g0+j+1)*128], identb[:]), reads=[b_P, b_idb], writes=[bpt_])
                if (g0 // 4) % 2:
                    P.op("dve", lambda e, pt_=pt_, g0=g0, gn=gn: e.tensor_copy(out=PT[:, g0:g0+gn, :].rearrange("p a d -> p (a d)"), in_=pt_[:, 0:gn*128]), reads=[bpt_], writes=[b_PT])
                else:
                    P.op("act", lambda e, pt_=pt_, g0=g0, gn=gn: e.copy(out=PT[:, g0:g0+gn, :].rearrange("p a d -> p (a d)"), in_=pt_[:, 0:gn*128]), reads=[bpt_], writes=[b_PT])
            po, bpo = nf()
            for kb in range(nkb):
                P.op("pe", lambda e, po=po, kb=kb, nkb=nkb: e.matmul(po[:, 0:128], PT[:, kb, :], vb[:, kb, :], start=(kb == 0), stop=(kb == nkb - 1)), reads=[b_PT, b_vb], writes=[bpo])
            os_ = qb % 2
            P.op("dve", lambda e, po=po, os_=os_: e.tensor_scalar(out=ob[os_][:], in0=po[:, 0:128], scalar1=c2[:, 3:4], scalar2=None, op0=ALU.mult), reads=[bpo, b_c2], writes=[b_ob[os_]])
            P.dma("sp", f"a_o{os_}", lambda e, os_=os_, qb=qb: e.dma_start(out=a_out[qb*128:(qb+1)*128, :], in_=ob[os_][:]), reads=[b_ob[os_]])

    units = globals().get("MIX_UNITS", "rsa")
    if "s" in units:
        s5_unit()
    if "r" in units:
        ret_unit()
    if "a" in units:
        att_unit()
    P.emit()
    return nc


def _order(d):
    if d == 0:
        return np.arange(T_ALL)
    return np.concatenate([np.arange(CTX)[::-1], CTX + np.arange(SEQ)[::-1]])


_CONST = {}


def mix_consts():
    if _CONST:
        return _CONST
    f64 = np.float64
    log_g = np.log(1.0 - 2.0 ** (-5.0 - np.arange(4, dtype=f64)))
    freqs = 10000.0 ** (-np.arange(0, 128, 2, dtype=f64) / 128)
    rt = {}
    for d in range(2):
        lg = log_g if d == 0 else log_g[::-1]
        order = _order(d)
        isl = order >= CTX
        pos = np.where(isl, order - CTX, 0).astype(f64)
        ang = pos[:, None] * freqs[None, :]
        cos = np.where(isl[:, None], np.cos(ang), 1.0)
        sin = np.where(isl[:, None], np.sin(ang), 0.0)
        cosf = np.concatenate([cos, cos], axis=1)
        sinf = np.concatenate([-sin, sin], axis=1)
        i = (np.arange(T_ALL) % 128).astype(f64)
        for h in range(4):
            gq = np.exp((i + 1.0) * lg[h])[:, None]
            gk = (128.0 ** -0.5) * np.exp(-(i + 1.0) * lg[h])[:, None]
            tab = np.stack([cosf * gq, sinf * gq, cosf * gk, sinf * gk], axis=1).astype(np.float32)
            rt[(h, d)] = (np.ascontiguousarray(tab), np.full((128, 1), np.exp(128.0 * lg[h]), np.float32))
    _CONST["ret"] = rt
    fr = 10000.0 ** (-np.arange(0, 64, 2, dtype=f64) / 64)
    pos = np.arange(SEQ)
    ar = (pos // 64).astype(f64)[:, None] * fr[None, :]
    ac = (pos % 64).astype(f64)[:, None] * fr[None, :]
    cosl = np.concatenate([np.cos(ar), np.cos(ar), np.cos(ac), np.cos(ac)], axis=1)
    sinl = np.concatenate([-np.sin(ar), np.sin(ar), -np.sin(ac), np.sin(ac)], axis=1)
    cosa = np.concatenate([np.ones((CTX, 128)), cosl], axis=0)
    sina = np.concatenate([np.zeros((CTX, 128)), sinl], axis=0)
    _CONST["att"] = np.ascontiguousarray(np.stack([cosa, sina], axis=1).astype(np.float32))
    _CONST["ident"] = np.eye(128, dtype=np.float32)
    _CONST["maskT"] = np.triu(np.ones((128, 128), np.float32))
    _CONST["iota"] = np.ascontiguousarray(np.broadcast_to(np.arange(1, SEG + 1, dtype=np.float32), (128, SEG)))
    return _CONST


def run_mix(z_all, prm):
    C = mix_consts()
    nc = build_mix()
    in_maps = []
    orders = [_order(0), _order(1)]
    for c in range(NCORE):
        m = {"ident": C["ident"], "maskT": C["maskT"]}
        h, d = c % 4, c // 4
        zo = z_all[orders[d]]
        m["r_qkv"] = np.ascontiguousarray(np.stack([zo[:, h*128:(h+1)*128], zo[:, 512+h*128:512+(h+1)*128], zo[:, 1024+h*128:1024+(h+1)*128]], axis=1))
        m["r_tab"], m["r_g"] = C["ret"][(h, d)]
        par = np.zeros((128, 4, 3), np.float32)
        sB = np.zeros((128, 2, 2, 32), np.float32)
        sC = np.zeros((128, 2, 2, 64), np.float32)
        uT = np.zeros((2, 2, 32, T_ALL), np.float32)
        for tl in range(2):
            for gi in range(2):
                g = 4 * c + 2 * tl + gi
                rows = slice(gi * 64, (gi + 1) * 64)
                for dd in range(2):
                    par[rows, dd*2+tl, 0] = prm["s5_a_re"][dd, g]
                    par[rows, dd*2+tl, 1] = prm["s5_a_im"][dd, g]
                    par[rows, dd*2+tl, 2] = prm["s5_log_step"][dd, g]
                sB[rows, tl, 0, gi*16:(gi+1)*16] = prm["s5_b_re"][g]
                sB[rows, tl, 1, gi*16:(gi+1)*16] = prm["s5_b_im"][g]
                sC[rows, tl, 0, tl*32+gi*16:tl*32+(gi+1)*16] = prm["s5_c_re"][g].T
                sC[rows, tl, 1, tl*32+gi*16:tl*32+(gi+1)*16] = prm["s5_c_im"][g].T
            for dd in range(2):
                ucols = z_all[orders[dd], 2560 + (4*c + 2*tl) * 16: 2560 + (4*c + 2*tl + 2) * 16]
                uT[dd, tl] = ucols.T
        m["s_par"], m["s_B"], m["s_C"], m["s_uT"], m["s_iota"] = par, sB, sC, uT, C["iota"]
        hq, half = c // 2, c % 2
        kvh = hq // 2
        A0 = 3072
        qsel = np.concatenate([np.arange(half*128, (half+1)*128), CTX + np.arange(half*4096, (half+1)*4096)])
        m["a_q"] = np.ascontiguousarray(z_all[qsel, A0 + hq*128: A0 + (hq+1)*128])
        m["a_k"] = np.ascontiguousarray(z_all[:, A0 + 512 + kvh*128: A0 + 512 + (kvh+1)*128])
        m["a_v"] = np.ascontiguousarray(z_all[:, A0 + 768 + kvh*128: A0 + 768 + (kvh+1)*128])
        m["a_qtab"] = np.ascontiguousarray(C["att"][qsel])
        m["a_ktab"] = C["att"]
        m["a_w"] = np.ascontiguousarray(np.stack([np.broadcast_to(prm["q_norm_w"], (128, 128)), np.broadcast_to(prm["k_norm_w"], (128, 128))], axis=1))
        in_maps.append(m)
    res = _run(nc, in_maps)
    ret = np.zeros((2, T_ALL, 512), np.float32)
    s5y = np.zeros((2, T_ALL, 512), np.float32)
    att = np.zeros((T_ALL, 512), np.float32)
    for c in range(NCORE):
        h, d = c % 4, c // 4
        if "r_out" in res[c]:
            ret[d][orders[d], h*128:(h+1)*128] = res[c]["r_out"]
        for dd in range(2):
            s5y[dd][orders[dd], c*64:(c+1)*64] = res[c]["s_out"][dd]
        hq, half = c // 2, c % 2
        qsel = np.concatenate([np.arange(half*128, (half+1)*128), CTX + np.arange(half*4096, (half+1)*4096)])
        att[qsel, hq*128:(hq+1)*128] = res[c]["a_out"]
    return ret, s5y, att


def build_out():
    nc = bass.Bass("TRN2", target_bir_lowering=False)
    di = lambda name, shape, dt=F32: nc.dram_tensor(name, list(shape), dt, kind="ExternalInput").ap()
    do = lambda name, shape, dt=F32: nc.dram_tensor(name, list(shape), dt, kind="ExternalOutput").ap()
    ident_d = di("ident", [128, 128])
    x = di("x", [TOK_PC, D])
    rg = di("rg", [TOK_PC, 4, 512])
    s5 = di("s5", [TOK_PC, 3, 512])
    att = di("att", [TOK_PC, 512])
    cv = di("cv", [TOK_PC, 7, 512])
    vecs = di("vecs", [128, 5, 512])
    w_glu = di("w_glu", [512, 512])
    w_out = di("w_out", [D, D])
    g1 = di("g1", [2, 128, D])
    modc = di("modc", [128, 16, 4])
    w_r = di("w_r", [128, 16, 32])
    b_r = di("b_r", [128, 32])
    xmid = do("xmid", [TOK_PC, D])
    fT = do("fT", [D, TOK_PC], BF16)
    gates = do("gates", [TOK_PC, 32])

    P = Prog(nc)
    NF = 5
    psf = [P.ps(f"psf{i}", [128, 512], F32) for i in range(NF)]; b_psf = [P.buf() for _ in range(NF)]
    psb = [P.ps(f"psb{i}", [128, 512], BF16) for i in range(2)]; b_psb = [P.buf() for _ in range(2)]
    cnt = {"f": 0, "b": 0}

    def nf():
        i = cnt["f"] % NF; cnt["f"] += 1
        return psf[i], b_psf[i]

    def nb():
        i = cnt["b"] % 2; cnt["b"] += 1
        return psb[i], b_psb[i]

    ident = P.sb("identt", [128, 128], F32); b_id = P.buf()
    identb = P.sb("identb", [128, 128], BF16); b_idb = P.buf()
    P.dma("sp", "ident", lambda e: e.dma_start(out=ident[:], in_=ident_d), writes=[b_id])
    P.op("dve", lambda e: e.tensor_copy(out=identb[:], in_=ident[:]), reads=[b_id], writes=[b_idb])
    vt = P.sb("vecs_t", [128, 5, 512], F32); b_vt = P.buf()
    P.dma("sp", "vecs", lambda e: e.dma_start(out=vt[:], in_=vecs), writes=[b_vt])
    g1t = P.sb("g1t", [128, 2, D], F32); b_g1 = P.buf()
    P.dma("sp", "g1", lambda e: e.dma_start(out=g1t[:], in_=g1.rearrange("r p d -> p r d")), writes=[b_g1])
    mc = P.sb("mc", [128, 16, 4], F32); b_mc = P.buf()
    P.dma("sp", "mc", lambda e: e.dma_start(out=mc[:], in_=modc), writes=[b_mc])
    P.op("dve", lambda e: e.tensor_scalar(out=mc[:, :, 1], in0=mc[:, :, 1], scalar1=1.0, scalar2=None, op0=ALU.add), reads=[b_mc], writes=[b_mc])
    P.op("dve", lambda e: e.tensor_scalar(out=mc[:, :, 3], in0=mc[:, :, 3], scalar1=1.0, scalar2=None, op0=ALU.add), reads=[b_mc], writes=[b_mc])
    wr = P.sb("wr", [128, 16, 32], F32); b_wr = P.buf()
    P.dma("sp", "wr", lambda e: e.dma_start(out=wr[:], in_=w_r), writes=[b_wr])
    brt = P.sb("brt", [128, 32], F32); b_br = P.buf()
    P.dma("sp", "brt", lambda e: e.dma_start(out=brt[:], in_=b_r), writes=[b_br])
    wst = [P.sb(f"wst{i}", [128, 4, 512], F32) for i in range(2)]; b_wst = [P.buf() for _ in range(2)]
    wg = P.sb("wg", [128, 4, 512], BF16); b_wg = P.buf()
    P.dma("sp", "wst0", lambda e: e.dma_start(out=wst[0][:], in_=w_glu.rearrange("(k p) n -> p k n", p=128)), writes=[b_wst[0]])
    P.op("pool", lambda e: e.tensor_copy(out=wg[:], in_=wst[0][:]), reads=[b_wst[0]], writes=[b_wg])

    mixT = P.sb("mixT", [128, 16, TOK_PC], BF16); b_mixT = [P.buf() for _ in TILES_PC]
    rgt = P.sb("rgt", [128, 4, 512], F32); b_rg = P.buf()
    s5t = P.sb("s5t", [128, 3, 512], F32); b_s5 = P.buf()
    att_t = P.sb("att_t", [128, 512], F32); b_att = P.buf()
    cvt = P.sb("cvt", [128, 7, 512], F32); b_cv = P.buf()
    tm = [P.sb(f"tm{i}", [128, 512], F32) for i in range(5)]; b_tm = [P.buf() for _ in range(5)]
    yb16 = P.sb("yb16", [128, 512], BF16); b_yb16 = P.buf()
    yT = P.sb("yT", [128, 4, 128], BF16); b_yT = P.buf()
    mix = P.sb("mix", [128, D], BF16); b_mix = [P.buf() for _ in range(4)]

    def tt(eng, o, a, b, op, rd, wr_):
        P.op(eng, lambda e: e.tensor_tensor(out=o, in0=a, in1=b, op=op), reads=rd, writes=wr_)

    for ti, (r0, n) in enumerate(TILES_PC):
        P.dma("sp", "rgt", lambda e, r0=r0, n=n: e.dma_start(out=rgt[0:n], in_=rg[r0:r0+n]), writes=[b_rg])
        P.dma("sp", "s5t", lambda e, r0=r0, n=n: e.dma_start(out=s5t[0:n], in_=s5[r0:r0+n]), writes=[b_s5])
        P.dma("sp", "att_t", lambda e, r0=r0, n=n: e.dma_start(out=att_t[0:n], in_=att[r0:r0+n]), writes=[b_att])
        P.dma("sp", "cvt", lambda e, r0=r0, n=n: e.dma_start(out=cvt[0:n], in_=cv[r0:r0+n]), writes=[b_cv])
        P.op("act", lambda e, n=n: e.activation(out=tm[0][0:n], in_=rgt[0:n, 2, :], func=AF.Silu), reads=[b_rg], writes=[b_tm[0]])
        P.op("act", lambda e, n=n: e.activation(out=tm[1][0:n], in_=rgt[0:n, 3, :], func=AF.Silu), reads=[b_rg], writes=[b_tm[1]])
        tt("dve", tm[0][0:n], tm[0][0:n], rgt[0:n, 0, :], ALU.mult, [b_tm[0], b_rg], [b_tm[0]])
        tt("pool", tm[1][0:n], tm[1][0:n], rgt[0:n, 1, :], ALU.mult, [b_tm[1], b_rg], [b_tm[1]])
        tt("dve", mix[0:n, 0:512], tm[0][0:n], tm[1][0:n], ALU.add, [b_tm[0], b_tm[1]], [b_mix[0]])
        tt("pool", tm[2][0:n], s5t[0:n, 0, :], s5t[0:n, 1, :], ALU.add, [b_s5], [b_tm[2]])
        tt("dve", tm[3][0:n], s5t[0:n, 2, :], vt[0:n, 0, :], ALU.mult, [b_s5, b_vt], [b_tm[3]])
        tt("pool", tm[2][0:n], tm[2][0:n], tm[3][0:n], ALU.add, [b_tm[2], b_tm[3]], [b_tm[2]])
        tt("pool", tm[3][0:n], tm[2][0:n], tm[2][0:n], ALU.mult, [b_tm[2]], [b_tm[3]])
        P.op("dve", lambda e, n=n: e.tensor_scalar(out=tm[3][0:n], in0=tm[3][0:n], scalar1=0.044715, scalar2=1.0, op0=ALU.mult, op1=ALU.add), reads=[b_tm[3]], writes=[b_tm[3]])
        tt("dve", tm[3][0:n], tm[3][0:n], tm[2][0:n], ALU.mult, [b_tm[3], b_tm[2]], [b_tm[3]])
        P.op("act", lambda e, n=n: e.activation(out=tm[3][0:n], in_=tm[3][0:n], func=AF.Sigmoid, scale=1.5957691216057308), reads=[b_tm[3]], writes=[b_tm[3]])
        tt("dve", tm[2][0:n], tm[2][0:n], tm[3][0:n], ALU.mult, [b_tm[2], b_tm[3]], [b_tm[2]])
        P.op("act", lambda e, n=n: e.copy(out=yb16[0:n], in_=tm[2][0:n]), reads=[b_tm[2]], writes=[b_yb16])
        pt_, bpt_ = nb()
        for k in range(4):
            P.op("pe", lambda e, pt_=pt_, k=k, n=n: e.transpose(pt_[:, k*128:k*128+n], yb16[0:n, k*128:(k+1)*128], identb[0:n, 0:n]), reads=[b_yb16, b_idb], writes=[bpt_])
        P.op("act", lambda e, pt_=pt_, n=n: e.copy(out=yT[:, :, 0:n], in_=pt_[:, 0:512].rearrange("p (a d) -> p a d", a=4)[:, :, 0:n]), reads=[bpt_], writes=[b_yT])
        pg, bpg = nf()
        for k in range(4):
            P.op("pe", lambda e, pg=pg, k=k, n=n: e.matmul(pg[0:n, :], yT[:, k, 0:n], wg[:, k, :], start=(k == 0), stop=(k == 3)), reads=[b_yT, b_wg], writes=[bpg])
        tt("dve", tm[3][0:n], pg[0:n, :], vt[0:n, 1, :], ALU.add, [bpg, b_vt], [b_tm[3]])
        P.op("act", lambda e, n=n: e.activation(out=tm[3][0:n], in_=tm[3][0:n], func=AF.Sigmoid), reads=[b_tm[3]], writes=[b_tm[3]])
        tt("dve", mix[0:n, 512:1024], tm[2][0:n], tm[3][0:n], ALU.mult, [b_tm[2], b_tm[3]], [b_mix[1]])
        P.op("act", lambda e, n=n: e.copy(out=mix[0:n, 1024:1536], in_=att_t[0:n]), reads=[b_att], writes=[b_mix[2]])
        tt("pool", tm[0][0:n], cvt[0:n, 1, :], cvt[0:n, 2, :], ALU.mult, [b_cv], [b_tm[0]])
        tt("dve", tm[1][0:n], cvt[0:n, 3, :], cvt[0:n, 4, :], ALU.mult, [b_cv], [b_tm[1]])
        tt("pool", tm[4][0:n], cvt[0:n, 5, :], cvt[0:n, 6, :], ALU.mult, [b_cv], [b_tm[4]])
        tt("dve", tm[0][0:n], tm[0][0:n], vt[0:n, 2, :], ALU.mult, [b_tm[0], b_vt], [b_tm[0]])
        tt("pool", tm[1][0:n], tm[1][0:n], vt[0:n, 3, :], ALU.mult, [b_tm[1], b_vt], [b_tm[1]])
        tt("dve", tm[4][0:n], tm[4][0:n], vt[0:n, 4, :], ALU.mult, [b_tm[4], b_vt], [b_tm[4]])
        tt("pool", tm[0][0:n], tm[0][0:n], tm[1][0:n], ALU.add, [b_tm[0], b_tm[1]], [b_tm[0]])
        tt("dve", tm[0][0:n], tm[0][0:n], tm[4][0:n], ALU.add, [b_tm[0], b_tm[4]], [b_tm[0]])
        tt("dve", mix[0:n, 1536:2048], tm[0][0:n], cvt[0:n, 0, :], ALU.mult, [b_tm[0], b_cv], [b_mix[3]])
        for kg in range(4):
            pt_, bpt_ = nb()
            for kk in range(4):
                k = kg * 4 + kk
                P.op("pe", lambda e, pt_=pt_, kk=kk, k=k, n=n: e.transpose(pt_[:, kk*128:kk*128+n], mix[0:n, k*128:(k+1)*128], identb[0:n, 0:n]), reads=[b_mix[kg], b_idb], writes=[bpt_])
            eng = "act" if kg % 2 else "dve"
            if eng == "act":
                P.op("act", lambda e, pt_=pt_, kg=kg, n=n, r0=r0: e.copy(out=mixT[:, kg*4:(kg+1)*4, r0:r0+n], in_=pt_[:, 0:512].rearrange("p (a d) -> p a d", a=4)[:, :, 0:n]), reads=[bpt_], writes=[b_mixT[ti]])
            else:
                P.op("dve", lambda e, pt_=pt_, kg=kg, n=n, r0=r0: e.tensor_copy(out=mixT[:, kg*4:(kg+1)*4, r0:r0+n], in_=pt_[:, 0:512].rearrange("p (a d) -> p a d", a=4)[:, :, 0:n]), reads=[bpt_], writes=[b_mixT[ti]])

    wb = [P.sb(f"wb{i}", [128, 16, 512], BF16) for i in range(2)]; b_wb = [P.buf() for _ in range(2)]
    xp = [P.sb(f"xp{i}", [128, 512], F32) for i in range(3)]; b_xp = [P.buf() for _ in range(3)]
    b_xm_dram = [P.buf() for _ in TILES_PC]
    w_v = w_out.rearrange("(k p) n -> p k n", p=128)
    wsi = 1; xi = 0
    for cb in range(4):
        s = cb % 2
        for kq in range(4):
            ws_ = wsi % 2; wsi += 1
            P.dma("sp", f"wst{ws_}", lambda e, ws_=ws_, cb=cb, kq=kq: e.dma_start(out=wst[ws_][:], in_=w_v[:, kq*4:(kq+1)*4, cb*512:(cb+1)*512]), writes=[b_wst[ws_]])
            P.op("pool", lambda e, ws_=ws_, s=s, kq=kq: e.tensor_copy(out=wb[s][:, kq*4:(kq+1)*4, :], in_=wst[ws_][:]), reads=[b_wst[ws_]], writes=[b_wb[s]])
        for ti, (r0, n) in enumerate(TILES_PC):
            isctx = 1 if r0 >= LAT_PC else 0
            xs_ = xi % 3; xi += 1
            P.dma("sp", f"xp{xs_}", lambda e, xs_=xs_, r0=r0, n=n, cb=cb: e.dma_start(out=xp[xs_][0:n], in_=x[r0:r0+n, cb*512:(cb+1)*512]), writes=[b_xp[xs_]])
            po, bpo = nf()
            for k in range(16):
                P.op("pe", lambda e, po=po, k=k, n=n, r0=r0, s=s: e.matmul(po[0:n, :], mixT[:, k, r0:r0+n], wb[s][:, k, :], start=(k == 0), stop=(k == 15)), reads=[b_mixT[ti], b_wb[s]], writes=[bpo])
            t_ = tm[xi % 2]; bt_ = b_tm[xi % 2]
            tt("dve", t_[0:n], po[0:n, :], g1t[0:n, isctx, cb*512:(cb+1)*512], ALU.mult, [bpo, b_g1], [bt_])
            tt("pool", xp[xs_][0:n], xp[xs_][0:n], t_[0:n], ALU.add, [b_xp[xs_], bt_], [b_xp[xs_]])
            P.dma("sp", f"xpo{xs_}", lambda e, xs_=xs_, r0=r0, n=n, cb=cb: e.dma_start(out=xmid[r0:r0+n, cb*512:(cb+1)*512], in_=xp[xs_][0:n]), reads=[b_xp[xs_]], writes=[b_xm_dram[ti]])

    xt = [P.sb(f"xt{i}", [128, D], F32) for i in range(1)]; b_xt = [P.buf() for _ in range(1)]
    xn = P.sb("xn", [128, D], F32); b_xn = P.buf()
    junk = mix
    ss = P.sb("ss", [128, 2], F32); b_ss = P.buf()
    f32t = P.sb("f32t", [128, 16, 128], F32); b_f32 = P.buf()
    lg = P.sb("lg", [128, 32], F32); b_lg = P.buf()
    rc = P.sb("rc", [128, 16], F32); b_rc = P.buf()
    ex = P.sb("ex", [128, 32], F32); b_ex = P.buf()
    gt = [P.sb(f"gt{i}", [128, 32], F32) for i in range(2)]; b_gt = [P.buf() for _ in range(2)]
    fT_v = fT.rearrange("(k p) t -> p k t", p=128)
    for ti, (r0, n) in enumerate(TILES_PC):
        s = ti % 2
        isctx = 1 if r0 >= LAT_PC else 0
        X = xt[0]; bX = b_xt[0]
        P.dma("sp", "xt0", lambda e, X=X, r0=r0, n=n: e.dma_start(out=X[0:n, :], in_=xmid[r0:r0+n, :]), reads=[b_xm_dram[ti]], writes=[bX])
        P.op("act", lambda e, X=X, n=n: e.activation(out=junk[0:n, :], in_=X[0:n, :], func=AF.Square, accum_out=ss[0:n, 0:1]), reads=[bX], writes=b_mix + [b_ss])
        P.op("act", lambda e, n=n: e.activation(out=ss[0:n, 1:2], in_=ss[0:n, 0:1], func=AF.Sqrt, scale=1.0 / D, bias=EPS), reads=[b_ss], writes=[b_ss])
        P.op("dve", lambda e, n=n: e.reciprocal(out=ss[0:n, 1:2], in_=ss[0:n, 1:2]), reads=[b_ss], writes=[b_ss])
        P.op("dve", lambda e, X=X, n=n: e.tensor_scalar(out=xn[0:n, :], in0=X[0:n, :], scalar1=ss[0:n, 1:2], scalar2=None, op0=ALU.mult), reads=[bX, b_ss], writes=[b_xn])
        for kg in range(4):
            pt_, bpt_ = nf()
            for kk in range(4):
                k = kg * 4 + kk
                P.op("pe", lambda e, pt_=pt_, kk=kk, k=k, n=n: e.transpose(pt_[:, kk*128:kk*128+n], xn[0:n, k*128:(k+1)*128], ident[0:n, 0:n]), reads=[b_xn, b_id], writes=[bpt_])
            for kk in range(4):
                k = kg * 4 + kk
                if kk % 2 == 0:
                    P.op("dve", lambda e, pt_=pt_, kk=kk, k=k, n=n, isctx=isctx: e.tensor_scalar(
                        out=f32t[:, k, 0:n], in0=pt_[:, kk*128:kk*128+n], scalar1=mc[:, k, 2*isctx+1:2*isctx+2], scalar2=mc[:, k, 2*isctx:2*isctx+1], op0=ALU.mult, op1=ALU.add),
                        reads=[bpt_, b_mc], writes=[b_f32])
                else:
                    P.op("act", lambda e, pt_=pt_, kk=kk, k=k, n=n, isctx=isctx: e.activation(
                        out=f32t[:, k, 0:n], in_=pt_[:, kk*128:kk*128+n], func=AF.Identity, scale=mc[:, k, 2*isctx+1:2*isctx+2], bias=mc[:, k, 2*isctx:2*isctx+1]),
                        reads=[bpt_, b_mc], writes=[b_f32])
        P.op("pool", lambda e, n=n, r0=r0: e.tensor_copy(out=mixT[:, :, r0:r0+n], in_=f32t[:, :, 0:n]), reads=[b_f32], writes=[b_mixT[ti]])
        pl, bpl = nf()
        for k in range(16):
            P.op("pe", lambda e, pl=pl, k=k, n=n: e.matmul(pl[0:n, 0:32], f32t[:, k, 0:n], wr[:, k, :], start=(k == 0), stop=(k == 15)), reads=[b_f32, b_wr], writes=[bpl])
        tt("dve", lg[0:n], pl[0:n, 0:32], brt[0:n], ALU.add, [bpl, b_br], [b_lg])
        P.op("dve", lambda e, n=n: e.max(out=rc[0:n, 0:8], in_=lg[0:n]), reads=[b_lg], writes=[b_rc])
        P.op("dve", lambda e, n=n: e.tensor_scalar(out=rc[0:n, 8:9], in0=rc[0:n, 0:1], scalar1=-1.0, scalar2=None, op0=ALU.mult), reads=[b_rc], writes=[b_rc])
        P.op("act", lambda e, n=n: e.activation(out=ex[0:n], in_=lg[0:n], func=AF.Exp, bias=rc[0:n, 8:9], scale=1.0), reads=[b_lg, b_rc], writes=[b_ex])
        P.op("dve", lambda e, n=n: e.tensor_scalar(out=lg[0:n], in0=lg[0:n], scalar1=rc[0:n, 3:4], scalar2=None, op0=ALU.is_ge), reads=[b_lg, b_rc], writes=[b_lg])
        tt("dve", ex[0:n], ex[0:n], lg[0:n], ALU.mult, [b_ex, b_lg], [b_ex])
        P.op("dve", lambda e, n=n: e.reduce_sum(out=rc[0:n, 9:10], in_=ex[0:n], axis=AX.X), reads=[b_ex], writes=[b_rc])
        P.op("dve", lambda e, n=n: e.reciprocal(out=rc[0:n, 10:11], in_=rc[0:n, 9:10]), reads=[b_rc], writes=[b_rc])
        P.op("dve", lambda e, n=n, s=s: e.tensor_scalar(out=gt[s][0:n], in0=ex[0:n], scalar1=rc[0:n, 10:11], scalar2=None, op0=ALU.mult), reads=[b_ex, b_rc], writes=[b_gt[s]])
        P.dma("sp", f"gto{s}", lambda e, s=s, n=n, r0=r0: e.dma_start(out=gates[r0:r0+n, :], in_=gt[s][0:n]), reads=[b_gt[s]])
    for k in range(16):
        P.dma("sp", "f16o", lambda e, k=k: e.dma_start(out=fT_v[:, k, :], in_=mixT[:, k, :]), reads=b_mixT)
    P.emit()
    return nc


def _shift(a, k):
    out = np.zeros_like(a)
    if k == -1:
        out[1:] = a[:-1]
    elif k == 1:
        out[:-1] = a[1:]
    return out


def run_out(x_shards, z_all, ret, s5y, att, mod_l, prm):
    C = mix_consts()
    nc = build_out()
    lat = lambda a: a[CTX:]
    ctx = lambda a: a[:CTX]
    sh = lambda a: shard_tokens(lat(a), ctx(a))
    gf = z_all[:, 1536:2048]; gb = z_all[:, 2048:2560]
    rg_s = sh(np.stack([ret[0], ret[1], gf, gb], axis=1))
    s5_s = sh(np.stack([s5y[0], s5y[1], z_all[:, 2560:3072]], axis=1))
    att_s = sh(att)
    zc = z_all[:, 4096:5632]
    bg, cg, hh = zc[:, 0:512], zc[:, 512:1024], zc[:, 1024:1536]

    def sh3(a):
        parts = []
        for k in (-1, 0, 1):
            parts.append(np.concatenate([_shift(ctx(a), k), _shift(lat(a), k)], axis=0) if k else a)
        return parts
    cs_, hs_ = sh3(cg), sh3(hh)
    cv_s = sh(np.stack([bg, cs_[0], hs_[0], cs_[1], hs_[1], cs_[2], hs_[2]], axis=1))
    rep = lambda v: np.broadcast_to(v, (128,) + v.shape)
    vecs = np.ascontiguousarray(np.stack([rep(prm["s5_d"]), rep(prm["s5_b_glu"]), rep(prm["conv_w"][0]), rep(prm["conv_w"][1]), rep(prm["conv_w"][2])], axis=1))
    g1 = np.ascontiguousarray(np.stack([rep(mod_l[0, 2*D:3*D]), rep(mod_l[1, 2*D:3*D])]))
    modc = np.ascontiguousarray(np.stack([cols128(mod_l[0, 3*D:4*D]), cols128(mod_l[0, 4*D:5*D]), cols128(mod_l[1, 3*D:4*D]), cols128(mod_l[1, 4*D:5*D])], axis=-1))
    b_r = np.ascontiguousarray(rep(prm["b_router"]))
    in_maps = []
    for c in range(NCORE):
        in_maps.append({"ident": C["ident"], "x": x_shards[c], "rg": rg_s[c], "s5": s5_s[c], "att": att_s[c], "cv": cv_s[c],
                        "vecs": vecs, "w_glu": prm["s5_w_glu"], "w_out": prm["w_out"], "g1": g1, "modc": modc,
                        "w_r": np.ascontiguousarray(prm["w_router"].reshape(16, 128, 32).transpose(1, 0, 2)), "b_r": b_r})
    res = _run(nc, in_maps)
    return [r["xmid"] for r in res], [r["fT"] for r in res], [r["gates"] for r in res]


E_PC = 4
NCORE_E = 32 // E_PC
DE = 1024
E_TILES = [(i * 512, 512) for i in range(16)] + [(8192, 256)]


def build_moe():
    nc = bass.Bass("TRN2", target_bir_lowering=False)
    di = lambda name, shape, dt=F32: nc.dram_tensor(name, list(shape), dt, kind="ExternalInput").ap()
    fT = di("fT", [D, T_ALL], BF16)
    gb = di("gb", [E_PC, 128, T_ALL])
    wg = di("wg", [E_PC, D, DE]); wu = di("wu", [E_PC, D, DE]); wd = di("wd", [E_PC, DE, D])
    bgu = di("bgu", [128, E_PC, 16]); bd = di("bd", [128, E_PC, 16])
    yT = nc.dram_tensor("yT", [D, T_ALL], F32, kind="ExternalOutput").ap()
    P = Prog(nc)
    NF = 8
    psf = [P.ps(f"psf{i}", [128, 512], F32) for i in range(NF)]; b_psf = [P.buf() for _ in range(NF)]
    cnt = {"f": 0}

    def nf():
        i = cnt["f"] % NF; cnt["f"] += 1
        return psf[i], b_psf[i]

    bgut = P.sb("bgut", [128, E_PC, 16], F32); b_bgu = P.buf()
    bdt = P.sb("bdt", [128, E_PC, 16], F32); b_bd = P.buf()
    P.dma("sp", "bgu", lambda e: e.dma_start(out=bgut[:], in_=bgu), writes=[b_bgu])
    P.dma("sp", "bd", lambda e: e.dma_start(out=bdt[:], in_=bd), writes=[b_bd])
    wgb = P.sb("wgb", [128, 16, DE], BF16); wub = P.sb("wub", [128, 16, DE], BF16); wdb = P.sb("wdb", [128, 8, D], BF16)
    b_wgb = P.buf(); b_wub = P.buf(); b_wdb = P.buf()
    wst = [P.sb(f"wst{i}", [128, 4, 512], F32) for i in range(2)]; b_wst = [P.buf() for _ in range(2)]
    ft = [P.sb(f"ft{i}", [128, 16, 512], BF16) for i in range(2)]; b_ft = [P.buf() for _ in range(2)]
    gtile = [P.sb(f"gtile{i}", [128, 512], F32) for i in range(2)]; b_gtile = [P.buf() for _ in range(2)]
    actT = P.sb("actT", [128, 8, 512], BF16); b_actT = P.buf()
    tg = [P.sb(f"tg{i}", [128, 512], F32) for i in range(2)]; b_tg = [P.buf() for _ in range(2)]
    tsg = [P.sb(f"tsg{i}", [128, 512], F32) for i in range(2)]; b_tsg = [P.buf() for _ in range(2)]
    tu = [P.sb(f"tu{i}", [128, 512], F32) for i in range(2)]; b_tu = [P.buf() for _ in range(2)]
    yp = [P.sb(f"yp{i}", [128, 512], F32) for i in range(3)]; b_yp = [P.buf() for _ in range(3)]
    yo = [P.sb(f"yo{i}", [128, 512], F32) for i in range(3)]; b_yo = [P.buf() for _ in range(3)]
    b_dram = [[P.buf() for _ in E_TILES] for _ in range(16)]
    fT_v = fT.rearrange("(k p) t -> p k t", p=128)
    yT_v = yT.rearrange("(m p) t -> p m t", p=128)
    wsi = 0; fi = 0; oi = 0; mi = 0
    ne = globals().get("MOE_NE", E_PC)
    for ex in range(ne):
        for (src, dst, bdst, nk, ncol) in ((wg, wgb, b_wgb, 16, DE), (wu, wub, b_wub, 16, DE), (wd, wdb, b_wdb, 8, D)):
            sv = src[ex].rearrange("(k p) n -> p k n", p=128)
            for kq in range(nk // 4):
                for cbk in range(ncol // 512):
                    ws_ = wsi % 2; wsi += 1
                    P.dma("sp", f"wst{ws_}_{ex % 2}", lambda e, ws_=ws_, sv=sv, kq=kq, cbk=cbk: e.dma_start(out=wst[ws_][:], in_=sv[:, kq*4:(kq+1)*4, cbk*512:(cbk+1)*512]), writes=[b_wst[ws_]])
                    eng = "pool" if wsi % 2 else "act"
                    if eng == "pool":
                        P.op("pool", lambda e, ws_=ws_, dst=dst, kq=kq, cbk=cbk: e.tensor_copy(out=dst[:, kq*4:(kq+1)*4, cbk*512:(cbk+1)*512], in_=wst[ws_][:]), reads=[b_wst[ws_]], writes=[bdst])
                    else:
                        P.op("act", lambda e, ws_=ws_, dst=dst, kq=kq, cbk=cbk: e.copy(out=dst[:, kq*4:(kq+1)*4, cbk*512:(cbk+1)*512], in_=wst[ws_][:]), reads=[b_wst[ws_]], writes=[bdst])
        for tix, (c0, w) in enumerate(E_TILES):
            fs = fi % 2; fi += 1
            for kq in range(4):
                P.dma("sp", f"ft{fs}_{ex % 2}", lambda e, fs=fs, c0=c0, w=w, kq=kq: e.dma_start(out=ft[fs][:, kq*4:(kq+1)*4, 0:w], in_=fT_v[:, kq*4:(kq+1)*4, c0:c0+w]), writes=[b_ft[fs]])
            P.dma("sp", f"gtile{fs}_{ex % 2}", lambda e, fs=fs, c0=c0, w=w, ex=ex: e.dma_start(out=gtile[fs][:, 0:w], in_=gb[ex, :, c0:c0+w]), writes=[b_gtile[fs]])
            for m in range(8):
                psg, bpsg = nf(); psu, bpsu = nf()
                for k in range(16):
                    P.op("pe", lambda e, psg=psg, k=k, m=m, fs=fs, w=w: e.matmul(psg[:, 0:w], wgb[:, k, m*128:(m+1)*128], ft[fs][:, k, 0:w], start=(k == 0), stop=(k == 15)), reads=[b_wgb, b_ft[fs]], writes=[bpsg])
                for k in range(16):
                    P.op("pe", lambda e, psu=psu, k=k, m=m, fs=fs, w=w: e.matmul(psu[:, 0:w], wub[:, k, m*128:(m+1)*128], ft[fs][:, k, 0:w], start=(k == 0), stop=(k == 15)), reads=[b_wub, b_ft[fs]], writes=[bpsu])
                s = mi % 2; mi += 1
                P.op("dve", lambda e, psg=psg, s=s, m=m, w=w, ex=ex: e.tensor_scalar(out=tg[s][:, 0:w], in0=psg[:, 0:w], scalar1=bgut[:, ex, m:m+1], scalar2=7.0, op0=ALU.add, op1=ALU.min), reads=[bpsg, b_bgu], writes=[b_tg[s]])
                P.op("act", lambda e, s=s, w=w: e.activation(out=tsg[s][:, 0:w], in_=tg[s][:, 0:w], func=AF.Sigmoid, scale=1.702), reads=[b_tg[s]], writes=[b_tsg[s]])
                P.op("dve", lambda e, psu=psu, s=s, m=m, w=w, ex=ex: e.tensor_scalar(out=tu[s][:, 0:w], in0=psu[:, 0:w], scalar1=bgut[:, ex, 8+m:9+m], scalar2=7.0, op0=ALU.add, op1=ALU.min), reads=[bpsu, b_bgu], writes=[b_tu[s]])
                P.op("dve", lambda e, s=s, w=w: e.tensor_scalar(out=tu[s][:, 0:w], in0=tu[s][:, 0:w], scalar1=-7.0, scalar2=1.0, op0=ALU.max, op1=ALU.add), reads=[b_tu[s]], writes=[b_tu[s]])
                P.op("pool", lambda e, s=s, w=w: e.tensor_tensor(out=tg[s][:, 0:w], in0=tg[s][:, 0:w], in1=tsg[s][:, 0:w], op=ALU.mult), reads=[b_tg[s], b_tsg[s]], writes=[b_tg[s]])
                P.op("dve", lambda e, s=s, m=m, w=w: e.tensor_tensor(out=actT[:, m, 0:w], in0=tg[s][:, 0:w], in1=tu[s][:, 0:w], op=ALU.mult), reads=[b_tg[s], b_tu[s]], writes=[b_actT])
            for m2 in range(16):
                psy, bpsy = nf()
                for k in range(8):
                    P.op("pe", lambda e, psy=psy, k=k, m2=m2, w=w: e.matmul(psy[:, 0:w], wdb[:, k, m2*128:(m2+1)*128], actT[:, k, 0:w], start=(k == 0), stop=(k == 7)), reads=[b_wdb, b_actT], writes=[bpsy])
                os_ = oi % 3; oi += 1
                bdr = b_dram[m2][tix]
                if ex > 0:
                    P.dma("sp", f"yp{os_}_{ex % 2}", lambda e, os_=os_, m2=m2, c0=c0, w=w: e.dma_start(out=yp[os_][:, 0:w], in_=yT_v[:, m2, c0:c0+w]), reads=[bdr], writes=[b_yp[os_]])
                P.op("dve", lambda e, psy=psy, os_=os_, m2=m2, w=w, fs=fs, ex=ex: e.scalar_tensor_tensor(out=yo[os_][:, 0:w], in0=psy[:, 0:w], scalar=bdt[:, ex, m2:m2+1], in1=gtile[fs][:, 0:w], op0=ALU.add, op1=ALU.mult),
                     reads=[bpsy, b_bd, b_gtile[fs]], writes=[b_yo[os_]])
                if ex > 0:
                    P.op("pool", lambda e, os_=os_, w=w: e.tensor_tensor(out=yo[os_][:, 0:w], in0=yo[os_][:, 0:w], in1=yp[os_][:, 0:w], op=ALU.add), reads=[b_yo[os_], b_yp[os_]], writes=[b_yo[os_]])
                P.dma("sp", f"yo{os_}_{ex % 2}", lambda e, os_=os_, m2=m2, c0=c0, w=w: e.dma_start(out=yT_v[:, m2, c0:c0+w], in_=yo[os_][:, 0:w]), reads=[b_yo[os_]], writes=[bdr])
    P.emit()
    return nc


def run_moe(fT_shards, gate_shards, prm):
    nc = build_moe()
    fT = np.ascontiguousarray(np.concatenate(fT_shards, axis=1))
    gates = np.concatenate(gate_shards, axis=0)
    in_maps = []
    for c in range(NCORE_E):
        es = slice(E_PC * c, E_PC * (c + 1))
        wgu = prm["w_gate_up"][es]
        bgu = prm["b_gate_up"][es]
        bg = bgu[:, 0::2].reshape(E_PC, 8, 128); bu = bgu[:, 1::2].reshape(E_PC, 8, 128)
        bgu_l = np.ascontiguousarray(np.concatenate([bg, bu], axis=1).transpose(2, 0, 1))
        bd_l = np.ascontiguousarray(prm["b_down"][es].reshape(E_PC, 16, 128).transpose(2, 0, 1))
        gbc = np.ascontiguousarray(np.broadcast_to(gates[:, es].T[:, None, :], (E_PC, 128, T_ALL)))
        in_maps.append({"fT": fT, "gb": gbc, "wg": np.ascontiguousarray(wgu[:, :, 0::2]), "wu": np.ascontiguousarray(wgu[:, :, 1::2]),
                        "wd": prm["w_down"][es], "bgu": bgu_l, "bd": bd_l})
    res = _run(nc, in_maps)
    parts = []
    for cp in range(NCORE):
        parts.append(np.ascontiguousarray(np.stack([res[c]["yT"][:, cp*TOK_PC:(cp+1)*TOK_PC].T for c in range(NCORE_E)], axis=0)))
    return parts


_LAYER_KEYS = ["w_in", "w_out", "s5_a_re", "s5_a_im", "s5_log_step", "s5_b_re", "s5_b_im", "s5_c_re", "s5_c_im",
               "s5_d", "s5_w_glu", "s5_b_glu", "q_norm_w", "k_norm_w", "conv_w", "w_router", "b_router",
               "w_gate_up", "b_gate_up", "w_down", "b_down"]


def _combine_maps(x_shards, parts, g2pair):
    rep = lambda v: np.broadcast_to(v, (128, D))
    g2 = np.ascontiguousarray(np.stack([rep(g2pair[0]), rep(g2pair[1])]))
    return g2


def run_proj2(x_shards, mod_l, w_in_l, parts=None, g2pair=None, project=True):
    combine = parts is not None
    nc = build_proj(combine, project)
    ident = np.eye(128, dtype=np.float32)
    in_maps = []
    for i in range(NCORE):
        m = {"x": x_shards[i], "ident": ident}
        if project:
            m["modc"] = np.ascontiguousarray(np.stack([cols128(mod_l[0, 0:D]), cols128(mod_l[0, D:2*D]), cols128(mod_l[1, 0:D]), cols128(mod_l[1, D:2*D])], axis=-1))
            m["w_in"] = w_in_l
        if combine:
            m["part"] = parts[i]
            m["g2"] = _combine_maps(x_shards, parts, g2pair)
        in_maps.append(m)
    res = _run(nc, in_maps)
    z = [r["z"] for r in res] if project else None
    xo = [r["xo"] for r in res] if combine else None
    return z, xo


def kernel(**inputs):
    inp = {k: np.asarray(v) for k, v in inputs.items()}
    mod = run_mod(inp["c"], inp["c_ctx"], inp["w_mod"], inp["b_mod"])
    x_shards = shard_tokens(inp["x"][0], inp["ctx"][0])
    parts = None
    g2pair = None
    for l in range(2):
        prm = {k: inp[k][l] for k in _LAYER_KEYS}
        z, xo = run_proj2(x_shards, mod[l], prm["w_in"], parts, g2pair)
        if xo is not None:
            x_shards = xo
        zl, zc = unshard_tokens(z)
        z_all = np.concatenate([zc, zl], axis=0)
        ret, s5y, att = run_mix(z_all, prm)
        xmid, fT, gates = run_out(x_shards, z_all, ret, s5y, att, mod[l], prm)
        parts = run_moe(fT, gates, prm)
        x_shards = xmid
        g2pair = (mod[l][0, 5*D:6*D], mod[l][1, 5*D:6*D])
    _, xo = run_proj2(x_shards, None, None, parts, g2pair, project=False)
    lat, _ = unshard_tokens(xo)
    return lat[None].astype(np.float32)
```

```python
import contextlib
import numpy as np
import ml_dtypes
import concourse.bass as bass
import concourse.mybir as mybir
from concourse.bass_utils import run_bass_kernel_spmd

F32 = mybir.dt.float32
BF16 = mybir.dt.bfloat16
I32 = mybir.dt.int32
ALU = mybir.AluOpType
AF = mybir.ActivationFunctionType
AX = mybir.AxisListType


class Buf:
    __slots__ = ("name", "w", "r")

    def __init__(self, name):
        self.name = name
        self.w = None
        self.r = []


class Prog:
    ENG = ("pe", "dve", "act", "pool", "sp")

    def __init__(self, nc):
        self.nc = nc
        self.stream = {e: [] for e in self.ENG}
        self.seen = {e: {} for e in self.ENG}
        self.needed = {e: set() for e in self.ENG}
        self.stack = contextlib.ExitStack()
        self.esem = {e: self.stack.enter_context(nc.semaphore("s_" + e))
                     for e in ("pe", "dve", "act", "pool")}
        self.dsem = {}
        self.dtoks = []
        self.nbuf = 0

    def buf(self, name=None):
        self.nbuf += 1
        return Buf(name or f"b{self.nbuf}")

    def sb(self, name, shape, dt):
        return self.stack.enter_context(self.nc.sbuf_tensor(name, list(shape), dt))

    def ps(self, name, shape, dt):
        return self.stack.enter_context(self.nc.psum_tensor(name, list(shape), dt))

    def _waits(self, eng, reads, writes):
        toks = []
        for b in reads:
            if b.w is not None:
                toks.append(b.w + (True,))
        for b in writes:
            if b.w is not None:
                toks.append(b.w + (False,))
            toks.extend(t + (False,) for t in b.r)
        need = {}
        for kind, src, val, raw in toks:
            if kind == "eng" and src == eng and (not raw or eng == "pe"):
                continue
            key = (kind, src)
            if self.seen[eng].get(key, -1) >= val:
                continue
            if need.get(key, -1) < val:
                need[key] = val
        for key, val in need.items():
            self.seen[eng][key] = val
            if key[0] == "eng":
                self.needed[key[1]].add(val)
        return list(need.items())

    def op(self, eng, fn, reads=(), writes=()):
        waits = self._waits(eng, reads, writes)
        idx = len(self.stream[eng])
        tok = ("eng", eng, idx)
        self.stream[eng].append((waits, fn, None))
        for b in reads:
            b.r.append(tok)
        for b in writes:
            b.w = tok
            b.r = []
        return tok

    def dma(self, q, semkey, fn, reads=(), writes=()):
        waits = self._waits(q, reads, writes)
        if semkey not in self.dsem:
            self.dsem[semkey] = [self.stack.enter_context(self.nc.semaphore("d_" + semkey)), 0]
        self.dsem[semkey][1] += 16
        tok = ("dma", semkey, self.dsem[semkey][1])
        self.stream[q].append((waits, fn, semkey))
        for b in reads:
            b.r.append(tok)
        for b in writes:
            b.w = tok
            b.r = []
        self.dtoks.append(tok)
        return tok

    def finish(self):
        fin = Buf("fin")
        for k, (s, v) in self.dsem.items():
            fin.r.append(("dma", k, v))
        for e in ("pe", "dve", "act", "pool"):
            if self.stream[e]:
                fin.r.append(("eng", e, len(self.stream[e]) - 1))
        waits = self._waits("sp", (), (fin,))
        self.stream["sp"].append((waits, None, None))

    def emit(self):
        nc = self.nc
        self.finish()
        val = {}
        for e in self.ENG:
            c = 0
            for idx in range(len(self.stream[e])):
                if idx in self.needed[e]:
                    c += 1
                    val[(e, idx)] = c
        with nc.Block() as block:
            decos = {"pe": block.tensor, "dve": block.vector, "act": block.scalar,
                     "pool": block.gpsimd, "sp": block.sync}
            for ename in self.ENG:
                items = self.stream[ename]

                def body(e, items=items, ename=ename):
                    for idx, (waits, fn, semkey) in enumerate(items):
                        for (kind, src), v in waits:
                            if kind == "eng":
                                e.wait_ge(self.esem[src], val[(src, v)])
                            else:
                                e.wait_ge(self.dsem[src][0], v)
                        if fn is None:
                            continue
                        ins = fn(e)
                        if semkey is not None:
                            ins.then_inc(self.dsem[semkey][0], 16)
                        elif idx in self.needed[ename]:
                            ins.then_inc(self.esem[ename], 1)

                decos[ename](body)
        self.stack.close()


D = 2048
SEQ = 8192
CTX = 256
NCORE = 8
LAT_PC = SEQ // NCORE
CTX_PC = CTX // NCORE
TOK_PC = LAT_PC + CTX_PC
T_ALL = SEQ + CTX
IN_COLS = 5632
EPS = 1e-6
TILES_PC = [(i * 128, 128) for i in range(8)] + [(1024, 32)]
NPART = 8


def _run(nc, in_maps):
    res = run_bass_kernel_spmd(nc, in_maps, core_ids=list(range(len(in_maps))))
    return res.results


def build_mod():
    nc = bass.Bass("TRN2", target_bir_lowering=False)
    cT = nc.dram_tensor("cT", [128, 16, 2], F32, kind="ExternalInput").ap()
    wm = nc.dram_tensor("wm", [2, 2048, 1536], F32, kind="ExternalInput").ap()
    bm = nc.dram_tensor("bm", [2, 2, 1536], F32, kind="ExternalInput").ap()
    out = nc.dram_tensor("mod", [2, 2, 1536], F32, kind="ExternalOutput").ap()
    P = Prog(nc)
    ct = P.sb("ct", [128, 16, 2], F32); b_ct = P.buf()
    av = P.sb("av", [128, 16, 2], F32); b_av = P.buf()
    bt = P.sb("bt", [2, 2, 1536], F32); b_bt = P.buf()
    ot = P.sb("ot", [2, 2, 1536], F32); b_ot = P.buf()
    NW = 4
    wt = [P.sb(f"wt{i}", [128, 1536], F32) for i in range(NW)]; b_wt = [P.buf() for _ in range(NW)]
    pst = [P.ps(f"ps{i}", [128, 512], F32) for i in range(3)]; b_ps = [P.buf() for _ in range(3)]
    P.dma("sp", "ct", lambda e: e.dma_start(out=ct[:], in_=cT), writes=[b_ct])
    P.dma("sp", "bt", lambda e: e.dma_start(out=bt[:], in_=bm.rearrange("l r n -> r l n")), writes=[b_bt])
    P.op("act", lambda e: e.activation(out=av[:], in_=ct[:], func=AF.Silu), reads=[b_ct], writes=[b_av])
    i = 0
    for l in range(2):
        for k in range(16):
            s = i % NW; i += 1
            P.dma("sp", f"wt{s}", lambda e, s=s, l=l, k=k: e.dma_start(out=wt[s][:], in_=wm[l, k*128:(k+1)*128, :]), writes=[b_wt[s]])
            for n in range(3):
                P.op("pe", lambda e, s=s, n=n, k=k: e.matmul(pst[n][0:2, :], av[:, k, :], wt[s][:, n*512:(n+1)*512], start=(k == 0), stop=(k == 15)),
                     reads=[b_av, b_wt[s]], writes=[b_ps[n]])
        for n in range(3):
            P.op("dve", lambda e, n=n, l=l: e.tensor_tensor(out=ot[:, l, n*512:(n+1)*512], in0=pst[n][0:2, :], in1=bt[:, l, n*512:(n+1)*512], op=ALU.add),
                 reads=[b_ps[n], b_bt], writes=[b_ot])
    P.dma("sp", "ot", lambda e: e.dma_start(out=out.rearrange("l r n -> r l n"), in_=ot[:]), reads=[b_ot])
    P.emit()
    return nc


def run_mod(c, c_ctx, w_mod, b_mod):
    cc = np.stack([c[0], c_ctx], axis=-1)
    cT = np.ascontiguousarray(cc.reshape(16, 128, 2).transpose(1, 0, 2))
    nc = build_mod()
    in_maps = []
    for i in range(NCORE):
        sl = slice(i * 1536, (i + 1) * 1536)
        in_maps.append({"cT": cT, "wm": np.ascontiguousarray(w_mod[:, :, sl]),
                        "bm": np.ascontiguousarray(np.broadcast_to(b_mod[:, None, sl], (2, 2, 1536)))})
    res = _run(nc, in_maps)
    return np.concatenate([r["mod"] for r in res], axis=-1)


def cols128(v):
    return np.ascontiguousarray(v.reshape(16, 128).T)


def build_proj(combine, project=True):
    nc = bass.Bass("TRN2", target_bir_lowering=False)
    x = nc.dram_tensor("x", [TOK_PC, D], F32, kind="ExternalInput").ap()
    ident_d = nc.dram_tensor("ident", [128, 128], F32, kind="ExternalInput").ap()
    P = Prog(nc)
    if project:
        modc = nc.dram_tensor("modc", [128, 16, 4], F32, kind="ExternalInput").ap()
        w_in = nc.dram_tensor("w_in", [D, IN_COLS], F32, kind="ExternalInput").ap()
        z = nc.dram_tensor("z", [TOK_PC, IN_COLS], F32, kind="ExternalOutput").ap()
    if combine:
        part = nc.dram_tensor("part", [NPART, TOK_PC, D], F32, kind="ExternalInput").ap()
        g2 = nc.dram_tensor("g2", [2, 128, D], F32, kind="ExternalInput").ap()
        xo = nc.dram_tensor("xo", [TOK_PC, D], F32, kind="ExternalOutput").ap()
        g2t = P.sb("g2t", [128, 2, D], F32); b_g2 = P.buf()
        P.dma("sp", "g2", lambda e: e.dma_start(out=g2t[:], in_=g2.rearrange("r p d -> p r d")), writes=[b_g2])
        pt = [P.sb(f"pt{i}", [128, D], F32) for i in range(3)]; b_pt = [P.buf() for _ in range(3)]
        acc = P.sb("acc", [128, D], F32); b_acc = P.buf()
    ident = P.sb("identt", [128, 128], F32); b_id = P.buf()
    P.dma("sp", "ident", lambda e: e.dma_start(out=ident[:], in_=ident_d), writes=[b_id])
    xt = [P.sb(f"xt{i}", [128, D], F32) for i in range(2)]; b_xt = [P.buf() for _ in range(2)]
    if project:
        mc = P.sb("mc", [128, 16, 4], F32); b_mc = P.buf()
        P.dma("sp", "mc", lambda e: e.dma_start(out=mc[:], in_=modc), writes=[b_mc])
        P.op("dve", lambda e: e.tensor_scalar(out=mc[:, :, 1], in0=mc[:, :, 1], scalar1=1.0, scalar2=None, op0=ALU.add), reads=[b_mc], writes=[b_mc])
        P.op("dve", lambda e: e.tensor_scalar(out=mc[:, :, 3], in0=mc[:, :, 3], scalar1=1.0, scalar2=None, op0=ALU.add), reads=[b_mc], writes=[b_mc])
        xn = P.sb("xn", [128, D], F32); b_xn = P.buf()
        junk = P.sb("junk", [128, D], BF16); b_junk = P.buf()
        ss = P.sb("ss", [128, 2], F32); b_ss = P.buf()
        xmT = P.sb("xmT", [128, 16, TOK_PC], BF16); b_xmT = [P.buf() for _ in TILES_PC]
        wb = [P.sb(f"wb{i}", [128, 16, 512], BF16) for i in range(2)]; b_wb = [P.buf() for _ in range(2)]
        zt = [P.sb(f"zt{i}", [128, 512], F32) for i in range(4)]; b_zt = [P.buf() for _ in range(4)]
        pst = [P.ps(f"ps{i}", [128, 512], F32) for i in range(8)]; b_ps = [P.buf() for _ in range(8)]
    psi = 0
    for ti, (r0, n) in enumerate(TILES_PC):
        s = ti % 2
        X = xt[s]; bX = b_xt[s]
        P.dma("sp", f"xt{s}", lambda e, X=X, r0=r0, n=n: e.dma_start(out=X[0:n, :], in_=x[r0:r0+n, :]), writes=[bX])
        if combine:
            isctx = 1 if r0 >= LAT_PC else 0
            for c in range(NPART):
                ps_ = c % 3
                P.dma("sp", f"pt{ps_}", lambda e, ps_=ps_, c=c, r0=r0, n=n: e.dma_start(out=pt[ps_][0:n, :], in_=part[c, r0:r0+n, :]), writes=[b_pt[ps_]])
                if c == 0:
                    P.op("pool", lambda e, ps_=ps_, n=n: e.tensor_copy(out=acc[0:n, :], in_=pt[ps_][0:n, :]), reads=[b_pt[ps_]], writes=[b_acc])
                else:
                    eng = "dve" if c % 2 else "pool"
                    P.op(eng, lambda e, ps_=ps_, n=n: e.tensor_tensor(out=acc[0:n, :], in0=acc[0:n, :], in1=pt[ps_][0:n, :], op=ALU.add), reads=[b_pt[ps_], b_acc], writes=[b_acc])
            P.op("dve", lambda e, n=n, isctx=isctx: e.tensor_tensor(out=acc[0:n, :], in0=acc[0:n, :], in1=g2t[0:n, isctx, :], op=ALU.mult), reads=[b_acc, b_g2], writes=[b_acc])
            P.op("dve", lambda e, X=X, n=n: e.tensor_tensor(out=X[0:n, :], in0=X[0:n, :], in1=acc[0:n, :], op=ALU.add), reads=[b_acc, bX], writes=[bX])
            P.dma("sp", f"xo{s}", lambda e, X=X, r0=r0, n=n: e.dma_start(out=xo[r0:r0+n, :], in_=X[0:n, :]), reads=[bX])
        if not project:
            continue
        isctx = 1 if r0 >= LAT_PC else 0
        P.op("act", lambda e, X=X, n=n: e.activation(out=junk[0:n, :], in_=X[0:n, :], func=AF.Square, accum_out=ss[0:n, 0:1]), reads=[bX], writes=[b_junk, b_ss])
        P.op("act", lambda e, n=n: e.activation(out=ss[0:n, 1:2], in_=ss[0:n, 0:1], func=AF.Sqrt, scale=1.0 / D, bias=EPS), reads=[b_ss], writes=[b_ss])
        P.op("dve", lambda e, n=n: e.reciprocal(out=ss[0:n, 1:2], in_=ss[0:n, 1:2]), reads=[b_ss], writes=[b_ss])
        P.op("dve", lambda e, X=X, n=n: e.tensor_scalar(out=xn[0:n, :], in0=X[0:n, :], scalar1=ss[0:n, 1:2], scalar2=None, op0=ALU.mult), reads=[bX, b_ss], writes=[b_xn])
        for kg in range(4):
            pb = psi % 8; psi += 1
            for kk in range(4):
                k = kg * 4 + kk
                P.op("pe", lambda e, pb=pb, kk=kk, k=k, n=n: e.transpose(pst[pb][:, kk*128:kk*128+n], xn[0:n, k*128:(k+1)*128], ident[0:n, 0:n]),
                     reads=[b_xn, b_id], writes=[b_ps[pb]])
            for kk in range(4):
                k = kg * 4 + kk
                if kk % 2 == 0:
                    P.op("dve", lambda e, pb=pb, kk=kk, k=k, n=n, r0=r0, isctx=isctx: e.tensor_scalar(
                        out=xmT[:, k, r0:r0+n], in0=pst[pb][:, kk*128:kk*128+n], scalar1=mc[:, k, 2*isctx+1:2*isctx+2], scalar2=mc[:, k, 2*isctx:2*isctx+1], op0=ALU.mult, op1=ALU.add),
                        reads=[b_ps[pb], b_mc], writes=[b_xmT[ti]])
                else:
                    P.op("act", lambda e, pb=pb, kk=kk, k=k, n=n, r0=r0, isctx=isctx: e.activation(
                        out=xmT[:, k, r0:r0+n], in_=pst[pb][:, kk*128:kk*128+n], func=AF.Identity, scale=mc[:, k, 2*isctx+1:2*isctx+2], bias=mc[:, k, 2*isctx:2*isctx+1]),
                        reads=[b_ps[pb], b_mc], writes=[b_xmT[ti]])
    if project:
        w_v = w_in.rearrange("(k p) n -> p k n", p=128)
        zi = 0
        wsi = 0
        wst = [P.sb(f"wst{i}", [128, 4, 512], F32) for i in range(3)]; b_wst = [P.buf() for _ in range(3)]
        NCB = IN_COLS // 512

        def load_w(cb_):
            nonlocal wsi
            s_ = cb_ % 2
            for kq in range(4):
                ws_ = wsi % 3; wsi += 1
                P.dma("sp", f"wst{ws_}", lambda e, ws_=ws_, cb_=cb_, kq=kq: e.dma_start(out=wst[ws_][:], in_=w_v[:, kq*4:(kq+1)*4, cb_*512:(cb_+1)*512]), writes=[b_wst[ws_]])
                P.op("pool", lambda e, ws_=ws_, s_=s_, kq=kq: e.tensor_copy(out=wb[s_][:, kq*4:(kq+1)*4, :], in_=wst[ws_][:]), reads=[b_wst[ws_]], writes=[b_wb[s_]])

        load_w(0)
        for cb in range(NCB):
            s = cb % 2
            if cb + 1 < NCB:
                load_w(cb + 1)
            for ti, (r0, n) in enumerate(TILES_PC):
                pb = psi % 8; psi += 1
                for k in range(16):
                    P.op("pe", lambda e, pb=pb, k=k, n=n, r0=r0, s=s: e.matmul(pst[pb][0:n, :], xmT[:, k, r0:r0+n], wb[s][:, k, :], start=(k == 0), stop=(k == 15)),
                         reads=[b_xmT[ti], b_wb[s]], writes=[b_ps[pb]])
                zs = zi % 4; zi += 1
                if zi % 2:
                    P.op("dve", lambda e, pb=pb, zs=zs, n=n: e.tensor_copy(out=zt[zs][0:n, :], in_=pst[pb][0:n, :]), reads=[b_ps[pb]], writes=[b_zt[zs]])
                else:
                    P.op("act", lambda e, pb=pb, zs=zs, n=n: e.copy(out=zt[zs][0:n, :], in_=pst[pb][0:n, :]), reads=[b_ps[pb]], writes=[b_zt[zs]])
                P.dma("sp", f"zt{zs}", lambda e, zs=zs, n=n, r0=r0, cb=cb: e.dma_start(out=z[r0:r0+n, cb*512:(cb+1)*512], in_=zt[zs][0:n, :]), reads=[b_zt[zs]])
    P.emit()
    return nc


def shard_tokens(lat, ctx):
    return [np.ascontiguousarray(np.concatenate([lat[i*LAT_PC:(i+1)*LAT_PC], ctx[i*CTX_PC:(i+1)*CTX_PC]], axis=0)) for i in range(NCORE)]


def unshard_tokens(per_core):
    lat = np.concatenate([p[:LAT_PC] for p in per_core], axis=0)
    ctx = np.concatenate([p[LAT_PC:] for p in per_core], axis=0)
    return lat, ctx


def run_proj(x_shards, mod_l, w_in_l, parts=None, project=True):
    combine = parts is not None
    nc = build_proj(combine, project)
    ident = np.eye(128, dtype=np.float32)
    in_maps = []
    for i in range(NCORE):
        m = {"x": x_shards[i], "ident": ident}
        if project:
            sh1, sc1 = mod_l[0, 0:D], mod_l[0, D:2*D]
            csh1, csc1 = mod_l[1, 0:D], mod_l[1, D:2*D]
            m["modc"] = np.ascontiguousarray(np.stack([cols128(sh1), cols128(sc1), cols128(csh1), cols128(csc1)], axis=-1))
            m["w_in"] = w_in_l
        if combine:
            m["part"] = parts[i]
            g2 = np.stack([np.broadcast_to(mod_l_prev_g2[0], (128, D)), np.broadcast_to(mod_l_prev_g2[1], (128, D))])
            m["g2"] = np.ascontiguousarray(g2)
        in_maps.append(m)
    res = _run(nc, in_maps)
    z = [r["z"] for r in res] if project else None
    xo = [r["xo"] for r in res] if combine else None
    return z, xo


NCH = T_ALL // 128
SEG = 384
NSEG = T_ALL // SEG
NQ = 128 + SEQ // 2
NQB = NQ // 128
ATT_SCALE = 128 ** -0.5


def build_mix():
    nc = bass.Bass("TRN2", target_bir_lowering=False)
    di = lambda name, shape, dt=F32: nc.dram_tensor(name, list(shape), dt, kind="ExternalInput").ap()
    do = lambda name, shape, dt=F32: nc.dram_tensor(name, list(shape), dt, kind="ExternalOutput").ap()
    ident_d = di("ident", [128, 128])
    maskT_d = di("maskT", [128, 128])
    r_qkv = di("r_qkv", [T_ALL, 3, 128])
    r_tab = di("r_tab", [T_ALL, 4, 128])
    r_g = di("r_g", [128, 1])
    r_out = do("r_out", [T_ALL, 128])
    s_par = di("s_par", [128, 4, 3])
    s_B = di("s_B", [128, 2, 2, 32])
    s_C = di("s_C", [128, 2, 2, 64])
    s_uT = di("s_uT", [2, 2, 32, T_ALL])
    s_iota = di("s_iota", [128, SEG])
    s_out = do("s_out", [2, T_ALL, 64])
    a_q = di("a_q", [NQ, 128]); a_k = di("a_k", [T_ALL, 128]); a_v = di("a_v", [T_ALL, 128])
    a_qtab = di("a_qtab", [NQ, 2, 128]); a_ktab = di("a_ktab", [T_ALL, 2, 128])
    a_w = di("a_w", [128, 2, 128])
    a_out = do("a_out", [NQ, 128])

    P = Prog(nc)
    NF = 6
    psf = [P.ps(f"psf{i}", [128, 512], F32) for i in range(NF)]; b_psf = [P.buf() for _ in range(NF)]
    psb = [P.ps(f"psb{i}", [128, 512], BF16) for i in range(2)]; b_psb = [P.buf() for _ in range(2)]
    cnt = {"f": 0, "b": 0}

    def nf():
        i = cnt["f"] % NF; cnt["f"] += 1
        return psf[i], b_psf[i]

    def nb():
        i = cnt["b"] % 2; cnt["b"] += 1
        return psb[i], b_psb[i]

    ident = P.sb("identt", [128, 128], F32); b_id = P.buf()
    identb = P.sb("identb", [128, 128], BF16); b_idb = P.buf()
    maskT = P.sb("maskTt", [128, 128], F32); b_mask = P.buf()
    P.dma("sp", "ident", lambda e: e.dma_start(out=ident[:], in_=ident_d), writes=[b_id])
    P.dma("sp", "maskT", lambda e: e.dma_start(out=maskT[:], in_=maskT_d), writes=[b_mask])
    P.op("dve", lambda e: e.tensor_copy(out=identb[:], in_=ident[:]), reads=[b_id], writes=[b_idb])

    def s5_unit():
        par = P.sb("s_par_t", [128, 4, 3], F32); b_par = P.buf()
        Bt = P.sb("s_B_t", [128, 2, 2, 32], F32); b_B = P.buf()
        Ct = P.sb("s_C_t", [128, 2, 2, 64], F32); b_C = P.buf()
        io = P.sb("s_iota_t", [128, SEG], F32); b_io = P.buf()
        P.dma("sp", "s_par", lambda e: e.dma_start(out=par[:], in_=s_par), writes=[b_par])
        P.dma("sp", "s_B", lambda e: e.dma_start(out=Bt[:], in_=s_B), writes=[b_B])
        P.dma("sp", "s_C", lambda e: e.dma_start(out=Ct[:], in_=s_C), writes=[b_C])
        P.dma("sp", "s_iota", lambda e: e.dma_start(out=io[:], in_=s_iota), writes=[b_io])
        P.op("dve", lambda e: e.tensor_scalar(out=Ct[:, :, 1, :], in0=Ct[:, :, 1, :], scalar1=-1.0, scalar2=None, op0=ALU.mult), reads=[b_C], writes=[b_C])
        cs = P.sb("s_cs", [128, 4, SEG], F32); sn = P.sb("s_sn", [128, 4, SEG], F32); rb = P.sb("s_rb", [128, 4, SEG], F32)
        b_tab = [P.buf() for _ in range(4)]
        col = P.sb("s_col", [128, 4, 24], F32); b_col = [P.buf() for _ in range(4)]
        ph = P.sb("s_ph", [128, SEG], F32); b_ph = P.buf()
        ph2 = P.sb("s_ph2", [128, SEG], F32); b_ph2 = P.buf()
        phi = P.sb("s_phi", [128, SEG], I32); b_phi = P.buf()
        BpT = P.sb("s_BpT", [32, 4, 2, 128], F32); b_BpT = [P.buf() for _ in range(4)]
        Bp = P.sb("s_Bp", [128, 2, 32], F32); b_Bp = P.buf()
        tmpB = P.sb("s_tmpB", [128, 32], F32); b_tmpB = P.buf()
        st = P.sb("s_st", [128, 4, 2], F32); b_st = [P.buf() for _ in range(4)]

        def frac_sin(dst, src, bsrc, bdst_list):
            P.op("dve", lambda e: e.tensor_copy(out=phi[:], in_=src), reads=[bsrc], writes=[b_phi])
            P.op("dve", lambda e: e.tensor_tensor(out=ph2[:], in0=src, in1=phi[:], op=ALU.subtract), reads=[bsrc, b_phi], writes=[b_ph2])
            P.op("dve", lambda e: e.tensor_scalar(out=phi[:], in0=ph2[:], scalar1=0.5, scalar2=None, op0=ALU.is_gt), reads=[b_ph2], writes=[b_phi])
            P.op("dve", lambda e: e.tensor_tensor(out=ph2[:], in0=ph2[:], in1=phi[:], op=ALU.subtract), reads=[b_ph2, b_phi], writes=[b_ph2])
            P.op("dve", lambda e: e.tensor_scalar(out=phi[:], in0=ph2[:], scalar1=-0.5, scalar2=None, op0=ALU.is_lt), reads=[b_ph2], writes=[b_phi])
            P.op("dve", lambda e: e.tensor_tensor(out=ph2[:], in0=ph2[:], in1=phi[:], op=ALU.add), reads=[b_ph2, b_phi], writes=[b_ph2])
            P.op("act", lambda e: e.activation(out=dst, in_=ph2[:], func=AF.Sin, scale=2.0 * 3.14159265), reads=[b_ph2], writes=bdst_list)

        for cb in range(4):
            tl = cb % 2
            c_ = lambda j, cb=cb: col[:, cb, j:j+1]
            bc = b_col[cb]
            a_re = par[:, cb, 0:1]; a_im = par[:, cb, 1:2]; lst = par[:, cb, 2:3]
            P.op("act", lambda e, c_=c_, lst=lst: e.activation(out=c_(0), in_=lst, func=AF.Exp), reads=[b_par], writes=[bc])
            P.op("dve", lambda e, c_=c_, a_re=a_re: e.tensor_tensor(out=c_(1), in0=a_re, in1=c_(0), op=ALU.mult), reads=[b_par, bc], writes=[bc])
            P.op("act", lambda e, c_=c_: e.activation(out=c_(2), in_=c_(1), func=AF.Exp), reads=[bc], writes=[bc])
            P.op("dve", lambda e, c_=c_, a_im=a_im: e.tensor_tensor(out=c_(3), in0=a_im, in1=c_(0), op=ALU.mult), reads=[b_par, bc], writes=[bc])
            P.op("dve", lambda e, c_=c_: e.tensor_scalar(out=c_(3), in0=c_(3), scalar1=1.0 / (2.0 * np.pi), scalar2=None, op0=ALU.mult), reads=[bc], writes=[bc])
            P.op("dve", lambda e, c_=c_: e.tensor_scalar(out=ph[:], in0=io[:], scalar1=c_(3), scalar2=None, op0=ALU.mult), reads=[b_io, bc], writes=[b_ph])
            frac_sin(sn[:, cb, :], ph[:], b_ph, [b_tab[cb]])
            P.op("dve", lambda e: e.tensor_scalar(out=ph[:], in0=ph[:], scalar1=0.25, scalar2=None, op0=ALU.add), reads=[b_ph], writes=[b_ph])
            frac_sin(cs[:, cb, :], ph[:], b_ph, [b_tab[cb]])
            P.op("pool", lambda e, cb=cb: e.memset(rb[:, cb, :], 1.0), writes=[b_tab[cb]])
            P.op("dve", lambda e, cb=cb, c_=c_: e.tensor_scalar(out=rb[:, cb, :], in0=rb[:, cb, :], scalar1=c_(2), scalar2=None, op0=ALU.mult), reads=[bc, b_tab[cb]], writes=[b_tab[cb]])
            tt = lambda o, a, b, op, c_=c_: P.op("dve", lambda e: e.tensor_tensor(out=o, in0=a, in1=b, op=op), reads=[bc, b_par, b_tab[cb]], writes=[bc])
            tt(c_(4), c_(2), cs[:, cb, 0:1], ALU.mult)
            tt(c_(5), c_(2), sn[:, cb, 0:1], ALU.mult)
            P.op("dve", lambda e, c_=c_: e.tensor_scalar(out=c_(6), in0=c_(4), scalar1=-1.0, scalar2=None, op0=ALU.add), reads=[bc], writes=[bc])
            tt(c_(7), c_(6), a_re, ALU.mult)
            tt(c_(8), c_(5), a_im, ALU.mult)
            tt(c_(9), c_(7), c_(8), ALU.add)
            tt(c_(10), c_(5), a_re, ALU.mult)
            tt(c_(11), c_(6), a_im, ALU.mult)
            tt(c_(12), c_(10), c_(11), ALU.subtract)
            tt(c_(13), a_re, a_re, ALU.mult)
            tt(c_(14), a_im, a_im, ALU.mult)
            tt(c_(15), c_(13), c_(14), ALU.add)
            P.op("dve", lambda e, c_=c_: e.reciprocal(out=c_(16), in_=c_(15)), reads=[bc], writes=[bc])
            tt(c_(17), c_(9), c_(16), ALU.mult)
            tt(c_(18), c_(12), c_(16), ALU.mult)
            P.op("dve", lambda e, c_=c_, tl=tl: e.tensor_scalar(out=tmpB[:], in0=Bt[:, tl, 1, :], scalar1=c_(18), scalar2=None, op0=ALU.mult), reads=[bc, b_B], writes=[b_tmpB])
            P.op("dve", lambda e, c_=c_, tl=tl: e.scalar_tensor_tensor(out=Bp[:, 0, :], in0=Bt[:, tl, 0, :], scalar=c_(17), in1=tmpB[:], op0=ALU.mult, op1=ALU.subtract), reads=[bc, b_B, b_tmpB], writes=[b_Bp])
            P.op("dve", lambda e, c_=c_, tl=tl: e.tensor_scalar(out=tmpB[:], in0=Bt[:, tl, 0, :], scalar1=c_(18), scalar2=None, op0=ALU.mult), reads=[bc, b_B], writes=[b_tmpB])
            P.op("dve", lambda e, c_=c_, tl=tl: e.scalar_tensor_tensor(out=Bp[:, 1, :], in0=Bt[:, tl, 1, :], scalar=c_(17), in1=tmpB[:], op0=ALU.mult, op1=ALU.add), reads=[bc, b_B, b_tmpB], writes=[b_Bp])
            for ri in range(2):
                pt_, bpt_ = nf()
                P.op("pe", lambda e, pt_=pt_, ri=ri: e.transpose(pt_[0:32, 0:128], Bp[:, ri, :], ident[:]), reads=[b_Bp, b_id], writes=[bpt_])
                P.op("act", lambda e, pt_=pt_, ri=ri, cb=cb: e.copy(out=BpT[:, cb, ri, :], in_=pt_[0:32, 0:128]), reads=[bpt_], writes=[b_BpT[cb]])

        NU = 3
        ut = [P.sb(f"s_ut{i}", [32, SEG], F32) for i in range(NU)]; b_ut = [P.buf() for _ in range(NU)]
        m = [P.sb(f"s_m{i}", [128, SEG], F32) for i in range(4)]; b_m = [P.buf() for _ in range(4)]
        dr = [P.sb(f"s_dr{i}", [128, SEG], F32) for i in range(2)]; b_dr = [P.buf() for _ in range(2)]
        xs_ = [P.sb(f"s_xs{i}", [128, SEG], F32) for i in range(2)]; b_xs = [P.buf() for _ in range(2)]
        xr = [[P.sb(f"s_xr{tl}{ri}", [128, SEG], F32) for ri in range(2)] for tl in range(2)]
        b_xr = [[P.buf() for _ in range(2)] for _ in range(2)]
        ysb = [P.sb(f"s_y{i}", [128, 3, 64], F32) for i in range(2)]; b_ysb = [P.buf() for _ in range(2)]
        ui = 0; yi = 0
        for d in range(2):
            for sg in range(NSEG):
                for tl in range(2):
                    cb = d * 2 + tl
                    us = ui % NU; ui += 1
                    P.dma("sp", f"s_ut{us}", lambda e, us=us, d=d, tl=tl, sg=sg: e.dma_start(out=ut[us][:], in_=s_uT[d, tl, :, sg*SEG:(sg+1)*SEG]), writes=[b_ut[us]])
                    pre, bpre = nf(); pim, bpim = nf()
                    P.op("pe", lambda e, pre=pre, us=us, cb=cb: e.matmul(pre[:, 0:SEG], BpT[:, cb, 0, :], ut[us][:], start=True, stop=True), reads=[b_BpT[cb], b_ut[us]], writes=[bpre])
                    P.op("pe", lambda e, pim=pim, us=us, cb=cb: e.matmul(pim[:, 0:SEG], BpT[:, cb, 1, :], ut[us][:], start=True, stop=True), reads=[b_BpT[cb], b_ut[us]], writes=[bpim])
                    C_ = cs[:, cb, :]; S_ = sn[:, cb, :]
                    P.op("dve", lambda e, pre=pre, C_=C_: e.tensor_tensor(out=m[0][:], in0=pre[:, 0:SEG], in1=C_, op=ALU.mult), reads=[bpre, b_tab[cb]], writes=[b_m[0]])
                    P.op("dve", lambda e, pim=pim, S_=S_: e.tensor_tensor(out=m[1][:], in0=pim[:, 0:SEG], in1=S_, op=ALU.mult), reads=[bpim, b_tab[cb]], writes=[b_m[1]])
                    P.op("dve", lambda e, pim=pim, C_=C_: e.tensor_tensor(out=m[2][:], in0=pim[:, 0:SEG], in1=C_, op=ALU.mult), reads=[bpim, b_tab[cb]], writes=[b_m[2]])
                    P.op("dve", lambda e, pre=pre, S_=S_: e.tensor_tensor(out=m[3][:], in0=pre[:, 0:SEG], in1=S_, op=ALU.mult), reads=[bpre, b_tab[cb]], writes=[b_m[3]])
                    P.op("pool", lambda e: e.tensor_tensor(out=dr[0][:], in0=m[0][:], in1=m[1][:], op=ALU.add), reads=[b_m[0], b_m[1]], writes=[b_dr[0]])
                    P.op("pool", lambda e: e.tensor_tensor(out=dr[1][:], in0=m[2][:], in1=m[3][:], op=ALU.subtract), reads=[b_m[2], b_m[3]], writes=[b_dr[1]])
                    for ri in range(2):
                        init = 0.0 if sg == 0 else st[:, cb, ri:ri+1]
                        P.op("dve", lambda e, ri=ri, init=init, cb=cb: e.tensor_tensor_scan(out=xs_[ri][:], data0=rb[:, cb, :], data1=dr[ri][:], initial=init, op0=ALU.mult, op1=ALU.add),
                             reads=[b_tab[cb], b_dr[ri], b_st[cb]], writes=[b_xs[ri]])
                    P.op("pool", lambda e, C_=C_: e.tensor_tensor(out=m[0][:], in0=xs_[0][:], in1=C_, op=ALU.mult), reads=[b_xs[0], b_tab[cb]], writes=[b_m[0]])
                    P.op("dve", lambda e, S_=S_: e.tensor_tensor(out=m[1][:], in0=xs_[1][:], in1=S_, op=ALU.mult), reads=[b_xs[1], b_tab[cb]], writes=[b_m[1]])
                    P.op("pool", lambda e, S_=S_: e.tensor_tensor(out=m[2][:], in0=xs_[0][:], in1=S_, op=ALU.mult), reads=[b_xs[0], b_tab[cb]], writes=[b_m[2]])
                    P.op("dve", lambda e, C_=C_: e.tensor_tensor(out=m[3][:], in0=xs_[1][:], in1=C_, op=ALU.mult), reads=[b_xs[1], b_tab[cb]], writes=[b_m[3]])
                    P.op("pool", lambda e, tl=tl: e.tensor_tensor(out=xr[tl][0][:], in0=m[0][:], in1=m[1][:], op=ALU.subtract), reads=[b_m[0], b_m[1]], writes=[b_xr[tl][0]])
                    P.op("pool", lambda e, tl=tl: e.tensor_tensor(out=xr[tl][1][:], in0=m[2][:], in1=m[3][:], op=ALU.add), reads=[b_m[2], b_m[3]], writes=[b_xr[tl][1]])
                    for ri in range(2):
                        P.op("act", lambda e, tl=tl, ri=ri, cb=cb: e.copy(out=st[:, cb, ri:ri+1], in_=xr[tl][ri][:, SEG-1:SEG]), reads=[b_xr[tl][ri]], writes=[b_st[cb]])
                ys = yi % 2; yi += 1
                for blk in range(3):
                    py, bpy = nf()
                    j = 0
                    for tl in range(2):
                        for ri in range(2):
                            P.op("pe", lambda e, py=py, tl=tl, ri=ri, blk=blk, j=j: e.matmul(py[:, 0:64], xr[tl][ri][:, blk*128:(blk+1)*128], Ct[:, tl, ri, :], start=(j == 0), stop=(j == 3)),
                                 reads=[b_xr[tl][ri], b_C], writes=[bpy])
                            j += 1
                    P.op("act", lambda e, py=py, ys=ys, blk=blk: e.copy(out=ysb[ys][:, blk, :], in_=py[:, 0:64]), reads=[bpy], writes=[b_ysb[ys]])
                P.dma("sp", f"s_y{ys}", lambda e, ys=ys, d=d, sg=sg: e.dma_start(out=s_out[d, sg*SEG:(sg+1)*SEG, :].rearrange("(b p) c -> p b c", p=128), in_=ysb[ys][:]), reads=[b_ysb[ys]])
                yield

    def ret_unit():
        g = P.sb("r_g_t", [128, 1], F32); b_g = P.buf()
        P.dma("sp", "r_g", lambda e: e.dma_start(out=g[:], in_=r_g), writes=[b_g])
        NB = 2
        qkv = [P.sb(f"r_qkv{i}", [128, 3, 128], F32) for i in range(NB)]; b_qkv = [P.buf() for _ in range(NB)]
        tab = [P.sb(f"r_tab{i}", [128, 4, 128], F32) for i in range(NB)]; b_tb = [P.buf() for _ in range(NB)]
        sw = P.sb("r_sw", [128, 2, 128], F32); b_sw = P.buf()
        t1 = P.sb("r_t1", [128, 2, 128], F32); b_t1 = P.buf()
        t2 = P.sb("r_t2", [128, 2, 128], F32); b_t2 = P.buf()
        qk = P.sb("r_qk", [128, 2, 128], BF16); b_qk = P.buf()
        qkT = P.sb("r_qkT", [128, 2, 128], BF16); b_qkT = P.buf()
        vb = P.sb("r_vb", [128, 128], BF16); b_vb = P.buf()
        sm = P.sb("r_sm", [128, 128], BF16); b_sm = P.buf()
        S32 = P.sb("r_S32", [128, 128], F32); b_S32 = P.buf()
        Sb = P.sb("r_Sb", [128, 128], BF16); b_Sb = P.buf()
        stt = P.sb("r_stt", [128, 6], F32); b_stt = P.buf()
        mv = P.sb("r_mv", [128, 4], F32); b_mv = P.buf()
        yo = [P.sb(f"r_yo{i}", [128, 128], F32) for i in range(2)]; b_yo = [P.buf() for _ in range(2)]
        P.op("pool", lambda e: e.memset(S32[:], 0.0), writes=[b_S32])
        P.op("pool", lambda e: e.memset(Sb[:], 0.0), writes=[b_Sb])
        for n in range(NCH):
            s = n % NB
            Q = qkv[s]; TB = tab[s]
            P.dma("sp", f"r_qkv{s}", lambda e, Q=Q, n=n: e.dma_start(out=Q[:], in_=r_qkv[n*128:(n+1)*128]), writes=[b_qkv[s]])
            P.dma("sp", f"r_tab{s}", lambda e, TB=TB, n=n: e.dma_start(out=TB[:], in_=r_tab[n*128:(n+1)*128]), writes=[b_tb[s]])
            P.op("pool", lambda e, Q=Q: e.tensor_copy(out=sw[:, :, 0:64], in_=Q[:, 0:2, 64:128]), reads=[b_qkv[s]], writes=[b_sw])
            P.op("pool", lambda e, Q=Q: e.tensor_copy(out=sw[:, :, 64:128], in_=Q[:, 0:2, 0:64]), reads=[b_qkv[s]], writes=[b_sw])
            TBv = TB[:].rearrange("p (a b) d -> p a b d", b=2)
            P.op("dve", lambda e, Q=Q, TBv=TBv: e.tensor_tensor(out=t1[:], in0=Q[:, 0:2, :], in1=TBv[:, :, 0, :], op=ALU.mult), reads=[b_qkv[s], b_tb[s]], writes=[b_t1])
            P.op("pool", lambda e, TBv=TBv: e.tensor_tensor(out=t2[:], in0=sw[:], in1=TBv[:, :, 1, :], op=ALU.mult), reads=[b_sw, b_tb[s]], writes=[b_t2])
            P.op("dve", lambda e: e.tensor_tensor(out=qk[:], in0=t1[:], in1=t2[:], op=ALU.add), reads=[b_t1, b_t2], writes=[b_qk])
            P.op("act", lambda e, Q=Q: e.copy(out=vb[:], in_=Q[:, 2, :]), reads=[b_qkv[s]], writes=[b_vb])
            pt_, bpt_ = nb()
            P.op("pe", lambda e, pt_=pt_: e.transpose(pt_[:, 0:128], qk[:, 0, :], identb[:]), reads=[b_qk, b_idb], writes=[bpt_])
            P.op("pe", lambda e, pt_=pt_: e.transpose(pt_[:, 128:256], qk[:, 1, :], identb[:]), reads=[b_qk, b_idb], writes=[bpt_])
            P.op("act", lambda e, pt_=pt_: e.copy(out=qkT[:].rearrange("p a d -> p (a d)"), in_=pt_[:, 0:256]), reads=[bpt_], writes=[b_qkT])
            ps_s, bps_s = nf()
            P.op("pe", lambda e, ps_s=ps_s: e.matmul(ps_s[:, 0:128], qkT[:, 1, :], qkT[:, 0, :], start=True, stop=True), reads=[b_qkT], writes=[bps_s])
            P.op("dve", lambda e, ps_s=ps_s: e.tensor_tensor(out=sm[:], in0=ps_s[:, 0:128], in1=maskT[:], op=ALU.mult), reads=[bps_s, b_mask], writes=[b_sm])
            ps_y, bps_y = nf()
            P.op("pe", lambda e, ps_y=ps_y: e.matmul(ps_y[:, 0:128], sm[:], vb[:], start=True, stop=False), reads=[b_sm, b_vb], writes=[bps_y])
            P.op("pe", lambda e, ps_y=ps_y: e.matmul(ps_y[:, 0:128], qkT[:, 0, :], Sb[:], start=False, stop=True), reads=[b_qkT, b_Sb], writes=[bps_y])
            ps_kv, bps_kv = nf()
            P.op("pe", lambda e, ps_kv=ps_kv: e.matmul(ps_kv[:, 0:128], qk[:, 1, :], vb[:], start=True, stop=True), reads=[b_qk, b_vb], writes=[bps_kv])
            P.op("dve", lambda e, ps_kv=ps_kv: e.tensor_tensor(out=S32[:], in0=ps_kv[:, 0:128], in1=S32[:], op=ALU.add), reads=[bps_kv, b_S32], writes=[b_S32])
            P.op("dve", lambda e: e.tensor_scalar(out=S32[:], in0=S32[:], scalar1=g[:, 0:1], scalar2=None, op0=ALU.mult), reads=[b_S32, b_g], writes=[b_S32])
            P.op("act", lambda e: e.copy(out=Sb[:], in_=S32[:]), reads=[b_S32], writes=[b_Sb])
            P.op("dve", lambda e, ps_y=ps_y: e.bn_stats(out=stt[:], in_=ps_y[:, 0:128]), reads=[bps_y], writes=[b_stt])
            P.op("dve", lambda e: e.bn_aggr(out=mv[:, 0:2], in_=stt[:]), reads=[b_stt], writes=[b_mv])
            P.op("act", lambda e: e.activation(out=mv[:, 2:3], in_=mv[:, 1:2], func=AF.Sqrt, bias=EPS, scale=1.0), reads=[b_mv], writes=[b_mv])
            P.op("dve", lambda e: e.reciprocal(out=mv[:, 3:4], in_=mv[:, 2:3]), reads=[b_mv], writes=[b_mv])
            ys = n % 2
            P.op("dve", lambda e, ps_y=ps_y, ys=ys: e.tensor_scalar(out=yo[ys][:], in0=ps_y[:, 0:128], scalar1=mv[:, 0:1], scalar2=mv[:, 3:4], op0=ALU.subtract, op1=ALU.mult), reads=[bps_y, b_mv], writes=[b_yo[ys]])
            P.dma("sp", f"r_yo{ys}", lambda e, ys=ys, n=n: e.dma_start(out=r_out[n*128:(n+1)*128, :], in_=yo[ys][:]), reads=[b_yo[ys]])
            yield

    def att_unit():
        wt = P.sb("a_w_t", [128, 2, 128], F32); b_w = P.buf()
        P.dma("sp", "a_w", lambda e: e.dma_start(out=wt[:], in_=a_w), writes=[b_w])
        kT = P.sb("a_kT", [128, T_ALL], BF16); b_kT = P.buf()
        qT = P.sb("a_qT", [128, NQ], BF16); b_qT = P.buf()
        vb = P.sb("a_vb", [128, NCH, 128], BF16); b_vb = P.buf()
        xin = [P.sb(f"a_xin{i}", [128, 128], F32) for i in range(2)]; b_xin = [P.buf() for _ in range(2)]
        tb = [P.sb(f"a_tb{i}", [128, 2, 128], F32) for i in range(2)]; b_tb = [P.buf() for _ in range(2)]
        junk = P.sb("a_junk", [128, 128], F32); b_junk = P.buf()
        col = P.sb("a_col", [128, 4], F32); b_col = P.buf()
        xn = P.sb("a_xn", [128, 128], F32); b_xn = P.buf()
        sw = P.sb("a_sw", [128, 128], F32); b_sw = P.buf()
        t1 = P.sb("a_t1", [128, 128], F32); b_t1 = P.buf()
        t2 = P.sb("a_t2", [128, 128], F32); b_t2 = P.buf()
        xr = P.sb("a_xr", [128, 128], BF16); b_xr = P.buf()
        vst = [P.sb(f"a_vst{i}", [128, 6, 128], F32) for i in range(2)]; b_vst = [P.buf() for _ in range(2)]
        a_v_v = a_v.rearrange("(b p) d -> p b d", p=128)
        for i in range(NCH // 6):
            s = i % 2
            P.dma("sp", f"a_vst{s}", lambda e, s=s, i=i: e.dma_start(out=vst[s][:], in_=a_v_v[:, i*6:(i+1)*6, :]), writes=[b_vst[s]])
            P.op("pool", lambda e, s=s, i=i: e.tensor_copy(out=vb[:, i*6:(i+1)*6, :], in_=vst[s][:]), reads=[b_vst[s]], writes=[b_vb])

        def prep(src, tabsrc, nblk, wi, dstT, b_dstT):
            for blk in range(nblk):
                s = blk % 2
                X = xin[s]; TB = tb[s]
                P.dma("sp", f"a_xin{s}", lambda e, X=X, blk=blk: e.dma_start(out=X[:], in_=src[blk*128:(blk+1)*128, :]), writes=[b_xin[s]])
                P.dma("sp", f"a_tb{s}", lambda e, TB=TB, blk=blk: e.dma_start(out=TB[:], in_=tabsrc[blk*128:(blk+1)*128]), writes=[b_tb[s]])
                P.op("act", lambda e, X=X: e.activation(out=junk[:], in_=X[:], func=AF.Square, accum_out=col[:, 0:1]), reads=[b_xin[s]], writes=[b_junk, b_col])
                P.op("act", lambda e: e.activation(out=col[:, 1:2], in_=col[:, 0:1], func=AF.Sqrt, scale=1.0 / 128, bias=EPS), reads=[b_col], writes=[b_col])
                P.op("dve", lambda e: e.reciprocal(out=col[:, 2:3], in_=col[:, 1:2]), reads=[b_col], writes=[b_col])
                P.op("dve", lambda e, X=X: e.scalar_tensor_tensor(out=xn[:], in0=X[:], scalar=col[:, 2:3], in1=wt[:, wi, :], op0=ALU.mult, op1=ALU.mult), reads=[b_xin[s], b_col, b_w], writes=[b_xn])
                xv = xn[:].rearrange("p (a b d) -> p a b d", a=2, b=2)
                sv = sw[:].rearrange("p (a b d) -> p a b d", a=2, b=2)
                P.op("pool", lambda e, xv=xv, sv=sv: e.tensor_copy(out=sv[:, :, 0, :], in_=xv[:, :, 1, :]), reads=[b_xn], writes=[b_sw])
                P.op("pool", lambda e, xv=xv, sv=sv: e.tensor_copy(out=sv[:, :, 1, :], in_=xv[:, :, 0, :]), reads=[b_xn], writes=[b_sw])
                P.op("dve", lambda e, TB=TB: e.tensor_tensor(out=t1[:], in0=xn[:], in1=TB[:, 0, :], op=ALU.mult), reads=[b_xn, b_tb[s]], writes=[b_t1])
                P.op("pool", lambda e, TB=TB: e.tensor_tensor(out=t2[:], in0=sw[:], in1=TB[:, 1, :], op=ALU.mult), reads=[b_sw, b_tb[s]], writes=[b_t2])
                P.op("dve", lambda e: e.tensor_tensor(out=xr[:], in0=t1[:], in1=t2[:], op=ALU.add), reads=[b_t1, b_t2], writes=[b_xr])
                pt_, bpt_ = nb()
                P.op("pe", lambda e, pt_=pt_: e.transpose(pt_[:, 0:128], xr[:], identb[:]), reads=[b_xr, b_idb], writes=[bpt_])
                P.op("act", lambda e, pt_=pt_, blk=blk: e.copy(out=dstT[:, blk*128:(blk+1)*128], in_=pt_[:, 0:128]), reads=[bpt_], writes=[b_dstT])
                yield

        yield from prep(a_k, a_ktab, NCH, 1, kT, b_kT)
        yield from prep(a_q, a_qtab, NQB, 0, qT, b_qT)

        Ssb = P.sb("a_S", [128, T_ALL], F32); b_S = P.buf()
        Pb = P.sb("a_P", [128, T_ALL], BF16); b_P = P.buf()
        PT = P.sb("a_PT", [128, NCH, 128], BF16); b_PT = P.buf()
        c2 = P.sb("a_c2", [128, 4], F32); b_c2 = P.buf()
        ob = [P.sb(f"a_o{i}", [128, 128], F32) for i in range(2)]; b_ob = [P.buf() for _ in range(2)]
        for qb in range(NQB):
            nk = CTX if qb == 0 else T_ALL
            nkt = (nk + 511) // 512
            for kt in range(nkt):
                w = min(512, nk - kt * 512)
                ps_, bps_ = nf()
                P.op("pe", lambda e, ps_=ps_, qb=qb, kt=kt, w=w: e.matmul(ps_[:, 0:w], qT[:, qb*128:(qb+1)*128], kT[:, kt*512:kt*512+w], start=True, stop=True), reads=[b_qT, b_kT], writes=[bps_])
                if kt % 2:
                    P.op("dve", lambda e, ps_=ps_, kt=kt, w=w: e.tensor_copy(out=Ssb[:, kt*512:kt*512+w], in_=ps_[:, 0:w]), reads=[bps_], writes=[b_S])
                else:
                    P.op("act", lambda e, ps_=ps_, kt=kt, w=w: e.copy(out=Ssb[:, kt*512:kt*512+w], in_=ps_[:, 0:w]), reads=[bps_], writes=[b_S])
            P.op("dve", lambda e, nk=nk: e.reduce_max(out=c2[:, 0:1], in_=Ssb[:, 0:nk], axis=AX.X), reads=[b_S], writes=[b_c2])
            P.op("dve", lambda e: e.tensor_scalar(out=c2[:, 1:2], in0=c2[:, 0:1], scalar1=-ATT_SCALE, scalar2=None, op0=ALU.mult), reads=[b_c2], writes=[b_c2])
            P.op("act", lambda e, nk=nk: e.activation(out=Pb[:, 0:nk], in_=Ssb[:, 0:nk], func=AF.Exp, scale=ATT_SCALE, bias=c2[:, 1:2], accum_out=c2[:, 2:3]), reads=[b_S, b_c2], writes=[b_P, b_c2])
            P.op("dve", lambda e: e.reciprocal(out=c2[:, 3:4], in_=c2[:, 2:3]), reads=[b_c2], writes=[b_c2])
            nkb = nk // 128
            for g0 in range(0, nkb, 4):
                gn = min(4, nkb - g0)
                pt_, bpt_ = nb()
                for j in range(gn):
                    P.op("pe", lambda e, pt_=pt_, j=j, g0=g0: e.transpose(pt_[:, j*128:(j+1)*128], Pb[:, (g0+j)*128:(g0+j+1)*128], identb[:]), reads=[b_P, b_idb], writes=[bpt_])
                if (g0 // 4) % 2:
                    P.op("dve", lambda e, pt_=pt_, g0=g0, gn=gn: e.tensor_copy(out=PT[:, g0:g0+gn, :].rearrange("p a d -> p (a d)"), in_=pt_[:, 0:gn*128]), reads=[bpt_], writes=[b_PT])
                else:
                    P.op("act", lambda e, pt_=pt_, g0=g0, gn=gn: e.copy(out=PT[:, g0:g0+gn, :].rearrange("p a d -> p (a d)"), in_=pt_[:, 0:gn*128]), reads=[bpt_], writes=[b_PT])
            po, bpo = nf()
            for kb in range(nkb):
                P.op("pe", lambda e, po=po, kb=kb, nkb=nkb: e.matmul(po[:, 0:128], PT[:, kb, :], vb[:, kb, :], start=(kb == 0), stop=(kb == nkb - 1)), reads=[b_PT, b_vb], writes=[bpo])
            os_ = qb % 2
            P.op("dve", lambda e, po=po, os_=os_: e.tensor_scalar(out=ob[os_][:], in0=po[:, 0:128], scalar1=c2[:, 3:4], scalar2=None, op0=ALU.mult), reads=[bpo, b_c2], writes=[b_ob[os_]])
            P.dma("sp", f"a_o{os_}", lambda e, os_=os_, qb=qb: e.dma_start(out=a_out[qb*128:(qb+1)*128, :], in_=ob[os_][:]), reads=[b_ob[os_]])
            yield

    units = globals().get("MIX_UNITS", "rsa")
    gens = []
    if "a" in units:
        gens.append(att_unit())
    if "r" in units:
        gens.append(ret_unit())
    if "s" in units:
        gens.append(s5_unit())
    while gens:
        for g_ in list(gens):
            try:
                next(g_)
            except StopIteration:
                gens.remove(g_)
    P.emit()
    return nc


def _order(d):
    if d == 0:
        return np.arange(T_ALL)
    return np.concatenate([np.arange(CTX)[::-1], CTX + np.arange(SEQ)[::-1]])


_CONST = {}


def mix_consts():
    if _CONST:
        return _CONST
    f64 = np.float64
    log_g = np.log(1.0 - 2.0 ** (-5.0 - np.arange(4, dtype=f64)))
    freqs = 10000.0 ** (-np.arange(0, 128, 2, dtype=f64) / 128)
    rt = {}
    for d in range(2):
        lg = log_g if d == 0 else log_g[::-1]
        order = _order(d)
        isl = order >= CTX
        pos = np.where(isl, order - CTX, 0).astype(f64)
        ang = pos[:, None] * freqs[None, :]
        cos = np.where(isl[:, None], np.cos(ang), 1.0)
        sin = np.where(isl[:, None], np.sin(ang), 0.0)
        cosf = np.concatenate([cos, cos], axis=1)
        sinf = np.concatenate([-sin, sin], axis=1)
        i = (np.arange(T_ALL) % 128).astype(f64)
        for h in range(4):
            gq = np.exp((i + 1.0) * lg[h])[:, None]
            gk = (128.0 ** -0.5) * np.exp(-(i + 1.0) * lg[h])[:, None]
            tab = np.stack([cosf * gq, sinf * gq, cosf * gk, sinf * gk], axis=1).astype(np.float32)
            rt[(h, d)] = (np.ascontiguousarray(tab), np.full((128, 1), np.exp(128.0 * lg[h]), np.float32))
    _CONST["ret"] = rt
    fr = 10000.0 ** (-np.arange(0, 64, 2, dtype=f64) / 64)
    pos = np.arange(SEQ)
    ar = (pos // 64).astype(f64)[:, None] * fr[None, :]
    ac = (pos % 64).astype(f64)[:, None] * fr[None, :]
    cosl = np.concatenate([np.cos(ar), np.cos(ar), np.cos(ac), np.cos(ac)], axis=1)
    sinl = np.concatenate([-np.sin(ar), np.sin(ar), -np.sin(ac), np.sin(ac)], axis=1)
    cosa = np.concatenate([np.ones((CTX, 128)), cosl], axis=0)
    sina = np.concatenate([np.zeros((CTX, 128)), sinl], axis=0)
    _CONST["att"] = np.ascontiguousarray(np.stack([cosa, sina], axis=1).astype(np.float32))
    _CONST["ident"] = np.eye(128, dtype=np.float32)
    _CONST["maskT"] = np.triu(np.ones((128, 128), np.float32))
    _CONST["iota"] = np.ascontiguousarray(np.broadcast_to(np.arange(1, SEG + 1, dtype=np.float32), (128, SEG)))
    return _CONST


def run_mix(z_all, prm):
    C = mix_consts()
    nc = build_mix()
    in_maps = []
    orders = [_order(0), _order(1)]
    for c in range(NCORE):
        m = {"ident": C["ident"], "maskT": C["maskT"]}
        h, d = c % 4, c // 4
        zo = z_all[orders[d]]
        m["r_qkv"] = np.ascontiguousarray(np.stack([zo[:, h*128:(h+1)*128], zo[:, 512+h*128:512+(h+1)*128], zo[:, 1024+h*128:1024+(h+1)*128]], axis=1))
        m["r_tab"], m["r_g"] = C["ret"][(h, d)]
        par = np.zeros((128, 4, 3), np.float32)
        sB = np.zeros((128, 2, 2, 32), np.float32)
        sC = np.zeros((128, 2, 2, 64), np.float32)
        uT = np.zeros((2, 2, 32, T_ALL), np.float32)
        for tl in range(2):
            for gi in range(2):
                g = 4 * c + 2 * tl + gi
                rows = slice(gi * 64, (gi + 1) * 64)
                for dd in range(2):
                    par[rows, dd*2+tl, 0] = prm["s5_a_re"][dd, g]
                    par[rows, dd*2+tl, 1] = prm["s5_a_im"][dd, g]
                    par[rows, dd*2+tl, 2] = prm["s5_log_step"][dd, g]
                sB[rows, tl, 0, gi*16:(gi+1)*16] = prm["s5_b_re"][g]
                sB[rows, tl, 1, gi*16:(gi+1)*16] = prm["s5_b_im"][g]
                sC[rows, tl, 0, tl*32+gi*16:tl*32+(gi+1)*16] = prm["s5_c_re"][g].T
                sC[rows, tl, 1, tl*32+gi*16:tl*32+(gi+1)*16] = prm["s5_c_im"][g].T
            for dd in range(2):
                ucols = z_all[orders[dd], 2560 + (4*c + 2*tl) * 16: 2560 + (4*c + 2*tl + 2) * 16]
                uT[dd, tl] = ucols.T
        m["s_par"], m["s_B"], m["s_C"], m["s_uT"], m["s_iota"] = par, sB, sC, uT, C["iota"]
        hq, half = c // 2, c % 2
        kvh = hq // 2
        A0 = 3072
        qsel = np.concatenate([np.arange(half*128, (half+1)*128), CTX + np.arange(half*4096, (half+1)*4096)])
        m["a_q"] = np.ascontiguousarray(z_all[qsel, A0 + hq*128: A0 + (hq+1)*128])
        m["a_k"] = np.ascontiguousarray(z_all[:, A0 + 512 + kvh*128: A0 + 512 + (kvh+1)*128])
        m["a_v"] = np.ascontiguousarray(z_all[:, A0 + 768 + kvh*128: A0 + 768 + (kvh+1)*128])
        m["a_qtab"] = np.ascontiguousarray(C["att"][qsel])
        m["a_ktab"] = C["att"]
        m["a_w"] = np.ascontiguousarray(np.stack([np.broadcast_to(prm["q_norm_w"], (128, 128)), np.broadcast_to(prm["k_norm_w"], (128, 128))], axis=1))
        in_maps.append(m)
    res = _run(nc, in_maps)
    ret = np.zeros((2, T_ALL, 512), np.float32)
    s5y = np.zeros((2, T_ALL, 512), np.float32)
    att = np.zeros((T_ALL, 512), np.float32)
    for c in range(NCORE):
        h, d = c % 4, c // 4
        if "r_out" in res[c]:
            ret[d][orders[d], h*128:(h+1)*128] = res[c]["r_out"]
        for dd in range(2):
            s5y[dd][orders[dd], c*64:(c+1)*64] = res[c]["s_out"][dd]
        hq, half = c // 2, c % 2
        qsel = np.concatenate([np.arange(half*128, (half+1)*128), CTX + np.arange(half*4096, (half+1)*4096)])
        att[qsel, hq*128:(hq+1)*128] = res[c]["a_out"]
    return ret, s5y, att


def build_out():
    nc = bass.Bass("TRN2", target_bir_lowering=False)
    di = lambda name, shape, dt=F32: nc.dram_tensor(name, list(shape), dt, kind="ExternalInput").ap()
    do = lambda name, shape, dt=F32: nc.dram_tensor(name, list(shape), dt, kind="ExternalOutput").ap()
    ident_d = di("ident", [128, 128])
    x = di("x", [TOK_PC, D])
    rg = di("rg", [TOK_PC, 4, 512])
    s5 = di("s5", [TOK_PC, 3, 512])
    att = di("att", [TOK_PC, 512])
    cv = di("cv", [TOK_PC, 7, 512])
    vecs = di("vecs", [128, 5, 512])
    w_glu = di("w_glu", [512, 512])
    w_out = di("w_out", [D, D])
    g1 = di("g1", [2, 128, D])
    modc = di("modc", [128, 16, 4])
    w_r = di("w_r", [128, 16, 32])
    b_r = di("b_r", [128, 32])
    xmid = do("xmid", [TOK_PC, D])
    fT = do("fT", [D, TOK_PC], BF16)
    gates = do("gates", [TOK_PC, 32])

    P = Prog(nc)
    NF = 5
    psf = [P.ps(f"psf{i}", [128, 512], F32) for i in range(NF)]; b_psf = [P.buf() for _ in range(NF)]
    psb = [P.ps(f"psb{i}", [128, 512], BF16) for i in range(2)]; b_psb = [P.buf() for _ in range(2)]
    cnt = {"f": 0, "b": 0}

    def nf():
        i = cnt["f"] % NF; cnt["f"] += 1
        return psf[i], b_psf[i]

    def nb():
        i = cnt["b"] % 2; cnt["b"] += 1
        return psb[i], b_psb[i]

    ident = P.sb("identt", [128, 128], F32); b_id = P.buf()
    identb = P.sb("identb", [128, 128], BF16); b_idb = P.buf()
    P.dma("sp", "ident", lambda e: e.dma_start(out=ident[:], in_=ident_d), writes=[b_id])
    P.op("dve", lambda e: e.tensor_copy(out=identb[:], in_=ident[:]), reads=[b_id], writes=[b_idb])
    vt = P.sb("vecs_t", [128, 5, 512], F32); b_vt = P.buf()
    P.dma("sp", "vecs", lambda e: e.dma_start(out=vt[:], in_=vecs), writes=[b_vt])
    g1t = P.sb("g1t", [128, 2, D], F32); b_g1 = P.buf()
    P.dma("sp", "g1", lambda e: e.dma_start(out=g1t[:], in_=g1.rearrange("r p d -> p r d")), writes=[b_g1])
    mc = P.sb("mc", [128, 16, 4], F32); b_mc = P.buf()
    P.dma("sp", "mc", lambda e: e.dma_start(out=mc[:], in_=modc), writes=[b_mc])
    P.op("dve", lambda e: e.tensor_scalar(out=mc[:, :, 1], in0=mc[:, :, 1], scalar1=1.0, scalar2=None, op0=ALU.add), reads=[b_mc], writes=[b_mc])
    P.op("dve", lambda e: e.tensor_scalar(out=mc[:, :, 3], in0=mc[:, :, 3], scalar1=1.0, scalar2=None, op0=ALU.add), reads=[b_mc], writes=[b_mc])
    wr = P.sb("wr", [128, 16, 32], F32); b_wr = P.buf()
    P.dma("sp", "wr", lambda e: e.dma_start(out=wr[:], in_=w_r), writes=[b_wr])
    brt = P.sb("brt", [128, 32], F32); b_br = P.buf()
    P.dma("sp", "brt", lambda e: e.dma_start(out=brt[:], in_=b_r), writes=[b_br])
    wst = [P.sb(f"wst{i}", [128, 4, 512], F32) for i in range(2)]; b_wst = [P.buf() for _ in range(2)]
    wg = P.sb("wg", [128, 4, 512], BF16); b_wg = P.buf()
    P.dma("sp", "wst0", lambda e: e.dma_start(out=wst[0][:], in_=w_glu.rearrange("(k p) n -> p k n", p=128)), writes=[b_wst[0]])
    P.op("pool", lambda e: e.tensor_copy(out=wg[:], in_=wst[0][:]), reads=[b_wst[0]], writes=[b_wg])

    mixT = P.sb("mixT", [128, 16, TOK_PC], BF16); b_mixT = [P.buf() for _ in TILES_PC]
    rgt = P.sb("rgt", [128, 4, 512], F32); b_rg = P.buf()
    s5t = P.sb("s5t", [128, 3, 512], F32); b_s5 = P.buf()
    att_t = P.sb("att_t", [128, 512], F32); b_att = P.buf()
    cvt = P.sb("cvt", [128, 7, 512], F32); b_cv = P.buf()
    tm = [P.sb(f"tm{i}", [128, 512], F32) for i in range(5)]; b_tm = [P.buf() for _ in range(5)]
    yb16 = P.sb("yb16", [128, 512], BF16); b_yb16 = P.buf()
    yT = P.sb("yT", [128, 4, 128], BF16); b_yT = P.buf()
    mix = P.sb("mix", [128, D], BF16); b_mix = [P.buf() for _ in range(4)]

    def tt(eng, o, a, b, op, rd, wr_):
        P.op(eng, lambda e: e.tensor_tensor(out=o, in0=a, in1=b, op=op), reads=rd, writes=wr_)

    for ti, (r0, n) in enumerate(TILES_PC):
        P.dma("sp", "rgt", lambda e, r0=r0, n=n: e.dma_start(out=rgt[0:n], in_=rg[r0:r0+n]), writes=[b_rg])
        P.dma("sp", "s5t", lambda e, r0=r0, n=n: e.dma_start(out=s5t[0:n], in_=s5[r0:r0+n]), writes=[b_s5])
        P.dma("sp", "att_t", lambda e, r0=r0, n=n: e.dma_start(out=att_t[0:n], in_=att[r0:r0+n]), writes=[b_att])
        P.dma("sp", "cvt", lambda e, r0=r0, n=n: e.dma_start(out=cvt[0:n], in_=cv[r0:r0+n]), writes=[b_cv])
        P.op("act", lambda e, n=n: e.activation(out=tm[0][0:n], in_=rgt[0:n, 2, :], func=AF.Silu), reads=[b_rg], writes=[b_tm[0]])
        P.op("act", lambda e, n=n: e.activation(out=tm[1][0:n], in_=rgt[0:n, 3, :], func=AF.Silu), reads=[b_rg], writes=[b_tm[1]])
        tt("dve", tm[0][0:n], tm[0][0:n], rgt[0:n, 0, :], ALU.mult, [b_tm[0], b_rg], [b_tm[0]])
        tt("pool", tm[1][0:n], tm[1][0:n], rgt[0:n, 1, :], ALU.mult, [b_tm[1], b_rg], [b_tm[1]])
        tt("dve", mix[0:n, 0:512], tm[0][0:n], tm[1][0:n], ALU.add, [b_tm[0], b_tm[1]], [b_mix[0]])
        tt("pool", tm[2][0:n], s5t[0:n, 0, :], s5t[0:n, 1, :], ALU.add, [b_s5], [b_tm[2]])
        tt("dve", tm[3][0:n], s5t[0:n, 2, :], vt[0:n, 0, :], ALU.mult, [b_s5, b_vt], [b_tm[3]])
        tt("pool", tm[2][0:n], tm[2][0:n], tm[3][0:n], ALU.add, [b_tm[2], b_tm[3]], [b_tm[2]])
        tt("pool", tm[3][0:n], tm[2][0:n], tm[2][0:n], ALU.mult, [b_tm[2]], [b_tm[3]])
        P.op("dve", lambda e, n=n: e.tensor_scalar(out=tm[3][0:n], in0=tm[3][0:n], scalar1=0.044715, scalar2=1.0, op0=ALU.mult, op1=ALU.add), reads=[b_tm[3]], writes=[b_tm[3]])
        tt("dve", tm[3][0:n], tm[3][0:n], tm[2][0:n], ALU.mult, [b_tm[3], b_tm[2]], [b_tm[3]])
        P.op("act", lambda e, n=n: e.activation(out=tm[3][0:n], in_=tm[3][0:n], func=AF.Sigmoid, scale=1.5957691216057308), reads=[b_tm[3]], writes=[b_tm[3]])
        tt("dve", tm[2][0:n], tm[2][0:n], tm[3][0:n], ALU.mult, [b_tm[2], b_tm[3]], [b_tm[2]])
        P.op("act", lambda e, n=n: e.copy(out=yb16[0:n], in_=tm[2][0:n]), reads=[b_tm[2]], writes=[b_yb16])
        pt_, bpt_ = nb()
        for k in range(4):
            P.op("pe", lambda e, pt_=pt_, k=k, n=n: e.transpose(pt_[:, k*128:k*128+n], yb16[0:n, k*128:(k+1)*128], identb[0:n, 0:n]), reads=[b_yb16, b_idb], writes=[bpt_])
        P.op("act", lambda e, pt_=pt_, n=n: e.copy(out=yT[:, :, 0:n], in_=pt_[:, 0:512].rearrange("p (a d) -> p a d", a=4)[:, :, 0:n]), reads=[bpt_], writes=[b_yT])
        pg, bpg = nf()
        for k in range(4):
            P.op("pe", lambda e, pg=pg, k=k, n=n: e.matmul(pg[0:n, :], yT[:, k, 0:n], wg[:, k, :], start=(k == 0), stop=(k == 3)), reads=[b_yT, b_wg], writes=[bpg])
        tt("dve", tm[3][0:n], pg[0:n, :], vt[0:n, 1, :], ALU.add, [bpg, b_vt], [b_tm[3]])
        P.op("act", lambda e, n=n: e.activation(out=tm[3][0:n], in_=tm[3][0:n], func=AF.Sigmoid), reads=[b_tm[3]], writes=[b_tm[3]])
        tt("dve", mix[0:n, 512:1024], tm[2][0:n], tm[3][0:n], ALU.mult, [b_tm[2], b_tm[3]], [b_mix[1]])
        P.op("act", lambda e, n=n: e.copy(out=mix[0:n, 1024:1536], in_=att_t[0:n]), reads=[b_att], writes=[b_mix[2]])
        tt("pool", tm[0][0:n], cvt[0:n, 1, :], cvt[0:n, 2, :], ALU.mult, [b_cv], [b_tm[0]])
        tt("dve", tm[1][0:n], cvt[0:n, 3, :], cvt[0:n, 4, :], ALU.mult, [b_cv], [b_tm[1]])
        tt("pool", tm[4][0:n], cvt[0:n, 5, :], cvt[0:n, 6, :], ALU.mult, [b_cv], [b_tm[4]])
        tt("dve", tm[0][0:n], tm[0][0:n], vt[0:n, 2, :], ALU.mult, [b_tm[0], b_vt], [b_tm[0]])
        tt("pool", tm[1][0:n], tm[1][0:n], vt[0:n, 3, :], ALU.mult, [b_tm[1], b_vt], [b_tm[1]])
        tt("dve", tm[4][0:n], tm[4][0:n], vt[0:n, 4, :], ALU.mult, [b_tm[4], b_vt], [b_tm[4]])
        tt("pool", tm[0][0:n], tm[0][0:n], tm[1][0:n], ALU.add, [b_tm[0], b_tm[1]], [b_tm[0]])
        tt("dve", tm[0][0:n], tm[0][0:n], tm[4][0:n], ALU.add, [b_tm[0], b_tm[4]], [b_tm[0]])
        tt("dve", mix[0:n, 1536:2048], tm[0][0:n], cvt[0:n, 0, :], ALU.mult, [b_tm[0], b_cv], [b_mix[3]])
        for kg in range(4):
            pt_, bpt_ = nb()
            for kk in range(4):
                k = kg * 4 + kk
                P.op("pe", lambda e, pt_=pt_, kk=kk, k=k, n=n: e.transpose(pt_[:, kk*128:kk*128+n], mix[0:n, k*128:(k+1)*128], identb[0:n, 0:n]), reads=[b_mix[kg], b_idb], writes=[bpt_])
            eng = "act" if kg % 2 else "dve"
            if eng == "act":
                P.op("act", lambda e, pt_=pt_, kg=kg, n=n, r0=r0: e.copy(out=mixT[:, kg*4:(kg+1)*4, r0:r0+n], in_=pt_[:, 0:512].rearrange("p (a d) -> p a d", a=4)[:, :, 0:n]), reads=[bpt_], writes=[b_mixT[ti]])
            else:
                P.op("dve", lambda e, pt_=pt_, kg=kg, n=n, r0=r0: e.tensor_copy(out=mixT[:, kg*4:(kg+1)*4, r0:r0+n], in_=pt_[:, 0:512].rearrange("p (a d) -> p a d", a=4)[:, :, 0:n]), reads=[bpt_], writes=[b_mixT[ti]])

    wb = [P.sb(f"wb{i}", [128, 16, 512], BF16) for i in range(2)]; b_wb = [P.buf() for _ in range(2)]
    xp = [P.sb(f"xp{i}", [128, 512], F32) for i in range(3)]; b_xp = [P.buf() for _ in range(3)]
    b_xm_dram = [P.buf() for _ in TILES_PC]
    w_v = w_out.rearrange("(k p) n -> p k n", p=128)
    wsi = 1; xi = 0
    def load_wo(cb_):
        nonlocal wsi
        s_ = cb_ % 2
        for kq in range(4):
            ws_ = wsi % 2; wsi += 1
            P.dma("sp", f"wst{ws_}", lambda e, ws_=ws_, cb_=cb_, kq=kq: e.dma_start(out=wst[ws_][:], in_=w_v[:, kq*4:(kq+1)*4, cb_*512:(cb_+1)*512]), writes=[b_wst[ws_]])
            P.op("pool", lambda e, ws_=ws_, s_=s_, kq=kq: e.tensor_copy(out=wb[s_][:, kq*4:(kq+1)*4, :], in_=wst[ws_][:]), reads=[b_wst[ws_]], writes=[b_wb[s_]])

    load_wo(0)
    for cb in range(4):
        s = cb % 2
        if cb + 1 < 4:
            load_wo(cb + 1)
        for ti, (r0, n) in enumerate(TILES_PC):
            isctx = 1 if r0 >= LAT_PC else 0
            xs_ = xi % 3; xi += 1
            P.dma("sp", f"xp{xs_}", lambda e, xs_=xs_, r0=r0, n=n, cb=cb: e.dma_start(out=xp[xs_][0:n], in_=x[r0:r0+n, cb*512:(cb+1)*512]), writes=[b_xp[xs_]])
            po, bpo = nf()
            for k in range(16):
                P.op("pe", lambda e, po=po, k=k, n=n, r0=r0, s=s: e.matmul(po[0:n, :], mixT[:, k, r0:r0+n], wb[s][:, k, :], start=(k == 0), stop=(k == 15)), reads=[b_mixT[ti], b_wb[s]], writes=[bpo])
            t_ = tm[xi % 2]; bt_ = b_tm[xi % 2]
            tt("dve", t_[0:n], po[0:n, :], g1t[0:n, isctx, cb*512:(cb+1)*512], ALU.mult, [bpo, b_g1], [bt_])
            tt("pool", xp[xs_][0:n], xp[xs_][0:n], t_[0:n], ALU.add, [b_xp[xs_], bt_], [b_xp[xs_]])
            P.dma("sp", f"xpo{xs_}", lambda e, xs_=xs_, r0=r0, n=n, cb=cb: e.dma_start(out=xmid[r0:r0+n, cb*512:(cb+1)*512], in_=xp[xs_][0:n]), reads=[b_xp[xs_]], writes=[b_xm_dram[ti]])

    xt = [P.sb(f"xt{i}", [128, D], F32) for i in range(1)]; b_xt = [P.buf() for _ in range(1)]
    xn = P.sb("xn", [128, D], F32); b_xn = P.buf()
    junk = mix
    ss = P.sb("ss", [128, 2], F32); b_ss = P.buf()
    f32t = P.sb("f32t", [128, 16, 128], F32); b_f32 = P.buf()
    lg = P.sb("lg", [128, 32], F32); b_lg = P.buf()
    rc = P.sb("rc", [128, 16], F32); b_rc = P.buf()
    ex = P.sb("ex", [128, 32], F32); b_ex = P.buf()
    gt = [P.sb(f"gt{i}", [128, 32], F32) for i in range(2)]; b_gt = [P.buf() for _ in range(2)]
    fT_v = fT.rearrange("(k p) t -> p k t", p=128)
    for ti, (r0, n) in enumerate(TILES_PC):
        s = ti % 2
        isctx = 1 if r0 >= LAT_PC else 0
        X = xt[0]; bX = b_xt[0]
        P.dma("sp", "xt0", lambda e, X=X, r0=r0, n=n: e.dma_start(out=X[0:n, :], in_=xmid[r0:r0+n, :]), reads=[b_xm_dram[ti]], writes=[bX])
        P.op("act", lambda e, X=X, n=n: e.activation(out=junk[0:n, :], in_=X[0:n, :], func=AF.Square, accum_out=ss[0:n, 0:1]), reads=[bX], writes=b_mix + [b_ss])
        P.op("act", lambda e, n=n: e.activation(out=ss[0:n, 1:2], in_=ss[0:n, 0:1], func=AF.Sqrt, scale=1.0 / D, bias=EPS), reads=[b_ss], writes=[b_ss])
        P.op("dve", lambda e, n=n: e.reciprocal(out=ss[0:n, 1:2], in_=ss[0:n, 1:2]), reads=[b_ss], writes=[b_ss])
        P.op("dve", lambda e, X=X, n=n: e.tensor_scalar(out=xn[0:n, :], in0=X[0:n, :], scalar1=ss[0:n, 1:2], scalar2=None, op0=ALU.mult), reads=[bX, b_ss], writes=[b_xn])
        for kg in range(4):
            pt_, bpt_ = nf()
            for kk in range(4):
                k = kg * 4 + kk
                P.op("pe", lambda e, pt_=pt_, kk=kk, k=k, n=n: e.transpose(pt_[:, kk*128:kk*128+n], xn[0:n, k*128:(k+1)*128], ident[0:n, 0:n]), reads=[b_xn, b_id], writes=[bpt_])
            for kk in range(4):
                k = kg * 4 + kk
                if kk % 2 == 0:
                    P.op("dve", lambda e, pt_=pt_, kk=kk, k=k, n=n, isctx=isctx: e.tensor_scalar(
                        out=f32t[:, k, 0:n], in0=pt_[:, kk*128:kk*128+n], scalar1=mc[:, k, 2*isctx+1:2*isctx+2], scalar2=mc[:, k, 2*isctx:2*isctx+1], op0=ALU.mult, op1=ALU.add),
                        reads=[bpt_, b_mc], writes=[b_f32])
                else:
                    P.op("act", lambda e, pt_=pt_, kk=kk, k=k, n=n, isctx=isctx: e.activation(
                        out=f32t[:, k, 0:n], in_=pt_[:, kk*128:kk*128+n], func=AF.Identity, scale=mc[:, k, 2*isctx+1:2*isctx+2], bias=mc[:, k, 2*isctx:2*isctx+1]),
                        reads=[bpt_, b_mc], writes=[b_f32])
        P.op("pool", lambda e, n=n, r0=r0: e.tensor_copy(out=mixT[:, :, r0:r0+n], in_=f32t[:, :, 0:n]), reads=[b_f32], writes=[b_mixT[ti]])
        pl, bpl = nf()
        for k in range(16):
            P.op("pe", lambda e, pl=pl, k=k, n=n: e.matmul(pl[0:n, 0:32], f32t[:, k, 0:n], wr[:, k, :], start=(k == 0), stop=(k == 15)), reads=[b_f32, b_wr], writes=[bpl])
        tt("dve", lg[0:n], pl[0:n, 0:32], brt[0:n], ALU.add, [bpl, b_br], [b_lg])
        P.op("dve", lambda e, n=n: e.max(out=rc[0:n, 0:8], in_=lg[0:n]), reads=[b_lg], writes=[b_rc])
        P.op("dve", lambda e, n=n: e.tensor_scalar(out=rc[0:n, 8:9], in0=rc[0:n, 0:1], scalar1=-1.0, scalar2=None, op0=ALU.mult), reads=[b_rc], writes=[b_rc])
        P.op("act", lambda e, n=n: e.activation(out=ex[0:n], in_=lg[0:n], func=AF.Exp, bias=rc[0:n, 8:9], scale=1.0), reads=[b_lg, b_rc], writes=[b_ex])
        P.op("dve", lambda e, n=n: e.tensor_scalar(out=lg[0:n], in0=lg[0:n], scalar1=rc[0:n, 3:4], scalar2=None, op0=ALU.is_ge), reads=[b_lg, b_rc], writes=[b_lg])
        tt("dve", ex[0:n], ex[0:n], lg[0:n], ALU.mult, [b_ex, b_lg], [b_ex])
        P.op("dve", lambda e, n=n: e.reduce_sum(out=rc[0:n, 9:10], in_=ex[0:n], axis=AX.X), reads=[b_ex], writes=[b_rc])
        P.op("dve", lambda e, n=n: e.reciprocal(out=rc[0:n, 10:11], in_=rc[0:n, 9:10]), reads=[b_rc], writes=[b_rc])
        P.op("dve", lambda e, n=n, s=s: e.tensor_scalar(out=gt[s][0:n], in0=ex[0:n], scalar1=rc[0:n, 10:11], scalar2=None, op0=ALU.mult), reads=[b_ex, b_rc], writes=[b_gt[s]])
        P.dma("sp", f"gto{s}", lambda e, s=s, n=n, r0=r0: e.dma_start(out=gates[r0:r0+n, :], in_=gt[s][0:n]), reads=[b_gt[s]])
    for k in range(16):
        P.dma("sp", "f16o", lambda e, k=k: e.dma_start(out=fT_v[:, k, :], in_=mixT[:, k, :]), reads=b_mixT)
    P.emit()
    return nc


def _shift(a, k):
    out = np.zeros_like(a)
    if k == -1:
        out[1:] = a[:-1]
    elif k == 1:
        out[:-1] = a[1:]
    return out


def run_out(x_shards, z_all, ret, s5y, att, mod_l, prm):
    C = mix_consts()
    nc = build_out()
    lat = lambda a: a[CTX:]
    ctx = lambda a: a[:CTX]
    sh = lambda a: shard_tokens(lat(a), ctx(a))
    gf = z_all[:, 1536:2048]; gb = z_all[:, 2048:2560]
    rg_s = sh(np.stack([ret[0], ret[1], gf, gb], axis=1))
    s5_s = sh(np.stack([s5y[0], s5y[1], z_all[:, 2560:3072]], axis=1))
    att_s = sh(att)
    zc = z_all[:, 4096:5632]
    bg, cg, hh = zc[:, 0:512], zc[:, 512:1024], zc[:, 1024:1536]

    def sh3(a):
        parts = []
        for k in (-1, 0, 1):
            parts.append(np.concatenate([_shift(ctx(a), k), _shift(lat(a), k)], axis=0) if k else a)
        return parts
    cs_, hs_ = sh3(cg), sh3(hh)
    cv_s = sh(np.stack([bg, cs_[0], hs_[0], cs_[1], hs_[1], cs_[2], hs_[2]], axis=1))
    rep = lambda v: np.broadcast_to(v, (128,) + v.shape)
    vecs = np.ascontiguousarray(np.stack([rep(prm["s5_d"]), rep(prm["s5_b_glu"]), rep(prm["conv_w"][0]), rep(prm["conv_w"][1]), rep(prm["conv_w"][2])], axis=1))
    g1 = np.ascontiguousarray(np.stack([rep(mod_l[0, 2*D:3*D]), rep(mod_l[1, 2*D:3*D])]))
    modc = np.ascontiguousarray(np.stack([cols128(mod_l[0, 3*D:4*D]), cols128(mod_l[0, 4*D:5*D]), cols128(mod_l[1, 3*D:4*D]), cols128(mod_l[1, 4*D:5*D])], axis=-1))
    b_r = np.ascontiguousarray(rep(prm["b_router"]))
    in_maps = []
    for c in range(NCORE):
        in_maps.append({"ident": C["ident"], "x": x_shards[c], "rg": rg_s[c], "s5": s5_s[c], "att": att_s[c], "cv": cv_s[c],
                        "vecs": vecs, "w_glu": prm["s5_w_glu"], "w_out": prm["w_out"], "g1": g1, "modc": modc,
                        "w_r": np.ascontiguousarray(prm["w_router"].reshape(16, 128, 32).transpose(1, 0, 2)), "b_r": b_r})
    res = _run(nc, in_maps)
    return [r["xmid"] for r in res], [r["fT"] for r in res], [r["gates"] for r in res]


E_PC = 4
NCORE_E = 32 // E_PC
DE = 1024
E_TILES = [(i * 512, 512) for i in range(16)] + [(8192, 256)]


def build_moe():
    nc = bass.Bass("TRN2", target_bir_lowering=False)
    di = lambda name, shape, dt=F32: nc.dram_tensor(name, list(shape), dt, kind="ExternalInput").ap()
    fT = di("fT", [D, T_ALL], BF16)
    gb = di("gb", [E_PC, 128, T_ALL])
    wg = di("wg", [E_PC, D, DE]); wu = di("wu", [E_PC, D, DE]); wd = di("wd", [E_PC, DE, D])
    bgu = di("bgu", [128, E_PC, 16]); bd = di("bd", [128, E_PC, 16])
    yT = nc.dram_tensor("yT", [D, T_ALL], F32, kind="ExternalOutput").ap()
    P = Prog(nc)
    NF = 8
    psf = [P.ps(f"psf{i}", [128, 512], F32) for i in range(NF)]; b_psf = [P.buf() for _ in range(NF)]
    cnt = {"f": 0}

    def nf():
        i = cnt["f"] % NF; cnt["f"] += 1
        return psf[i], b_psf[i]

    bgut = P.sb("bgut", [128, E_PC, 16], F32); b_bgu = P.buf()
    bdt = P.sb("bdt", [128, E_PC, 16], F32); b_bd = P.buf()
    P.dma("sp", "bgu", lambda e: e.dma_start(out=bgut[:], in_=bgu), writes=[b_bgu])
    P.dma("sp", "bd", lambda e: e.dma_start(out=bdt[:], in_=bd), writes=[b_bd])
    wgb = P.sb("wgb", [128, 16, DE], BF16); wub = P.sb("wub", [128, 16, DE], BF16); wdb = P.sb("wdb", [128, 8, D], BF16)
    b_wgb = P.buf(); b_wub = P.buf(); b_wdb = P.buf()
    wst = [P.sb(f"wst{i}", [128, 4, 512], F32) for i in range(2)]; b_wst = [P.buf() for _ in range(2)]
    ft = [P.sb(f"ft{i}", [128, 16, 512], BF16) for i in range(2)]; b_ft = [P.buf() for _ in range(2)]
    gtile = [P.sb(f"gtile{i}", [128, 512], F32) for i in range(2)]; b_gtile = [P.buf() for _ in range(2)]
    actT = P.sb("actT", [128, 8, 512], BF16); b_actT = P.buf()
    tg = [P.sb(f"tg{i}", [128, 512], F32) for i in range(2)]; b_tg = [P.buf() for _ in range(2)]
    tsg = [P.sb(f"tsg{i}", [128, 512], F32) for i in range(2)]; b_tsg = [P.buf() for _ in range(2)]
    tu = [P.sb(f"tu{i}", [128, 512], F32) for i in range(2)]; b_tu = [P.buf() for _ in range(2)]
    yp = [P.sb(f"yp{i}", [128, 512], F32) for i in range(3)]; b_yp = [P.buf() for _ in range(3)]
    yo = [P.sb(f"yo{i}", [128, 512], F32) for i in range(3)]; b_yo = [P.buf() for _ in range(3)]
    b_dram = [[P.buf() for _ in E_TILES] for _ in range(16)]
    fT_v = fT.rearrange("(k p) t -> p k t", p=128)
    yT_v = yT.rearrange("(m p) t -> p m t", p=128)
    wsi = 0; fi = 0; oi = 0; mi = 0
    ne = globals().get("MOE_NE", E_PC)
    def load_tile(i):
        ex_, tix_ = divmod(i, len(E_TILES))
        c0_, w_ = E_TILES[tix_]
        fs_ = i % 2
        for kq in range(4):
            P.dma("sp", f"ft{fs_}_{ex_ % 2}", lambda e, fs_=fs_, c0_=c0_, w_=w_, kq=kq: e.dma_start(out=ft[fs_][:, kq*4:(kq+1)*4, 0:w_], in_=fT_v[:, kq*4:(kq+1)*4, c0_:c0_+w_]), writes=[b_ft[fs_]])
        P.dma("sp", f"gtile{fs_}_{ex_ % 2}", lambda e, fs_=fs_, c0_=c0_, w_=w_, ex_=ex_: e.dma_start(out=gtile[fs_][:, 0:w_], in_=gb[ex_, :, c0_:c0_+w_]), writes=[b_gtile[fs_]])

    n_tiles_total = ne * len(E_TILES)
    load_tile(0)
    for ex in range(ne):
        for (src, dst, bdst, nk, ncol) in ((wg, wgb, b_wgb, 16, DE), (wu, wub, b_wub, 16, DE), (wd, wdb, b_wdb, 8, D)):
            sv = src[ex].rearrange("(k p) n -> p k n", p=128)
            for kq in range(nk // 4):
                for cbk in range(ncol // 512):
                    ws_ = wsi % 2; wsi += 1
                    P.dma("sp", f"wst{ws_}_{ex % 2}", lambda e, ws_=ws_, sv=sv, kq=kq, cbk=cbk: e.dma_start(out=wst[ws_][:], in_=sv[:, kq*4:(kq+1)*4, cbk*512:(cbk+1)*512]), writes=[b_wst[ws_]])
                    eng = "pool" if wsi % 2 else "act"
                    if eng == "pool":
                        P.op("pool", lambda e, ws_=ws_, dst=dst, kq=kq, cbk=cbk: e.tensor_copy(out=dst[:, kq*4:(kq+1)*4, cbk*512:(cbk+1)*512], in_=wst[ws_][:]), reads=[b_wst[ws_]], writes=[bdst])
                    else:
                        P.op("act", lambda e, ws_=ws_, dst=dst, kq=kq, cbk=cbk: e.copy(out=dst[:, kq*4:(kq+1)*4, cbk*512:(cbk+1)*512], in_=wst[ws_][:]), reads=[b_wst[ws_]], writes=[bdst])
        for tix, (c0, w) in enumerate(E_TILES):
            fs = fi % 2; fi += 1
            if fi < n_tiles_total:
                load_tile(fi)
            for m in range(8):
                psg, bpsg = nf(); psu, bpsu = nf()
                for k in range(16):
                    P.op("pe", lambda e, psg=psg, k=k, m=m, fs=fs, w=w: e.matmul(psg[:, 0:w], wgb[:, k, m*128:(m+1)*128], ft[fs][:, k, 0:w], start=(k == 0), stop=(k == 15)), reads=[b_wgb, b_ft[fs]], writes=[bpsg])
                for k in range(16):
                    P.op("pe", lambda e, psu=psu, k=k, m=m, fs=fs, w=w: e.matmul(psu[:, 0:w], wub[:, k, m*128:(m+1)*128], ft[fs][:, k, 0:w], start=(k == 0), stop=(k == 15)), reads=[b_wub, b_ft[fs]], writes=[bpsu])
                s = mi % 2; mi += 1
                P.op("dve", lambda e, psg=psg, s=s, m=m, w=w, ex=ex: e.tensor_scalar(out=tg[s][:, 0:w], in0=psg[:, 0:w], scalar1=bgut[:, ex, m:m+1], scalar2=7.0, op0=ALU.add, op1=ALU.min), reads=[bpsg, b_bgu], writes=[b_tg[s]])
                P.op("act", lambda e, s=s, w=w: e.activation(out=tsg[s][:, 0:w], in_=tg[s][:, 0:w], func=AF.Sigmoid, scale=1.702), reads=[b_tg[s]], writes=[b_tsg[s]])
                P.op("dve", lambda e, psu=psu, s=s, m=m, w=w, ex=ex: e.tensor_scalar(out=tu[s][:, 0:w], in0=psu[:, 0:w], scalar1=bgut[:, ex, 8+m:9+m], scalar2=7.0, op0=ALU.add, op1=ALU.min), reads=[bpsu, b_bgu], writes=[b_tu[s]])
                P.op("dve", lambda e, s=s, w=w: e.tensor_scalar(out=tu[s][:, 0:w], in0=tu[s][:, 0:w], scalar1=-7.0, scalar2=1.0, op0=ALU.max, op1=ALU.add), reads=[b_tu[s]], writes=[b_tu[s]])
                P.op("pool", lambda e, s=s, w=w: e.tensor_tensor(out=tg[s][:, 0:w], in0=tg[s][:, 0:w], in1=tsg[s][:, 0:w], op=ALU.mult), reads=[b_tg[s], b_tsg[s]], writes=[b_tg[s]])
                P.op("dve", lambda e, s=s, m=m, w=w: e.tensor_tensor(out=actT[:, m, 0:w], in0=tg[s][:, 0:w], in1=tu[s][:, 0:w], op=ALU.mult), reads=[b_tg[s], b_tu[s]], writes=[b_actT])
            for m2 in range(16):
                psy, bpsy = nf()
                for k in range(8):
                    P.op("pe", lambda e, psy=psy, k=k, m2=m2, w=w: e.matmul(psy[:, 0:w], wdb[:, k, m2*128:(m2+1)*128], actT[:, k, 0:w], start=(k == 0), stop=(k == 7)), reads=[b_wdb, b_actT], writes=[bpsy])
                os_ = oi % 3; oi += 1
                bdr = b_dram[m2][tix]
                if ex > 0:
                    P.dma("sp", f"yp{os_}_{ex % 2}", lambda e, os_=os_, m2=m2, c0=c0, w=w: e.dma_start(out=yp[os_][:, 0:w], in_=yT_v[:, m2, c0:c0+w]), reads=[bdr], writes=[b_yp[os_]])
                P.op("dve", lambda e, psy=psy, os_=os_, m2=m2, w=w, fs=fs, ex=ex: e.scalar_tensor_tensor(out=yo[os_][:, 0:w], in0=psy[:, 0:w], scalar=bdt[:, ex, m2:m2+1], in1=gtile[fs][:, 0:w], op0=ALU.add, op1=ALU.mult),
                     reads=[bpsy, b_bd, b_gtile[fs]], writes=[b_yo[os_]])
                if ex > 0:
                    P.op("pool", lambda e, os_=os_, w=w: e.tensor_tensor(out=yo[os_][:, 0:w], in0=yo[os_][:, 0:w], in1=yp[os_][:, 0:w], op=ALU.add), reads=[b_yo[os_], b_yp[os_]], writes=[b_yo[os_]])
                P.dma("sp", f"yo{os_}_{ex % 2}", lambda e, os_=os_, m2=m2, c0=c0, w=w: e.dma_start(out=yT_v[:, m2, c0:c0+w], in_=yo[os_][:, 0:w]), reads=[b_yo[os_]], writes=[bdr])
    P.emit()
    return nc


def run_moe(fT_shards, gate_shards, prm):
    nc = build_moe()
    fT = np.ascontiguousarray(np.concatenate(fT_shards, axis=1))
    gates = np.concatenate(gate_shards, axis=0)
    in_maps = []
    for c in range(NCORE_E):
        es = slice(E_PC * c, E_PC * (c + 1))
        wgu = prm["w_gate_up"][es]
        bgu = prm["b_gate_up"][es]
        bg = bgu[:, 0::2].reshape(E_PC, 8, 128); bu = bgu[:, 1::2].reshape(E_PC, 8, 128)
        bgu_l = np.ascontiguousarray(np.concatenate([bg, bu], axis=1).transpose(2, 0, 1))
        bd_l = np.ascontiguousarray(prm["b_down"][es].reshape(E_PC, 16, 128).transpose(2, 0, 1))
        gbc = np.ascontiguousarray(np.broadcast_to(gates[:, es].T[:, None, :], (E_PC, 128, T_ALL)))
        in_maps.append({"fT": fT, "gb": gbc, "wg": np.ascontiguousarray(wgu[:, :, 0::2]), "wu": np.ascontiguousarray(wgu[:, :, 1::2]),
                        "wd": prm["w_down"][es], "bgu": bgu_l, "bd": bd_l})
    res = _run(nc, in_maps)
    parts = []
    for cp in range(NCORE):
        parts.append(np.ascontiguousarray(np.stack([res[c]["yT"][:, cp*TOK_PC:(cp+1)*TOK_PC].T for c in range(NCORE_E)], axis=0)))
    return parts


_LAYER_KEYS = ["w_in", "w_out", "s5_a_re", "s5_a_im", "s5_log_step", "s5_b_re", "s5_b_im", "s5_c_re", "s5_c_im",
               "s5_d", "s5_w_glu", "s5_b_glu", "q_norm_w", "k_norm_w", "conv_w", "w_router", "b_router",
               "w_gate_up", "b_gate_up", "w_down", "b_down"]


def _combine_maps(x_shards, parts, g2pair):
    rep = lambda v: np.broadcast_to(v, (128, D))
    g2 = np.ascontiguousarray(np.stack([rep(g2pair[0]), rep(g2pair[1])]))
    return g2


def run_proj2(x_shards, mod_l, w_in_l, parts=None, g2pair=None, project=True):
    combine = parts is not None
    nc = build_proj(combine, project)
    ident = np.eye(128, dtype=np.float32)
    in_maps = []
    for i in range(NCORE):
        m = {"x": x_shards[i], "ident": ident}
        if project:
            m["modc"] = np.ascontiguousarray(np.stack([cols128(mod_l[0, 0:D]), cols128(mod_l[0, D:2*D]), cols128(mod_l[1, 0:D]), cols128(mod_l[1, D:2*D])], axis=-1))
            m["w_in"] = w_in_l
        if combine:
            m["part"] = parts[i]
            m["g2"] = _combine_maps(x_shards, parts, g2pair)
        in_maps.append(m)
    res = _run(nc, in_maps)
    z = [r["z"] for r in res] if project else None
    xo = [r["xo"] for r in res] if combine else None
    return z, xo


def kernel(**inputs):
    inp = {k: np.asarray(v) for k, v in inputs.items()}
    mod = run_mod(inp["c"], inp["c_ctx"], inp["w_mod"], inp["b_mod"])
    x_shards = shard_tokens(inp["x"][0], inp["ctx"][0])
    parts = None
    g2pair = None
    for l in range(2):
        prm = {k: inp[k][l] for k in _LAYER_KEYS}
        z, xo = run_proj2(x_shards, mod[l], prm["w_in"], parts, g2pair)
        if xo is not None:
            x_shards = xo
        zl, zc = unshard_tokens(z)
        z_all = np.concatenate([zc, zl], axis=0)
        ret, s5y, att = run_mix(z_all, prm)
        xmid, fT, gates = run_out(x_shards, z_all, ret, s5y, att, mod[l], prm)
        parts = run_moe(fT, gates, prm)
        x_shards = xmid
        g2pair = (mod[l][0, 5*D:6*D], mod[l][1, 5*D:6*D])
    _, xo = run_proj2(x_shards, None, None, parts, g2pair, project=False)
    lat, _ = unshard_tokens(xo)
    return lat[None].astype(np.float32)
```

```python
import contextlib
import numpy as np
import ml_dtypes
import concourse.bass as bass
import concourse.mybir as mybir
from concourse.bass_utils import run_bass_kernel_spmd

F32 = mybir.dt.float32
BF16 = mybir.dt.bfloat16
I32 = mybir.dt.int32
ALU = mybir.AluOpType
AF = mybir.ActivationFunctionType
AX = mybir.AxisListType


class Buf:
    __slots__ = ("name", "w", "r")

    def __init__(self, name):
        self.name = name
        self.w = None
        self.r = []


class Prog:
    ENG = ("pe", "dve", "act", "pool", "sp")

    def __init__(self, nc):
        self.nc = nc
        self.stream = {e: [] for e in self.ENG}
        self.seen = {e: {} for e in self.ENG}
        self.needed = {e: set() for e in self.ENG}
        self.stack = contextlib.ExitStack()
        self.esem = {e: self.stack.enter_context(nc.semaphore("s_" + e))
                     for e in ("pe", "dve", "act", "pool")}
        self.dsem = {}
        self.dtoks = []
        self.nbuf = 0

    def buf(self, name=None):
        self.nbuf += 1
        return Buf(name or f"b{self.nbuf}")

    def sb(self, name, shape, dt):
        return self.stack.enter_context(self.nc.sbuf_tensor(name, list(shape), dt))

    def ps(self, name, shape, dt):
        return self.stack.enter_context(self.nc.psum_tensor(name, list(shape), dt))

    def _waits(self, eng, reads, writes):
        toks = []
        for b in reads:
            if b.w is not None:
                toks.append(b.w + (True,))
        for b in writes:
            if b.w is not None:
                toks.append(b.w + (False,))
            toks.extend(t + (False,) for t in b.r)
        need = {}
        for kind, src, val, raw in toks:
            if kind == "eng" and src == eng and (not raw or eng == "pe"):
                continue
            key = (kind, src)
            if self.seen[eng].get(key, -1) >= val:
                continue
            if need.get(key, -1) < val:
                need[key] = val
        for key, val in need.items():
            self.seen[eng][key] = val
            if key[0] == "eng":
                self.needed[key[1]].add(val)
        return list(need.items())

    def op(self, eng, fn, reads=(), writes=()):
        waits = self._waits(eng, reads, writes)
        idx = len(self.stream[eng])
        tok = ("eng", eng, idx)
        self.stream[eng].append((waits, fn, None))
        for b in reads:
            b.r.append(tok)
        for b in writes:
            b.w = tok
            b.r = []
        return tok

    def dma(self, q, semkey, fn, reads=(), writes=()):
        waits = self._waits(q, reads, writes)
        if semkey not in self.dsem:
            self.dsem[semkey] = [self.stack.enter_context(self.nc.semaphore("d_" + semkey)), 0]
        self.dsem[semkey][1] += 16
        tok = ("dma", semkey, self.dsem[semkey][1])
        self.stream[q].append((waits, fn, semkey))
        for b in reads:
            b.r.append(tok)
        for b in writes:
            b.w = tok
            b.r = []
        self.dtoks.append(tok)
        return tok

    def finish(self):
        fin = Buf("fin")
        for k, (s, v) in self.dsem.items():
            fin.r.append(("dma", k, v))
        for e in ("pe", "dve", "act", "pool"):
            if self.stream[e]:
                fin.r.append(("eng", e, len(self.stream[e]) - 1))
        waits = self._waits("sp", (), (fin,))
        self.stream["sp"].append((waits, None, None))

    def emit(self):
        nc = self.nc
        self.finish()
        val = {}
        for e in self.ENG:
            c = 0
            for idx in range(len(self.stream[e])):
                if idx in self.needed[e]:
                    c += 1
                    val[(e, idx)] = c
        with nc.Block() as block:
            decos = {"pe": block.tensor, "dve": block.vector, "act": block.scalar,
                     "pool": block.gpsimd, "sp": block.sync}
            for ename in self.ENG:
                items = self.stream[ename]

                def body(e, items=items, ename=ename):
                    for idx, (waits, fn, semkey) in enumerate(items):
                        for (kind, src), v in waits:
                            if kind == "eng":
                                e.wait_ge(self.esem[src], val[(src, v)])
                            else:
                                e.wait_ge(self.dsem[src][0], v)
                        if fn is None:
                            continue
                        ins = fn(e)
                        if semkey is not None:
                            ins.then_inc(self.dsem[semkey][0], 16)
                        elif idx in self.needed[ename]:
                            ins.then_inc(self.esem[ename], 1)

                decos[ename](body)
        self.stack.close()


D = 2048
SEQ = 8192
CTX = 256
NCORE = 8
LAT_PC = SEQ // NCORE
CTX_PC = CTX // NCORE
TOK_PC = LAT_PC + CTX_PC
T_ALL = SEQ + CTX
IN_COLS = 5632
EPS = 1e-6
TILES_PC = [(i * 128, 128) for i in range(8)] + [(1024, 32)]
NPART = 8


def _run(nc, in_maps):
    res = run_bass_kernel_spmd(nc, in_maps, core_ids=list(range(len(in_maps))))
    return res.results


def build_mod():
    nc = bass.Bass("TRN2", target_bir_lowering=False)
    cT = nc.dram_tensor("cT", [128, 16, 2], F32, kind="ExternalInput").ap()
    wm = nc.dram_tensor("wm", [2, 2048, 1536], F32, kind="ExternalInput").ap()
    bm = nc.dram_tensor("bm", [2, 2, 1536], F32, kind="ExternalInput").ap()
    out = nc.dram_tensor("mod", [2, 2, 1536], F32, kind="ExternalOutput").ap()
    P = Prog(nc)
    ct = P.sb("ct", [128, 16, 2], F32); b_ct = P.buf()
    av = P.sb("av", [128, 16, 2], F32); b_av = P.buf()
    bt = P.sb("bt", [2, 2, 1536], F32); b_bt = P.buf()
    ot = P.sb("ot", [2, 2, 1536], F32); b_ot = P.buf()
    NW = 4
    wt = [P.sb(f"wt{i}", [128, 1536], F32) for i in range(NW)]; b_wt = [P.buf() for _ in range(NW)]
    pst = [P.ps(f"ps{i}", [128, 512], F32) for i in range(3)]; b_ps = [P.buf() for _ in range(3)]
    P.dma("sp", "ct", lambda e: e.dma_start(out=ct[:], in_=cT), writes=[b_ct])
    P.dma("sp", "bt", lambda e: e.dma_start(out=bt[:], in_=bm.rearrange("l r n -> r l n")), writes=[b_bt])
    P.op("act", lambda e: e.activation(out=av[:], in_=ct[:], func=AF.Silu), reads=[b_ct], writes=[b_av])
    i = 0
    for l in range(2):
        for k in range(16):
            s = i % NW; i += 1
            P.dma("sp", f"wt{s}", lambda e, s=s, l=l, k=k: e.dma_start(out=wt[s][:], in_=wm[l, k*128:(k+1)*128, :]), writes=[b_wt[s]])
            for n in range(3):
                P.op("pe", lambda e, s=s, n=n, k=k: e.matmul(pst[n][0:2, :], av[:, k, :], wt[s][:, n*512:(n+1)*512], start=(k == 0), stop=(k == 15)),
                     reads=[b_av, b_wt[s]], writes=[b_ps[n]])
        for n in range(3):
            P.op("dve", lambda e, n=n, l=l: e.tensor_tensor(out=ot[:, l, n*512:(n+1)*512], in0=pst[n][0:2, :], in1=bt[:, l, n*512:(n+1)*512], op=ALU.add),
                 reads=[b_ps[n], b_bt], writes=[b_ot])
    P.dma("sp", "ot", lambda e: e.dma_start(out=out.rearrange("l r n -> r l n"), in_=ot[:]), reads=[b_ot])
    P.emit()
    return nc


def run_mod(c, c_ctx, w_mod, b_mod):
    cc = np.stack([c[0], c_ctx], axis=-1)
    cT = np.ascontiguousarray(cc.reshape(16, 128, 2).transpose(1, 0, 2))
    nc = build_mod()
    in_maps = []
    for i in range(NCORE):
        sl = slice(i * 1536, (i + 1) * 1536)
        in_maps.append({"cT": cT, "wm": np.ascontiguousarray(w_mod[:, :, sl]),
                        "bm": np.ascontiguousarray(np.broadcast_to(b_mod[:, None, sl], (2, 2, 1536)))})
    res = _run(nc, in_maps)
    return np.concatenate([r["mod"] for r in res], axis=-1)


def cols128(v):
    return np.ascontiguousarray(v.reshape(16, 128).T)


def build_proj(combine, project=True):
    nc = bass.Bass("TRN2", target_bir_lowering=False)
    x = nc.dram_tensor("x", [TOK_PC, D], F32, kind="ExternalInput").ap()
    ident_d = nc.dram_tensor("ident", [128, 128], F32, kind="ExternalInput").ap()
    P = Prog(nc)
    if project:
        modc = nc.dram_tensor("modc", [128, 16, 4], F32, kind="ExternalInput").ap()
        w_in = nc.dram_tensor("w_in", [D, IN_COLS], F32, kind="ExternalInput").ap()
        z = nc.dram_tensor("z", [TOK_PC, IN_COLS], F32, kind="ExternalOutput").ap()
    if combine:
        part = nc.dram_tensor("part", [NPART, TOK_PC, D], F32, kind="ExternalInput").ap()
        g2 = nc.dram_tensor("g2", [2, 128, D], F32, kind="ExternalInput").ap()
        xo = nc.dram_tensor("xo", [TOK_PC, D], F32, kind="ExternalOutput").ap()
        g2t = P.sb("g2t", [128, 2, D], F32); b_g2 = P.buf()
        P.dma("sp", "g2", lambda e: e.dma_start(out=g2t[:], in_=g2.rearrange("r p d -> p r d")), writes=[b_g2])
        pt = [P.sb(f"pt{i}", [128, D], F32) for i in range(3)]; b_pt = [P.buf() for _ in range(3)]
        acc = P.sb("acc", [128, D], F32); b_acc = P.buf()
    ident = P.sb("identt", [128, 128], F32); b_id = P.buf()
    P.dma("sp", "ident", lambda e: e.dma_start(out=ident[:], in_=ident_d), writes=[b_id])
    xt = [P.sb(f"xt{i}", [128, D], F32) for i in range(2)]; b_xt = [P.buf() for _ in range(2)]
    if project:
        mc = P.sb("mc", [128, 16, 4], F32); b_mc = P.buf()
        P.dma("sp", "mc", lambda e: e.dma_start(out=mc[:], in_=modc), writes=[b_mc])
        P.op("dve", lambda e: e.tensor_scalar(out=mc[:, :, 1], in0=mc[:, :, 1], scalar1=1.0, scalar2=None, op0=ALU.add), reads=[b_mc], writes=[b_mc])
        P.op("dve", lambda e: e.tensor_scalar(out=mc[:, :, 3], in0=mc[:, :, 3], scalar1=1.0, scalar2=None, op0=ALU.add), reads=[b_mc], writes=[b_mc])
        xn = P.sb("xn", [128, D], F32); b_xn = P.buf()
        junk = P.sb("junk", [128, D], BF16); b_junk = P.buf()
        ss = P.sb("ss", [128, 2], F32); b_ss = P.buf()
        xmT = P.sb("xmT", [128, 16, TOK_PC], BF16); b_xmT = [P.buf() for _ in TILES_PC]
        wb = [P.sb(f"wb{i}", [128, 16, 512], BF16) for i in range(2)]; b_wb = [P.buf() for _ in range(2)]
        zt = [P.sb(f"zt{i}", [128, 512], F32) for i in range(4)]; b_zt = [P.buf() for _ in range(4)]
        pst = [P.ps(f"ps{i}", [128, 512], F32) for i in range(8)]; b_ps = [P.buf() for _ in range(8)]
    psi = 0
    for ti, (r0, n) in enumerate(TILES_PC):
        s = ti % 2
        X = xt[s]; bX = b_xt[s]
        P.dma("sp", f"xt{s}", lambda e, X=X, r0=r0, n=n: e.dma_start(out=X[0:n, :], in_=x[r0:r0+n, :]), writes=[bX])
        if combine:
            isctx = 1 if r0 >= LAT_PC else 0
            for c in range(NPART):
                ps_ = c % 3
                P.dma("sp", f"pt{ps_}", lambda e, ps_=ps_, c=c, r0=r0, n=n: e.dma_start(out=pt[ps_][0:n, :], in_=part[c, r0:r0+n, :]), writes=[b_pt[ps_]])
                if c == 0:
                    P.op("pool", lambda e, ps_=ps_, n=n: e.tensor_copy(out=acc[0:n, :], in_=pt[ps_][0:n, :]), reads=[b_pt[ps_]], writes=[b_acc])
                else:
                    eng = "dve" if c % 2 else "pool"
                    P.op(eng, lambda e, ps_=ps_, n=n: e.tensor_tensor(out=acc[0:n, :], in0=acc[0:n, :], in1=pt[ps_][0:n, :], op=ALU.add), reads=[b_pt[ps_], b_acc], writes=[b_acc])
            P.op("dve", lambda e, n=n, isctx=isctx: e.tensor_tensor(out=acc[0:n, :], in0=acc[0:n, :], in1=g2t[0:n, isctx, :], op=ALU.mult), reads=[b_acc, b_g2], writes=[b_acc])
            P.op("dve", lambda e, X=X, n=n: e.tensor_tensor(out=X[0:n, :], in0=X[0:n, :], in1=acc[0:n, :], op=ALU.add), reads=[b_acc, bX], writes=[bX])
            P.dma("sp", f"xo{s}", lambda e, X=X, r0=r0, n=n: e.dma_start(out=xo[r0:r0+n, :], in_=X[0:n, :]), reads=[bX])
        if not project:
            continue
        isctx = 1 if r0 >= LAT_PC else 0
        P.op("act", lambda e, X=X, n=n: e.activation(out=junk[0:n, :], in_=X[0:n, :], func=AF.Square, accum_out=ss[0:n, 0:1]), reads=[bX], writes=[b_junk, b_ss])
        P.op("act", lambda e, n=n: e.activation(out=ss[0:n, 1:2], in_=ss[0:n, 0:1], func=AF.Sqrt, scale=1.0 / D, bias=EPS), reads=[b_ss], writes=[b_ss])
        P.op("dve", lambda e, n=n: e.reciprocal(out=ss[0:n, 1:2], in_=ss[0:n, 1:2]), reads=[b_ss], writes=[b_ss])
        P.op("dve", lambda e, X=X, n=n: e.tensor_scalar(out=xn[0:n, :], in0=X[0:n, :], scalar1=ss[0:n, 1:2], scalar2=None, op0=ALU.mult), reads=[bX, b_ss], writes=[b_xn])
        for kg in range(4):
            pb = psi % 8; psi += 1
            for kk in range(4):
                k = kg * 4 + kk
                P.op("pe", lambda e, pb=pb, kk=kk, k=k, n=n: e.transpose(pst[pb][:, kk*128:kk*128+n], xn[0:n, k*128:(k+1)*128], ident[0:n, 0:n]),
                     reads=[b_xn, b_id], writes=[b_ps[pb]])
            for kk in range(4):
                k = kg * 4 + kk
                if kk % 2 == 0:
                    P.op("dve", lambda e, pb=pb, kk=kk, k=k, n=n, r0=r0, isctx=isctx: e.tensor_scalar(
                        out=xmT[:, k, r0:r0+n], in0=pst[pb][:, kk*128:kk*128+n], scalar1=mc[:, k, 2*isctx+1:2*isctx+2], scalar2=mc[:, k, 2*isctx:2*isctx+1], op0=ALU.mult, op1=ALU.add),
                        reads=[b_ps[pb], b_mc], writes=[b_xmT[ti]])
                else:
                    P.op("act", lambda e, pb=pb, kk=kk, k=k, n=n, r0=r0, isctx=isctx: e.activation(
                        out=xmT[:, k, r0:r0+n], in_=pst[pb][:, kk*128:kk*128+n], func=AF.Identity, scale=mc[:, k, 2*isctx+1:2*isctx+2], bias=mc[:, k, 2*isctx:2*isctx+1]),
                        reads=[b_ps[pb], b_mc], writes=[b_xmT[ti]])
    if project:
        w_v = w_in.rearrange("(k p) n -> p k n", p=128)
        zi = 0
        wsi = 0
        wst = [P.sb(f"wst{i}", [128, 4, 512], F32) for i in range(3)]; b_wst = [P.buf() for _ in range(3)]
        NCB = IN_COLS // 512

        def load_w(cb_):
            nonlocal wsi
            s_ = cb_ % 2
            for kq in range(4):
                ws_ = wsi % 3; wsi += 1
                P.dma("sp", f"wst{ws_}", lambda e, ws_=ws_, cb_=cb_, kq=kq: e.dma_start(out=wst[ws_][:], in_=w_v[:, kq*4:(kq+1)*4, cb_*512:(cb_+1)*512]), writes=[b_wst[ws_]])
                P.op("pool", lambda e, ws_=ws_, s_=s_, kq=kq: e.tensor_copy(out=wb[s_][:, kq*4:(kq+1)*4, :], in_=wst[ws_][:]), reads=[b_wst[ws_]], writes=[b_wb[s_]])

        load_w(0)
        for cb in range(NCB):
            s = cb % 2
            if cb + 1 < NCB:
                load_w(cb + 1)
            for ti, (r0, n) in enumerate(TILES_PC):
                pb = psi % 8; psi += 1
                for k in range(16):
                    P.op("pe", lambda e, pb=pb, k=k, n=n, r0=r0, s=s: e.matmul(pst[pb][0:n, :], xmT[:, k, r0:r0+n], wb[s][:, k, :], start=(k == 0), stop=(k == 15)),
                         reads=[b_xmT[ti], b_wb[s]], writes=[b_ps[pb]])
                zs = zi % 4; zi += 1
                if zi % 2:
                    P.op("dve", lambda e, pb=pb, zs=zs, n=n: e.tensor_copy(out=zt[zs][0:n, :], in_=pst[pb][0:n, :]), reads=[b_ps[pb]], writes=[b_zt[zs]])
                else:
                    P.op("act", lambda e, pb=pb, zs=zs, n=n: e.copy(out=zt[zs][0:n, :], in_=pst[pb][0:n, :]), reads=[b_ps[pb]], writes=[b_zt[zs]])
                P.dma("sp", f"zt{zs}", lambda e, zs=zs, n=n, r0=r0, cb=cb: e.dma_start(out=z[r0:r0+n, cb*512:(cb+1)*512], in_=zt[zs][0:n, :]), reads=[b_zt[zs]])
    P.emit()
    return nc


def shard_tokens(lat, ctx):
    return [np.ascontiguousarray(np.concatenate([lat[i*LAT_PC:(i+1)*LAT_PC], ctx[i*CTX_PC:(i+1)*CTX_PC]], axis=0)) for i in range(NCORE)]


def unshard_tokens(per_core):
    lat = np.concatenate([p[:LAT_PC] for p in per_core], axis=0)
    ctx = np.concatenate([p[LAT_PC:] for p in per_core], axis=0)
    return lat, ctx


def run_proj(x_shards, mod_l, w_in_l, parts=None, project=True):
    combine = parts is not None
    nc = build_proj(combine, project)
    ident = np.eye(128, dtype=np.float32)
    in_maps = []
    for i in range(NCORE):
        m = {"x": x_shards[i], "ident": ident}
        if project:
            sh1, sc1 = mod_l[0, 0:D], mod_l[0, D:2*D]
            csh1, csc1 = mod_l[1, 0:D], mod_l[1, D:2*D]
            m["modc"] = np.ascontiguousarray(np.stack([cols128(sh1), cols128(sc1), cols128(csh1), cols128(csc1)], axis=-1))
            m["w_in"] = w_in_l
        if combine:
            m["part"] = parts[i]
            g2 = np.stack([np.broadcast_to(mod_l_prev_g2[0], (128, D)), np.broadcast_to(mod_l_prev_g2[1], (128, D))])
            m["g2"] = np.ascontiguousarray(g2)
        in_maps.append(m)
    res = _run(nc, in_maps)
    z = [r["z"] for r in res] if project else None
    xo = [r["xo"] for r in res] if combine else None
    return z, xo


NCH = T_ALL // 128
SEG = 384
NSEG = T_ALL // SEG
NQ = 128 + SEQ // 2
NQB = NQ // 128
ATT_SCALE = 128 ** -0.5


def build_mix():
    nc = bass.Bass("TRN2", target_bir_lowering=False)
    di = lambda name, shape, dt=F32: nc.dram_tensor(name, list(shape), dt, kind="ExternalInput").ap()
    do = lambda name, shape, dt=F32: nc.dram_tensor(name, list(shape), dt, kind="ExternalOutput").ap()
    ident_d = di("ident", [128, 128])
    maskT_d = di("maskT", [128, 128])
    r_qkv = di("r_qkv", [T_ALL, 3, 128])
    r_tab = di("r_tab", [T_ALL, 4, 128])
    r_g = di("r_g", [128, 1])
    r_out = do("r_out", [T_ALL, 128])
    s_par = di("s_par", [128, 4, 3])
    s_B = di("s_B", [128, 2, 2, 32])
    s_C = di("s_C", [128, 2, 2, 64])
    s_uT = di("s_uT", [2, 2, 32, T_ALL])
    s_iota = di("s_iota", [128, SEG])
    s_out = do("s_out", [2, T_ALL, 64])
    a_q = di("a_q", [NQ, 128]); a_k = di("a_k", [T_ALL, 128]); a_v = di("a_v", [T_ALL, 128])
    a_qtab = di("a_qtab", [NQ, 2, 128]); a_ktab = di("a_ktab", [T_ALL, 2, 128])
    a_w = di("a_w", [128, 2, 128])
    a_out = do("a_out", [NQ, 128])

    P = Prog(nc)
    NF = 6
    psf = [P.ps(f"psf{i}", [128, 512], F32) for i in range(NF)]; b_psf = [P.buf() for _ in range(NF)]
    psb = [P.ps(f"psb{i}", [128, 512], BF16) for i in range(2)]; b_psb = [P.buf() for _ in range(2)]
    cnt = {"f": 0, "b": 0}

    def nf():
        i = cnt["f"] % NF; cnt["f"] += 1
        return psf[i], b_psf[i]

    def nb():
        i = cnt["b"] % 2; cnt["b"] += 1
        return psb[i], b_psb[i]

    ident = P.sb("identt", [128, 128], F32); b_id = P.buf()
    identb = P.sb("identb", [128, 128], BF16); b_idb = P.buf()
    maskT = P.sb("maskTt", [128, 128], F32); b_mask = P.buf()
    P.dma("sp", "ident", lambda e: e.dma_start(out=ident[:], in_=ident_d), writes=[b_id])
    P.dma("sp", "maskT", lambda e: e.dma_start(out=maskT[:], in_=maskT_d), writes=[b_mask])
    P.op("dve", lambda e: e.tensor_copy(out=identb[:], in_=ident[:]), reads=[b_id], writes=[b_idb])

    def s5_unit():
        par = P.sb("s_par_t", [128, 4, 3], F32); b_par = P.buf()
        Bt = P.sb("s_B_t", [128, 2, 2, 32], F32); b_B = P.buf()
        Ct = P.sb("s_C_t", [128, 2, 2, 64], F32); b_C = P.buf()
        io = P.sb("s_iota_t", [128, SEG], F32); b_io = P.buf()
        P.dma("sp", "s_par", lambda e: e.dma_start(out=par[:], in_=s_par), writes=[b_par])
        P.dma("sp", "s_B", lambda e: e.dma_start(out=Bt[:], in_=s_B), writes=[b_B])
        P.dma("sp", "s_C", lambda e: e.dma_start(out=Ct[:], in_=s_C), writes=[b_C])
        P.dma("sp", "s_iota", lambda e: e.dma_start(out=io[:], in_=s_iota), writes=[b_io])
        P.op("dve", lambda e: e.tensor_scalar(out=Ct[:, :, 1, :], in0=Ct[:, :, 1, :], scalar1=-1.0, scalar2=None, op0=ALU.mult), reads=[b_C], writes=[b_C])
        cs = P.sb("s_cs", [128, 4, SEG], F32); sn = P.sb("s_sn", [128, 4, SEG], F32); rb = P.sb("s_rb", [128, 4, SEG], F32)
        b_tab = [P.buf() for _ in range(4)]
        col = P.sb("s_col", [128, 4, 24], F32); b_col = [P.buf() for _ in range(4)]
        ph = P.sb("s_ph", [128, SEG], F32); b_ph = P.buf()
        ph2 = P.sb("s_ph2", [128, SEG], F32); b_ph2 = P.buf()
        phi = P.sb("s_phi", [128, SEG], I32); b_phi = P.buf()
        BpT = P.sb("s_BpT", [32, 4, 2, 128], F32); b_BpT = [P.buf() for _ in range(4)]
        Bp = P.sb("s_Bp", [128, 2, 32], F32); b_Bp = P.buf()
        tmpB = P.sb("s_tmpB", [128, 32], F32); b_tmpB = P.buf()
        st = P.sb("s_st", [128, 4, 2], F32); b_st = [P.buf() for _ in range(4)]

        def frac_sin(dst, src, bsrc, bdst_list):
            P.op("dve", lambda e: e.tensor_copy(out=phi[:], in_=src), reads=[bsrc], writes=[b_phi])
            P.op("dve", lambda e: e.tensor_tensor(out=ph2[:], in0=src, in1=phi[:], op=ALU.subtract), reads=[bsrc, b_phi], writes=[b_ph2])
            P.op("dve", lambda e: e.tensor_scalar(out=phi[:], in0=ph2[:], scalar1=0.5, scalar2=None, op0=ALU.is_gt), reads=[b_ph2], writes=[b_phi])
            P.op("dve", lambda e: e.tensor_tensor(out=ph2[:], in0=ph2[:], in1=phi[:], op=ALU.subtract), reads=[b_ph2, b_phi], writes=[b_ph2])
            P.op("dve", lambda e: e.tensor_scalar(out=phi[:], in0=ph2[:], scalar1=-0.5, scalar2=None, op0=ALU.is_lt), reads=[b_ph2], writes=[b_phi])
            P.op("dve", lambda e: e.tensor_tensor(out=ph2[:], in0=ph2[:], in1=phi[:], op=ALU.add), reads=[b_ph2, b_phi], writes=[b_ph2])
            P.op("act", lambda e: e.activation(out=dst, in_=ph2[:], func=AF.Sin, scale=2.0 * 3.14159265), reads=[b_ph2], writes=bdst_list)

        for cb in range(4):
            tl = cb % 2
            c_ = lambda j, cb=cb: col[:, cb, j:j+1]
            bc = b_col[cb]
            a_re = par[:, cb, 0:1]; a_im = par[:, cb, 1:2]; lst = par[:, cb, 2:3]
            P.op("act", lambda e, c_=c_, lst=lst: e.activation(out=c_(0), in_=lst, func=AF.Exp), reads=[b_par], writes=[bc])
            P.op("dve", lambda e, c_=c_, a_re=a_re: e.tensor_tensor(out=c_(1), in0=a_re, in1=c_(0), op=ALU.mult), reads=[b_par, bc], writes=[bc])
            P.op("act", lambda e, c_=c_: e.activation(out=c_(2), in_=c_(1), func=AF.Exp), reads=[bc], writes=[bc])
            P.op("dve", lambda e, c_=c_, a_im=a_im: e.tensor_tensor(out=c_(3), in0=a_im, in1=c_(0), op=ALU.mult), reads=[b_par, bc], writes=[bc])
            P.op("dve", lambda e, c_=c_: e.tensor_scalar(out=c_(3), in0=c_(3), scalar1=1.0 / (2.0 * np.pi), scalar2=None, op0=ALU.mult), reads=[bc], writes=[bc])
            P.op("dve", lambda e, c_=c_: e.tensor_scalar(out=ph[:], in0=io[:], scalar1=c_(3), scalar2=None, op0=ALU.mult), reads=[b_io, bc], writes=[b_ph])
            frac_sin(sn[:, cb, :], ph[:], b_ph, [b_tab[cb]])
            P.op("dve", lambda e: e.tensor_scalar(out=ph[:], in0=ph[:], scalar1=0.25, scalar2=None, op0=ALU.add), reads=[b_ph], writes=[b_ph])
            frac_sin(cs[:, cb, :], ph[:], b_ph, [b_tab[cb]])
            P.op("pool", lambda e, cb=cb: e.memset(rb[:, cb, :], 1.0), writes=[b_tab[cb]])
            P.op("dve", lambda e, cb=cb, c_=c_: e.tensor_scalar(out=rb[:, cb, :], in0=rb[:, cb, :], scalar1=c_(2), scalar2=None, op0=ALU.mult), reads=[bc, b_tab[cb]], writes=[b_tab[cb]])
            tt = lambda o, a, b, op, c_=c_: P.op("dve", lambda e: e.tensor_tensor(out=o, in0=a, in1=b, op=op), reads=[bc, b_par, b_tab[cb]], writes=[bc])
            tt(c_(4), c_(2), cs[:, cb, 0:1], ALU.mult)
            tt(c_(5), c_(2), sn[:, cb, 0:1], ALU.mult)
            P.op("dve", lambda e, c_=c_: e.tensor_scalar(out=c_(6), in0=c_(4), scalar1=-1.0, scalar2=None, op0=ALU.add), reads=[bc], writes=[bc])
            tt(c_(7), c_(6), a_re, ALU.mult)
            tt(c_(8), c_(5), a_im, ALU.mult)
            tt(c_(9), c_(7), c_(8), ALU.add)
            tt(c_(10), c_(5), a_re, ALU.mult)
            tt(c_(11), c_(6), a_im, ALU.mult)
            tt(c_(12), c_(10), c_(11), ALU.subtract)
            tt(c_(13), a_re, a_re, ALU.mult)
            tt(c_(14), a_im, a_im, ALU.mult)
            tt(c_(15), c_(13), c_(14), ALU.add)
            P.op("dve", lambda e, c_=c_: e.reciprocal(out=c_(16), in_=c_(15)), reads=[bc], writes=[bc])
            tt(c_(17), c_(9), c_(16), ALU.mult)
            tt(c_(18), c_(12), c_(16), ALU.mult)
            P.op("dve", lambda e, c_=c_, tl=tl: e.tensor_scalar(out=tmpB[:], in0=Bt[:, tl, 1, :], scalar1=c_(18), scalar2=None, op0=ALU.mult), reads=[bc, b_B], writes=[b_tmpB])
            P.op("dve", lambda e, c_=c_, tl=tl: e.scalar_tensor_tensor(out=Bp[:, 0, :], in0=Bt[:, tl, 0, :], scalar=c_(17), in1=tmpB[:], op0=ALU.mult, op1=ALU.subtract), reads=[bc, b_B, b_tmpB], writes=[b_Bp])
            P.op("dve", lambda e, c_=c_, tl=tl: e.tensor_scalar(out=tmpB[:], in0=Bt[:, tl, 0, :], scalar1=c_(18), scalar2=None, op0=ALU.mult), reads=[bc, b_B], writes=[b_tmpB])
            P.op("dve", lambda e, c_=c_, tl=tl: e.scalar_tensor_tensor(out=Bp[:, 1, :], in0=Bt[:, tl, 1, :], scalar=c_(17), in1=tmpB[:], op0=ALU.mult, op1=ALU.add), reads=[bc, b_B, b_tmpB], writes=[b_Bp])
            for ri in range(2):
                pt_, bpt_ = nf()
                P.op("pe", lambda e, pt_=pt_, ri=ri: e.transpose(pt_[0:32, 0:128], Bp[:, ri, :], ident[:]), reads=[b_Bp, b_id], writes=[bpt_])
                P.op("act", lambda e, pt_=pt_, ri=ri, cb=cb: e.copy(out=BpT[:, cb, ri, :], in_=pt_[0:32, 0:128]), reads=[bpt_], writes=[b_BpT[cb]])

        NU = 3
        ut = [P.sb(f"s_ut{i}", [32, SEG], F32) for i in range(NU)]; b_ut = [P.buf() for _ in range(NU)]
        m = [P.sb(f"s_m{i}", [128, SEG], F32) for i in range(4)]; b_m = [P.buf() for _ in range(4)]
        dr = [P.sb(f"s_dr{i}", [128, SEG], F32) for i in range(2)]; b_dr = [P.buf() for _ in range(2)]
        xs_ = [P.sb(f"s_xs{i}", [128, SEG], F32) for i in range(2)]; b_xs = [P.buf() for _ in range(2)]
        xr = [[P.sb(f"s_xr{tl}{ri}", [128, SEG], F32) for ri in range(2)] for tl in range(2)]
        b_xr = [[P.buf() for _ in range(2)] for _ in range(2)]
        ysb = [P.sb(f"s_y{i}", [128, 3, 64], F32) for i in range(2)]; b_ysb = [P.buf() for _ in range(2)]
        ui = 0; yi = 0
        for d in range(2):
            for sg in range(NSEG):
                for tl in range(2):
                    cb = d * 2 + tl
                    us = ui % NU; ui += 1
                    P.dma("sp", f"s_ut{us}", lambda e, us=us, d=d, tl=tl, sg=sg: e.dma_start(out=ut[us][:], in_=s_uT[d, tl, :, sg*SEG:(sg+1)*SEG]), writes=[b_ut[us]])
                    pre, bpre = nf(); pim, bpim = nf()
                    P.op("pe", lambda e, pre=pre, us=us, cb=cb: e.matmul(pre[:, 0:SEG], BpT[:, cb, 0, :], ut[us][:], start=True, stop=True), reads=[b_BpT[cb], b_ut[us]], writes=[bpre])
                    P.op("pe", lambda e, pim=pim, us=us, cb=cb: e.matmul(pim[:, 0:SEG], BpT[:, cb, 1, :], ut[us][:], start=True, stop=True), reads=[b_BpT[cb], b_ut[us]], writes=[bpim])
                    C_ = cs[:, cb, :]; S_ = sn[:, cb, :]
                    P.op("dve", lambda e, pre=pre, C_=C_: e.tensor_tensor(out=m[0][:], in0=pre[:, 0:SEG], in1=C_, op=ALU.mult), reads=[bpre, b_tab[cb]], writes=[b_m[0]])
                    P.op("dve", lambda e, pim=pim, S_=S_: e.tensor_tensor(out=m[1][:], in0=pim[:, 0:SEG], in1=S_, op=ALU.mult), reads=[bpim, b_tab[cb]], writes=[b_m[1]])
                    P.op("dve", lambda e, pim=pim, C_=C_: e.tensor_tensor(out=m[2][:], in0=pim[:, 0:SEG], in1=C_, op=ALU.mult), reads=[bpim, b_tab[cb]], writes=[b_m[2]])
                    P.op("dve", lambda e, pre=pre, S_=S_: e.tensor_tensor(out=m[3][:], in0=pre[:, 0:SEG], in1=S_, op=ALU.mult), reads=[bpre, b_tab[cb]], writes=[b_m[3]])
                    P.op("pool", lambda e: e.tensor_tensor(out=dr[0][:], in0=m[0][:], in1=m[1][:], op=ALU.add), reads=[b_m[0], b_m[1]], writes=[b_dr[0]])
                    P.op("pool", lambda e: e.tensor_tensor(out=dr[1][:], in0=m[2][:], in1=m[3][:], op=ALU.subtract), reads=[b_m[2], b_m[3]], writes=[b_dr[1]])
                    for ri in range(2):
                        init = 0.0 if sg == 0 else st[:, cb, ri:ri+1]
                        P.op("dve", lambda e, ri=ri, init=init, cb=cb: e.tensor_tensor_scan(out=xs_[ri][:], data0=rb[:, cb, :], data1=dr[ri][:], initial=init, op0=ALU.mult, op1=ALU.add),
                             reads=[b_tab[cb], b_dr[ri], b_st[cb]], writes=[b_xs[ri]])
                    P.op("pool", lambda e, C_=C_: e.tensor_tensor(out=m[0][:], in0=xs_[0][:], in1=C_, op=ALU.mult), reads=[b_xs[0], b_tab[cb]], writes=[b_m[0]])
                    P.op("dve", lambda e, S_=S_: e.tensor_tensor(out=m[1][:], in0=xs_[1][:], in1=S_, op=ALU.mult), reads=[b_xs[1], b_tab[cb]], writes=[b_m[1]])
                    P.op("pool", lambda e, S_=S_: e.tensor_tensor(out=m[2][:], in0=xs_[0][:], in1=S_, op=ALU.mult), reads=[b_xs[0], b_tab[cb]], writes=[b_m[2]])
                    P.op("dve", lambda e, C_=C_: e.tensor_tensor(out=m[3][:], in0=xs_[1][:], in1=C_, op=ALU.mult), reads=[b_xs[1], b_tab[cb]], writes=[b_m[3]])
                    P.op("pool", lambda e, tl=tl: e.tensor_tensor(out=xr[tl][0][:], in0=m[0][:], in1=m[1][:], op=ALU.subtract), reads=[b_m[0], b_m[1]], writes=[b_xr[tl][0]])
                    P.op("pool", lambda e, tl=tl: e.tensor_tensor(out=xr[tl][1][:], in0=m[2][:], in1=m[3][:], op=ALU.add), reads=[b_m[2], b_m[3]], writes=[b_xr[tl][1]])
                    for ri in range(2):
                        P.op("act", lambda e, tl=tl, ri=ri, cb=cb: e.copy(out=st[:, cb, ri:ri+1], in_=xr[tl][ri][:, SEG-1:SEG]), reads=[b_xr[tl][ri]], writes=[b_st[cb]])
                ys = yi % 2; yi += 1
                for blk in range(3):
                    py, bpy = nf()
                    j = 0
                    for tl in range(2):
                        for ri in range(2):
                            P.op("pe", lambda e, py=py, tl=tl, ri=ri, blk=blk, j=j: e.matmul(py[:, 0:64], xr[tl][ri][:, blk*128:(blk+1)*128], Ct[:, tl, ri, :], start=(j == 0), stop=(j == 3)),
                                 reads=[b_xr[tl][ri], b_C], writes=[bpy])
                            j += 1
                    P.op("act", lambda e, py=py, ys=ys, blk=blk: e.copy(out=ysb[ys][:, blk, :], in_=py[:, 0:64]), reads=[bpy], writes=[b_ysb[ys]])
                P.dma("sp", f"s_y{ys}", lambda e, ys=ys, d=d, sg=sg: e.dma_start(out=s_out[d, sg*SEG:(sg+1)*SEG, :].rearrange("(b p) c -> p b c", p=128), in_=ysb[ys][:]), reads=[b_ysb[ys]])
                yield

    def ret_unit():
        g = P.sb("r_g_t", [128, 1], F32); b_g = P.buf()
        P.dma("sp", "r_g", lambda e: e.dma_start(out=g[:], in_=r_g), writes=[b_g])
        NB = 2
        qkv = [P.sb(f"r_qkv{i}", [128, 3, 128], F32) for i in range(NB)]; b_qkv = [P.buf() for _ in range(NB)]
        tab = [P.sb(f"r_tab{i}", [128, 4, 128], F32) for i in range(NB)]; b_tb = [P.buf() for _ in range(NB)]
        NS = 2
        sw_l = [P.sb(f"r_sw{i}", [128, 2, 128], F32) for i in range(NS)]; b_sw_l = [P.buf() for _ in range(NS)]
        t1_l = [P.sb(f"r_t1{i}", [128, 2, 128], F32) for i in range(NS)]; b_t1_l = [P.buf() for _ in range(NS)]
        t2_l = [P.sb(f"r_t2{i}", [128, 2, 128], F32) for i in range(NS)]; b_t2_l = [P.buf() for _ in range(NS)]
        qk_l = [P.sb(f"r_qk{i}", [128, 2, 128], BF16) for i in range(NS)]; b_qk_l = [P.buf() for _ in range(NS)]
        qkT_l = [P.sb(f"r_qkT{i}", [128, 2, 128], BF16) for i in range(NS)]; b_qkT_l = [P.buf() for _ in range(NS)]
        vb_l = [P.sb(f"r_vb{i}", [128, 128], BF16) for i in range(NS)]; b_vb_l = [P.buf() for _ in range(NS)]
        sm_l = [P.sb(f"r_sm{i}", [128, 128], BF16) for i in range(NS)]; b_sm_l = [P.buf() for _ in range(NS)]
        S32 = P.sb("r_S32", [128, 128], F32); b_S32 = P.buf()
        Sb = P.sb("r_Sb", [128, 128], BF16); b_Sb = P.buf()
        stt_l = [P.sb(f"r_stt{i}", [128, 6], F32) for i in range(NS)]; b_stt_l = [P.buf() for _ in range(NS)]
        mv_l = [P.sb(f"r_mv{i}", [128, 4], F32) for i in range(NS)]; b_mv_l = [P.buf() for _ in range(NS)]
        yo = [P.sb(f"r_yo{i}", [128, 128], F32) for i in range(2)]; b_yo = [P.buf() for _ in range(2)]
        P.op("pool", lambda e: e.memset(S32[:], 0.0), writes=[b_S32])
        P.op("pool", lambda e: e.memset(Sb[:], 0.0), writes=[b_Sb])
        for n in range(NCH):
            s = n % NB
            Q = qkv[s]; TB = tab[s]
            z_ = n % NS
            sw, t1, t2, qk, qkT, vb, sm, stt, mv = sw_l[z_], t1_l[z_], t2_l[z_], qk_l[z_], qkT_l[z_], vb_l[z_], sm_l[z_], stt_l[z_], mv_l[z_]
            b_sw, b_t1, b_t2, b_qk, b_qkT, b_vb, b_sm, b_stt, b_mv = b_sw_l[z_], b_t1_l[z_], b_t2_l[z_], b_qk_l[z_], b_qkT_l[z_], b_vb_l[z_], b_sm_l[z_], b_stt_l[z_], b_mv_l[z_]
            P.dma("sp", f"r_qkv{s}", lambda e, sw=sw, t1=t1, t2=t2, qk=qk, qkT=qkT, vb=vb, sm=sm, stt=stt, mv=mv, Q=Q, n=n: e.dma_start(out=Q[:], in_=r_qkv[n*128:(n+1)*128]), writes=[b_qkv[s]])
            P.dma("sp", f"r_tab{s}", lambda e, sw=sw, t1=t1, t2=t2, qk=qk, qkT=qkT, vb=vb, sm=sm, stt=stt, mv=mv, TB=TB, n=n: e.dma_start(out=TB[:], in_=r_tab[n*128:(n+1)*128]), writes=[b_tb[s]])
            P.op("pool", lambda e, sw=sw, t1=t1, t2=t2, qk=qk, qkT=qkT, vb=vb, sm=sm, stt=stt, mv=mv, Q=Q: e.tensor_copy(out=sw[:, :, 0:64], in_=Q[:, 0:2, 64:128]), reads=[b_qkv[s]], writes=[b_sw])
            P.op("pool", lambda e, sw=sw, t1=t1, t2=t2, qk=qk, qkT=qkT, vb=vb, sm=sm, stt=stt, mv=mv, Q=Q: e.tensor_copy(out=sw[:, :, 64:128], in_=Q[:, 0:2, 0:64]), reads=[b_qkv[s]], writes=[b_sw])
            TBv = TB[:].rearrange("p (a b) d -> p a b d", b=2)
            P.op("dve", lambda e, sw=sw, t1=t1, t2=t2, qk=qk, qkT=qkT, vb=vb, sm=sm, stt=stt, mv=mv, Q=Q, TBv=TBv: e.tensor_tensor(out=t1[:], in0=Q[:, 0:2, :], in1=TBv[:, :, 0, :], op=ALU.mult), reads=[b_qkv[s], b_tb[s]], writes=[b_t1])
            P.op("pool", lambda e, sw=sw, t1=t1, t2=t2, qk=qk, qkT=qkT, vb=vb, sm=sm, stt=stt, mv=mv, TBv=TBv: e.tensor_tensor(out=t2[:], in0=sw[:], in1=TBv[:, :, 1, :], op=ALU.mult), reads=[b_sw, b_tb[s]], writes=[b_t2])
            P.op("dve", lambda e, sw=sw, t1=t1, t2=t2, qk=qk, qkT=qkT, vb=vb, sm=sm, stt=stt, mv=mv: e.tensor_tensor(out=qk[:], in0=t1[:], in1=t2[:], op=ALU.add), reads=[b_t1, b_t2], writes=[b_qk])
            P.op("act", lambda e, sw=sw, t1=t1, t2=t2, qk=qk, qkT=qkT, vb=vb, sm=sm, stt=stt, mv=mv, Q=Q: e.copy(out=vb[:], in_=Q[:, 2, :]), reads=[b_qkv[s]], writes=[b_vb])
            pt_, bpt_ = nb()
            P.op("pe", lambda e, sw=sw, t1=t1, t2=t2, qk=qk, qkT=qkT, vb=vb, sm=sm, stt=stt, mv=mv, pt_=pt_: e.transpose(pt_[:, 0:128], qk[:, 0, :], identb[:]), reads=[b_qk, b_idb], writes=[bpt_])
            P.op("pe", lambda e, sw=sw, t1=t1, t2=t2, qk=qk, qkT=qkT, vb=vb, sm=sm, stt=stt, mv=mv, pt_=pt_: e.transpose(pt_[:, 128:256], qk[:, 1, :], identb[:]), reads=[b_qk, b_idb], writes=[bpt_])
            P.op("act", lambda e, sw=sw, t1=t1, t2=t2, qk=qk, qkT=qkT, vb=vb, sm=sm, stt=stt, mv=mv, pt_=pt_: e.copy(out=qkT[:].rearrange("p a d -> p (a d)"), in_=pt_[:, 0:256]), reads=[bpt_], writes=[b_qkT])
            ps_s, bps_s = nf()
            P.op("pe", lambda e, sw=sw, t1=t1, t2=t2, qk=qk, qkT=qkT, vb=vb, sm=sm, stt=stt, mv=mv, ps_s=ps_s: e.matmul(ps_s[:, 0:128], qkT[:, 1, :], qkT[:, 0, :], start=True, stop=True), reads=[b_qkT], writes=[bps_s])
            P.op("dve", lambda e, sw=sw, t1=t1, t2=t2, qk=qk, qkT=qkT, vb=vb, sm=sm, stt=stt, mv=mv, ps_s=ps_s: e.tensor_tensor(out=sm[:], in0=ps_s[:, 0:128], in1=maskT[:], op=ALU.mult), reads=[bps_s, b_mask], writes=[b_sm])
            ps_y, bps_y = nf()
            P.op("pe", lambda e, sw=sw, t1=t1, t2=t2, qk=qk, qkT=qkT, vb=vb, sm=sm, stt=stt, mv=mv, ps_y=ps_y: e.matmul(ps_y[:, 0:128], sm[:], vb[:], start=True, stop=False), reads=[b_sm, b_vb], writes=[bps_y])
            P.op("pe", lambda e, sw=sw, t1=t1, t2=t2, qk=qk, qkT=qkT, vb=vb, sm=sm, stt=stt, mv=mv, ps_y=ps_y: e.matmul(ps_y[:, 0:128], qkT[:, 0, :], Sb[:], start=False, stop=True), reads=[b_qkT, b_Sb], writes=[bps_y])
            ps_kv, bps_kv = nf()
            P.op("pe", lambda e, sw=sw, t1=t1, t2=t2, qk=qk, qkT=qkT, vb=vb, sm=sm, stt=stt, mv=mv, ps_kv=ps_kv: e.matmul(ps_kv[:, 0:128], qk[:, 1, :], vb[:], start=True, stop=True), reads=[b_qk, b_vb], writes=[bps_kv])
            P.op("dve", lambda e, sw=sw, t1=t1, t2=t2, qk=qk, qkT=qkT, vb=vb, sm=sm, stt=stt, mv=mv, ps_kv=ps_kv: e.tensor_tensor(out=S32[:], in0=ps_kv[:, 0:128], in1=S32[:], op=ALU.add), reads=[bps_kv, b_S32], writes=[b_S32])
            P.op("dve", lambda e, sw=sw, t1=t1, t2=t2, qk=qk, qkT=qkT, vb=vb, sm=sm, stt=stt, mv=mv: e.tensor_scalar(out=S32[:], in0=S32[:], scalar1=g[:, 0:1], scalar2=None, op0=ALU.mult), reads=[b_S32, b_g], writes=[b_S32])
            P.op("act", lambda e, sw=sw, t1=t1, t2=t2, qk=qk, qkT=qkT, vb=vb, sm=sm, stt=stt, mv=mv: e.copy(out=Sb[:], in_=S32[:]), reads=[b_S32], writes=[b_Sb])
            P.op("dve", lambda e, sw=sw, t1=t1, t2=t2, qk=qk, qkT=qkT, vb=vb, sm=sm, stt=stt, mv=mv, ps_y=ps_y: e.bn_stats(out=stt[:], in_=ps_y[:, 0:128]), reads=[bps_y], writes=[b_stt])
            P.op("dve", lambda e, sw=sw, t1=t1, t2=t2, qk=qk, qkT=qkT, vb=vb, sm=sm, stt=stt, mv=mv: e.bn_aggr(out=mv[:, 0:2], in_=stt[:]), reads=[b_stt], writes=[b_mv])
            P.op("act", lambda e, sw=sw, t1=t1, t2=t2, qk=qk, qkT=qkT, vb=vb, sm=sm, stt=stt, mv=mv: e.activation(out=mv[:, 2:3], in_=mv[:, 1:2], func=AF.Sqrt, bias=EPS, scale=1.0), reads=[b_mv], writes=[b_mv])
            P.op("dve", lambda e, sw=sw, t1=t1, t2=t2, qk=qk, qkT=qkT, vb=vb, sm=sm, stt=stt, mv=mv: e.reciprocal(out=mv[:, 3:4], in_=mv[:, 2:3]), reads=[b_mv], writes=[b_mv])
            ys = n % 2
            P.op("dve", lambda e, sw=sw, t1=t1, t2=t2, qk=qk, qkT=qkT, vb=vb, sm=sm, stt=stt, mv=mv, ps_y=ps_y, ys=ys: e.tensor_scalar(out=yo[ys][:], in0=ps_y[:, 0:128], scalar1=mv[:, 0:1], scalar2=mv[:, 3:4], op0=ALU.subtract, op1=ALU.mult), reads=[bps_y, b_mv], writes=[b_yo[ys]])
            P.dma("sp", f"r_yo{ys}", lambda e, sw=sw, t1=t1, t2=t2, qk=qk, qkT=qkT, vb=vb, sm=sm, stt=stt, mv=mv, ys=ys, n=n: e.dma_start(out=r_out[n*128:(n+1)*128, :], in_=yo[ys][:]), reads=[b_yo[ys]])
            yield

    def att_unit():
        wt = P.sb("a_w_t", [128, 2, 128], F32); b_w = P.buf()
        P.dma("sp", "a_w", lambda e: e.dma_start(out=wt[:], in_=a_w), writes=[b_w])
        kT = P.sb("a_kT", [128, T_ALL], BF16); b_kT = P.buf()
        qT = P.sb("a_qT", [128, NQ], BF16); b_qT = P.buf()
        vb = P.sb("a_vb", [128, NCH, 128], BF16); b_vb = P.buf()
        xin = [P.sb(f"a_xin{i}", [128, 128], F32) for i in range(2)]; b_xin = [P.buf() for _ in range(2)]
        tb = [P.sb(f"a_tb{i}", [128, 2, 128], F32) for i in range(2)]; b_tb = [P.buf() for _ in range(2)]
        NS = 2
        mk = lambda nm, shp, dt: ([P.sb(f"{nm}{i}", shp, dt) for i in range(NS)], [P.buf() for _ in range(NS)])
        junk_l, b_junk_l = mk("a_junk", [128, 128], F32)
        col_l, b_col_l = mk("a_col", [128, 4], F32)
        xn_l, b_xn_l = mk("a_xn", [128, 128], F32)
        sw_l, b_sw_l = mk("a_sw", [128, 128], F32)
        t1_l, b_t1_l = mk("a_t1", [128, 128], F32)
        t2_l, b_t2_l = mk("a_t2", [128, 128], F32)
        xr_l, b_xr_l = mk("a_xr", [128, 128], BF16)
        vst = [P.sb(f"a_vst{i}", [128, 6, 128], F32) for i in range(2)]; b_vst = [P.buf() for _ in range(2)]
        a_v_v = a_v.rearrange("(b p) d -> p b d", p=128)
        for i in range(NCH // 6):
            s = i % 2
            P.dma("sp", f"a_vst{s}", lambda e, s=s, i=i: e.dma_start(out=vst[s][:], in_=a_v_v[:, i*6:(i+1)*6, :]), writes=[b_vst[s]])
            P.op("pool", lambda e, s=s, i=i: e.tensor_copy(out=vb[:, i*6:(i+1)*6, :], in_=vst[s][:]), reads=[b_vst[s]], writes=[b_vb])

        def prep(src, tabsrc, nblk, wi, dstT, b_dstT):
            for blk in range(nblk):
                s = blk % 2
                X = xin[s]; TB = tb[s]
                junk, col, xn, sw, t1, t2, xr = junk_l[s], col_l[s], xn_l[s], sw_l[s], t1_l[s], t2_l[s], xr_l[s]
                b_junk, b_col, b_xn, b_sw, b_t1, b_t2, b_xr = b_junk_l[s], b_col_l[s], b_xn_l[s], b_sw_l[s], b_t1_l[s], b_t2_l[s], b_xr_l[s]
                P.dma("sp", f"a_xin{s}", lambda e, junk=junk, col=col, xn=xn, sw=sw, t1=t1, t2=t2, xr=xr, X=X, blk=blk: e.dma_start(out=X[:], in_=src[blk*128:(blk+1)*128, :]), writes=[b_xin[s]])
                P.dma("sp", f"a_tb{s}", lambda e, junk=junk, col=col, xn=xn, sw=sw, t1=t1, t2=t2, xr=xr, TB=TB, blk=blk: e.dma_start(out=TB[:], in_=tabsrc[blk*128:(blk+1)*128]), writes=[b_tb[s]])
                P.op("act", lambda e, junk=junk, col=col, xn=xn, sw=sw, t1=t1, t2=t2, xr=xr, X=X: e.activation(out=junk[:], in_=X[:], func=AF.Square, accum_out=col[:, 0:1]), reads=[b_xin[s]], writes=[b_junk, b_col])
                P.op("act", lambda e, junk=junk, col=col, xn=xn, sw=sw, t1=t1, t2=t2, xr=xr: e.activation(out=col[:, 1:2], in_=col[:, 0:1], func=AF.Sqrt, scale=1.0 / 128, bias=EPS), reads=[b_col], writes=[b_col])
                P.op("dve", lambda e, junk=junk, col=col, xn=xn, sw=sw, t1=t1, t2=t2, xr=xr: e.reciprocal(out=col[:, 2:3], in_=col[:, 1:2]), reads=[b_col], writes=[b_col])
                P.op("dve", lambda e, junk=junk, col=col, xn=xn, sw=sw, t1=t1, t2=t2, xr=xr, X=X: e.scalar_tensor_tensor(out=xn[:], in0=X[:], scalar=col[:, 2:3], in1=wt[:, wi, :], op0=ALU.mult, op1=ALU.mult), reads=[b_xin[s], b_col, b_w], writes=[b_xn])
                xv = xn[:].rearrange("p (a b d) -> p a b d", a=2, b=2)
                sv = sw[:].rearrange("p (a b d) -> p a b d", a=2, b=2)
                P.op("pool", lambda e, junk=junk, col=col, xn=xn, sw=sw, t1=t1, t2=t2, xr=xr, xv=xv, sv=sv: e.tensor_copy(out=sv[:, :, 0, :], in_=xv[:, :, 1, :]), reads=[b_xn], writes=[b_sw])
                P.op("pool", lambda e, junk=junk, col=col, xn=xn, sw=sw, t1=t1, t2=t2, xr=xr, xv=xv, sv=sv: e.tensor_copy(out=sv[:, :, 1, :], in_=xv[:, :, 0, :]), reads=[b_xn], writes=[b_sw])
                P.op("dve", lambda e, junk=junk, col=col, xn=xn, sw=sw, t1=t1, t2=t2, xr=xr, TB=TB: e.tensor_tensor(out=t1[:], in0=xn[:], in1=TB[:, 0, :], op=ALU.mult), reads=[b_xn, b_tb[s]], writes=[b_t1])
                P.op("pool", lambda e, junk=junk, col=col, xn=xn, sw=sw, t1=t1, t2=t2, xr=xr, TB=TB: e.tensor_tensor(out=t2[:], in0=sw[:], in1=TB[:, 1, :], op=ALU.mult), reads=[b_sw, b_tb[s]], writes=[b_t2])
                P.op("dve", lambda e, junk=junk, col=col, xn=xn, sw=sw, t1=t1, t2=t2, xr=xr: e.tensor_tensor(out=xr[:], in0=t1[:], in1=t2[:], op=ALU.add), reads=[b_t1, b_t2], writes=[b_xr])
                pt_, bpt_ = nb()
                P.op("pe", lambda e, junk=junk, col=col, xn=xn, sw=sw, t1=t1, t2=t2, xr=xr, pt_=pt_: e.transpose(pt_[:, 0:128], xr[:], identb[:]), reads=[b_xr, b_idb], writes=[bpt_])
                P.op("act", lambda e, junk=junk, col=col, xn=xn, sw=sw, t1=t1, t2=t2, xr=xr, pt_=pt_, blk=blk: e.copy(out=dstT[:, blk*128:(blk+1)*128], in_=pt_[:, 0:128]), reads=[bpt_], writes=[b_dstT])
                yield

        yield from prep(a_k, a_ktab, NCH, 1, kT, b_kT)
        yield from prep(a_q, a_qtab, NQB, 0, qT, b_qT)

        Ssb = P.sb("a_S", [128, T_ALL], F32); b_S = P.buf()
        Pb = P.sb("a_P", [128, T_ALL], BF16); b_P = P.buf()
        PT = P.sb("a_PT", [128, NCH, 128], BF16); b_PT = P.buf()
        c2 = P.sb("a_c2", [128, 4], F32); b_c2 = P.buf()
        ob = [P.sb(f"a_o{i}", [128, 128], F32) for i in range(2)]; b_ob = [P.buf() for _ in range(2)]
        for qb in range(NQB):
            nk = CTX if qb == 0 else T_ALL
            nkt = (nk + 511) // 512
            for kt in range(nkt):
                w = min(512, nk - kt * 512)
                ps_, bps_ = nf()
                P.op("pe", lambda e, ps_=ps_, qb=qb, kt=kt, w=w: e.matmul(ps_[:, 0:w], qT[:, qb*128:(qb+1)*128], kT[:, kt*512:kt*512+w], start=True, stop=True), reads=[b_qT, b_kT], writes=[bps_])
                if kt % 2:
                    P.op("dve", lambda e, ps_=ps_, kt=kt, w=w: e.tensor_copy(out=Ssb[:, kt*512:kt*512+w], in_=ps_[:, 0:w]), reads=[bps_], writes=[b_S])
                else:
                    P.op("act", lambda e, ps_=ps_, kt=kt, w=w: e.copy(out=Ssb[:, kt*512:kt*512+w], in_=ps_[:, 0:w]), reads=[bps_], writes=[b_S])
            P.op("dve", lambda e, nk=nk: e.reduce_max(out=c2[:, 0:1], in_=Ssb[:, 0:nk], axis=AX.X), reads=[b_S], writes=[b_c2])
            P.op("dve", lambda e: e.tensor_scalar(out=c2[:, 1:2], in0=c2[:, 0:1], scalar1=-ATT_SCALE, scalar2=None, op0=ALU.mult), reads=[b_c2], writes=[b_c2])
            P.op("act", lambda e, nk=nk: e.activation(out=Pb[:, 0:nk], in_=Ssb[:, 0:nk], func=AF.Exp, scale=ATT_SCALE, bias=c2[:, 1:2], accum_out=c2[:, 2:3]), reads=[b_S, b_c2], writes=[b_P, b_c2])
            P.op("dve", lambda e: e.reciprocal(out=c2[:, 3:4], in_=c2[:, 2:3]), reads=[b_c2], writes=[b_c2])
            nkb = nk // 128
            for g0 in range(0, nkb, 4):
                gn = min(4, nkb - g0)
                pt_, bpt_ = nb()
                for j in range(gn):
                    P.op("pe", lambda e, pt_=pt_, j=j, g0=g0: e.transpose(pt_[:, j*128:(j+1)*128], Pb[:, (g0+j)*128:(g0+j+1)*128], identb[:]), reads=[b_P, b_idb], writes=[bpt_])
                if (g0 // 4) % 2:
                    P.op("dve", lambda e, pt_=pt_, g0=g0, gn=gn: e.tensor_copy(out=PT[:, g0:g0+gn, :].rearrange("p a d -> p (a d)"), in_=pt_[:, 0:gn*128]), reads=[bpt_], writes=[b_PT])
                else:
                    P.op("act", lambda e, pt_=pt_, g0=g0, gn=gn: e.copy(out=PT[:, g0:g0+gn, :].rearrange("p a d -> p (a d)"), in_=pt_[:, 0:gn*128]), reads=[bpt_], writes=[b_PT])
            po, bpo = nf()
            for kb in range(nkb):
                P.op("pe", lambda e, po=po, kb=kb, nkb=nkb: e.matmul(po[:, 0:128], PT[:, kb, :], vb[:, kb, :], start=(kb == 0), stop=(kb == nkb - 1)), reads=[b_PT, b_vb], writes=[bpo])
            os_ = qb % 2
            P.op("dve", lambda e, po=po, os_=os_: e.tensor_scalar(out=ob[os_][:], in0=po[:, 0:128], scalar1=c2[:, 3:4], scalar2=None, op0=ALU.mult), reads=[bpo, b_c2], writes=[b_ob[os_]])
            P.dma("sp", f"a_o{os_}", lambda e, os_=os_, qb=qb: e.dma_start(out=a_out[qb*128:(qb+1)*128, :], in_=ob[os_][:]), reads=[b_ob[os_]])
            yield

    units = globals().get("MIX_UNITS", "rsa")
    gens = []
    if "a" in units:
        gens.append(att_unit())
    if "r" in units:
        gens.append(ret_unit())
    if "s" in units:
        gens.append(s5_unit())
    while gens:
        for g_ in list(gens):
            try:
                next(g_)
            except StopIteration:
                gens.remove(g_)
    P.emit()
    return nc


def _order(d):
    if d == 0:
        return np.arange(T_ALL)
    return np.concatenate([np.arange(CTX)[::-1], CTX + np.arange(SEQ)[::-1]])


_CONST = {}


def mix_consts():
    if _CONST:
        return _CONST
    f64 = np.float64
    log_g = np.log(1.0 - 2.0 ** (-5.0 - np.arange(4, dtype=f64)))
    freqs = 10000.0 ** (-np.arange(0, 128, 2, dtype=f64) / 128)
    rt = {}
    for d in range(2):
        lg = log_g if d == 0 else log_g[::-1]
        order = _order(d)
        isl = order >= CTX
        pos = np.where(isl, order - CTX, 0).astype(f64)
        ang = pos[:, None] * freqs[None, :]
        cos = np.where(isl[:, None], np.cos(ang), 1.0)
        sin = np.where(isl[:, None], np.sin(ang), 0.0)
        cosf = np.concatenate([cos, cos], axis=1)
        sinf = np.concatenate([-sin, sin], axis=1)
        i = (np.arange(T_ALL) % 128).astype(f64)
        for h in range(4):
            gq = np.exp((i + 1.0) * lg[h])[:, None]
            gk = (128.0 ** -0.5) * np.exp(-(i + 1.0) * lg[h])[:, None]
            tab = np.stack([cosf * gq, sinf * gq, cosf * gk, sinf * gk], axis=1).astype(np.float32)
            rt[(h, d)] = (np.ascontiguousarray(tab), np.full((128, 1), np.exp(128.0 * lg[h]), np.float32))
    _CONST["ret"] = rt
    fr = 10000.0 ** (-np.arange(0, 64, 2, dtype=f64) / 64)
    pos = np.arange(SEQ)
    ar = (pos // 64).astype(f64)[:, None] * fr[None, :]
    ac = (pos % 64).astype(f64)[:, None] * fr[None, :]
    cosl = np.concatenate([np.cos(ar), np.cos(ar), np.cos(ac), np.cos(ac)], axis=1)
    sinl = np.concatenate([-np.sin(ar), np.sin(ar), -np.sin(ac), np.sin(ac)], axis=1)
    cosa = np.concatenate([np.ones((CTX, 128)), cosl], axis=0)
    sina = np.concatenate([np.zeros((CTX, 128)), sinl], axis=0)
    _CONST["att"] = np.ascontiguousarray(np.stack([cosa, sina], axis=1).astype(np.float32))
    _CONST["ident"] = np.eye(128, dtype=np.float32)
    _CONST["maskT"] = np.triu(np.ones((128, 128), np.float32))
    _CONST["iota"] = np.ascontiguousarray(np.broadcast_to(np.arange(1, SEG + 1, dtype=np.float32), (128, SEG)))
    return _CONST


def run_mix(z_all, prm):
    C = mix_consts()
    nc = build_mix()
    in_maps = []
    orders = [_order(0), _order(1)]
    for c in range(NCORE):
        m = {"ident": C["ident"], "maskT": C["maskT"]}
        h, d = c % 4, c // 4
        zo = z_all[orders[d]]
        m["r_qkv"] = np.ascontiguousarray(np.stack([zo[:, h*128:(h+1)*128], zo[:, 512+h*128:512+(h+1)*128], zo[:, 1024+h*128:1024+(h+1)*128]], axis=1))
        m["r_tab"], m["r_g"] = C["ret"][(h, d)]
        par = np.zeros((128, 4, 3), np.float32)
        sB = np.zeros((128, 2, 2, 32), np.float32)
        sC = np.zeros((128, 2, 2, 64), np.float32)
        uT = np.zeros((2, 2, 32, T_ALL), np.float32)
        for tl in range(2):
            for gi in range(2):
                g = 4 * c + 2 * tl + gi
                rows = slice(gi * 64, (gi + 1) * 64)
                for dd in range(2):
                    par[rows, dd*2+tl, 0] = prm["s5_a_re"][dd, g]
                    par[rows, dd*2+tl, 1] = prm["s5_a_im"][dd, g]
                    par[rows, dd*2+tl, 2] = prm["s5_log_step"][dd, g]
                sB[rows, tl, 0, gi*16:(gi+1)*16] = prm["s5_b_re"][g]
                sB[rows, tl, 1, gi*16:(gi+1)*16] = prm["s5_b_im"][g]
                sC[rows, tl, 0, tl*32+gi*16:tl*32+(gi+1)*16] = prm["s5_c_re"][g].T
                sC[rows, tl, 1, tl*32+gi*16:tl*32+(gi+1)*16] = prm["s5_c_im"][g].T
            for dd in range(2):
                ucols = z_all[orders[dd], 2560 + (4*c + 2*tl) * 16: 2560 + (4*c + 2*tl + 2) * 16]
                uT[dd, tl] = ucols.T
        m["s_par"], m["s_B"], m["s_C"], m["s_uT"], m["s_iota"] = par, sB, sC, uT, C["iota"]
        hq, half = c // 2, c % 2
        kvh = hq // 2
        A0 = 3072
        qsel = np.concatenate([np.arange(half*128, (half+1)*128), CTX + np.arange(half*4096, (half+1)*4096)])
        m["a_q"] = np.ascontiguousarray(z_all[qsel, A0 + hq*128: A0 + (hq+1)*128])
        m["a_k"] = np.ascontiguousarray(z_all[:, A0 + 512 + kvh*128: A0 + 512 + (kvh+1)*128])
        m["a_v"] = np.ascontiguousarray(z_all[:, A0 + 768 + kvh*128: A0 + 768 + (kvh+1)*128])
        m["a_qtab"] = np.ascontiguousarray(C["att"][qsel])
        m["a_ktab"] = C["att"]
        m["a_w"] = np.ascontiguousarray(np.stack([np.broadcast_to(prm["q_norm_w"], (128, 128)), np.broadcast_to(prm["k_norm_w"], (128, 128))], axis=1))
        in_maps.append(m)
    res = _run(nc, in_maps)
    ret = np.zeros((2, T_ALL, 512), np.float32)
    s5y = np.zeros((2, T_ALL, 512), np.float32)
    att = np.zeros((T_ALL, 512), np.float32)
    for c in range(NCORE):
        h, d = c % 4, c // 4
        if "r_out" in res[c]:
            ret[d][orders[d], h*128:(h+1)*128] = res[c]["r_out"]
        for dd in range(2):
            s5y[dd][orders[dd], c*64:(c+1)*64] = res[c]["s_out"][dd]
        hq, half = c // 2, c % 2
        qsel = np.concatenate([np.arange(half*128, (half+1)*128), CTX + np.arange(half*4096, (half+1)*4096)])
        att[qsel, hq*128:(hq+1)*128] = res[c]["a_out"]
    return ret, s5y, att


def build_out():
    nc = bass.Bass("TRN2", target_bir_lowering=False)
    di = lambda name, shape, dt=F32: nc.dram_tensor(name, list(shape), dt, kind="ExternalInput").ap()
    do = lambda name, shape, dt=F32: nc.dram_tensor(name, list(shape), dt, kind="ExternalOutput").ap()
    ident_d = di("ident", [128, 128])
    x = di("x", [TOK_PC, D])
    rg = di("rg", [TOK_PC, 4, 512])
    s5 = di("s5", [TOK_PC, 3, 512])
    att = di("att", [TOK_PC, 512])
    cv = di("cv", [TOK_PC, 7, 512])
    vecs = di("vecs", [128, 5, 512])
    w_glu = di("w_glu", [512, 512])
    w_out = di("w_out", [D, D])
    g1 = di("g1", [2, 128, D])
    modc = di("modc", [128, 16, 4])
    w_r = di("w_r", [128, 16, 32])
    b_r = di("b_r", [128, 32])
    xmid = do("xmid", [TOK_PC, D])
    fT = do("fT", [D, TOK_PC], BF16)
    gates = do("gates", [TOK_PC, 32])

    P = Prog(nc)
    NF = 5
    psf = [P.ps(f"psf{i}", [128, 512], F32) for i in range(NF)]; b_psf = [P.buf() for _ in range(NF)]
    psb = [P.ps(f"psb{i}", [128, 512], BF16) for i in range(2)]; b_psb = [P.buf() for _ in range(2)]
    cnt = {"f": 0, "b": 0}

    def nf():
        i = cnt["f"] % NF; cnt["f"] += 1
        return psf[i], b_psf[i]

    def nb():
        i = cnt["b"] % 2; cnt["b"] += 1
        return psb[i], b_psb[i]

    ident = P.sb("identt", [128, 128], F32); b_id = P.buf()
    identb = P.sb("identb", [128, 128], BF16); b_idb = P.buf()
    P.dma("sp", "ident", lambda e: e.dma_start(out=ident[:], in_=ident_d), writes=[b_id])
    P.op("dve", lambda e: e.tensor_copy(out=identb[:], in_=ident[:]), reads=[b_id], writes=[b_idb])
    vt = P.sb("vecs_t", [128, 5, 512], F32); b_vt = P.buf()
    P.dma("sp", "vecs", lambda e: e.dma_start(out=vt[:], in_=vecs), writes=[b_vt])
    g1t = P.sb("g1t", [128, 2, D], F32); b_g1 = P.buf()
    P.dma("sp", "g1", lambda e: e.dma_start(out=g1t[:], in_=g1.rearrange("r p d -> p r d")), writes=[b_g1])
    mc = P.sb("mc", [128, 16, 4], F32); b_mc = P.buf()
    P.dma("sp", "mc", lambda e: e.dma_start(out=mc[:], in_=modc), writes=[b_mc])
    P.op("dve", lambda e: e.tensor_scalar(out=mc[:, :, 1], in0=mc[:, :, 1], scalar1=1.0, scalar2=None, op0=ALU.add), reads=[b_mc], writes=[b_mc])
    P.op("dve", lambda e: e.tensor_scalar(out=mc[:, :, 3], in0=mc[:, :, 3], scalar1=1.0, scalar2=None, op0=ALU.add), reads=[b_mc], writes=[b_mc])
    wr = P.sb("wr", [128, 16, 32], F32); b_wr = P.buf()
    P.dma("sp", "wr", lambda e: e.dma_start(out=wr[:], in_=w_r), writes=[b_wr])
    brt = P.sb("brt", [128, 32], F32); b_br = P.buf()
    P.dma("sp", "brt", lambda e: e.dma_start(out=brt[:], in_=b_r), writes=[b_br])
    wst = [P.sb(f"wst{i}", [128, 4, 512], F32) for i in range(2)]; b_wst = [P.buf() for _ in range(2)]
    wg = P.sb("wg", [128, 4, 512], BF16); b_wg = P.buf()
    P.dma("sp", "wst0", lambda e: e.dma_start(out=wst[0][:], in_=w_glu.rearrange("(k p) n -> p k n", p=128)), writes=[b_wst[0]])
    P.op("pool", lambda e: e.tensor_copy(out=wg[:], in_=wst[0][:]), reads=[b_wst[0]], writes=[b_wg])

    mixT = P.sb("mixT", [128, 16, TOK_PC], BF16); b_mixT = [P.buf() for _ in TILES_PC]
    rgt = P.sb("rgt", [128, 4, 512], F32); b_rg = P.buf()
    s5t = P.sb("s5t", [128, 3, 512], F32); b_s5 = P.buf()
    att_t = P.sb("att_t", [128, 512], F32); b_att = P.buf()
    cvt = P.sb("cvt", [128, 7, 512], F32); b_cv = P.buf()
    tm = [P.sb(f"tm{i}", [128, 512], F32) for i in range(5)]; b_tm = [P.buf() for _ in range(5)]
    yb16 = P.sb("yb16", [128, 512], BF16); b_yb16 = P.buf()
    yT = P.sb("yT", [128, 4, 128], BF16); b_yT = P.buf()
    mix = P.sb("mix", [128, D], BF16); b_mix = [P.buf() for _ in range(4)]

    def tt(eng, o, a, b, op, rd, wr_):
        P.op(eng, lambda e: e.tensor_tensor(out=o, in0=a, in1=b, op=op), reads=rd, writes=wr_)

    for ti, (r0, n) in enumerate(TILES_PC):
        P.dma("sp", "rgt", lambda e, r0=r0, n=n: e.dma_start(out=rgt[0:n], in_=rg[r0:r0+n]), writes=[b_rg])
        P.dma("sp", "s5t", lambda e, r0=r0, n=n: e.dma_start(out=s5t[0:n], in_=s5[r0:r0+n]), writes=[b_s5])
        P.dma("sp", "att_t", lambda e, r0=r0, n=n: e.dma_start(out=att_t[0:n], in_=att[r0:r0+n]), writes=[b_att])
        P.dma("sp", "cvt", lambda e, r0=r0, n=n: e.dma_start(out=cvt[0:n], in_=cv[r0:r0+n]), writes=[b_cv])
        P.op("act", lambda e, n=n: e.activation(out=tm[0][0:n], in_=rgt[0:n, 2, :], func=AF.Silu), reads=[b_rg], writes=[b_tm[0]])
        P.op("act", lambda e, n=n: e.activation(out=tm[1][0:n], in_=rgt[0:n, 3, :], func=AF.Silu), reads=[b_rg], writes=[b_tm[1]])
        tt("dve", tm[0][0:n], tm[0][0:n], rgt[0:n, 0, :], ALU.mult, [b_tm[0], b_rg], [b_tm[0]])
        tt("pool", tm[1][0:n], tm[1][0:n], rgt[0:n, 1, :], ALU.mult, [b_tm[1], b_rg], [b_tm[1]])
        tt("dve", mix[0:n, 0:512], tm[0][0:n], tm[1][0:n], ALU.add, [b_tm[0], b_tm[1]], [b_mix[0]])
        tt("pool", tm[2][0:n], s5t[0:n, 0, :], s5t[0:n, 1, :], ALU.add, [b_s5], [b_tm[2]])
        tt("dve", tm[3][0:n], s5t[0:n, 2, :], vt[0:n, 0, :], ALU.mult, [b_s5, b_vt], [b_tm[3]])
        tt("pool", tm[2][0:n], tm[2][0:n], tm[3][0:n], ALU.add, [b_tm[2], b_tm[3]], [b_tm[2]])
        tt("pool", tm[3][0:n], tm[2][0:n], tm[2][0:n], ALU.mult, [b_tm[2]], [b_tm[3]])
        P.op("dve", lambda e, n=n: e.tensor_scalar(out=tm[3][0:n], in0=tm[3][0:n], scalar1=0.044715, scalar2=1.0, op0=ALU.mult, op1=ALU.add), reads=[b_tm[3]], writes=[b_tm[3]])
        tt("dve", tm[3][0:n], tm[3][0:n], tm[2][0:n], ALU.mult, [b_tm[3], b_tm[2]], [b_tm[3]])
        P.op("act", lambda e, n=n: e.activation(out=tm[3][0:n], in_=tm[3][0:n], func=AF.Sigmoid, scale=1.5957691216057308), reads=[b_tm[3]], writes=[b_tm[3]])
        tt("dve", tm[2][0:n], tm[2][0:n], tm[3][0:n], ALU.mult, [b_tm[2], b_tm[3]], [b_tm[2]])
        P.op("act", lambda e, n=n: e.copy(out=yb16[0:n], in_=tm[2][0:n]), reads=[b_tm[2]], writes=[b_yb16])
        pt_, bpt_ = nb()
        for k in range(4):
            P.op("pe", lambda e, pt_=pt_, k=k, n=n: e.transpose(pt_[:, k*128:k*128+n], yb16[0:n, k*128:(k+1)*128], identb[0:n, 0:n]), reads=[b_yb16, b_idb], writes=[bpt_])
        P.op("act", lambda e, pt_=pt_, n=n: e.copy(out=yT[:, :, 0:n], in_=pt_[:, 0:512].rearrange("p (a d) -> p a d", a=4)[:, :, 0:n]), reads=[bpt_], writes=[b_yT])
        pg, bpg = nf()
        for k in range(4):
            P.op("pe", lambda e, pg=pg, k=k, n=n: e.matmul(pg[0:n, :], yT[:, k, 0:n], wg[:, k, :], start=(k == 0), stop=(k == 3)), reads=[b_yT, b_wg], writes=[bpg])
        tt("dve", tm[3][0:n], pg[0:n, :], vt[0:n, 1, :], ALU.add, [bpg, b_vt], [b_tm[3]])
        P.op("act", lambda e, n=n: e.activation(out=tm[3][0:n], in_=tm[3][0:n], func=AF.Sigmoid), reads=[b_tm[3]], writes=[b_tm[3]])
        tt("dve", mix[0:n, 512:1024], tm[2][0:n], tm[3][0:n], ALU.mult, [b_tm[2], b_tm[3]], [b_mix[1]])
        P.op("act", lambda e, n=n: e.copy(out=mix[0:n, 1024:1536], in_=att_t[0:n]), reads=[b_att], writes=[b_mix[2]])
        tt("pool", tm[0][0:n], cvt[0:n, 1, :], cvt[0:n, 2, :], ALU.mult, [b_cv], [b_tm[0]])
        tt("dve", tm[1][0:n], cvt[0:n, 3, :], cvt[0:n, 4, :], ALU.mult, [b_cv], [b_tm[1]])
        tt("pool", tm[4][0:n], cvt[0:n, 5, :], cvt[0:n, 6, :], ALU.mult, [b_cv], [b_tm[4]])
        tt("dve", tm[0][0:n], tm[0][0:n], vt[0:n, 2, :], ALU.mult, [b_tm[0], b_vt], [b_tm[0]])
        tt("pool", tm[1][0:n], tm[1][0:n], vt[0:n, 3, :], ALU.mult, [b_tm[1], b_vt], [b_tm[1]])
        tt("dve", tm[4][0:n], tm[4][0:n], vt[0:n, 4, :], ALU.mult, [b_tm[4], b_vt], [b_tm[4]])
        tt("pool", tm[0][0:n], tm[0][0:n], tm[1][0:n], ALU.add, [b_tm[0], b_tm[1]], [b_tm[0]])
        tt("dve", tm[0][0:n], tm[0][0:n], tm[4][0:n], ALU.add, [b_tm[0], b_tm[4]], [b_tm[0]])
        tt("dve", mix[0:n, 1536:2048], tm[0][0:n], cvt[0:n, 0, :], ALU.mult, [b_tm[0], b_cv], [b_mix[3]])
        for kg in range(4):
            pt_, bpt_ = nb()
            for kk in range(4):
                k = kg * 4 + kk
                P.op("pe", lambda e, pt_=pt_, kk=kk, k=k, n=n: e.transpose(pt_[:, kk*128:kk*128+n], mix[0:n, k*128:(k+1)*128], identb[0:n, 0:n]), reads=[b_mix[kg], b_idb], writes=[bpt_])
            eng = "act" if kg % 2 else "dve"
            if eng == "act":
                P.op("act", lambda e, pt_=pt_, kg=kg, n=n, r0=r0: e.copy(out=mixT[:, kg*4:(kg+1)*4, r0:r0+n], in_=pt_[:, 0:512].rearrange("p (a d) -> p a d", a=4)[:, :, 0:n]), reads=[bpt_], writes=[b_mixT[ti]])
            else:
                P.op("dve", lambda e, pt_=pt_, kg=kg, n=n, r0=r0: e.tensor_copy(out=mixT[:, kg*4:(kg+1)*4, r0:r0+n], in_=pt_[:, 0:512].rearrange("p (a d) -> p a d", a=4)[:, :, 0:n]), reads=[bpt_], writes=[b_mixT[ti]])

    wb = [P.sb(f"wb{i}", [128, 16, 512], BF16) for i in range(2)]; b_wb = [P.buf() for _ in range(2)]
    xp = [P.sb(f"xp{i}", [128, 512], F32) for i in range(3)]; b_xp = [P.buf() for _ in range(3)]
    b_xm_dram = [P.buf() for _ in TILES_PC]
    w_v = w_out.rearrange("(k p) n -> p k n", p=128)
    wsi = 1; xi = 0
    def load_wo(cb_):
        nonlocal wsi
        s_ = cb_ % 2
        for kq in range(4):
            ws_ = wsi % 2; wsi += 1
            P.dma("sp", f"wst{ws_}", lambda e, ws_=ws_, cb_=cb_, kq=kq: e.dma_start(out=wst[ws_][:], in_=w_v[:, kq*4:(kq+1)*4, cb_*512:(cb_+1)*512]), writes=[b_wst[ws_]])
            P.op("pool", lambda e, ws_=ws_, s_=s_, kq=kq: e.tensor_copy(out=wb[s_][:, kq*4:(kq+1)*4, :], in_=wst[ws_][:]), reads=[b_wst[ws_]], writes=[b_wb[s_]])

    load_wo(0)
    for cb in range(4):
        s = cb % 2
        if cb + 1 < 4:
            load_wo(cb + 1)
        for ti, (r0, n) in enumerate(TILES_PC):
            isctx = 1 if r0 >= LAT_PC else 0
            xs_ = xi % 3; xi += 1
            P.dma("sp", f"xp{xs_}", lambda e, xs_=xs_, r0=r0, n=n, cb=cb: e.dma_start(out=xp[xs_][0:n], in_=x[r0:r0+n, cb*512:(cb+1)*512]), writes=[b_xp[xs_]])
            po, bpo = nf()
            for k in range(16):
                P.op("pe", lambda e, po=po, k=k, n=n, r0=r0, s=s: e.matmul(po[0:n, :], mixT[:, k, r0:r0+n], wb[s][:, k, :], start=(k == 0), stop=(k == 15)), reads=[b_mixT[ti], b_wb[s]], writes=[bpo])
            t_ = tm[xi % 2]; bt_ = b_tm[xi % 2]
            tt("dve", t_[0:n], po[0:n, :], g1t[0:n, isctx, cb*512:(cb+1)*512], ALU.mult, [bpo, b_g1], [bt_])
            tt("pool", xp[xs_][0:n], xp[xs_][0:n], t_[0:n], ALU.add, [b_xp[xs_], bt_], [b_xp[xs_]])
            P.dma("sp", f"xpo{xs_}", lambda e, xs_=xs_, r0=r0, n=n, cb=cb: e.dma_start(out=xmid[r0:r0+n, cb*512:(cb+1)*512], in_=xp[xs_][0:n]), reads=[b_xp[xs_]], writes=[b_xm_dram[ti]])

    xt = [P.sb(f"xt{i}", [128, D], F32) for i in range(1)]; b_xt = [P.buf() for _ in range(1)]
    xn = P.sb("xn", [128, D], F32); b_xn = P.buf()
    junk = mix
    ss = P.sb("ss", [128, 2], F32); b_ss = P.buf()
    f32t = P.sb("f32t", [128, 16, 128], F32); b_f32 = P.buf()
    lg = P.sb("lg", [128, 32], F32); b_lg = P.buf()
    rc = P.sb("rc", [128, 16], F32); b_rc = P.buf()
    ex = P.sb("ex", [128, 32], F32); b_ex = P.buf()
    gt = [P.sb(f"gt{i}", [128, 32], F32) for i in range(2)]; b_gt = [P.buf() for _ in range(2)]
    fT_v = fT.rearrange("(k p) t -> p k t", p=128)
    for ti, (r0, n) in enumerate(TILES_PC):
        s = ti % 2
        isctx = 1 if r0 >= LAT_PC else 0
        X = xt[0]; bX = b_xt[0]
        P.dma("sp", "xt0", lambda e, X=X, r0=r0, n=n: e.dma_start(out=X[0:n, :], in_=xmid[r0:r0+n, :]), reads=[b_xm_dram[ti]], writes=[bX])
        P.op("act", lambda e, X=X, n=n: e.activation(out=junk[0:n, :], in_=X[0:n, :], func=AF.Square, accum_out=ss[0:n, 0:1]), reads=[bX], writes=b_mix + [b_ss])
        P.op("act", lambda e, n=n: e.activation(out=ss[0:n, 1:2], in_=ss[0:n, 0:1], func=AF.Sqrt, scale=1.0 / D, bias=EPS), reads=[b_ss], writes=[b_ss])
        P.op("dve", lambda e, n=n: e.reciprocal(out=ss[0:n, 1:2], in_=ss[0:n, 1:2]), reads=[b_ss], writes=[b_ss])
        P.op("dve", lambda e, X=X, n=n: e.tensor_scalar(out=xn[0:n, :], in0=X[0:n, :], scalar1=ss[0:n, 1:2], scalar2=None, op0=ALU.mult), reads=[bX, b_ss], writes=[b_xn])
        for kg in range(4):
            pt_, bpt_ = nf()
            for kk in range(4):
                k = kg * 4 + kk
                P.op("pe", lambda e, pt_=pt_, kk=kk, k=k, n=n: e.transpose(pt_[:, kk*128:kk*128+n], xn[0:n, k*128:(k+1)*128], ident[0:n, 0:n]), reads=[b_xn, b_id], writes=[bpt_])
            for kk in range(4):
                k = kg * 4 + kk
                if kk % 2 == 0:
                    P.op("dve", lambda e, pt_=pt_, kk=kk, k=k, n=n, isctx=isctx: e.tensor_scalar(
                        out=f32t[:, k, 0:n], in0=pt_[:, kk*128:kk*128+n], scalar1=mc[:, k, 2*isctx+1:2*isctx+2], scalar2=mc[:, k, 2*isctx:2*isctx+1], op0=ALU.mult, op1=ALU.add),
                        reads=[bpt_, b_mc], writes=[b_f32])
                else:
                    P.op("act", lambda e, pt_=pt_, kk=kk, k=k, n=n, isctx=isctx: e.activation(
                        out=f32t[:, k, 0:n], in_=pt_[:, kk*128:kk*128+n], func=AF.Identity, scale=mc[:, k, 2*isctx+1:2*isctx+2], bias=mc[:, k, 2*isctx:2*isctx+1]),
                        reads=[bpt_, b_mc], writes=[b_f32])
        P.op("pool", lambda e, n=n, r0=r0: e.tensor_copy(out=mixT[:, :, r0:r0+n], in_=f32t[:, :, 0:n]), reads=[b_f32], writes=[b_mixT[ti]])
        pl, bpl = nf()
        for k in range(16):
            P.op("pe", lambda e, pl=pl, k=k, n=n: e.matmul(pl[0:n, 0:32], f32t[:, k, 0:n], wr[:, k, :], start=(k == 0), stop=(k == 15)), reads=[b_f32, b_wr], writes=[bpl])
        tt("dve", lg[0:n], pl[0:n, 0:32], brt[0:n], ALU.add, [bpl, b_br], [b_lg])
        P.op("dve", lambda e, n=n: e.max(out=rc[0:n, 0:8], in_=lg[0:n]), reads=[b_lg], writes=[b_rc])
        P.op("dve", lambda e, n=n: e.tensor_scalar(out=rc[0:n, 8:9], in0=rc[0:n, 0:1], scalar1=-1.0, scalar2=None, op0=ALU.mult), reads=[b_rc], writes=[b_rc])
        P.op("act", lambda e, n=n: e.activation(out=ex[0:n], in_=lg[0:n], func=AF.Exp, bias=rc[0:n, 8:9], scale=1.0), reads=[b_lg, b_rc], writes=[b_ex])
        P.op("dve", lambda e, n=n: e.tensor_scalar(out=lg[0:n], in0=lg[0:n], scalar1=rc[0:n, 3:4], scalar2=None, op0=ALU.is_ge), reads=[b_lg, b_rc], writes=[b_lg])
        tt("dve", ex[0:n], ex[0:n], lg[0:n], ALU.mult, [b_ex, b_lg], [b_ex])
        P.op("dve", lambda e, n=n: e.reduce_sum(out=rc[0:n, 9:10], in_=ex[0:n], axis=AX.X), reads=[b_ex], writes=[b_rc])
        P.op("dve", lambda e, n=n: e.reciprocal(out=rc[0:n, 10:11], in_=rc[0:n, 9:10]), reads=[b_rc], writes=[b_rc])
        P.op("dve", lambda e, n=n, s=s: e.tensor_scalar(out=gt[s][0:n], in0=ex[0:n], scalar1=rc[0:n, 10:11], scalar2=None, op0=ALU.mult), reads=[b_ex, b_rc], writes=[b_gt[s]])
        P.dma("sp", f"gto{s}", lambda e, s=s, n=n, r0=r0: e.dma_start(out=gates[r0:r0+n, :], in_=gt[s][0:n]), reads=[b_gt[s]])
    for k in range(16):
        P.dma("sp", "f16o", lambda e, k=k: e.dma_start(out=fT_v[:, k, :], in_=mixT[:, k, :]), reads=b_mixT)
    P.emit()
    return nc


def _shift(a, k):
    out = np.zeros_like(a)
    if k == -1:
        out[1:] = a[:-1]
    elif k == 1:
        out[:-1] = a[1:]
    return out


def run_out(x_shards, z_all, ret, s5y, att, mod_l, prm):
    C = mix_consts()
    nc = build_out()
    lat = lambda a: a[CTX:]
    ctx = lambda a: a[:CTX]
    sh = lambda a: shard_tokens(lat(a), ctx(a))
    gf = z_all[:, 1536:2048]; gb = z_all[:, 2048:2560]
    rg_s = sh(np.stack([ret[0], ret[1], gf, gb], axis=1))
    s5_s = sh(np.stack([s5y[0], s5y[1], z_all[:, 2560:3072]], axis=1))
    att_s = sh(att)
    zc = z_all[:, 4096:5632]
    bg, cg, hh = zc[:, 0:512], zc[:, 512:1024], zc[:, 1024:1536]

    def sh3(a):
        parts = []
        for k in (-1, 0, 1):
            parts.append(np.concatenate([_shift(ctx(a), k), _shift(lat(a), k)], axis=0) if k else a)
        return parts
    cs_, hs_ = sh3(cg), sh3(hh)
    cv_s = sh(np.stack([bg, cs_[0], hs_[0], cs_[1], hs_[1], cs_[2], hs_[2]], axis=1))
    rep = lambda v: np.broadcast_to(v, (128,) + v.shape)
    vecs = np.ascontiguousarray(np.stack([rep(prm["s5_d"]), rep(prm["s5_b_glu"]), rep(prm["conv_w"][0]), rep(prm["conv_w"][1]), rep(prm["conv_w"][2])], axis=1))
    g1 = np.ascontiguousarray(np.stack([rep(mod_l[0, 2*D:3*D]), rep(mod_l[1, 2*D:3*D])]))
    modc = np.ascontiguousarray(np.stack([cols128(mod_l[0, 3*D:4*D]), cols128(mod_l[0, 4*D:5*D]), cols128(mod_l[1, 3*D:4*D]), cols128(mod_l[1, 4*D:5*D])], axis=-1))
    b_r = np.ascontiguousarray(rep(prm["b_router"]))
    in_maps = []
    for c in range(NCORE):
        in_maps.append({"ident": C["ident"], "x": x_shards[c], "rg": rg_s[c], "s5": s5_s[c], "att": att_s[c], "cv": cv_s[c],
                        "vecs": vecs, "w_glu": prm["s5_w_glu"], "w_out": prm["w_out"], "g1": g1, "modc": modc,
                        "w_r": np.ascontiguousarray(prm["w_router"].reshape(16, 128, 32).transpose(1, 0, 2)), "b_r": b_r})
    res = _run(nc, in_maps)
    return [r["xmid"] for r in res], [r["fT"] for r in res], [r["gates"] for r in res]


E_PC = 4
NCORE_E = 32 // E_PC
DE = 1024
E_TILES = [(i * 512, 512) for i in range(16)] + [(8192, 256)]


def build_moe():
    nc = bass.Bass("TRN2", target_bir_lowering=False)
    di = lambda name, shape, dt=F32: nc.dram_tensor(name, list(shape), dt, kind="ExternalInput").ap()
    fT = di("fT", [D, T_ALL], BF16)
    gb = di("gb", [E_PC, 128, T_ALL])
    wg = di("wg", [E_PC, D, DE]); wu = di("wu", [E_PC, D, DE]); wd = di("wd", [E_PC, DE, D])
    bgu = di("bgu", [128, E_PC, 16]); bd = di("bd", [128, E_PC, 16])
    yT = nc.dram_tensor("yT", [D, T_ALL], F32, kind="ExternalOutput").ap()
    P = Prog(nc)
    NF = 8
    psf = [P.ps(f"psf{i}", [128, 512], F32) for i in range(NF)]; b_psf = [P.buf() for _ in range(NF)]
    cnt = {"f": 0}

    def nf():
        i = cnt["f"] % NF; cnt["f"] += 1
        return psf[i], b_psf[i]

    bgut = P.sb("bgut", [128, E_PC, 16], F32); b_bgu = P.buf()
    bdt = P.sb("bdt", [128, E_PC, 16], F32); b_bd = P.buf()
    P.dma("sp", "bgu", lambda e: e.dma_start(out=bgut[:], in_=bgu), writes=[b_bgu])
    P.dma("sp", "bd", lambda e: e.dma_start(out=bdt[:], in_=bd), writes=[b_bd])
    wgb = P.sb("wgb", [128, 16, DE], BF16); wub = P.sb("wub", [128, 16, DE], BF16); wdb = P.sb("wdb", [128, 8, D], BF16)
    b_wgb = P.buf(); b_wub = P.buf(); b_wdb = P.buf()
    wst = [P.sb(f"wst{i}", [128, 4, 512], F32) for i in range(3)]; b_wst = [P.buf() for _ in range(3)]
    ft = [P.sb(f"ft{i}", [128, 16, 512], BF16) for i in range(2)]; b_ft = [P.buf() for _ in range(2)]
    gtile = [P.sb(f"gtile{i}", [128, 512], F32) for i in range(2)]; b_gtile = [P.buf() for _ in range(2)]
    actT = P.sb("actT", [128, 8, 512], BF16); b_actT = P.buf()
    tg = [P.sb(f"tg{i}", [128, 512], F32) for i in range(2)]; b_tg = [P.buf() for _ in range(2)]
    tsg = [P.sb(f"tsg{i}", [128, 512], F32) for i in range(2)]; b_tsg = [P.buf() for _ in range(2)]
    tu = [P.sb(f"tu{i}", [128, 512], F32) for i in range(2)]; b_tu = [P.buf() for _ in range(2)]
    yp = [P.sb(f"yp{i}", [128, 512], F32) for i in range(3)]; b_yp = [P.buf() for _ in range(3)]
    yo = [P.sb(f"yo{i}", [128, 512], F32) for i in range(3)]; b_yo = [P.buf() for _ in range(3)]
    b_dram = [[P.buf() for _ in E_TILES] for _ in range(16)]
    fT_v = fT.rearrange("(k p) t -> p k t", p=128)
    yT_v = yT.rearrange("(m p) t -> p m t", p=128)
    wsi = 0; fi = 0; oi = 0; mi = 0
    ne = globals().get("MOE_NE", E_PC)
    def load_tile(i):
        ex_, tix_ = divmod(i, len(E_TILES))
        c0_, w_ = E_TILES[tix_]
        fs_ = i % 2
        for kq in range(4):
            P.dma("sp", f"ft{fs_}_{ex_ % 2}", lambda e, fs_=fs_, c0_=c0_, w_=w_, kq=kq: e.dma_start(out=ft[fs_][:, kq*4:(kq+1)*4, 0:w_], in_=fT_v[:, kq*4:(kq+1)*4, c0_:c0_+w_]), writes=[b_ft[fs_]])
        P.dma("sp", f"gtile{fs_}_{ex_ % 2}", lambda e, fs_=fs_, c0_=c0_, w_=w_, ex_=ex_: e.dma_start(out=gtile[fs_][:, 0:w_], in_=gb[ex_, :, c0_:c0_+w_]), writes=[b_gtile[fs_]])

    n_tiles_total = ne * len(E_TILES)
    load_tile(0)
    for ex in range(ne):
        for (src, dst, bdst, nk, ncol) in ((wg, wgb, b_wgb, 16, DE), (wu, wub, b_wub, 16, DE), (wd, wdb, b_wdb, 8, D)):
            sv = src[ex].rearrange("(k p) n -> p k n", p=128)
            for kq in range(nk // 4):
                for cbk in range(ncol // 512):
                    ws_ = wsi % 3; wsi += 1
                    P.dma("sp", f"wst{ws_}_{ex % 2}", lambda e, ws_=ws_, sv=sv, kq=kq, cbk=cbk: e.dma_start(out=wst[ws_][:], in_=sv[:, kq*4:(kq+1)*4, cbk*512:(cbk+1)*512]), writes=[b_wst[ws_]])
                    eng = "pool" if wsi % 2 else "act"
                    if eng == "pool":
                        P.op("pool", lambda e, ws_=ws_, dst=dst, kq=kq, cbk=cbk: e.tensor_copy(out=dst[:, kq*4:(kq+1)*4, cbk*512:(cbk+1)*512], in_=wst[ws_][:]), reads=[b_wst[ws_]], writes=[bdst])
                    else:
                        P.op("act", lambda e, ws_=ws_, dst=dst, kq=kq, cbk=cbk: e.copy(out=dst[:, kq*4:(kq+1)*4, cbk*512:(cbk+1)*512], in_=wst[ws_][:]), reads=[b_wst[ws_]], writes=[bdst])
        for tix, (c0, w) in enumerate(E_TILES):
            fs = fi % 2; fi += 1
            if fi < n_tiles_total:
                load_tile(fi)
            for m in range(8):
                psg, bpsg = nf(); psu, bpsu = nf()
                for k in range(16):
                    P.op("pe", lambda e, psg=psg, k=k, m=m, fs=fs, w=w: e.matmul(psg[:, 0:w], wgb[:, k, m*128:(m+1)*128], ft[fs][:, k, 0:w], start=(k == 0), stop=(k == 15)), reads=[b_wgb, b_ft[fs]], writes=[bpsg])
                for k in range(16):
                    P.op("pe", lambda e, psu=psu, k=k, m=m, fs=fs, w=w: e.matmul(psu[:, 0:w], wub[:, k, m*128:(m+1)*128], ft[fs][:, k, 0:w], start=(k == 0), stop=(k == 15)), reads=[b_wub, b_ft[fs]], writes=[bpsu])
                s = mi % 2; mi += 1
                P.op("dve", lambda e, psg=psg, s=s, m=m, w=w, ex=ex: e.tensor_scalar(out=tg[s][:, 0:w], in0=psg[:, 0:w], scalar1=bgut[:, ex, m:m+1], scalar2=7.0, op0=ALU.add, op1=ALU.min), reads=[bpsg, b_bgu], writes=[b_tg[s]])
                P.op("act", lambda e, s=s, w=w: e.activation(out=tsg[s][:, 0:w], in_=tg[s][:, 0:w], func=AF.Sigmoid, scale=1.702), reads=[b_tg[s]], writes=[b_tsg[s]])
                P.op("dve", lambda e, psu=psu, s=s, m=m, w=w, ex=ex: e.tensor_scalar(out=tu[s][:, 0:w], in0=psu[:, 0:w], scalar1=bgut[:, ex, 8+m:9+m], scalar2=7.0, op0=ALU.add, op1=ALU.min), reads=[bpsu, b_bgu], writes=[b_tu[s]])
                P.op("dve", lambda e, s=s, w=w: e.tensor_scalar(out=tu[s][:, 0:w], in0=tu[s][:, 0:w], scalar1=-7.0, scalar2=1.0, op0=ALU.max, op1=ALU.add), reads=[b_tu[s]], writes=[b_tu[s]])
                P.op("pool", lambda e, s=s, w=w: e.tensor_tensor(out=tg[s][:, 0:w], in0=tg[s][:, 0:w], in1=tsg[s][:, 0:w], op=ALU.mult), reads=[b_tg[s], b_tsg[s]], writes=[b_tg[s]])
                P.op("dve", lambda e, s=s, m=m, w=w: e.tensor_tensor(out=actT[:, m, 0:w], in0=tg[s][:, 0:w], in1=tu[s][:, 0:w], op=ALU.mult), reads=[b_tg[s], b_tu[s]], writes=[b_actT])
            for m2 in range(16):
                psy, bpsy = nf()
                for k in range(8):
                    P.op("pe", lambda e, psy=psy, k=k, m2=m2, w=w: e.matmul(psy[:, 0:w], wdb[:, k, m2*128:(m2+1)*128], actT[:, k, 0:w], start=(k == 0), stop=(k == 7)), reads=[b_wdb, b_actT], writes=[bpsy])
                os_ = oi % 3; oi += 1
                bdr = b_dram[m2][tix]
                if ex > 0:
                    P.dma("sp", f"yp{os_}_{ex % 2}", lambda e, os_=os_, m2=m2, c0=c0, w=w: e.dma_start(out=yp[os_][:, 0:w], in_=yT_v[:, m2, c0:c0+w]), reads=[bdr], writes=[b_yp[os_]])
                P.op("dve", lambda e, psy=psy, os_=os_, m2=m2, w=w, fs=fs, ex=ex: e.scalar_tensor_tensor(out=yo[os_][:, 0:w], in0=psy[:, 0:w], scalar=bdt[:, ex, m2:m2+1], in1=gtile[fs][:, 0:w], op0=ALU.add, op1=ALU.mult),
                     reads=[bpsy, b_bd, b_gtile[fs]], writes=[b_yo[os_]])
                if ex > 0:
                    P.op("pool", lambda e, os_=os_, w=w: e.tensor_tensor(out=yo[os_][:, 0:w], in0=yo[os_][:, 0:w], in1=yp[os_][:, 0:w], op=ALU.add), reads=[b_yo[os_], b_yp[os_]], writes=[b_yo[os_]])
                P.dma("sp", f"yo{os_}_{ex % 2}", lambda e, os_=os_, m2=m2, c0=c0, w=w: e.dma_start(out=yT_v[:, m2, c0:c0+w], in_=yo[os_][:, 0:w]), reads=[b_yo[os_]], writes=[bdr])
    P.emit()
    return nc


def run_moe(fT_shards, gate_shards, prm):
    nc = build_moe()
    fT = np.ascontiguousarray(np.concatenate(fT_shards, axis=1))
    gates = np.concatenate(gate_shards, axis=0)
    in_maps = []
    for c in range(NCORE_E):
        es = slice(E_PC * c, E_PC * (c + 1))
        wgu = prm["w_gate_up"][es]
        bgu = prm["b_gate_up"][es]
        bg = bgu[:, 0::2].reshape(E_PC, 8, 128); bu = bgu[:, 1::2].reshape(E_PC, 8, 128)
        bgu_l = np.ascontiguousarray(np.concatenate([bg, bu], axis=1).transpose(2, 0, 1))
        bd_l = np.ascontiguousarray(prm["b_down"][es].reshape(E_PC, 16, 128).transpose(2, 0, 1))
        gbc = np.ascontiguousarray(np.broadcast_to(gates[:, es].T[:, None, :], (E_PC, 128, T_ALL)))
        in_maps.append({"fT": fT, "gb": gbc, "wg": np.ascontiguousarray(wgu[:, :, 0::2]), "wu": np.ascontiguousarray(wgu[:, :, 1::2]),
                        "wd": prm["w_down"][es], "bgu": bgu_l, "bd": bd_l})
    res = _run(nc, in_maps)
    parts = []
    for cp in range(NCORE):
        parts.append(np.ascontiguousarray(np.stack([res[c]["yT"][:, cp*TOK_PC:(cp+1)*TOK_PC].T for c in range(NCORE_E)], axis=0)))
    return parts


_LAYER_KEYS = ["w_in", "w_out", "s5_a_re", "s5_a_im", "s5_log_step", "s5_b_re", "s5_b_im", "s5_c_re", "s5_c_im",
               "s5_d", "s5_w_glu", "s5_b_glu", "q_norm_w", "k_norm_w", "conv_w", "w_router", "b_router",
               "w_gate_up", "b_gate_up", "w_down", "b_down"]


def _combine_maps(x_shards, parts, g2pair):
    rep = lambda v: np.broadcast_to(v, (128, D))
    g2 = np.ascontiguousarray(np.stack([rep(g2pair[0]), rep(g2pair[1])]))
    return g2


def run_proj2(x_shards, mod_l, w_in_l, parts=None, g2pair=None, project=True):
    combine = parts is not None
    nc = build_proj(combine, project)
    ident = np.eye(128, dtype=np.float32)
    in_maps = []
    for i in range(NCORE):
        m = {"x": x_shards[i], "ident": ident}
        if project:
            m["modc"] = np.ascontiguousarray(np.stack([cols128(mod_l[0, 0:D]), cols128(mod_l[0, D:2*D]), cols128(mod_l[1, 0:D]), cols128(mod_l[1, D:2*D])], axis=-1))
            m["w_in"] = w_in_l
        if combine:
            m["part"] = parts[i]
            m["g2"] = _combine_maps(x_shards, parts, g2pair)
        in_maps.append(m)
    res = _run(nc, in_maps)
    z = [r["z"] for r in res] if project else None
    xo = [r["xo"] for r in res] if combine else None
    return z, xo


def kernel(**inputs):
    inp = {k: np.asarray(v) for k, v in inputs.items()}
    mod = run_mod(inp["c"], inp["c_ctx"], inp["w_mod"], inp["b_mod"])
    x_shards = shard_tokens(inp["x"][0], inp["ctx"][0])
    parts = None
    g2pair = None
    for l in range(2):
        prm = {k: inp[k][l] for k in _LAYER_KEYS}
        z, xo = run_proj2(x_shards, mod[l], prm["w_in"], parts, g2pair)
        if xo is not None:
            x_shards = xo
        zl, zc = unshard_tokens(z)
        z_all = np.concatenate([zc, zl], axis=0)
        ret, s5y, att = run_mix(z_all, prm)
        xmid, fT, gates = run_out(x_shards, z_all, ret, s5y, att, mod[l], prm)
        parts = run_moe(fT, gates, prm)
        x_shards = xmid
        g2pair = (mod[l][0, 5*D:6*D], mod[l][1, 5*D:6*D])
    _, xo = run_proj2(x_shards, None, None, parts, g2pair, project=False)
    lat, _ = unshard_tokens(xo)
    return lat[None].astype(np.float32)
```

```python
import contextlib
import numpy as np
import ml_dtypes
import concourse.bass as bass
import concourse.mybir as mybir
from concourse.bass_utils import run_bass_kernel_spmd

F32 = mybir.dt.float32
BF16 = mybir.dt.bfloat16
I32 = mybir.dt.int32
ALU = mybir.AluOpType
AF = mybir.ActivationFunctionType
AX = mybir.AxisListType


class Buf:
    __slots__ = ("name", "w", "r")

    def __init__(self, name):
        self.name = name
        self.w = None
        self.r = []


class Prog:
    ENG = ("pe", "dve", "act", "pool", "sp")

    def __init__(self, nc):
        self.nc = nc
        self.stream = {e: [] for e in self.ENG}
        self.seen = {e: {} for e in self.ENG}
        self.needed = {e: set() for e in self.ENG}
        self.stack = contextlib.ExitStack()
        self.esem = {e: self.stack.enter_context(nc.semaphore("s_" + e))
                     for e in ("pe", "dve", "act", "pool")}
        self.dsem = {}
        self.dtoks = []
        self.nbuf = 0

    def buf(self, name=None):
        self.nbuf += 1
        return Buf(name or f"b{self.nbuf}")

    def sb(self, name, shape, dt):
        return self.stack.enter_context(self.nc.sbuf_tensor(name, list(shape), dt))

    def ps(self, name, shape, dt):
        return self.stack.enter_context(self.nc.psum_tensor(name, list(shape), dt))

    def _waits(self, eng, reads, writes):
        toks = []
        for b in reads:
            if b.w is not None:
                toks.append(b.w + (True,))
        for b in writes:
            if b.w is not None:
                toks.append(b.w + (False,))
            toks.extend(t + (False,) for t in b.r)
        need = {}
        for kind, src, val, raw in toks:
            if kind == "eng" and src == eng and (not raw or eng == "pe"):
                continue
            key = (kind, src)
            if self.seen[eng].get(key, -1) >= val:
                continue
            if need.get(key, -1) < val:
                need[key] = val
        for key, val in need.items():
            self.seen[eng][key] = val
            if key[0] == "eng":
                self.needed[key[1]].add(val)
        return list(need.items())

    def op(self, eng, fn, reads=(), writes=()):
        waits = self._waits(eng, reads, writes)
        idx = len(self.stream[eng])
        tok = ("eng", eng, idx)
        self.stream[eng].append((waits, fn, None))
        for b in reads:
            b.r.append(tok)
        for b in writes:
            b.w = tok
            b.r = []
        return tok

    def dma(self, q, semkey, fn, reads=(), writes=()):
        waits = self._waits(q, reads, writes)
        if semkey not in self.dsem:
            self.dsem[semkey] = [self.stack.enter_context(self.nc.semaphore("d_" + semkey)), 0]
        self.dsem[semkey][1] += 16
        tok = ("dma", semkey, self.dsem[semkey][1])
        self.stream[q].append((waits, fn, semkey))
        for b in reads:
            b.r.append(tok)
        for b in writes:
            b.w = tok
            b.r = []
        self.dtoks.append(tok)
        return tok

    def finish(self):
        fin = Buf("fin")
        for k, (s, v) in self.dsem.items():
            fin.r.append(("dma", k, v))
        for e in ("pe", "dve", "act", "pool"):
            if self.stream[e]:
                fin.r.append(("eng", e, len(self.stream[e]) - 1))
        waits = self._waits("sp", (), (fin,))
        self.stream["sp"].append((waits, None, None))

    def emit(self):
        nc = self.nc
        self.finish()
        val = {}
        for e in self.ENG:
            c = 0
            for idx in range(len(self.stream[e])):
                if idx in self.needed[e]:
                    c += 1
                    val[(e, idx)] = c
        with nc.Block() as block:
            decos = {"pe": block.tensor, "dve": block.vector, "act": block.scalar,
                     "pool": block.gpsimd, "sp": block.sync}
            for ename in self.ENG:
                items = self.stream[ename]

                def body(e, items=items, ename=ename):
                    for idx, (waits, fn, semkey) in enumerate(items):
                        for (kind, src), v in waits:
                            if kind == "eng":
                                e.wait_ge(self.esem[src], val[(src, v)])
                            else:
                                e.wait_ge(self.dsem[src][0], v)
                        if fn is None:
                            continue
                        ins = fn(e)
                        if semkey is not None:
                            ins.then_inc(self.dsem[semkey][0], 16)
                        elif idx in self.needed[ename]:
                            ins.then_inc(self.esem[ename], 1)

                decos[ename](body)
        self.stack.close()


D = 2048
SEQ = 8192
CTX = 256
NCORE = 8
LAT_PC = SEQ // NCORE
CTX_PC = CTX // NCORE
TOK_PC = LAT_PC + CTX_PC
T_ALL = SEQ + CTX
IN_COLS = 5632
EPS = 1e-6
TILES_PC = [(i * 128, 128) for i in range(8)] + [(1024, 32)]
NPART = 8


def _run(nc, in_maps):
    res = run_bass_kernel_spmd(nc, in_maps, core_ids=list(range(len(in_maps))))
    return res.results


def build_mod():
    nc = bass.Bass("TRN2", target_bir_lowering=False)
    cT = nc.dram_tensor("cT", [128, 16, 2], F32, kind="ExternalInput").ap()
    wm = nc.dram_tensor("wm", [2, 2048, 1536], F32, kind="ExternalInput").ap()
    bm = nc.dram_tensor("bm", [2, 2, 1536], F32, kind="ExternalInput").ap()
    out = nc.dram_tensor("mod", [2, 2, 1536], F32, kind="ExternalOutput").ap()
    P = Prog(nc)
    ct = P.sb("ct", [128, 16, 2], F32); b_ct = P.buf()
    av = P.sb("av", [128, 16, 2], F32); b_av = P.buf()
    bt = P.sb("bt", [2, 2, 1536], F32); b_bt = P.buf()
    ot = P.sb("ot", [2, 2, 1536], F32); b_ot = P.buf()
    NW = 4
    wt = [P.sb(f"wt{i}", [128, 1536], F32) for i in range(NW)]; b_wt = [P.buf() for _ in range(NW)]
    pst = [P.ps(f"ps{i}", [128, 512], F32) for i in range(3)]; b_ps = [P.buf() for _ in range(3)]
    P.dma("sp", "ct", lambda e: e.dma_start(out=ct[:], in_=cT), writes=[b_ct])
    P.dma("sp", "bt", lambda e: e.dma_start(out=bt[:], in_=bm.rearrange("l r n -> r l n")), writes=[b_bt])
    P.op("act", lambda e: e.activation(out=av[:], in_=ct[:], func=AF.Silu), reads=[b_ct], writes=[b_av])
    i = 0
    for l in range(2):
        for k in range(16):
            s = i % NW; i += 1
            P.dma("sp", f"wt{s}", lambda e, s=s, l=l, k=k: e.dma_start(out=wt[s][:], in_=wm[l, k*128:(k+1)*128, :]), writes=[b_wt[s]])
            for n in range(3):
                P.op("pe", lambda e, s=s, n=n, k=k: e.matmul(pst[n][0:2, :], av[:, k, :], wt[s][:, n*512:(n+1)*512], start=(k == 0), stop=(k == 15)),
                     reads=[b_av, b_wt[s]], writes=[b_ps[n]])
        for n in range(3):
            P.op("dve", lambda e, n=n, l=l: e.tensor_tensor(out=ot[:, l, n*512:(n+1)*512], in0=pst[n][0:2, :], in1=bt[:, l, n*512:(n+1)*512], op=ALU.add),
                 reads=[b_ps[n], b_bt], writes=[b_ot])
    P.dma("sp", "ot", lambda e: e.dma_start(out=out.rearrange("l r n -> r l n"), in_=ot[:]), reads=[b_ot])
    P.emit()
    return nc


def run_mod(c, c_ctx, w_mod, b_mod):
    cc = np.stack([c[0], c_ctx], axis=-1)
    cT = np.ascontiguousarray(cc.reshape(16, 128, 2).transpose(1, 0, 2))
    nc = build_mod()
    in_maps = []
    for i in range(NCORE):
        sl = slice(i * 1536, (i + 1) * 1536)
        in_maps.append({"cT": cT, "wm": np.ascontiguousarray(w_mod[:, :, sl]),
                        "bm": np.ascontiguousarray(np.broadcast_to(b_mod[:, None, sl], (2, 2, 1536)))})
    res = _run(nc, in_maps)
    return np.concatenate([r["mod"] for r in res], axis=-1)


def cols128(v):
    return np.ascontiguousarray(v.reshape(16, 128).T)


def build_proj(combine, project=True):
    nc = bass.Bass("TRN2", target_bir_lowering=False)
    x = nc.dram_tensor("x", [TOK_PC, D], F32, kind="ExternalInput").ap()
    ident_d = nc.dram_tensor("ident", [128, 128], F32, kind="ExternalInput").ap()
    P = Prog(nc)
    if project:
        modc = nc.dram_tensor("modc", [128, 16, 4], F32, kind="ExternalInput").ap()
        w_in = nc.dram_tensor("w_in", [D, IN_COLS], F32, kind="ExternalInput").ap()
        z = nc.dram_tensor("z", [TOK_PC, IN_COLS], F32, kind="ExternalOutput").ap()
    if combine:
        part = nc.dram_tensor("part", [NPART, TOK_PC, D], F32, kind="ExternalInput").ap()
        g2 = nc.dram_tensor("g2", [2, 128, D], F32, kind="ExternalInput").ap()
        xo = nc.dram_tensor("xo", [TOK_PC, D], F32, kind="ExternalOutput").ap()
        g2t = P.sb("g2t", [128, 2, D], F32); b_g2 = P.buf()
        P.dma("sp", "g2", lambda e: e.dma_start(out=g2t[:], in_=g2.rearrange("r p d -> p r d")), writes=[b_g2])
        pt = [P.sb(f"pt{i}", [128, D], F32) for i in range(3)]; b_pt = [P.buf() for _ in range(3)]
        acc = P.sb("acc", [128, D], F32); b_acc = P.buf()
    ident = P.sb("identt", [128, 128], F32); b_id = P.buf()
    P.dma("sp", "ident", lambda e: e.dma_start(out=ident[:], in_=ident_d), writes=[b_id])
    xt = [P.sb(f"xt{i}", [128, D], F32) for i in range(2)]; b_xt = [P.buf() for _ in range(2)]
    if project:
        mc = P.sb("mc", [128, 16, 4], F32); b_mc = P.buf()
        P.dma("sp", "mc", lambda e: e.dma_start(out=mc[:], in_=modc), writes=[b_mc])
        P.op("dve", lambda e: e.tensor_scalar(out=mc[:, :, 1], in0=mc[:, :, 1], scalar1=1.0, scalar2=None, op0=ALU.add), reads=[b_mc], writes=[b_mc])
        P.op("dve", lambda e: e.tensor_scalar(out=mc[:, :, 3], in0=mc[:, :, 3], scalar1=1.0, scalar2=None, op0=ALU.add), reads=[b_mc], writes=[b_mc])
        xn = P.sb("xn", [128, D], F32); b_xn = P.buf()
        junk = P.sb("junk", [128, D], BF16); b_junk = P.buf()
        ss = P.sb("ss", [128, 2], F32); b_ss = P.buf()
        xmT = P.sb("xmT", [128, 16, TOK_PC], BF16); b_xmT = [P.buf() for _ in TILES_PC]
        wb = [P.sb(f"wb{i}", [128, 16, 512], BF16) for i in range(2)]; b_wb = [P.buf() for _ in range(2)]
        zt = [P.sb(f"zt{i}", [128, 512], F32) for i in range(4)]; b_zt = [P.buf() for _ in range(4)]
        pst = [P.ps(f"ps{i}", [128, 512], F32) for i in range(8)]; b_ps = [P.buf() for _ in range(8)]
    psi = 0
    for ti, (r0, n) in enumerate(TILES_PC):
        s = ti % 2
        X = xt[s]; bX = b_xt[s]
        P.dma("sp", f"xt{s}", lambda e, X=X, r0=r0, n=n: e.dma_start(out=X[0:n, :], in_=x[r0:r0+n, :]), writes=[bX])
        if combine:
            isctx = 1 if r0 >= LAT_PC else 0
            for c in range(NPART):
                ps_ = c % 3
                P.dma("sp", f"pt{ps_}", lambda e, ps_=ps_, c=c, r0=r0, n=n: e.dma_start(out=pt[ps_][0:n, :], in_=part[c, r0:r0+n, :]), writes=[b_pt[ps_]])
                if c == 0:
                    P.op("pool", lambda e, ps_=ps_, n=n: e.tensor_copy(out=acc[0:n, :], in_=pt[ps_][0:n, :]), reads=[b_pt[ps_]], writes=[b_acc])
                else:
                    eng = "dve" if c % 2 else "pool"
                    P.op(eng, lambda e, ps_=ps_, n=n: e.tensor_tensor(out=acc[0:n, :], in0=acc[0:n, :], in1=pt[ps_][0:n, :], op=ALU.add), reads=[b_pt[ps_], b_acc], writes=[b_acc])
            P.op("dve", lambda e, n=n, isctx=isctx: e.tensor_tensor(out=acc[0:n, :], in0=acc[0:n, :], in1=g2t[0:n, isctx, :], op=ALU.mult), reads=[b_acc, b_g2], writes=[b_acc])
            P.op("dve", lambda e, X=X, n=n: e.tensor_tensor(out=X[0:n, :], in0=X[0:n, :], in1=acc[0:n, :], op=ALU.add), reads=[b_acc, bX], writes=[bX])
            P.dma("sp", f"xo{s}", lambda e, X=X, r0=r0, n=n: e.dma_start(out=xo[r0:r0+n, :], in_=X[0:n, :]), reads=[bX])
        if not project:
            continue
        isctx = 1 if r0 >= LAT_PC else 0
        P.op("act", lambda e, X=X, n=n: e.activation(out=junk[0:n, :], in_=X[0:n, :], func=AF.Square, accum_out=ss[0:n, 0:1]), reads=[bX], writes=[b_junk, b_ss])
        P.op("act", lambda e, n=n: e.activation(out=ss[0:n, 1:2], in_=ss[0:n, 0:1], func=AF.Sqrt, scale=1.0 / D, bias=EPS), reads=[b_ss], writes=[b_ss])
        P.op("dve", lambda e, n=n: e.reciprocal(out=ss[0:n, 1:2], in_=ss[0:n, 1:2]), reads=[b_ss], writes=[b_ss])
        P.op("dve", lambda e, X=X, n=n: e.tensor_scalar(out=xn[0:n, :], in0=X[0:n, :], scalar1=ss[0:n, 1:2], scalar2=None, op0=ALU.mult), reads=[bX, b_ss], writes=[b_xn])
        for kg in range(4):
            pb = psi % 8; psi += 1
            for kk in range(4):
                k = kg * 4 + kk
                P.op("pe", lambda e, pb=pb, kk=kk, k=k, n=n: e.transpose(pst[pb][:, kk*128:kk*128+n], xn[0:n, k*128:(k+1)*128], ident[0:n, 0:n]),
                     reads=[b_xn, b_id], writes=[b_ps[pb]])
            for kk in range(4):
                k = kg * 4 + kk
                if kk % 2 == 0:
                    P.op("dve", lambda e, pb=pb, kk=kk, k=k, n=n, r0=r0, isctx=isctx: e.tensor_scalar(
                        out=xmT[:, k, r0:r0+n], in0=pst[pb][:, kk*128:kk*128+n], scalar1=mc[:, k, 2*isctx+1:2*isctx+2], scalar2=mc[:, k, 2*isctx:2*isctx+1], op0=ALU.mult, op1=ALU.add),
                        reads=[b_ps[pb], b_mc], writes=[b_xmT[ti]])
                else:
                    P.op("act", lambda e, pb=pb, kk=kk, k=k, n=n, r0=r0, isctx=isctx: e.activation(
                        out=xmT[:, k, r0:r0+n], in_=pst[pb][:, kk*128:kk*128+n], func=AF.Identity, scale=mc[:, k, 2*isctx+1:2*isctx+2], bias=mc[:, k, 2*isctx:2*isctx+1]),
                        reads=[b_ps[pb], b_mc], writes=[b_xmT[ti]])
    if project:
        w_v = w_in.rearrange("(k p) n -> p k n", p=128)
        zi = 0
        wsi = 0
        wst = [P.sb(f"wst{i}", [128, 4, 512], F32) for i in range(3)]; b_wst = [P.buf() for _ in range(3)]
        NCB = IN_COLS // 512

        def load_w(cb_):
            nonlocal wsi
            s_ = cb_ % 2
            for kq in range(4):
                ws_ = wsi % 3; wsi += 1
                P.dma("sp", f"wst{ws_}", lambda e, ws_=ws_, cb_=cb_, kq=kq: e.dma_start(out=wst[ws_][:], in_=w_v[:, kq*4:(kq+1)*4, cb_*512:(cb_+1)*512]), writes=[b_wst[ws_]])
                P.op("pool", lambda e, ws_=ws_, s_=s_, kq=kq: e.tensor_copy(out=wb[s_][:, kq*4:(kq+1)*4, :], in_=wst[ws_][:]), reads=[b_wst[ws_]], writes=[b_wb[s_]])

        load_w(0)
        for cb in range(NCB):
            s = cb % 2
            if cb + 1 < NCB:
                load_w(cb + 1)
            for ti, (r0, n) in enumerate(TILES_PC):
                pb = psi % 8; psi += 1
                for k in range(16):
                    P.op("pe", lambda e, pb=pb, k=k, n=n, r0=r0, s=s: e.matmul(pst[pb][0:n, :], xmT[:, k, r0:r0+n], wb[s][:, k, :], start=(k == 0), stop=(k == 15)),
                         reads=[b_xmT[ti], b_wb[s]], writes=[b_ps[pb]])
                zs = zi % 4; zi += 1
                if zi % 2:
                    P.op("dve", lambda e, pb=pb, zs=zs, n=n: e.tensor_copy(out=zt[zs][0:n, :], in_=pst[pb][0:n, :]), reads=[b_ps[pb]], writes=[b_zt[zs]])
                else:
                    P.op("act", lambda e, pb=pb, zs=zs, n=n: e.copy(out=zt[zs][0:n, :], in_=pst[pb][0:n, :]), reads=[b_ps[pb]], writes=[b_zt[zs]])
                P.dma("sp", f"zt{zs}", lambda e, zs=zs, n=n, r0=r0, cb=cb: e.dma_start(out=z[r0:r0+n, cb*512:(cb+1)*512], in_=zt[zs][0:n, :]), reads=[b_zt[zs]])
    P.emit()
    return nc


def shard_tokens(lat, ctx):
    return [np.ascontiguousarray(np.concatenate([lat[i*LAT_PC:(i+1)*LAT_PC], ctx[i*CTX_PC:(i+1)*CTX_PC]], axis=0)) for i in range(NCORE)]


def unshard_tokens(per_core):
    lat = np.concatenate([p[:LAT_PC] for p in per_core], axis=0)
    ctx = np.concatenate([p[LAT_PC:] for p in per_core], axis=0)
    return lat, ctx


def run_proj(x_shards, mod_l, w_in_l, parts=None, project=True):
    combine = parts is not None
    nc = build_proj(combine, project)
    ident = np.eye(128, dtype=np.float32)
    in_maps = []
    for i in range(NCORE):
        m = {"x": x_shards[i], "ident": ident}
        if project:
            sh1, sc1 = mod_l[0, 0:D], mod_l[0, D:2*D]
            csh1, csc1 = mod_l[1, 0:D], mod_l[1, D:2*D]
            m["modc"] = np.ascontiguousarray(np.stack([cols128(sh1), cols128(sc1), cols128(csh1), cols128(csc1)], axis=-1))
            m["w_in"] = w_in_l
        if combine:
            m["part"] = parts[i]
            g2 = np.stack([np.broadcast_to(mod_l_prev_g2[0], (128, D)), np.broadcast_to(mod_l_prev_g2[1], (128, D))])
            m["g2"] = np.ascontiguousarray(g2)
        in_maps.append(m)
    res = _run(nc, in_maps)
    z = [r["z"] for r in res] if project else None
    xo = [r["xo"] for r in res] if combine else None
    return z, xo


NCH = T_ALL // 128
SEG = 384
NSEG = T_ALL // SEG
NQ = 128 + SEQ // 2
NQB = NQ // 128
ATT_SCALE = 128 ** -0.5


def build_mix():
    nc = bass.Bass("TRN2", target_bir_lowering=False)
    di = lambda name, shape, dt=F32: nc.dram_tensor(name, list(shape), dt, kind="ExternalInput").ap()
    do = lambda name, shape, dt=F32: nc.dram_tensor(name, list(shape), dt, kind="ExternalOutput").ap()
    ident_d = di("ident", [128, 128])
    maskT_d = di("maskT", [128, 128])
    r_qkv = di("r_qkv", [T_ALL, 3, 128])
    r_tab = di("r_tab", [T_ALL, 4, 128])
    r_g = di("r_g", [128, 1])
    r_out = do("r_out", [T_ALL, 128])
    s_par = di("s_par", [128, 4, 3])
    s_B = di("s_B", [128, 2, 2, 32])
    s_C = di("s_C", [128, 2, 2, 64])
    s_uT = di("s_uT", [2, 2, 32, T_ALL])
    s_iota = di("s_iota", [128, SEG])
    s_out = do("s_out", [2, T_ALL, 64])
    a_q = di("a_q", [NQ, 128]); a_k = di("a_k", [T_ALL, 128]); a_v = di("a_v", [T_ALL, 128])
    a_qtab = di("a_qtab", [NQ, 2, 128]); a_ktab = di("a_ktab", [T_ALL, 2, 128])
    a_w = di("a_w", [128, 2, 128])
    a_out = do("a_out", [NQ, 128])

    P = Prog(nc)
    NF = 6
    psf = [P.ps(f"psf{i}", [128, 512], F32) for i in range(NF)]; b_psf = [P.buf() for _ in range(NF)]
    psb = [P.ps(f"psb{i}", [128, 512], BF16) for i in range(2)]; b_psb = [P.buf() for _ in range(2)]
    cnt = {"f": 0, "b": 0}

    def nf():
        i = cnt["f"] % NF; cnt["f"] += 1
        return psf[i], b_psf[i]

    def nb():
        i = cnt["b"] % 2; cnt["b"] += 1
        return psb[i], b_psb[i]

    ident = P.sb("identt", [128, 128], F32); b_id = P.buf()
    identb = P.sb("identb", [128, 128], BF16); b_idb = P.buf()
    maskT = P.sb("maskTt", [128, 128], F32); b_mask = P.buf()
    P.dma("sp", "ident", lambda e: e.dma_start(out=ident[:], in_=ident_d), writes=[b_id])
    P.dma("sp", "maskT", lambda e: e.dma_start(out=maskT[:], in_=maskT_d), writes=[b_mask])
    P.op("dve", lambda e: e.tensor_copy(out=identb[:], in_=ident[:]), reads=[b_id], writes=[b_idb])

    def s5_unit():
        par = P.sb("s_par_t", [128, 4, 3], F32); b_par = P.buf()
        Bt = P.sb("s_B_t", [128, 2, 2, 32], F32); b_B = P.buf()
        Ct = P.sb("s_C_t", [128, 2, 2, 64], F32); b_C = P.buf()
        io = P.sb("s_iota_t", [128, SEG], F32); b_io = P.buf()
        P.dma("sp", "s_par", lambda e: e.dma_start(out=par[:], in_=s_par), writes=[b_par])
        P.dma("sp", "s_B", lambda e: e.dma_start(out=Bt[:], in_=s_B), writes=[b_B])
        P.dma("sp", "s_C", lambda e: e.dma_start(out=Ct[:], in_=s_C), writes=[b_C])
        P.dma("sp", "s_iota", lambda e: e.dma_start(out=io[:], in_=s_iota), writes=[b_io])
        P.op("dve", lambda e: e.tensor_scalar(out=Ct[:, :, 1, :], in0=Ct[:, :, 1, :], scalar1=-1.0, scalar2=None, op0=ALU.mult), reads=[b_C], writes=[b_C])
        cs = P.sb("s_cs", [128, 4, SEG], F32); sn = P.sb("s_sn", [128, 4, SEG], F32); rb = P.sb("s_rb", [128, 4, SEG], F32)
        b_tab = [P.buf() for _ in range(4)]
        col = P.sb("s_col", [128, 4, 24], F32); b_col = [P.buf() for _ in range(4)]
        ph = P.sb("s_ph", [128, SEG], F32); b_ph = P.buf()
        ph2 = P.sb("s_ph2", [128, SEG], F32); b_ph2 = P.buf()
        phi = P.sb("s_phi", [128, SEG], I32); b_phi = P.buf()
        BpT = P.sb("s_BpT", [32, 4, 2, 128], F32); b_BpT = [P.buf() for _ in range(4)]
        Bp = P.sb("s_Bp", [128, 2, 32], F32); b_Bp = P.buf()
        tmpB = P.sb("s_tmpB", [128, 32], F32); b_tmpB = P.buf()
        st = P.sb("s_st", [128, 4, 2], F32); b_st = [P.buf() for _ in range(4)]

        def frac_sin(dst, src, bsrc, bdst_list):
            P.op("dve", lambda e: e.tensor_copy(out=phi[:], in_=src), reads=[bsrc], writes=[b_phi])
            P.op("dve", lambda e: e.tensor_tensor(out=ph2[:], in0=src, in1=phi[:], op=ALU.subtract), reads=[bsrc, b_phi], writes=[b_ph2])
            P.op("dve", lambda e: e.tensor_scalar(out=phi[:], in0=ph2[:], scalar1=0.5, scalar2=None, op0=ALU.is_gt), reads=[b_ph2], writes=[b_phi])
            P.op("dve", lambda e: e.tensor_tensor(out=ph2[:], in0=ph2[:], in1=phi[:], op=ALU.subtract), reads=[b_ph2, b_phi], writes=[b_ph2])
            P.op("dve", lambda e: e.tensor_scalar(out=phi[:], in0=ph2[:], scalar1=-0.5, scalar2=None, op0=ALU.is_lt), reads=[b_ph2], writes=[b_phi])
            P.op("dve", lambda e: e.tensor_tensor(out=ph2[:], in0=ph2[:], in1=phi[:], op=ALU.add), reads=[b_ph2, b_phi], writes=[b_ph2])
            P.op("act", lambda e: e.activation(out=dst, in_=ph2[:], func=AF.Sin, scale=2.0 * 3.14159265), reads=[b_ph2], writes=bdst_list)

        for cb in range(4):
            tl = cb % 2
            c_ = lambda j, cb=cb: col[:, cb, j:j+1]
            bc = b_col[cb]
            a_re = par[:, cb, 0:1]; a_im = par[:, cb, 1:2]; lst = par[:, cb, 2:3]
            P.op("act", lambda e, c_=c_, lst=lst: e.activation(out=c_(0), in_=lst, func=AF.Exp), reads=[b_par], writes=[bc])
            P.op("dve", lambda e, c_=c_, a_re=a_re: e.tensor_tensor(out=c_(1), in0=a_re, in1=c_(0), op=ALU.mult), reads=[b_par, bc], writes=[bc])
            P.op("act", lambda e, c_=c_: e.activation(out=c_(2), in_=c_(1), func=AF.Exp), reads=[bc], writes=[bc])
            P.op("dve", lambda e, c_=c_, a_im=a_im: e.tensor_tensor(out=c_(3), in0=a_im, in1=c_(0), op=ALU.mult), reads=[b_par, bc], writes=[bc])
            P.op("dve", lambda e, c_=c_: e.tensor_scalar(out=c_(3), in0=c_(3), scalar1=1.0 / (2.0 * np.pi), scalar2=None, op0=ALU.mult), reads=[bc], writes=[bc])
            P.op("dve", lambda e, c_=c_: e.tensor_scalar(out=ph[:], in0=io[:], scalar1=c_(3), scalar2=None, op0=ALU.mult), reads=[b_io, bc], writes=[b_ph])
            frac_sin(sn[:, cb, :], ph[:], b_ph, [b_tab[cb]])
            P.op("dve", lambda e: e.tensor_scalar(out=ph[:], in0=ph[:], scalar1=0.25, scalar2=None, op0=ALU.add), reads=[b_ph], writes=[b_ph])
            frac_sin(cs[:, cb, :], ph[:], b_ph, [b_tab[cb]])
            P.op("pool", lambda e, cb=cb: e.memset(rb[:, cb, :], 1.0), writes=[b_tab[cb]])
            P.op("dve", lambda e, cb=cb, c_=c_: e.tensor_scalar(out=rb[:, cb, :], in0=rb[:, cb, :], scalar1=c_(2), scalar2=None, op0=ALU.mult), reads=[bc, b_tab[cb]], writes=[b_tab[cb]])
            tt = lambda o, a, b, op, c_=c_: P.op("dve", lambda e: e.tensor_tensor(out=o, in0=a, in1=b, op=op), reads=[bc, b_par, b_tab[cb]], writes=[bc])
            tt(c_(4), c_(2), cs[:, cb, 0:1], ALU.mult)
            tt(c_(5), c_(2), sn[:, cb, 0:1], ALU.mult)
            P.op("dve", lambda e, c_=c_: e.tensor_scalar(out=c_(6), in0=c_(4), scalar1=-1.0, scalar2=None, op0=ALU.add), reads=[bc], writes=[bc])
            tt(c_(7), c_(6), a_re, ALU.mult)
            tt(c_(8), c_(5), a_im, ALU.mult)
            tt(c_(9), c_(7), c_(8), ALU.add)
            tt(c_(10), c_(5), a_re, ALU.mult)
            tt(c_(11), c_(6), a_im, ALU.mult)
            tt(c_(12), c_(10), c_(11), ALU.subtract)
            tt(c_(13), a_re, a_re, ALU.mult)
            tt(c_(14), a_im, a_im, ALU.mult)
            tt(c_(15), c_(13), c_(14), ALU.add)
            P.op("dve", lambda e, c_=c_: e.reciprocal(out=c_(16), in_=c_(15)), reads=[bc], writes=[bc])
            tt(c_(17), c_(9), c_(16), ALU.mult)
            tt(c_(18), c_(12), c_(16), ALU.mult)
            P.op("dve", lambda e, c_=c_, tl=tl: e.tensor_scalar(out=tmpB[:], in0=Bt[:, tl, 1, :], scalar1=c_(18), scalar2=None, op0=ALU.mult), reads=[bc, b_B], writes=[b_tmpB])
            P.op("dve", lambda e, c_=c_, tl=tl: e.scalar_tensor_tensor(out=Bp[:, 0, :], in0=Bt[:, tl, 0, :], scalar=c_(17), in1=tmpB[:], op0=ALU.mult, op1=ALU.subtract), reads=[bc, b_B, b_tmpB], writes=[b_Bp])
            P.op("dve", lambda e, c_=c_, tl=tl: e.tensor_scalar(out=tmpB[:], in0=Bt[:, tl, 0, :], scalar1=c_(18), scalar2=None, op0=ALU.mult), reads=[bc, b_B], writes=[b_tmpB])
            P.op("dve", lambda e, c_=c_, tl=tl: e.scalar_tensor_tensor(out=Bp[:, 1, :], in0=Bt[:, tl, 1, :], scalar=c_(17), in1=tmpB[:], op0=ALU.mult, op1=ALU.add), reads=[bc, b_B, b_tmpB], writes=[b_Bp])
            for ri in range(2):
                pt_, bpt_ = nf()
                P.op("pe", lambda e, pt_=pt_, ri=ri: e.transpose(pt_[0:32, 0:128], Bp[:, ri, :], ident[:]), reads=[b_Bp, b_id], writes=[bpt_])
                P.op("act", lambda e, pt_=pt_, ri=ri, cb=cb: e.copy(out=BpT[:, cb, ri, :], in_=pt_[0:32, 0:128]), reads=[bpt_], writes=[b_BpT[cb]])

        NU = 3
        ut = [P.sb(f"s_ut{i}", [32, SEG], F32) for i in range(NU)]; b_ut = [P.buf() for _ in range(NU)]
        m = [P.sb(f"s_m{i}", [128, SEG], F32) for i in range(4)]; b_m = [P.buf() for _ in range(4)]
        dr = [P.sb(f"s_dr{i}", [128, SEG], F32) for i in range(2)]; b_dr = [P.buf() for _ in range(2)]
        xs_ = [P.sb(f"s_xs{i}", [128, SEG], F32) for i in range(2)]; b_xs = [P.buf() for _ in range(2)]
        xr = [[P.sb(f"s_xr{tl}{ri}", [128, SEG], F32) for ri in range(2)] for tl in range(2)]
        b_xr = [[P.buf() for _ in range(2)] for _ in range(2)]
        ysb = [P.sb(f"s_y{i}", [128, 3, 64], F32) for i in range(2)]; b_ysb = [P.buf() for _ in range(2)]
        ui = 0; yi = 0
        for d in range(2):
            for sg in range(NSEG):
                for tl in range(2):
                    cb = d * 2 + tl
                    us = ui % NU; ui += 1
                    P.dma("sp", f"s_ut{us}", lambda e, us=us, d=d, tl=tl, sg=sg: e.dma_start(out=ut[us][:], in_=s_uT[d, tl, :, sg*SEG:(sg+1)*SEG]), writes=[b_ut[us]])
                    pre, bpre = nf(); pim, bpim = nf()
                    P.op("pe", lambda e, pre=pre, us=us, cb=cb: e.matmul(pre[:, 0:SEG], BpT[:, cb, 0, :], ut[us][:], start=True, stop=True), reads=[b_BpT[cb], b_ut[us]], writes=[bpre])
                    P.op("pe", lambda e, pim=pim, us=us, cb=cb: e.matmul(pim[:, 0:SEG], BpT[:, cb, 1, :], ut[us][:], start=True, stop=True), reads=[b_BpT[cb], b_ut[us]], writes=[bpim])
                    C_ = cs[:, cb, :]; S_ = sn[:, cb, :]
                    P.op("dve", lambda e, pre=pre, C_=C_: e.tensor_tensor(out=m[0][:], in0=pre[:, 0:SEG], in1=C_, op=ALU.mult), reads=[bpre, b_tab[cb]], writes=[b_m[0]])
                    P.op("dve", lambda e, pim=pim, S_=S_: e.tensor_tensor(out=m[1][:], in0=pim[:, 0:SEG], in1=S_, op=ALU.mult), reads=[bpim, b_tab[cb]], writes=[b_m[1]])
                    P.op("dve", lambda e, pim=pim, C_=C_: e.tensor_tensor(out=m[2][:], in0=pim[:, 0:SEG], in1=C_, op=ALU.mult), reads=[bpim, b_tab[cb]], writes=[b_m[2]])
                    P.op("dve", lambda e, pre=pre, S_=S_: e.tensor_tensor(out=m[3][:], in0=pre[:, 0:SEG], in1=S_, op=ALU.mult), reads=[bpre, b_tab[cb]], writes=[b_m[3]])
                    P.op("pool", lambda e: e.tensor_tensor(out=dr[0][:], in0=m[0][:], in1=m[1][:], op=ALU.add), reads=[b_m[0], b_m[1]], writes=[b_dr[0]])
                    P.op("pool", lambda e: e.tensor_tensor(out=dr[1][:], in0=m[2][:], in1=m[3][:], op=ALU.subtract), reads=[b_m[2], b_m[3]], writes=[b_dr[1]])
                    for ri in range(2):
                        init = 0.0 if sg == 0 else st[:, cb, ri:ri+1]
                        P.op("dve", lambda e, ri=ri, init=init, cb=cb: e.tensor_tensor_scan(out=xs_[ri][:], data0=rb[:, cb, :], data1=dr[ri][:], initial=init, op0=ALU.mult, op1=ALU.add),
                             reads=[b_tab[cb], b_dr[ri], b_st[cb]], writes=[b_xs[ri]])
                    P.op("pool", lambda e, C_=C_: e.tensor_tensor(out=m[0][:], in0=xs_[0][:], in1=C_, op=ALU.mult), reads=[b_xs[0], b_tab[cb]], writes=[b_m[0]])
                    P.op("dve", lambda e, S_=S_: e.tensor_tensor(out=m[1][:], in0=xs_[1][:], in1=S_, op=ALU.mult), reads=[b_xs[1], b_tab[cb]], writes=[b_m[1]])
                    P.op("pool", lambda e, S_=S_: e.tensor_tensor(out=m[2][:], in0=xs_[0][:], in1=S_, op=ALU.mult), reads=[b_xs[0], b_tab[cb]], writes=[b_m[2]])
                    P.op("dve", lambda e, C_=C_: e.tensor_tensor(out=m[3][:], in0=xs_[1][:], in1=C_, op=ALU.mult), reads=[b_xs[1], b_tab[cb]], writes=[b_m[3]])
                    P.op("pool", lambda e, tl=tl: e.tensor_tensor(out=xr[tl][0][:], in0=m[0][:], in1=m[1][:], op=ALU.subtract), reads=[b_m[0], b_m[1]], writes=[b_xr[tl][0]])
                    P.op("pool", lambda e, tl=tl: e.tensor_tensor(out=xr[tl][1][:], in0=m[2][:], in1=m[3][:], op=ALU.add), reads=[b_m[2], b_m[3]], writes=[b_xr[tl][1]])
                    for ri in range(2):
                        P.op("act", lambda e, tl=tl, ri=ri, cb=cb: e.copy(out=st[:, cb, ri:ri+1], in_=xr[tl][ri][:, SEG-1:SEG]), reads=[b_xr[tl][ri]], writes=[b_st[cb]])
                ys = yi % 2; yi += 1
                for blk in range(3):
                    py, bpy = nf()
                    j = 0
                    for tl in range(2):
                        for ri in range(2):
                            P.op("pe", lambda e, py=py, tl=tl, ri=ri, blk=blk, j=j: e.matmul(py[:, 0:64], xr[tl][ri][:, blk*128:(blk+1)*128], Ct[:, tl, ri, :], start=(j == 0), stop=(j == 3)),
                                 reads=[b_xr[tl][ri], b_C], writes=[bpy])
                            j += 1
                    P.op("act", lambda e, py=py, ys=ys, blk=blk: e.copy(out=ysb[ys][:, blk, :], in_=py[:, 0:64]), reads=[bpy], writes=[b_ysb[ys]])
                P.dma("sp", f"s_y{ys}", lambda e, ys=ys, d=d, sg=sg: e.dma_start(out=s_out[d, sg*SEG:(sg+1)*SEG, :].rearrange("(b p) c -> p b c", p=128), in_=ysb[ys][:]), reads=[b_ysb[ys]])
                yield

    def ret_unit():
        g = P.sb("r_g_t", [128, 1], F32); b_g = P.buf()
        P.dma("sp", "r_g", lambda e: e.dma_start(out=g[:], in_=r_g), writes=[b_g])
        NB = 2
        qkv = [P.sb(f"r_qkv{i}", [128, 3, 128], F32) for i in range(NB)]; b_qkv = [P.buf() for _ in range(NB)]
        tab = [P.sb(f"r_tab{i}", [128, 4, 128], F32) for i in range(NB)]; b_tb = [P.buf() for _ in range(NB)]
        NS = 2
        sw_l = [P.sb(f"r_sw{i}", [128, 2, 128], F32) for i in range(NS)]; b_sw_l = [P.buf() for _ in range(NS)]
        t1_l = [P.sb(f"r_t1{i}", [128, 2, 128], F32) for i in range(NS)]; b_t1_l = [P.buf() for _ in range(NS)]
        t2_l = [P.sb(f"r_t2{i}", [128, 2, 128], F32) for i in range(NS)]; b_t2_l = [P.buf() for _ in range(NS)]
        qk_l = [P.sb(f"r_qk{i}", [128, 2, 128], BF16) for i in range(NS)]; b_qk_l = [P.buf() for _ in range(NS)]
        qkT_l = [P.sb(f"r_qkT{i}", [128, 2, 128], BF16) for i in range(NS)]; b_qkT_l = [P.buf() for _ in range(NS)]
        vb_l = [P.sb(f"r_vb{i}", [128, 128], BF16) for i in range(NS)]; b_vb_l = [P.buf() for _ in range(NS)]
        sm_l = [P.sb(f"r_sm{i}", [128, 128], BF16) for i in range(NS)]; b_sm_l = [P.buf() for _ in range(NS)]
        S32 = P.sb("r_S32", [128, 128], F32); b_S32 = P.buf()
        Sb = P.sb("r_Sb", [128, 128], BF16); b_Sb = P.buf()
        stt_l = [P.sb(f"r_stt{i}", [128, 6], F32) for i in range(NS)]; b_stt_l = [P.buf() for _ in range(NS)]
        mv_l = [P.sb(f"r_mv{i}", [128, 4], F32) for i in range(NS)]; b_mv_l = [P.buf() for _ in range(NS)]
        yo = [P.sb(f"r_yo{i}", [128, 128], F32) for i in range(2)]; b_yo = [P.buf() for _ in range(2)]
        P.op("pool", lambda e: e.memset(S32[:], 0.0), writes=[b_S32])
        P.op("pool", lambda e: e.memset(Sb[:], 0.0), writes=[b_Sb])
        for n in range(NCH):
            s = n % NB
            Q = qkv[s]; TB = tab[s]
            z_ = n % NS
            sw, t1, t2, qk, qkT, vb, sm, stt, mv = sw_l[z_], t1_l[z_], t2_l[z_], qk_l[z_], qkT_l[z_], vb_l[z_], sm_l[z_], stt_l[z_], mv_l[z_]
            b_sw, b_t1, b_t2, b_qk, b_qkT, b_vb, b_sm, b_stt, b_mv = b_sw_l[z_], b_t1_l[z_], b_t2_l[z_], b_qk_l[z_], b_qkT_l[z_], b_vb_l[z_], b_sm_l[z_], b_stt_l[z_], b_mv_l[z_]
            P.dma("sp", f"r_qkv{s}", lambda e, sw=sw, t1=t1, t2=t2, qk=qk, qkT=qkT, vb=vb, sm=sm, stt=stt, mv=mv, Q=Q, n=n: e.dma_start(out=Q[:], in_=r_qkv[n*128:(n+1)*128]), writes=[b_qkv[s]])
            P.dma("sp", f"r_tab{s}", lambda e, sw=sw, t1=t1, t2=t2, qk=qk, qkT=qkT, vb=vb, sm=sm, stt=stt, mv=mv, TB=TB, n=n: e.dma_start(out=TB[:], in_=r_tab[n*128:(n+1)*128]), writes=[b_tb[s]])
            P.op("pool", lambda e, sw=sw, t1=t1, t2=t2, qk=qk, qkT=qkT, vb=vb, sm=sm, stt=stt, mv=mv, Q=Q: e.tensor_copy(out=sw[:, :, 0:64], in_=Q[:, 0:2, 64:128]), reads=[b_qkv[s]], writes=[b_sw])
            P.op("pool", lambda e, sw=sw, t1=t1, t2=t2, qk=qk, qkT=qkT, vb=vb, sm=sm, stt=stt, mv=mv, Q=Q: e.tensor_copy(out=sw[:, :, 64:128], in_=Q[:, 0:2, 0:64]), reads=[b_qkv[s]], writes=[b_sw])
            TBv = TB[:].rearrange("p (a b) d -> p a b d", b=2)
            P.op("dve", lambda e, sw=sw, t1=t1, t2=t2, qk=qk, qkT=qkT, vb=vb, sm=sm, stt=stt, mv=mv, Q=Q, TBv=TBv: e.tensor_tensor(out=t1[:], in0=Q[:, 0:2, :], in1=TBv[:, :, 0, :], op=ALU.mult), reads=[b_qkv[s], b_tb[s]], writes=[b_t1])
            P.op("pool", lambda e, sw=sw, t1=t1, t2=t2, qk=qk, qkT=qkT, vb=vb, sm=sm, stt=stt, mv=mv, TBv=TBv: e.tensor_tensor(out=t2[:], in0=sw[:], in1=TBv[:, :, 1, :], op=ALU.mult), reads=[b_sw, b_tb[s]], writes=[b_t2])
            P.op("dve", lambda e, sw=sw, t1=t1, t2=t2, qk=qk, qkT=qkT, vb=vb, sm=sm, stt=stt, mv=mv: e.tensor_tensor(out=qk[:], in0=t1[:], in1=t2[:], op=ALU.add), reads=[b_t1, b_t2], writes=[b_qk])
            P.op("act", lambda e, sw=sw, t1=t1, t2=t2, qk=qk, qkT=qkT, vb=vb, sm=sm, stt=stt, mv=mv, Q=Q: e.copy(out=vb[:], in_=Q[:, 2, :]), reads=[b_qkv[s]], writes=[b_vb])
            pt_, bpt_ = nb()
            P.op("pe", lambda e, sw=sw, t1=t1, t2=t2, qk=qk, qkT=qkT, vb=vb, sm=sm, stt=stt, mv=mv, pt_=pt_: e.transpose(pt_[:, 0:128], qk[:, 0, :], identb[:]), reads=[b_qk, b_idb], writes=[bpt_])
            P.op("pe", lambda e, sw=sw, t1=t1, t2=t2, qk=qk, qkT=qkT, vb=vb, sm=sm, stt=stt, mv=mv, pt_=pt_: e.transpose(pt_[:, 128:256], qk[:, 1, :], identb[:]), reads=[b_qk, b_idb], writes=[bpt_])
            P.op("act", lambda e, sw=sw, t1=t1, t2=t2, qk=qk, qkT=qkT, vb=vb, sm=sm, stt=stt, mv=mv, pt_=pt_: e.copy(out=qkT[:].rearrange("p a d -> p (a d)"), in_=pt_[:, 0:256]), reads=[bpt_], writes=[b_qkT])
            ps_s, bps_s = nf()
            P.op("pe", lambda e, sw=sw, t1=t1, t2=t2, qk=qk, qkT=qkT, vb=vb, sm=sm, stt=stt, mv=mv, ps_s=ps_s: e.matmul(ps_s[:, 0:128], qkT[:, 1, :], qkT[:, 0, :], start=True, stop=True), reads=[b_qkT], writes=[bps_s])
            P.op("dve", lambda e, sw=sw, t1=t1, t2=t2, qk=qk, qkT=qkT, vb=vb, sm=sm, stt=stt, mv=mv, ps_s=ps_s: e.tensor_tensor(out=sm[:], in0=ps_s[:, 0:128], in1=maskT[:], op=ALU.mult), reads=[bps_s, b_mask], writes=[b_sm])
            ps_y, bps_y = nf()
            P.op("pe", lambda e, sw=sw, t1=t1, t2=t2, qk=qk, qkT=qkT, vb=vb, sm=sm, stt=stt, mv=mv, ps_y=ps_y: e.matmul(ps_y[:, 0:128], sm[:], vb[:], start=True, stop=False), reads=[b_sm, b_vb], writes=[bps_y])
            P.op("pe", lambda e, sw=sw, t1=t1, t2=t2, qk=qk, qkT=qkT, vb=vb, sm=sm, stt=stt, mv=mv, ps_y=ps_y: e.matmul(ps_y[:, 0:128], qkT[:, 0, :], Sb[:], start=False, stop=True), reads=[b_qkT, b_Sb], writes=[bps_y])
            ps_kv, bps_kv = nf()
            P.op("pe", lambda e, sw=sw, t1=t1, t2=t2, qk=qk, qkT=qkT, vb=vb, sm=sm, stt=stt, mv=mv, ps_kv=ps_kv: e.matmul(ps_kv[:, 0:128], qk[:, 1, :], vb[:], start=True, stop=True), reads=[b_qk, b_vb], writes=[bps_kv])
            P.op("dve", lambda e, sw=sw, t1=t1, t2=t2, qk=qk, qkT=qkT, vb=vb, sm=sm, stt=stt, mv=mv, ps_kv=ps_kv: e.tensor_tensor(out=S32[:], in0=ps_kv[:, 0:128], in1=S32[:], op=ALU.add), reads=[bps_kv, b_S32], writes=[b_S32])
            P.op("dve", lambda e, sw=sw, t1=t1, t2=t2, qk=qk, qkT=qkT, vb=vb, sm=sm, stt=stt, mv=mv: e.tensor_scalar(out=S32[:], in0=S32[:], scalar1=g[:, 0:1], scalar2=None, op0=ALU.mult), reads=[b_S32, b_g], writes=[b_S32])
            P.op("act", lambda e, sw=sw, t1=t1, t2=t2, qk=qk, qkT=qkT, vb=vb, sm=sm, stt=stt, mv=mv: e.copy(out=Sb[:], in_=S32[:]), reads=[b_S32], writes=[b_Sb])
            P.op("dve", lambda e, sw=sw, t1=t1, t2=t2, qk=qk, qkT=qkT, vb=vb, sm=sm, stt=stt, mv=mv, ps_y=ps_y: e.bn_stats(out=stt[:], in_=ps_y[:, 0:128]), reads=[bps_y], writes=[b_stt])
            P.op("dve", lambda e, sw=sw, t1=t1, t2=t2, qk=qk, qkT=qkT, vb=vb, sm=sm, stt=stt, mv=mv: e.bn_aggr(out=mv[:, 0:2], in_=stt[:]), reads=[b_stt], writes=[b_mv])
            P.op("act", lambda e, sw=sw, t1=t1, t2=t2, qk=qk, qkT=qkT, vb=vb, sm=sm, stt=stt, mv=mv: e.activation(out=mv[:, 2:3], in_=mv[:, 1:2], func=AF.Sqrt, bias=EPS, scale=1.0), reads=[b_mv], writes=[b_mv])
            P.op("dve", lambda e, sw=sw, t1=t1, t2=t2, qk=qk, qkT=qkT, vb=vb, sm=sm, stt=stt, mv=mv: e.reciprocal(out=mv[:, 3:4], in_=mv[:, 2:3]), reads=[b_mv], writes=[b_mv])
            ys = n % 2
            P.op("dve", lambda e, sw=sw, t1=t1, t2=t2, qk=qk, qkT=qkT, vb=vb, sm=sm, stt=stt, mv=mv, ps_y=ps_y, ys=ys: e.tensor_scalar(out=yo[ys][:], in0=ps_y[:, 0:128], scalar1=mv[:, 0:1], scalar2=mv[:, 3:4], op0=ALU.subtract, op1=ALU.mult), reads=[bps_y, b_mv], writes=[b_yo[ys]])
            P.dma("sp", f"r_yo{ys}", lambda e, sw=sw, t1=t1, t2=t2, qk=qk, qkT=qkT, vb=vb, sm=sm, stt=stt, mv=mv, ys=ys, n=n: e.dma_start(out=r_out[n*128:(n+1)*128, :], in_=yo[ys][:]), reads=[b_yo[ys]])
            yield

    def att_unit():
        wt = P.sb("a_w_t", [128, 2, 128], F32); b_w = P.buf()
        P.dma("sp", "a_w", lambda e: e.dma_start(out=wt[:], in_=a_w), writes=[b_w])
        kT = P.sb("a_kT", [128, T_ALL], BF16); b_kT = P.buf()
        qT = P.sb("a_qT", [128, NQ], BF16); b_qT = P.buf()
        vb = P.sb("a_vb", [128, NCH, 128], BF16); b_vb = P.buf()
        xin = [P.sb(f"a_xin{i}", [128, 128], F32) for i in range(2)]; b_xin = [P.buf() for _ in range(2)]
        tb = [P.sb(f"a_tb{i}", [128, 2, 128], F32) for i in range(2)]; b_tb = [P.buf() for _ in range(2)]
        NS = 2
        mk = lambda nm, shp, dt: ([P.sb(f"{nm}{i}", shp, dt) for i in range(NS)], [P.buf() for _ in range(NS)])
        junk_l, b_junk_l = mk("a_junk", [128, 128], F32)
        col_l, b_col_l = mk("a_col", [128, 4], F32)
        xn_l, b_xn_l = mk("a_xn", [128, 128], F32)
        sw_l, b_sw_l = mk("a_sw", [128, 128], F32)
        t1_l, b_t1_l = mk("a_t1", [128, 128], F32)
        t2_l, b_t2_l = mk("a_t2", [128, 128], F32)
        xr_l, b_xr_l = mk("a_xr", [128, 128], BF16)
        vst = [P.sb(f"a_vst{i}", [128, 6, 128], F32) for i in range(2)]; b_vst = [P.buf() for _ in range(2)]
        a_v_v = a_v.rearrange("(b p) d -> p b d", p=128)
        for i in range(NCH // 6):
            s = i % 2
            P.dma("sp", f"a_vst{s}", lambda e, s=s, i=i: e.dma_start(out=vst[s][:], in_=a_v_v[:, i*6:(i+1)*6, :]), writes=[b_vst[s]])
            P.op("pool", lambda e, s=s, i=i: e.tensor_copy(out=vb[:, i*6:(i+1)*6, :], in_=vst[s][:]), reads=[b_vst[s]], writes=[b_vb])

        def prep(src, tabsrc, nblk, wi, dstT, b_dstT):
            for blk in range(nblk):
                s = blk % 2
                X = xin[s]; TB = tb[s]
                junk, col, xn, sw, t1, t2, xr = junk_l[s], col_l[s], xn_l[s], sw_l[s], t1_l[s], t2_l[s], xr_l[s]
                b_junk, b_col, b_xn, b_sw, b_t1, b_t2, b_xr = b_junk_l[s], b_col_l[s], b_xn_l[s], b_sw_l[s], b_t1_l[s], b_t2_l[s], b_xr_l[s]
                P.dma("sp", f"a_xin{s}", lambda e, junk=junk, col=col, xn=xn, sw=sw, t1=t1, t2=t2, xr=xr, X=X, blk=blk: e.dma_start(out=X[:], in_=src[blk*128:(blk+1)*128, :]), writes=[b_xin[s]])
                P.dma("sp", f"a_tb{s}", lambda e, junk=junk, col=col, xn=xn, sw=sw, t1=t1, t2=t2, xr=xr, TB=TB, blk=blk: e.dma_start(out=TB[:], in_=tabsrc[blk*128:(blk+1)*128]), writes=[b_tb[s]])
                P.op("act", lambda e, junk=junk, col=col, xn=xn, sw=sw, t1=t1, t2=t2, xr=xr, X=X: e.activation(out=junk[:], in_=X[:], func=AF.Square, accum_out=col[:, 0:1]), reads=[b_xin[s]], writes=[b_junk, b_col])
                P.op("act", lambda e, junk=junk, col=col, xn=xn, sw=sw, t1=t1, t2=t2, xr=xr: e.activation(out=col[:, 1:2], in_=col[:, 0:1], func=AF.Sqrt, scale=1.0 / 128, bias=EPS), reads=[b_col], writes=[b_col])
                P.op("dve", lambda e, junk=junk, col=col, xn=xn, sw=sw, t1=t1, t2=t2, xr=xr: e.reciprocal(out=col[:, 2:3], in_=col[:, 1:2]), reads=[b_col], writes=[b_col])
                P.op("dve", lambda e, junk=junk, col=col, xn=xn, sw=sw, t1=t1, t2=t2, xr=xr, X=X: e.scalar_tensor_tensor(out=xn[:], in0=X[:], scalar=col[:, 2:3], in1=wt[:, wi, :], op0=ALU.mult, op1=ALU.mult), reads=[b_xin[s], b_col, b_w], writes=[b_xn])
                xv = xn[:].rearrange("p (a b d) -> p a b d", a=2, b=2)
                sv = sw[:].rearrange("p (a b d) -> p a b d", a=2, b=2)
                P.op("pool", lambda e, junk=junk, col=col, xn=xn, sw=sw, t1=t1, t2=t2, xr=xr, xv=xv, sv=sv: e.tensor_copy(out=sv[:, :, 0, :], in_=xv[:, :, 1, :]), reads=[b_xn], writes=[b_sw])
                P.op("pool", lambda e, junk=junk, col=col, xn=xn, sw=sw, t1=t1, t2=t2, xr=xr, xv=xv, sv=sv: e.tensor_copy(out=sv[:, :, 1, :], in_=xv[:, :, 0, :]), reads=[b_xn], writes=[b_sw])
                P.op("dve", lambda e, junk=junk, col=col, xn=xn, sw=sw, t1=t1, t2=t2, xr=xr, TB=TB: e.tensor_tensor(out=t1[:], in0=xn[:], in1=TB[:, 0, :], op=ALU.mult), reads=[b_xn, b_tb[s]], writes=[b_t1])
                P.op("pool", lambda e, junk=junk, col=col, xn=xn, sw=sw, t1=t1, t2=t2, xr=xr, TB=TB: e.tensor_tensor(out=t2[:], in0=sw[:], in1=TB[:, 1, :], op=ALU.mult), reads=[b_sw, b_tb[s]], writes=[b_t2])
                P.op("dve", lambda e, junk=junk, col=col, xn=xn, sw=sw, t1=t1, t2=t2, xr=xr: e.tensor_tensor(out=xr[:], in0=t1[:], in1=t2[:], op=ALU.add), reads=[b_t1, b_t2], writes=[b_xr])
                pt_, bpt_ = nb()
                P.op("pe", lambda e, junk=junk, col=col, xn=xn, sw=sw, t1=t1, t2=t2, xr=xr, pt_=pt_: e.transpose(pt_[:, 0:128], xr[:], identb[:]), reads=[b_xr, b_idb], writes=[bpt_])
                P.op("act", lambda e, junk=junk, col=col, xn=xn, sw=sw, t1=t1, t2=t2, xr=xr, pt_=pt_, blk=blk: e.copy(out=dstT[:, blk*128:(blk+1)*128], in_=pt_[:, 0:128]), reads=[bpt_], writes=[b_dstT])
                yield

        yield from prep(a_k, a_ktab, NCH, 1, kT, b_kT)
        yield from prep(a_q, a_qtab, NQB, 0, qT, b_qT)

        Ssb = P.sb("a_S", [128, T_ALL], F32); b_S = P.buf()
        Pb = P.sb("a_P", [128, T_ALL], BF16); b_P = P.buf()
        PT = P.sb("a_PT", [128, NCH, 128], BF16); b_PT = P.buf()
        c2 = P.sb("a_c2", [128, 4], F32); b_c2 = P.buf()
        ob = [P.sb(f"a_o{i}", [128, 128], F32) for i in range(2)]; b_ob = [P.buf() for _ in range(2)]
        for qb in range(NQB):
            nk = CTX if qb == 0 else T_ALL
            nkt = (nk + 511) // 512
            for kt in range(nkt):
                w = min(512, nk - kt * 512)
                ps_, bps_ = nf()
                P.op("pe", lambda e, ps_=ps_, qb=qb, kt=kt, w=w: e.matmul(ps_[:, 0:w], qT[:, qb*128:(qb+1)*128], kT[:, kt*512:kt*512+w], start=True, stop=True), reads=[b_qT, b_kT], writes=[bps_])
                if kt % 2:
                    P.op("dve", lambda e, ps_=ps_, kt=kt, w=w: e.tensor_copy(out=Ssb[:, kt*512:kt*512+w], in_=ps_[:, 0:w]), reads=[bps_], writes=[b_S])
                else:
                    P.op("act", lambda e, ps_=ps_, kt=kt, w=w: e.copy(out=Ssb[:, kt*512:kt*512+w], in_=ps_[:, 0:w]), reads=[bps_], writes=[b_S])
            P.op("dve", lambda e, nk=nk: e.reduce_max(out=c2[:, 0:1], in_=Ssb[:, 0:nk], axis=AX.X), reads=[b_S], writes=[b_c2])
            P.op("dve", lambda e: e.tensor_scalar(out=c2[:, 1:2], in0=c2[:, 0:1], scalar1=-ATT_SCALE, scalar2=None, op0=ALU.mult), reads=[b_c2], writes=[b_c2])
            P.op("act", lambda e, nk=nk: e.activation(out=Pb[:, 0:nk], in_=Ssb[:, 0:nk], func=AF.Exp, scale=ATT_SCALE, bias=c2[:, 1:2], accum_out=c2[:, 2:3]), reads=[b_S, b_c2], writes=[b_P, b_c2])
            P.op("dve", lambda e: e.reciprocal(out=c2[:, 3:4], in_=c2[:, 2:3]), reads=[b_c2], writes=[b_c2])
            nkb = nk // 128
            for g0 in range(0, nkb, 4):
                gn = min(4, nkb - g0)
                pt_, bpt_ = nb()
                for j in range(gn):
                    P.op("pe", lambda e, pt_=pt_, j=j, g0=g0: e.transpose(pt_[:, j*128:(j+1)*128], Pb[:, (g0+j)*128:(g0+j+1)*128], identb[:]), reads=[b_P, b_idb], writes=[bpt_])
                if (g0 // 4) % 2:
                    P.op("dve", lambda e, pt_=pt_, g0=g0, gn=gn: e.tensor_copy(out=PT[:, g0:g0+gn, :].rearrange("p a d -> p (a d)"), in_=pt_[:, 0:gn*128]), reads=[bpt_], writes=[b_PT])
                else:
                    P.op("act", lambda e, pt_=pt_, g0=g0, gn=gn: e.copy(out=PT[:, g0:g0+gn, :].rearrange("p a d -> p (a d)"), in_=pt_[:, 0:gn*128]), reads=[bpt_], writes=[b_PT])
            po, bpo = nf()
            for kb in range(nkb):
                P.op("pe", lambda e, po=po, kb=kb, nkb=nkb: e.matmul(po[:, 0:128], PT[:, kb, :], vb[:, kb, :], start=(kb == 0), stop=(kb == nkb - 1)), reads=[b_PT, b_vb], writes=[bpo])
            os_ = qb % 2
            P.op("dve", lambda e, po=po, os_=os_: e.tensor_scalar(out=ob[os_][:], in0=po[:, 0:128], scalar1=c2[:, 3:4], scalar2=None, op0=ALU.mult), reads=[bpo, b_c2], writes=[b_ob[os_]])
            P.dma("sp", f"a_o{os_}", lambda e, os_=os_, qb=qb: e.dma_start(out=a_out[qb*128:(qb+1)*128, :], in_=ob[os_][:]), reads=[b_ob[os_]])
            yield

    units = globals().get("MIX_UNITS", "rsa")
    gens = []
    if "a" in units:
        gens.append(att_unit())
    if "r" in units:
        gens.append(ret_unit())
    if "s" in units:
        gens.append(s5_unit())
    while gens:
        for g_ in list(gens):
            try:
                next(g_)
            except StopIteration:
                gens.remove(g_)
    P.emit()
    return nc


def _order(d):
    if d == 0:
        return np.arange(T_ALL)
    return np.concatenate([np.arange(CTX)[::-1], CTX + np.arange(SEQ)[::-1]])


_CONST = {}


def mix_consts():
    if _CONST:
        return _CONST
    f64 = np.float64
    log_g = np.log(1.0 - 2.0 ** (-5.0 - np.arange(4, dtype=f64)))
    freqs = 10000.0 ** (-np.arange(0, 128, 2, dtype=f64) / 128)
    rt = {}
    for d in range(2):
        lg = log_g if d == 0 else log_g[::-1]
        order = _order(d)
        isl = order >= CTX
        pos = np.where(isl, order - CTX, 0).astype(f64)
        ang = pos[:, None] * freqs[None, :]
        cos = np.where(isl[:, None], np.cos(ang), 1.0)
        sin = np.where(isl[:, None], np.sin(ang), 0.0)
        cosf = np.concatenate([cos, cos], axis=1)
        sinf = np.concatenate([-sin, sin], axis=1)
        i = (np.arange(T_ALL) % 128).astype(f64)
        for h in range(4):
            gq = np.exp((i + 1.0) * lg[h])[:, None]
            gk = (128.0 ** -0.5) * np.exp(-(i + 1.0) * lg[h])[:, None]
            tab = np.stack([cosf * gq, sinf * gq, cosf * gk, sinf * gk], axis=1).astype(np.float32)
            rt[(h, d)] = (np.ascontiguousarray(tab), np.full((128, 1), np.exp(128.0 * lg[h]), np.float32))
    _CONST["ret"] = rt
    fr = 10000.0 ** (-np.arange(0, 64, 2, dtype=f64) / 64)
    pos = np.arange(SEQ)
    ar = (pos // 64).astype(f64)[:, None] * fr[None, :]
    ac = (pos % 64).astype(f64)[:, None] * fr[None, :]
    cosl = np.concatenate([np.cos(ar), np.cos(ar), np.cos(ac), np.cos(ac)], axis=1)
    sinl = np.concatenate([-np.sin(ar), np.sin(ar), -np.sin(ac), np.sin(ac)], axis=1)
    cosa = np.concatenate([np.ones((CTX, 128)), cosl], axis=0)
    sina = np.concatenate([np.zeros((CTX, 128)), sinl], axis=0)
    _CONST["att"] = np.ascontiguousarray(np.stack([cosa, sina], axis=1).astype(np.float32))
    _CONST["ident"] = np.eye(128, dtype=np.float32)
    _CONST["maskT"] = np.triu(np.ones((128, 128), np.float32))
    _CONST["iota"] = np.ascontiguousarray(np.broadcast_to(np.arange(1, SEG + 1, dtype=np.float32), (128, SEG)))
    return _CONST


def run_mix(z_all, prm):
    C = mix_consts()
    nc = build_mix()
    in_maps = []
    orders = [_order(0), _order(1)]
    for c in range(NCORE):
        m = {"ident": C["ident"], "maskT": C["maskT"]}
        h, d = c % 4, c // 4
        zo = z_all[orders[d]]
        m["r_qkv"] = np.ascontiguousarray(np.stack([zo[:, h*128:(h+1)*128], zo[:, 512+h*128:512+(h+1)*128], zo[:, 1024+h*128:1024+(h+1)*128]], axis=1))
        m["r_tab"], m["r_g"] = C["ret"][(h, d)]
        par = np.zeros((128, 4, 3), np.float32)
        sB = np.zeros((128, 2, 2, 32), np.float32)
        sC = np.zeros((128, 2, 2, 64), np.float32)
        uT = np.zeros((2, 2, 32, T_ALL), np.float32)
        for tl in range(2):
            for gi in range(2):
                g = 4 * c + 2 * tl + gi
                rows = slice(gi * 64, (gi + 1) * 64)
                for dd in range(2):
                    par[rows, dd*2+tl, 0] = prm["s5_a_re"][dd, g]
                    par[rows, dd*2+tl, 1] = prm["s5_a_im"][dd, g]
                    par[rows, dd*2+tl, 2] = prm["s5_log_step"][dd, g]
                sB[rows, tl, 0, gi*16:(gi+1)*16] = prm["s5_b_re"][g]
                sB[rows, tl, 1, gi*16:(gi+1)*16] = prm["s5_b_im"][g]
                sC[rows, tl, 0, tl*32+gi*16:tl*32+(gi+1)*16] = prm["s5_c_re"][g].T
                sC[rows, tl, 1, tl*32+gi*16:tl*32+(gi+1)*16] = prm["s5_c_im"][g].T
            for dd in range(2):
                ucols = z_all[orders[dd], 2560 + (4*c + 2*tl) * 16: 2560 + (4*c + 2*tl + 2) * 16]
                uT[dd, tl] = ucols.T
        m["s_par"], m["s_B"], m["s_C"], m["s_uT"], m["s_iota"] = par, sB, sC, uT, C["iota"]
        hq, half = c // 2, c % 2
        kvh = hq // 2
        A0 = 3072
        qsel = np.concatenate([np.arange(half*128, (half+1)*128), CTX + np.arange(half*4096, (half+1)*4096)])
        m["a_q"] = np.ascontiguousarray(z_all[qsel, A0 + hq*128: A0 + (hq+1)*128])
        m["a_k"] = np.ascontiguousarray(z_all[:, A0 + 512 + kvh*128: A0 + 512 + (kvh+1)*128])
        m["a_v"] = np.ascontiguousarray(z_all[:, A0 + 768 + kvh*128: A0 + 768 + (kvh+1)*128])
        m["a_qtab"] = np.ascontiguousarray(C["att"][qsel])
        m["a_ktab"] = C["att"]
        m["a_w"] = np.ascontiguousarray(np.stack([np.broadcast_to(prm["q_norm_w"], (128, 128)), np.broadcast_to(prm["k_norm_w"], (128, 128))], axis=1))
        in_maps.append(m)
    res = _run(nc, in_maps)
    ret = np.zeros((2, T_ALL, 512), np.float32)
    s5y = np.zeros((2, T_ALL, 512), np.float32)
    att = np.zeros((T_ALL, 512), np.float32)
    for c in range(NCORE):
        h, d = c % 4, c // 4
        if "r_out" in res[c]:
            ret[d][orders[d], h*128:(h+1)*128] = res[c]["r_out"]
        for dd in range(2):
            s5y[dd][orders[dd], c*64:(c+1)*64] = res[c]["s_out"][dd]
        hq, half = c // 2, c % 2
        qsel = np.concatenate([np.arange(half*128, (half+1)*128), CTX + np.arange(half*4096, (half+1)*4096)])
        att[qsel, hq*128:(hq+1)*128] = res[c]["a_out"]
    return ret, s5y, att


def build_out():
    nc = bass.Bass("TRN2", target_bir_lowering=False)
    di = lambda name, shape, dt=F32: nc.dram_tensor(name, list(shape), dt, kind="ExternalInput").ap()
    do = lambda name, shape, dt=F32: nc.dram_tensor(name, list(shape), dt, kind="ExternalOutput").ap()
    ident_d = di("ident", [128, 128])
    x = di("x", [TOK_PC, D])
    rg = di("rg", [TOK_PC, 4, 512])
    s5 = di("s5", [TOK_PC, 3, 512])
    att = di("att", [TOK_PC, 512])
    cv = di("cv", [TOK_PC, 7, 512])
    vecs = di("vecs", [128, 5, 512])
    w_glu = di("w_glu", [512, 512])
    w_out = di("w_out", [D, D])
    g1 = di("g1", [2, 128, D])
    modc = di("modc", [128, 16, 4])
    w_r = di("w_r", [128, 16, 32])
    b_r = di("b_r", [128, 32])
    xmid = do("xmid", [TOK_PC, D])
    fT = do("fT", [D, TOK_PC], BF16)
    gates = do("gates", [TOK_PC, 32])

    P = Prog(nc)
    NF = 5
    psf = [P.ps(f"psf{i}", [128, 512], F32) for i in range(NF)]; b_psf = [P.buf() for _ in range(NF)]
    psb = [P.ps(f"psb{i}", [128, 512], BF16) for i in range(2)]; b_psb = [P.buf() for _ in range(2)]
    cnt = {"f": 0, "b": 0}

    def nf():
        i = cnt["f"] % NF; cnt["f"] += 1
        return psf[i], b_psf[i]

    def nb():
        i = cnt["b"] % 2; cnt["b"] += 1
        return psb[i], b_psb[i]

    ident = P.sb("identt", [128, 128], F32); b_id = P.buf()
    identb = P.sb("identb", [128, 128], BF16); b_idb = P.buf()
    P.dma("sp", "ident", lambda e: e.dma_start(out=ident[:], in_=ident_d), writes=[b_id])
    P.op("dve", lambda e: e.tensor_copy(out=identb[:], in_=ident[:]), reads=[b_id], writes=[b_idb])
    vt = P.sb("vecs_t", [128, 5, 512], F32); b_vt = P.buf()
    P.dma("sp", "vecs", lambda e: e.dma_start(out=vt[:], in_=vecs), writes=[b_vt])
    g1t = P.sb("g1t", [128, 2, D], F32); b_g1 = P.buf()
    P.dma("sp", "g1", lambda e: e.dma_start(out=g1t[:], in_=g1.rearrange("r p d -> p r d")), writes=[b_g1])
    mc = P.sb("mc", [128, 16, 4], F32); b_mc = P.buf()
    P.dma("sp", "mc", lambda e: e.dma_start(out=mc[:], in_=modc), writes=[b_mc])
    P.op("dve", lambda e: e.tensor_scalar(out=mc[:, :, 1], in0=mc[:, :, 1], scalar1=1.0, scalar2=None, op0=ALU.add), reads=[b_mc], writes=[b_mc])
    P.op("dve", lambda e: e.tensor_scalar(out=mc[:, :, 3], in0=mc[:, :, 3], scalar1=1.0, scalar2=None, op0=ALU.add), reads=[b_mc], writes=[b_mc])
    wr = P.sb("wr", [128, 16, 32], F32); b_wr = P.buf()
    P.dma("sp", "wr", lambda e: e.dma_start(out=wr[:], in_=w_r), writes=[b_wr])
    brt = P.sb("brt", [128, 32], F32); b_br = P.buf()
    P.dma("sp", "brt", lambda e: e.dma_start(out=brt[:], in_=b_r), writes=[b_br])
    wst = [P.sb(f"wst{i}", [128, 4, 512], F32) for i in range(2)]; b_wst = [P.buf() for _ in range(2)]
    wg = P.sb("wg", [128, 4, 512], BF16); b_wg = P.buf()
    P.dma("sp", "wst0", lambda e: e.dma_start(out=wst[0][:], in_=w_glu.rearrange("(k p) n -> p k n", p=128)), writes=[b_wst[0]])
    P.op("pool", lambda e: e.tensor_copy(out=wg[:], in_=wst[0][:]), reads=[b_wst[0]], writes=[b_wg])

    mixT = P.sb("mixT", [128, 16, TOK_PC], BF16); b_mixT = [P.buf() for _ in TILES_PC]
    rgt = P.sb("rgt", [128, 4, 512], F32); b_rg = P.buf()
    s5t = P.sb("s5t", [128, 3, 512], F32); b_s5 = P.buf()
    att_t = P.sb("att_t", [128, 512], F32); b_att = P.buf()
    cvt = P.sb("cvt", [128, 7, 512], F32); b_cv = P.buf()
    tm = [P.sb(f"tm{i}", [128, 512], F32) for i in range(5)]; b_tm = [P.buf() for _ in range(5)]
    yb16 = P.sb("yb16", [128, 512], BF16); b_yb16 = P.buf()
    yT = P.sb("yT", [128, 4, 128], BF16); b_yT = P.buf()
    mix = P.sb("mix", [128, D], BF16); b_mix = [P.buf() for _ in range(4)]

    def tt(eng, o, a, b, op, rd, wr_):
        P.op(eng, lambda e: e.tensor_tensor(out=o, in0=a, in1=b, op=op), reads=rd, writes=wr_)

    for ti, (r0, n) in enumerate(TILES_PC):
        P.dma("sp", "rgt", lambda e, r0=r0, n=n: e.dma_start(out=rgt[0:n], in_=rg[r0:r0+n]), writes=[b_rg])
        P.dma("sp", "s5t", lambda e, r0=r0, n=n: e.dma_start(out=s5t[0:n], in_=s5[r0:r0+n]), writes=[b_s5])
        P.dma("sp", "att_t", lambda e, r0=r0, n=n: e.dma_start(out=att_t[0:n], in_=att[r0:r0+n]), writes=[b_att])
        P.dma("sp", "cvt", lambda e, r0=r0, n=n: e.dma_start(out=cvt[0:n], in_=cv[r0:r0+n]), writes=[b_cv])
        P.op("act", lambda e, n=n: e.activation(out=tm[0][0:n], in_=rgt[0:n, 2, :], func=AF.Silu), reads=[b_rg], writes=[b_tm[0]])
        P.op("act", lambda e, n=n: e.activation(out=tm[1][0:n], in_=rgt[0:n, 3, :], func=AF.Silu), reads=[b_rg], writes=[b_tm[1]])
        tt("dve", tm[0][0:n], tm[0][0:n], rgt[0:n, 0, :], ALU.mult, [b_tm[0], b_rg], [b_tm[0]])
        tt("pool", tm[1][0:n], tm[1][0:n], rgt[0:n, 1, :], ALU.mult, [b_tm[1], b_rg], [b_tm[1]])
        tt("dve", mix[0:n, 0:512], tm[0][0:n], tm[1][0:n], ALU.add, [b_tm[0], b_tm[1]], [b_mix[0]])
        tt("pool", tm[2][0:n], s5t[0:n, 0, :], s5t[0:n, 1, :], ALU.add, [b_s5], [b_tm[2]])
        tt("dve", tm[3][0:n], s5t[0:n, 2, :], vt[0:n, 0, :], ALU.mult, [b_s5, b_vt], [b_tm[3]])
        tt("pool", tm[2][0:n], tm[2][0:n], tm[3][0:n], ALU.add, [b_tm[2], b_tm[3]], [b_tm[2]])
        tt("pool", tm[3][0:n], tm[2][0:n], tm[2][0:n], ALU.mult, [b_tm[2]], [b_tm[3]])
        P.op("dve", lambda e, n=n: e.tensor_scalar(out=tm[3][0:n], in0=tm[3][0:n], scalar1=0.044715, scalar2=1.0, op0=ALU.mult, op1=ALU.add), reads=[b_tm[3]], writes=[b_tm[3]])
        tt("dve", tm[3][0:n], tm[3][0:n], tm[2][0:n], ALU.mult, [b_tm[3], b_tm[2]], [b_tm[3]])
        P.op("act", lambda e, n=n: e.activation(out=tm[3][0:n], in_=tm[3][0:n], func=AF.Sigmoid, scale=1.5957691216057308), reads=[b_tm[3]], writes=[b_tm[3]])
        tt("dve", tm[2][0:n], tm[2][0:n], tm[3][0:n], ALU.mult, [b_tm[2], b_tm[3]], [b_tm[2]])
        P.op("act", lambda e, n=n: e.copy(out=yb16[0:n], in_=tm[2][0:n]), reads=[b_tm[2]], writes=[b_yb16])
        pt_, bpt_ = nb()
        for k in range(4):
            P.op("pe", lambda e, pt_=pt_, k=k, n=n: e.transpose(pt_[:, k*128:k*128+n], yb16[0:n, k*128:(k+1)*128], identb[0:n, 0:n]), reads=[b_yb16, b_idb], writes=[bpt_])
        P.op("act", lambda e, pt_=pt_, n=n: e.copy(out=yT[:, :, 0:n], in_=pt_[:, 0:512].rearrange("p (a d) -> p a d", a=4)[:, :, 0:n]), reads=[bpt_], writes=[b_yT])
        pg, bpg = nf()
        for k in range(4):
            P.op("pe", lambda e, pg=pg, k=k, n=n: e.matmul(pg[0:n, :], yT[:, k, 0:n], wg[:, k, :], start=(k == 0), stop=(k == 3)), reads=[b_yT, b_wg], writes=[bpg])
        tt("dve", tm[3][0:n], pg[0:n, :], vt[0:n, 1, :], ALU.add, [bpg, b_vt], [b_tm[3]])
        P.op("act", lambda e, n=n: e.activation(out=tm[3][0:n], in_=tm[3][0:n], func=AF.Sigmoid), reads=[b_tm[3]], writes=[b_tm[3]])
        tt("dve", mix[0:n, 512:1024], tm[2][0:n], tm[3][0:n], ALU.mult, [b_tm[2], b_tm[3]], [b_mix[1]])
        P.op("act", lambda e, n=n: e.copy(out=mix[0:n, 1024:1536], in_=att_t[0:n]), reads=[b_att], writes=[b_mix[2]])
        tt("pool", tm[0][0:n], cvt[0:n, 1, :], cvt[0:n, 2, :], ALU.mult, [b_cv], [b_tm[0]])
        tt("dve", tm[1][0:n], cvt[0:n, 3, :], cvt[0:n, 4, :], ALU.mult, [b_cv], [b_tm[1]])
        tt("pool", tm[4][0:n], cvt[0:n, 5, :], cvt[0:n, 6, :], ALU.mult, [b_cv], [b_tm[4]])
        tt("dve", tm[0][0:n], tm[0][0:n], vt[0:n, 2, :], ALU.mult, [b_tm[0], b_vt], [b_tm[0]])
        tt("pool", tm[1][0:n], tm[1][0:n], vt[0:n, 3, :], ALU.mult, [b_tm[1], b_vt], [b_tm[1]])
        tt("dve", tm[4][0:n], tm[4][0:n], vt[0:n, 4, :], ALU.mult, [b_tm[4], b_vt], [b_tm[4]])
        tt("pool", tm[0][0:n], tm[0][0:n], tm[1][0:n], ALU.add, [b_tm[0], b_tm[1]], [b_tm[0]])
        tt("dve", tm[0][0:n], tm[0][0:n], tm[4][0:n], ALU.add, [b_tm[0], b_tm[4]], [b_tm[0]])
        tt("dve", mix[0:n, 1536:2048], tm[0][0:n], cvt[0:n, 0, :], ALU.mult, [b_tm[0], b_cv], [b_mix[3]])
        for kg in range(4):
            pt_, bpt_ = nb()
            for kk in range(4):
                k = kg * 4 + kk
                P.op("pe", lambda e, pt_=pt_, kk=kk, k=k, n=n: e.transpose(pt_[:, kk*128:kk*128+n], mix[0:n, k*128:(k+1)*128], identb[0:n, 0:n]), reads=[b_mix[kg], b_idb], writes=[bpt_])
            eng = "act" if kg % 2 else "dve"
            if eng == "act":
                P.op("act", lambda e, pt_=pt_, kg=kg, n=n, r0=r0: e.copy(out=mixT[:, kg*4:(kg+1)*4, r0:r0+n], in_=pt_[:, 0:512].rearrange("p (a d) -> p a d", a=4)[:, :, 0:n]), reads=[bpt_], writes=[b_mixT[ti]])
            else:
                P.op("dve", lambda e, pt_=pt_, kg=kg, n=n, r0=r0: e.tensor_copy(out=mixT[:, kg*4:(kg+1)*4, r0:r0+n], in_=pt_[:, 0:512].rearrange("p (a d) -> p a d", a=4)[:, :, 0:n]), reads=[bpt_], writes=[b_mixT[ti]])

    wb = [P.sb(f"wb{i}", [128, 16, 512], BF16) for i in range(2)]; b_wb = [P.buf() for _ in range(2)]
    xp = [P.sb(f"xp{i}", [128, 512], F32) for i in range(3)]; b_xp = [P.buf() for _ in range(3)]
    b_xm_dram = [P.buf() for _ in TILES_PC]
    w_v = w_out.rearrange("(k p) n -> p k n", p=128)
    wsi = 1; xi = 0
    def load_wo(cb_):
        nonlocal wsi
        s_ = cb_ % 2
        for kq in range(4):
            ws_ = wsi % 2; wsi += 1
            P.dma("sp", f"wst{ws_}", lambda e, ws_=ws_, cb_=cb_, kq=kq: e.dma_start(out=wst[ws_][:], in_=w_v[:, kq*4:(kq+1)*4, cb_*512:(cb_+1)*512]), writes=[b_wst[ws_]])
            P.op("pool", lambda e, ws_=ws_, s_=s_, kq=kq: e.tensor_copy(out=wb[s_][:, kq*4:(kq+1)*4, :], in_=wst[ws_][:]), reads=[b_wst[ws_]], writes=[b_wb[s_]])

    load_wo(0)
    for cb in range(4):
        s = cb % 2
        if cb + 1 < 4:
            load_wo(cb + 1)
        for ti, (r0, n) in enumerate(TILES_PC):
            isctx = 1 if r0 >= LAT_PC else 0
            xs_ = xi % 3; xi += 1
            P.dma("sp", f"xp{xs_}", lambda e, xs_=xs_, r0=r0, n=n, cb=cb: e.dma_start(out=xp[xs_][0:n], in_=x[r0:r0+n, cb*512:(cb+1)*512]), writes=[b_xp[xs_]])
            po, bpo = nf()
            for k in range(16):
                P.op("pe", lambda e, po=po, k=k, n=n, r0=r0, s=s: e.matmul(po[0:n, :], mixT[:, k, r0:r0+n], wb[s][:, k, :], start=(k == 0), stop=(k == 15)), reads=[b_mixT[ti], b_wb[s]], writes=[bpo])
            t_ = tm[xi % 2]; bt_ = b_tm[xi % 2]
            tt("dve", t_[0:n], po[0:n, :], g1t[0:n, isctx, cb*512:(cb+1)*512], ALU.mult, [bpo, b_g1], [bt_])
            tt("pool", xp[xs_][0:n], xp[xs_][0:n], t_[0:n], ALU.add, [b_xp[xs_], bt_], [b_xp[xs_]])
            P.dma("sp", f"xpo{xs_}", lambda e, xs_=xs_, r0=r0, n=n, cb=cb: e.dma_start(out=xmid[r0:r0+n, cb*512:(cb+1)*512], in_=xp[xs_][0:n]), reads=[b_xp[xs_]], writes=[b_xm_dram[ti]])

    xt = [P.sb(f"xt{i}", [128, D], F32) for i in range(1)]; b_xt = [P.buf() for _ in range(1)]
    xn = P.sb("xn", [128, D], F32); b_xn = P.buf()
    junk = mix
    ss = P.sb("ss", [128, 2], F32); b_ss = P.buf()
    f32t = P.sb("f32t", [128, 16, 128], F32); b_f32 = P.buf()
    lg = P.sb("lg", [128, 32], F32); b_lg = P.buf()
    rc = P.sb("rc", [128, 16], F32); b_rc = P.buf()
    ex = P.sb("ex", [128, 32], F32); b_ex = P.buf()
    gt = [P.sb(f"gt{i}", [128, 32], F32) for i in range(2)]; b_gt = [P.buf() for _ in range(2)]
    fT_v = fT.rearrange("(k p) t -> p k t", p=128)
    for ti, (r0, n) in enumerate(TILES_PC):
        s = ti % 2
        isctx = 1 if r0 >= LAT_PC else 0
        X = xt[0]; bX = b_xt[0]
        P.dma("sp", "xt0", lambda e, X=X, r0=r0, n=n: e.dma_start(out=X[0:n, :], in_=xmid[r0:r0+n, :]), reads=[b_xm_dram[ti]], writes=[bX])
        P.op("act", lambda e, X=X, n=n: e.activation(out=junk[0:n, :], in_=X[0:n, :], func=AF.Square, accum_out=ss[0:n, 0:1]), reads=[bX], writes=b_mix + [b_ss])
        P.op("act", lambda e, n=n: e.activation(out=ss[0:n, 1:2], in_=ss[0:n, 0:1], func=AF.Sqrt, scale=1.0 / D, bias=EPS), reads=[b_ss], writes=[b_ss])
        P.op("dve", lambda e, n=n: e.reciprocal(out=ss[0:n, 1:2], in_=ss[0:n, 1:2]), reads=[b_ss], writes=[b_ss])
        P.op("dve", lambda e, X=X, n=n: e.tensor_scalar(out=xn[0:n, :], in0=X[0:n, :], scalar1=ss[0:n, 1:2], scalar2=None, op0=ALU.mult), reads=[bX, b_ss], writes=[b_xn])
        for kg in range(4):
            pt_, bpt_ = nf()
            for kk in range(4):
                k = kg * 4 + kk
                P.op("pe", lambda e, pt_=pt_, kk=kk, k=k, n=n: e.transpose(pt_[:, kk*128:kk*128+n], xn[0:n, k*128:(k+1)*128], ident[0:n, 0:n]), reads=[b_xn, b_id], writes=[bpt_])
            for kk in range(4):
                k = kg * 4 + kk
                if kk % 2 == 0:
                    P.op("dve", lambda e, pt_=pt_, kk=kk, k=k, n=n, isctx=isctx: e.tensor_scalar(
                        out=f32t[:, k, 0:n], in0=pt_[:, kk*128:kk*128+n], scalar1=mc[:, k, 2*isctx+1:2*isctx+2], scalar2=mc[:, k, 2*isctx:2*isctx+1], op0=ALU.mult, op1=ALU.add),
                        reads=[bpt_, b_mc], writes=[b_f32])
                else:
                    P.op("act", lambda e, pt_=pt_, kk=kk, k=k, n=n, isctx=isctx: e.activation(
                        out=f32t[:, k, 0:n], in_=pt_[:, kk*128:kk*128+n], func=AF.Identity, scale=mc[:, k, 2*isctx+1:2*isctx+2], bias=mc[:, k, 2*isctx:2*isctx+1]),
                        reads=[bpt_, b_mc], writes=[b_f32])
        P.op("pool", lambda e, n=n, r0=r0: e.tensor_copy(out=mixT[:, :, r0:r0+n], in_=f32t[:, :, 0:n]), reads=[b_f32], writes=[b_mixT[ti]])
        pl, bpl = nf()
        for k in range(16):
            P.op("pe", lambda e, pl=pl, k=k, n=n: e.matmul(pl[0:n, 0:32], f32t[:, k, 0:n], wr[:, k, :], start=(k == 0), stop=(k == 15)), reads=[b_f32, b_wr], writes=[bpl])
        tt("dve", lg[0:n], pl[0:n, 0:32], brt[0:n], ALU.add, [bpl, b_br], [b_lg])
        P.op("dve", lambda e, n=n: e.max(out=rc[0:n, 0:8], in_=lg[0:n]), reads=[b_lg], writes=[b_rc])
        P.op("dve", lambda e, n=n: e.tensor_scalar(out=rc[0:n, 8:9], in0=rc[0:n, 0:1], scalar1=-1.0, scalar2=None, op0=ALU.mult), reads=[b_rc], writes=[b_rc])
        P.op("act", lambda e, n=n: e.activation(out=ex[0:n], in_=lg[0:n], func=AF.Exp, bias=rc[0:n, 8:9], scale=1.0), reads=[b_lg, b_rc], writes=[b_ex])
        P.op("dve", lambda e, n=n: e.tensor_scalar(out=lg[0:n], in0=lg[0:n], scalar1=rc[0:n, 3:4], scalar2=None, op0=ALU.is_ge), reads=[b_lg, b_rc], writes=[b_lg])
        tt("dve", ex[0:n], ex[0:n], lg[0:n], ALU.mult, [b_ex, b_lg], [b_ex])
        P.op("dve", lambda e, n=n: e.reduce_sum(out=rc[0:n, 9:10], in_=ex[0:n], axis=AX.X), reads=[b_ex], writes=[b_rc])
        P.op("dve", lambda e, n=n: e.reciprocal(out=rc[0:n, 10:11], in_=rc[0:n, 9:10]), reads=[b_rc], writes=[b_rc])
        P.op("dve", lambda e, n=n, s=s: e.tensor_scalar(out=gt[s][0:n], in0=ex[0:n], scalar1=rc[0:n, 10:11], scalar2=None, op0=ALU.mult), reads=[b_ex, b_rc], writes=[b_gt[s]])
        P.dma("sp", f"gto{s}", lambda e, s=s, n=n, r0=r0: e.dma_start(out=gates[r0:r0+n, :], in_=gt[s][0:n]), reads=[b_gt[s]])
    for k in range(16):
        P.dma("sp", "f16o", lambda e, k=k: e.dma_start(out=fT_v[:, k, :], in_=mixT[:, k, :]), reads=b_mixT)
    P.emit()
    return nc


def _shift(a, k):
    out = np.zeros_like(a)
    if k == -1:
        out[1:] = a[:-1]
    elif k == 1:
        out[:-1] = a[1:]
    return out


def run_out(x_shards, z_all, ret, s5y, att, mod_l, prm):
    C = mix_consts()
    nc = build_out()
    lat = lambda a: a[CTX:]
    ctx = lambda a: a[:CTX]
    sh = lambda a: shard_tokens(lat(a), ctx(a))
    gf = z_all[:, 1536:2048]; gb = z_all[:, 2048:2560]
    rg_s = sh(np.stack([ret[0], ret[1], gf, gb], axis=1))
    s5_s = sh(np.stack([s5y[0], s5y[1], z_all[:, 2560:3072]], axis=1))
    att_s = sh(att)
    zc = z_all[:, 4096:5632]
    bg, cg, hh = zc[:, 0:512], zc[:, 512:1024], zc[:, 1024:1536]

    def sh3(a):
        parts = []
        for k in (-1, 0, 1):
            parts.append(np.concatenate([_shift(ctx(a), k), _shift(lat(a), k)], axis=0) if k else a)
        return parts
    cs_, hs_ = sh3(cg), sh3(hh)
    cv_s = sh(np.stack([bg, cs_[0], hs_[0], cs_[1], hs_[1], cs_[2], hs_[2]], axis=1))
    rep = lambda v: np.broadcast_to(v, (128,) + v.shape)
    vecs = np.ascontiguousarray(np.stack([rep(prm["s5_d"]), rep(prm["s5_b_glu"]), rep(prm["conv_w"][0]), rep(prm["conv_w"][1]), rep(prm["conv_w"][2])], axis=1))
    g1 = np.ascontiguousarray(np.stack([rep(mod_l[0, 2*D:3*D]), rep(mod_l[1, 2*D:3*D])]))
    modc = np.ascontiguousarray(np.stack([cols128(mod_l[0, 3*D:4*D]), cols128(mod_l[0, 4*D:5*D]), cols128(mod_l[1, 3*D:4*D]), cols128(mod_l[1, 4*D:5*D])], axis=-1))
    b_r = np.ascontiguousarray(rep(prm["b_router"]))
    in_maps = []
    for c in range(NCORE):
        in_maps.append({"ident": C["ident"], "x": x_shards[c], "rg": rg_s[c], "s5": s5_s[c], "att": att_s[c], "cv": cv_s[c],
                        "vecs": vecs, "w_glu": prm["s5_w_glu"], "w_out": prm["w_out"], "g1": g1, "modc": modc,
                        "w_r": np.ascontiguousarray(prm["w_router"].reshape(16, 128, 32).transpose(1, 0, 2)), "b_r": b_r})
    res = _run(nc, in_maps)
    return [r["xmid"] for r in res], [r["fT"] for r in res], [r["gates"] for r in res]


E_PC = 4
NCORE_E = 32 // E_PC
DE = 1024
E_TILES = [(i * 512, 512) for i in range(16)] + [(8192, 256)]


def build_moe():
    nc = bass.Bass("TRN2", target_bir_lowering=False)
    di = lambda name, shape, dt=F32: nc.dram_tensor(name, list(shape), dt, kind="ExternalInput").ap()
    fT = di("fT", [D, T_ALL], BF16)
    gb = di("gb", [E_PC, 128, T_ALL])
    wg = di("wg", [E_PC, D, DE]); wu = di("wu", [E_PC, D, DE]); wd = di("wd", [E_PC, DE, D])
    bgu = di("bgu", [128, E_PC, 16]); bd = di("bd", [128, E_PC, 16])
    yT = nc.dram_tensor("yT", [D, T_ALL], F32, kind="ExternalOutput").ap()
    P = Prog(nc)
    NF = 8
    psf = [P.ps(f"psf{i}", [128, 512], F32) for i in range(NF)]; b_psf = [P.buf() for _ in range(NF)]
    cnt = {"f": 0}

    def nf():
        i = cnt["f"] % NF; cnt["f"] += 1
        return psf[i], b_psf[i]

    bgut = P.sb("bgut", [128, E_PC, 16], F32); b_bgu = P.buf()
    bdt = P.sb("bdt", [128, E_PC, 16], F32); b_bd = P.buf()
    P.dma("sp", "bgu", lambda e: e.dma_start(out=bgut[:], in_=bgu), writes=[b_bgu])
    P.dma("sp", "bd", lambda e: e.dma_start(out=bdt[:], in_=bd), writes=[b_bd])
    wgb = P.sb("wgb", [128, 16, DE], BF16); wub = P.sb("wub", [128, 16, DE], BF16); wdb = P.sb("wdb", [128, 8, D], BF16)
    b_wgb = P.buf(); b_wub = P.buf(); b_wdb = P.buf()
    wst = [P.sb(f"wst{i}", [128, 4, 512], F32) for i in range(3)]; b_wst = [P.buf() for _ in range(3)]
    ft = [P.sb(f"ft{i}", [128, 16, 512], BF16) for i in range(2)]; b_ft = [P.buf() for _ in range(2)]
    gtile = [P.sb(f"gtile{i}", [128, 512], F32) for i in range(2)]; b_gtile = [P.buf() for _ in range(2)]
    actT = P.sb("actT", [128, 8, 512], BF16); b_actT = P.buf()
    tg = [P.sb(f"tg{i}", [128, 512], F32) for i in range(2)]; b_tg = [P.buf() for _ in range(2)]
    tsg = [P.sb(f"tsg{i}", [128, 512], F32) for i in range(2)]; b_tsg = [P.buf() for _ in range(2)]
    tu = [P.sb(f"tu{i}", [128, 512], F32) for i in range(2)]; b_tu = [P.buf() for _ in range(2)]
    NYP = 8
    yp = [P.sb(f"yp{i}", [128, 512], F32) for i in range(NYP)]; b_yp = [P.buf() for _ in range(NYP)]
    yo = [P.sb(f"yo{i}", [128, 512], F32) for i in range(4)]; b_yo = [P.buf() for _ in range(4)]
    b_dram = [[P.buf() for _ in E_TILES] for _ in range(16)]
    fT_v = fT.rearrange("(k p) t -> p k t", p=128)
    yT_v = yT.rearrange("(m p) t -> p m t", p=128)
    wsi = 0; fi = 0; oi = 0; mi = 0
    ne = globals().get("MOE_NE", E_PC)
    def load_tile(i):
        ex_, tix_ = divmod(i, len(E_TILES))
        c0_, w_ = E_TILES[tix_]
        fs_ = i % 2
        for kq in range(4):
            P.dma("sp", f"ft{fs_}_{ex_ % 2}", lambda e, fs_=fs_, c0_=c0_, w_=w_, kq=kq: e.dma_start(out=ft[fs_][:, kq*4:(kq+1)*4, 0:w_], in_=fT_v[:, kq*4:(kq+1)*4, c0_:c0_+w_]), writes=[b_ft[fs_]])
        P.dma("sp", f"gtile{fs_}_{ex_ % 2}", lambda e, fs_=fs_, c0_=c0_, w_=w_, ex_=ex_: e.dma_start(out=gtile[fs_][:, 0:w_], in_=gb[ex_, :, c0_:c0_+w_]), writes=[b_gtile[fs_]])

    n_tiles_total = ne * len(E_TILES)
    load_tile(0)
    for ex in range(ne):
        for (src, dst, bdst, nk, ncol) in ((wg, wgb, b_wgb, 16, DE), (wu, wub, b_wub, 16, DE), (wd, wdb, b_wdb, 8, D)):
            sv = src[ex].rearrange("(k p) n -> p k n", p=128)
            for kq in range(nk // 4):
                for cbk in range(ncol // 512):
                    ws_ = wsi % 3; wsi += 1
                    P.dma("sp", f"wst{ws_}_{ex % 2}", lambda e, ws_=ws_, sv=sv, kq=kq, cbk=cbk: e.dma_start(out=wst[ws_][:], in_=sv[:, kq*4:(kq+1)*4, cbk*512:(cbk+1)*512]), writes=[b_wst[ws_]])
                    eng = "pool" if wsi % 2 else "act"
                    if eng == "pool":
                        P.op("pool", lambda e, ws_=ws_, dst=dst, kq=kq, cbk=cbk: e.tensor_copy(out=dst[:, kq*4:(kq+1)*4, cbk*512:(cbk+1)*512], in_=wst[ws_][:]), reads=[b_wst[ws_]], writes=[bdst])
                    else:
                        P.op("act", lambda e, ws_=ws_, dst=dst, kq=kq, cbk=cbk: e.copy(out=dst[:, kq*4:(kq+1)*4, cbk*512:(cbk+1)*512], in_=wst[ws_][:]), reads=[b_wst[ws_]], writes=[bdst])
        for tix, (c0, w) in enumerate(E_TILES):
            fs = fi % 2; fi += 1
            if fi < n_tiles_total:
                load_tile(fi)
            for m in range(8):
                psg, bpsg = nf(); psu, bpsu = nf()
                for k in range(16):
                    P.op("pe", lambda e, psg=psg, k=k, m=m, fs=fs, w=w: e.matmul(psg[:, 0:w], wgb[:, k, m*128:(m+1)*128], ft[fs][:, k, 0:w], start=(k == 0), stop=(k == 15)), reads=[b_wgb, b_ft[fs]], writes=[bpsg])
                for k in range(16):
                    P.op("pe", lambda e, psu=psu, k=k, m=m, fs=fs, w=w: e.matmul(psu[:, 0:w], wub[:, k, m*128:(m+1)*128], ft[fs][:, k, 0:w], start=(k == 0), stop=(k == 15)), reads=[b_wub, b_ft[fs]], writes=[bpsu])
                s = mi % 2; mi += 1
                P.op("dve", lambda e, psg=psg, s=s, m=m, w=w, ex=ex: e.tensor_scalar(out=tg[s][:, 0:w], in0=psg[:, 0:w], scalar1=bgut[:, ex, m:m+1], scalar2=7.0, op0=ALU.add, op1=ALU.min), reads=[bpsg, b_bgu], writes=[b_tg[s]])
                P.op("act", lambda e, s=s, w=w: e.activation(out=tsg[s][:, 0:w], in_=tg[s][:, 0:w], func=AF.Sigmoid, scale=1.702), reads=[b_tg[s]], writes=[b_tsg[s]])
                P.op("dve", lambda e, psu=psu, s=s, m=m, w=w, ex=ex: e.tensor_scalar(out=tu[s][:, 0:w], in0=psu[:, 0:w], scalar1=bgut[:, ex, 8+m:9+m], scalar2=7.0, op0=ALU.add, op1=ALU.min), reads=[bpsu, b_bgu], writes=[b_tu[s]])
                P.op("dve", lambda e, s=s, w=w: e.tensor_scalar(out=tu[s][:, 0:w], in0=tu[s][:, 0:w], scalar1=-7.0, scalar2=1.0, op0=ALU.max, op1=ALU.add), reads=[b_tu[s]], writes=[b_tu[s]])
                P.op("pool", lambda e, s=s, w=w: e.tensor_tensor(out=tg[s][:, 0:w], in0=tg[s][:, 0:w], in1=tsg[s][:, 0:w], op=ALU.mult), reads=[b_tg[s], b_tsg[s]], writes=[b_tg[s]])
                P.op("dve", lambda e, s=s, m=m, w=w: e.tensor_tensor(out=actT[:, m, 0:w], in0=tg[s][:, 0:w], in1=tu[s][:, 0:w], op=ALU.mult), reads=[b_tg[s], b_tu[s]], writes=[b_actT])
            def issue_yp(m2_, tix=tix, c0=c0, w=w, ex=ex):
                ys_ = m2_ % NYP
                P.dma("sp", f"yp{ys_}_{ex % 2}", lambda e, ys_=ys_, m2_=m2_, c0=c0, w=w: e.dma_start(out=yp[ys_][:, 0:w], in_=yT_v[:, m2_, c0:c0+w]), reads=[b_dram[m2_][tix]], writes=[b_yp[ys_]])

            if ex > 0:
                for m2 in range(NYP):
                    issue_yp(m2)
            for m2 in range(16):
                psy, bpsy = nf()
                for k in range(8):
                    P.op("pe", lambda e, psy=psy, k=k, m2=m2, w=w: e.matmul(psy[:, 0:w], wdb[:, k, m2*128:(m2+1)*128], actT[:, k, 0:w], start=(k == 0), stop=(k == 7)), reads=[b_wdb, b_actT], writes=[bpsy])
                os_ = oi % 4; oi += 1
                ys_ = m2 % NYP
                bdr = b_dram[m2][tix]
                P.op("dve", lambda e, psy=psy, os_=os_, m2=m2, w=w, fs=fs, ex=ex: e.scalar_tensor_tensor(out=yo[os_][:, 0:w], in0=psy[:, 0:w], scalar=bdt[:, ex, m2:m2+1], in1=gtile[fs][:, 0:w], op0=ALU.add, op1=ALU.mult),
                     reads=[bpsy, b_bd, b_gtile[fs]], writes=[b_yo[os_]])
                if ex > 0:
                    P.op("dve", lambda e, os_=os_, ys_=ys_, w=w: e.tensor_tensor(out=yo[os_][:, 0:w], in0=yo[os_][:, 0:w], in1=yp[ys_][:, 0:w], op=ALU.add), reads=[b_yo[os_], b_yp[ys_]], writes=[b_yo[os_]])
                P.dma("sp", f"yo{os_}_{ex % 2}", lambda e, os_=os_, m2=m2, c0=c0, w=w: e.dma_start(out=yT_v[:, m2, c0:c0+w], in_=yo[os_][:, 0:w]), reads=[b_yo[os_]], writes=[bdr])
                if ex > 0 and m2 + NYP < 16:
                    issue_yp(m2 + NYP)
    P.emit()
    return nc


def run_moe(fT_shards, gate_shards, prm):
    nc = build_moe()
    fT = np.ascontiguousarray(np.concatenate(fT_shards, axis=1))
    gates = np.concatenate(gate_shards, axis=0)
    in_maps = []
    for c in range(NCORE_E):
        es = slice(E_PC * c, E_PC * (c + 1))
        wgu = prm["w_gate_up"][es]
        bgu = prm["b_gate_up"][es]
        bg = bgu[:, 0::2].reshape(E_PC, 8, 128); bu = bgu[:, 1::2].reshape(E_PC, 8, 128)
        bgu_l = np.ascontiguousarray(np.concatenate([bg, bu], axis=1).transpose(2, 0, 1))
        bd_l = np.ascontiguousarray(prm["b_down"][es].reshape(E_PC, 16, 128).transpose(2, 0, 1))
        gbc = np.ascontiguousarray(np.broadcast_to(gates[:, es].T[:, None, :], (E_PC, 128, T_ALL)))
        in_maps.append({"fT": fT, "gb": gbc, "wg": np.ascontiguousarray(wgu[:, :, 0::2]), "wu": np.ascontiguousarray(wgu[:, :, 1::2]),
                        "wd": prm["w_down"][es], "bgu": bgu_l, "bd": bd_l})
    res = _run(nc, in_maps)
    parts = []
    for cp in range(NCORE):
        parts.append(np.ascontiguousarray(np.stack([res[c]["yT"][:, cp*TOK_PC:(cp+1)*TOK_PC].T for c in range(NCORE_E)], axis=0)))
    return parts


_LAYER_KEYS = ["w_in", "w_out", "s5_a_re", "s5_a_im", "s5_log_step", "s5_b_re", "s5_b_im", "s5_c_re", "s5_c_im",
               "s5_d", "s5_w_glu", "s5_b_glu", "q_norm_w", "k_norm_w", "conv_w", "w_router", "b_router",
               "w_gate_up", "b_gate_up", "w_down", "b_down"]


def _combine_maps(x_shards, parts, g2pair):
    rep = lambda v: np.broadcast_to(v, (128, D))
    g2 = np.ascontiguousarray(np.stack([rep(g2pair[0]), rep(g2pair[1])]))
    return g2


def run_proj2(x_shards, mod_l, w_in_l, parts=None, g2pair=None, project=True):
    combine = parts is not None
    nc = build_proj(combine, project)
    ident = np.eye(128, dtype=np.float32)
    in_maps = []
    for i in range(NCORE):
        m = {"x": x_shards[i], "ident": ident}
        if project:
            m["modc"] = np.ascontiguousarray(np.stack([cols128(mod_l[0, 0:D]), cols128(mod_l[0, D:2*D]), cols128(mod_l[1, 0:D]), cols128(mod_l[1, D:2*D])], axis=-1))
            m["w_in"] = w_in_l
        if combine:
            m["part"] = parts[i]
            m["g2"] = _combine_maps(x_shards, parts, g2pair)
        in_maps.append(m)
    res = _run(nc, in_maps)
    z = [r["z"] for r in res] if project else None
    xo = [r["xo"] for r in res] if combine else None
    return z, xo


def kernel(**inputs):
    inp = {k: np.asarray(v) for k, v in inputs.items()}
    mod = run_mod(inp["c"], inp["c_ctx"], inp["w_mod"], inp["b_mod"])
    x_shards = shard_tokens(inp["x"][0], inp["ctx"][0])
    parts = None
    g2pair = None
    for l in range(2):
        prm = {k: inp[k][l] for k in _LAYER_KEYS}
        z, xo = run_proj2(x_shards, mod[l], prm["w_in"], parts, g2pair)
        if xo is not None:
            x_shards = xo
        zl, zc = unshard_tokens(z)
        z_all = np.concatenate([zc, zl], axis=0)
        ret, s5y, att = run_mix(z_all, prm)
        xmid, fT, gates = run_out(x_shards, z_all, ret, s5y, att, mod[l], prm)
        parts = run_moe(fT, gates, prm)
        x_shards = xmid
        g2pair = (mod[l][0, 5*D:6*D], mod[l][1, 5*D:6*D])
    _, xo = run_proj2(x_shards, None, None, parts, g2pair, project=False)
    lat, _ = unshard_tokens(xo)
    return lat[None].astype(np.float32)
```

```python
import contextlib
import numpy as np
import ml_dtypes
import concourse.bass as bass
import concourse.mybir as mybir
from concourse.bass_utils import run_bass_kernel_spmd

F32 = mybir.dt.float32
BF16 = mybir.dt.bfloat16
I32 = mybir.dt.int32
ALU = mybir.AluOpType
AF = mybir.ActivationFunctionType
AX = mybir.AxisListType


class Buf:
    __slots__ = ("name", "w", "r")

    def __init__(self, name):
        self.name = name
        self.w = None
        self.r = []


class Prog:
    ENG = ("pe", "dve", "act", "pool", "sp")

    def __init__(self, nc):
        self.nc = nc
        self.stream = {e: [] for e in self.ENG}
        self.seen = {e: {} for e in self.ENG}
        self.needed = {e: set() for e in self.ENG}
        self.stack = contextlib.ExitStack()
        self.esem = {e: self.stack.enter_context(nc.semaphore("s_" + e))
                     for e in ("pe", "dve", "act", "pool")}
        self.dsem = {}
        self.dtoks = []
        self.nbuf = 0

    def buf(self, name=None):
        self.nbuf += 1
        return Buf(name or f"b{self.nbuf}")

    def sb(self, name, shape, dt):
        return self.stack.enter_context(self.nc.sbuf_tensor(name, list(shape), dt))

    def ps(self, name, shape, dt):
        return self.stack.enter_context(self.nc.psum_tensor(name, list(shape), dt))

    def _waits(self, eng, reads, writes):
        toks = []
        for b in reads:
            if b.w is not None:
                toks.append(b.w + (True,))
        for b in writes:
            if b.w is not None:
                toks.append(b.w + (False,))
            toks.extend(t + (False,) for t in b.r)
        need = {}
        for kind, src, val, raw in toks:
            if kind == "eng" and src == eng and (not raw or eng == "pe"):
                continue
            key = (kind, src)
            if self.seen[eng].get(key, -1) >= val:
                continue
            if need.get(key, -1) < val:
                need[key] = val
        for key, val in need.items():
            self.seen[eng][key] = val
            if key[0] == "eng":
                self.needed[key[1]].add(val)
        return list(need.items())

    def op(self, eng, fn, reads=(), writes=()):
        waits = self._waits(eng, reads, writes)
        idx = len(self.stream[eng])
        tok = ("eng", eng, idx)
        self.stream[eng].append((waits, fn, None))
        for b in reads:
            b.r.append(tok)
        for b in writes:
            b.w = tok
            b.r = []
        return tok

    def dma(self, q, semkey, fn, reads=(), writes=()):
        waits = self._waits(q, reads, writes)
        if semkey not in self.dsem:
            self.dsem[semkey] = [self.stack.enter_context(self.nc.semaphore("d_" + semkey)), 0]
        self.dsem[semkey][1] += 16
        tok = ("dma", semkey, self.dsem[semkey][1])
        self.stream[q].append((waits, fn, semkey))
        for b in reads:
            b.r.append(tok)
        for b in writes:
            b.w = tok
            b.r = []
        self.dtoks.append(tok)
        return tok

    def finish(self):
        fin = Buf("fin")
        for k, (s, v) in self.dsem.items():
            fin.r.append(("dma", k, v))
        for e in ("pe", "dve", "act", "pool"):
            if self.stream[e]:
                fin.r.append(("eng", e, len(self.stream[e]) - 1))
        waits = self._waits("sp", (), (fin,))
        self.stream["sp"].append((waits, None, None))

    def emit(self):
        nc = self.nc
        self.finish()
        val = {}
        for e in self.ENG:
            c = 0
            for idx in range(len(self.stream[e])):
                if idx in self.needed[e]:
                    c += 1
                    val[(e, idx)] = c
        with nc.Block() as block:
            decos = {"pe": block.tensor, "dve": block.vector, "act": block.scalar,
                     "pool": block.gpsimd, "sp": block.sync}
            for ename in self.ENG:
                items = self.stream[ename]

                def body(e, items=items, ename=ename):
                    for idx, (waits, fn, semkey) in enumerate(items):
                        for (kind, src), v in waits:
                            if kind == "eng":
                                e.wait_ge(self.esem[src], val[(src, v)])
                            else:
                                e.wait_ge(self.dsem[src][0], v)
                        if fn is None:
                            continue
                        ins = fn(e)
                        if semkey is not None:
                            ins.then_inc(self.dsem[semkey][0], 16)
                        elif idx in self.needed[ename]:
                            ins.then_inc(self.esem[ename], 1)

                decos[ename](body)
        self.stack.close()


D = 2048
SEQ = 8192
CTX = 256
NCORE = 8
LAT_PC = SEQ // NCORE
CTX_PC = CTX // NCORE
TOK_PC = LAT_PC + CTX_PC
T_ALL = SEQ + CTX
IN_COLS = 5632
EPS = 1e-6
TILES_PC = [(i * 128, 128) for i in range(8)] + [(1024, 32)]
NPART = 8


def _run(nc, in_maps):
    res = run_bass_kernel_spmd(nc, in_maps, core_ids=list(range(len(in_maps))))
    return res.results


def build_mod():
    nc = bass.Bass("TRN2", target_bir_lowering=False)
    cT = nc.dram_tensor("cT", [128, 16, 2], F32, kind="ExternalInput").ap()
    wm = nc.dram_tensor("wm", [2, 2048, 1536], F32, kind="ExternalInput").ap()
    bm = nc.dram_tensor("bm", [2, 2, 1536], F32, kind="ExternalInput").ap()
    out = nc.dram_tensor("mod", [2, 2, 1536], F32, kind="ExternalOutput").ap()
    P = Prog(nc)
    ct = P.sb("ct", [128, 16, 2], F32); b_ct = P.buf()
    av = P.sb("av", [128, 16, 2], F32); b_av = P.buf()
    bt = P.sb("bt", [2, 2, 1536], F32); b_bt = P.buf()
    ot = P.sb("ot", [2, 2, 1536], F32); b_ot = P.buf()
    NW = 4
    wt = [P.sb(f"wt{i}", [128, 1536], F32) for i in range(NW)]; b_wt = [P.buf() for _ in range(NW)]
    pst = [P.ps(f"ps{i}", [128, 512], F32) for i in range(3)]; b_ps = [P.buf() for _ in range(3)]
    P.dma("sp", "ct", lambda e: e.dma_start(out=ct[:], in_=cT), writes=[b_ct])
    P.dma("sp", "bt", lambda e: e.dma_start(out=bt[:], in_=bm.rearrange("l r n -> r l n")), writes=[b_bt])
    P.op("act", lambda e: e.activation(out=av[:], in_=ct[:], func=AF.Silu), reads=[b_ct], writes=[b_av])
    i = 0
    for l in range(2):
        for k in range(16):
            s = i % NW; i += 1
            P.dma("sp", f"wt{s}", lambda e, s=s, l=l, k=k: e.dma_start(out=wt[s][:], in_=wm[l, k*128:(k+1)*128, :]), writes=[b_wt[s]])
            for n in range(3):
                P.op("pe", lambda e, s=s, n=n, k=k: e.matmul(pst[n][0:2, :], av[:, k, :], wt[s][:, n*512:(n+1)*512], start=(k == 0), stop=(k == 15)),
                     reads=[b_av, b_wt[s]], writes=[b_ps[n]])
        for n in range(3):
            P.op("dve", lambda e, n=n, l=l: e.tensor_tensor(out=ot[:, l, n*512:(n+1)*512], in0=pst[n][0:2, :], in1=bt[:, l, n*512:(n+1)*512], op=ALU.add),
                 reads=[b_ps[n], b_bt], writes=[b_ot])
    P.dma("sp", "ot", lambda e: e.dma_start(out=out.rearrange("l r n -> r l n"), in_=ot[:]), reads=[b_ot])
    P.emit()
    return nc


def run_mod(c, c_ctx, w_mod, b_mod):
    cc = np.stack([c[0], c_ctx], axis=-1)
    cT = np.ascontiguousarray(cc.reshape(16, 128, 2).transpose(1, 0, 2))
    nc = build_mod()
    in_maps = []
    for i in range(NCORE):
        sl = slice(i * 1536, (i + 1) * 1536)
        in_maps.append({"cT": cT, "wm": np.ascontiguousarray(w_mod[:, :, sl]),
                        "bm": np.ascontiguousarray(np.broadcast_to(b_mod[:, None, sl], (2, 2, 1536)))})
    res = _run(nc, in_maps)
    return np.concatenate([r["mod"] for r in res], axis=-1)


def cols128(v):
    return np.ascontiguousarray(v.reshape(16, 128).T)


def build_proj(combine, project=True):
    nc = bass.Bass("TRN2", target_bir_lowering=False)
    x = nc.dram_tensor("x", [TOK_PC, D], F32, kind="ExternalInput").ap()
    ident_d = nc.dram_tensor("ident", [128, 128], F32, kind="ExternalInput").ap()
    P = Prog(nc)
    if project:
        modc = nc.dram_tensor("modc", [128, 16, 4], F32, kind="ExternalInput").ap()
        w_in = nc.dram_tensor("w_in", [D, IN_COLS], F32, kind="ExternalInput").ap()
        z = nc.dram_tensor("z", [TOK_PC, IN_COLS], F32, kind="ExternalOutput").ap()
    if combine:
        part = nc.dram_tensor("part", [NPART, TOK_PC, D], F32, kind="ExternalInput").ap()
        g2 = nc.dram_tensor("g2", [2, 128, D], F32, kind="ExternalInput").ap()
        xo = nc.dram_tensor("xo", [TOK_PC, D], F32, kind="ExternalOutput").ap()
        g2t = P.sb("g2t", [128, 2, D], F32); b_g2 = P.buf()
        P.dma("sp", "g2", lambda e: e.dma_start(out=g2t[:], in_=g2.rearrange("r p d -> p r d")), writes=[b_g2])
        pt = [P.sb(f"pt{i}", [128, D], F32) for i in range(3)]; b_pt = [P.buf() for _ in range(3)]
        acc = P.sb("acc", [128, D], F32); b_acc = P.buf()
    ident = P.sb("identt", [128, 128], F32); b_id = P.buf()
    P.dma("sp", "ident", lambda e: e.dma_start(out=ident[:], in_=ident_d), writes=[b_id])
    xt = [P.sb(f"xt{i}", [128, D], F32) for i in range(2)]; b_xt = [P.buf() for _ in range(2)]
    if project:
        mc = P.sb("mc", [128, 16, 4], F32); b_mc = P.buf()
        P.dma("sp", "mc", lambda e: e.dma_start(out=mc[:], in_=modc), writes=[b_mc])
        P.op("dve", lambda e: e.tensor_scalar(out=mc[:, :, 1], in0=mc[:, :, 1], scalar1=1.0, scalar2=None, op0=ALU.add), reads=[b_mc], writes=[b_mc])
        P.op("dve", lambda e: e.tensor_scalar(out=mc[:, :, 3], in0=mc[:, :, 3], scalar1=1.0, scalar2=None, op0=ALU.add), reads=[b_mc], writes=[b_mc])
        xn = P.sb("xn", [128, D], F32); b_xn = P.buf()
        junk = P.sb("junk", [128, D], BF16); b_junk = P.buf()
        ss = P.sb("ss", [128, 2], F32); b_ss = P.buf()
        xmT = P.sb("xmT", [128, 16, TOK_PC], BF16); b_xmT = [P.buf() for _ in TILES_PC]
        wb = [P.sb(f"wb{i}", [128, 16, 512], BF16) for i in range(2)]; b_wb = [P.buf() for _ in range(2)]
        zt = [P.sb(f"zt{i}", [128, 512], F32) for i in range(4)]; b_zt = [P.buf() for _ in range(4)]
        pst = [P.ps(f"ps{i}", [128, 512], F32) for i in range(8)]; b_ps = [P.buf() for _ in range(8)]
    psi = 0
    for ti, (r0, n) in enumerate(TILES_PC):
        s = ti % 2
        X = xt[s]; bX = b_xt[s]
        P.dma("sp", f"xt{s}", lambda e, X=X, r0=r0, n=n: e.dma_start(out=X[0:n, :], in_=x[r0:r0+n, :]), writes=[bX])
        if combine:
            isctx = 1 if r0 >= LAT_PC else 0
            for c in range(NPART):
                ps_ = c % 3
                P.dma("sp", f"pt{ps_}", lambda e, ps_=ps_, c=c, r0=r0, n=n: e.dma_start(out=pt[ps_][0:n, :], in_=part[c, r0:r0+n, :]), writes=[b_pt[ps_]])
                if c == 0:
                    P.op("pool", lambda e, ps_=ps_, n=n: e.tensor_copy(out=acc[0:n, :], in_=pt[ps_][0:n, :]), reads=[b_pt[ps_]], writes=[b_acc])
                else:
                    eng = "dve" if c % 2 else "pool"
                    P.op(eng, lambda e, ps_=ps_, n=n: e.tensor_tensor(out=acc[0:n, :], in0=acc[0:n, :], in1=pt[ps_][0:n, :], op=ALU.add), reads=[b_pt[ps_], b_acc], writes=[b_acc])
            P.op("dve", lambda e, n=n, isctx=isctx: e.tensor_tensor(out=acc[0:n, :], in0=acc[0:n, :], in1=g2t[0:n, isctx, :], op=ALU.mult), reads=[b_acc, b_g2], writes=[b_acc])
            P.op("dve", lambda e, X=X, n=n: e.tensor_tensor(out=X[0:n, :], in0=X[0:n, :], in1=acc[0:n, :], op=ALU.add), reads=[b_acc, bX], writes=[bX])
            P.dma("sp", f"xo{s}", lambda e, X=X, r0=r0, n=n: e.dma_start(out=xo[r0:r0+n, :], in_=X[0:n, :]), reads=[bX])
        if not project:
            continue
        isctx = 1 if r0 >= LAT_PC else 0
        P.op("act", lambda e, X=X, n=n: e.activation(out=junk[0:n, :], in_=X[0:n, :], func=AF.Square, accum_out=ss[0:n, 0:1]), reads=[bX], writes=[b_junk, b_ss])
        P.op("act", lambda e, n=n: e.activation(out=ss[0:n, 1:2], in_=ss[0:n, 0:1], func=AF.Sqrt, scale=1.0 / D, bias=EPS), reads=[b_ss], writes=[b_ss])
        P.op("dve", lambda e, n=n: e.reciprocal(out=ss[0:n, 1:2], in_=ss[0:n, 1:2]), reads=[b_ss], writes=[b_ss])
        P.op("dve", lambda e, X=X, n=n: e.tensor_scalar(out=xn[0:n, :], in0=X[0:n, :], scalar1=ss[0:n, 1:2], scalar2=None, op0=ALU.mult), reads=[bX, b_ss], writes=[b_xn])
        for kg in range(4):
            pb = psi % 8; psi += 1
            for kk in range(4):
                k = kg * 4 + kk
                P.op("pe", lambda e, pb=pb, kk=kk, k=k, n=n: e.transpose(pst[pb][:, kk*128:kk*128+n], xn[0:n, k*128:(k+1)*128], ident[0:n, 0:n]),
                     reads=[b_xn, b_id], writes=[b_ps[pb]])
            for kk in range(4):
                k = kg * 4 + kk
                if kk % 2 == 0:
                    P.op("dve", lambda e, pb=pb, kk=kk, k=k, n=n, r0=r0, isctx=isctx: e.tensor_scalar(
                        out=xmT[:, k, r0:r0+n], in0=pst[pb][:, kk*128:kk*128+n], scalar1=mc[:, k, 2*isctx+1:2*isctx+2], scalar2=mc[:, k, 2*isctx:2*isctx+1], op0=ALU.mult, op1=ALU.add),
                        reads=[b_ps[pb], b_mc], writes=[b_xmT[ti]])
                else:
                    P.op("act", lambda e, pb=pb, kk=kk, k=k, n=n, r0=r0, isctx=isctx: e.activation(
                        out=xmT[:, k, r0:r0+n], in_=pst[pb][:, kk*128:kk*128+n], func=AF.Identity, scale=mc[:, k, 2*isctx+1:2*isctx+2], bias=mc[:, k, 2*isctx:2*isctx+1]),
                        reads=[b_ps[pb], b_mc], writes=[b_xmT[ti]])
    if project:
        w_v = w_in.rearrange("(k p) n -> p k n", p=128)
        zi = 0
        wsi = 0
        wst = [P.sb(f"wst{i}", [128, 4, 512], F32) for i in range(3)]; b_wst = [P.buf() for _ in range(3)]
        NCB = IN_COLS // 512

        def load_w(cb_):
            nonlocal wsi
            s_ = cb_ % 2
            for kq in range(4):
                ws_ = wsi % 3; wsi += 1
                P.dma("sp", f"wst{ws_}", lambda e, ws_=ws_, cb_=cb_, kq=kq: e.dma_start(out=wst[ws_][:], in_=w_v[:, kq*4:(kq+1)*4, cb_*512:(cb_+1)*512]), writes=[b_wst[ws_]])
                P.op("pool", lambda e, ws_=ws_, s_=s_, kq=kq: e.tensor_copy(out=wb[s_][:, kq*4:(kq+1)*4, :], in_=wst[ws_][:]), reads=[b_wst[ws_]], writes=[b_wb[s_]])

        load_w(0)
        for cb in range(NCB):
            s = cb % 2
            if cb + 1 < NCB:
                load_w(cb + 1)
            for ti, (r0, n) in enumerate(TILES_PC):
                pb = psi % 8; psi += 1
                for k in range(16):
                    P.op("pe", lambda e, pb=pb, k=k, n=n, r0=r0, s=s: e.matmul(pst[pb][0:n, :], xmT[:, k, r0:r0+n], wb[s][:, k, :], start=(k == 0), stop=(k == 15)),
                         reads=[b_xmT[ti], b_wb[s]], writes=[b_ps[pb]])
                zs = zi % 4; zi += 1
                if zi % 2:
                    P.op("dve", lambda e, pb=pb, zs=zs, n=n: e.tensor_copy(out=zt[zs][0:n, :], in_=pst[pb][0:n, :]), reads=[b_ps[pb]], writes=[b_zt[zs]])
                else:
                    P.op("act", lambda e, pb=pb, zs=zs, n=n: e.copy(out=zt[zs][0:n, :], in_=pst[pb][0:n, :]), reads=[b_ps[pb]], writes=[b_zt[zs]])
                P.dma("sp", f"zt{zs}", lambda e, zs=zs, n=n, r0=r0, cb=cb: e.dma_start(out=z[r0:r0+n, cb*512:(cb+1)*512], in_=zt[zs][0:n, :]), reads=[b_zt[zs]])
    P.emit()
    return nc


def shard_tokens(lat, ctx):
    return [np.ascontiguousarray(np.concatenate([lat[i*LAT_PC:(i+1)*LAT_PC], ctx[i*CTX_PC:(i+1)*CTX_PC]], axis=0)) for i in range(NCORE)]


def unshard_tokens(per_core):
    lat = np.concatenate([p[:LAT_PC] for p in per_core], axis=0)
    ctx = np.concatenate([p[LAT_PC:] for p in per_core], axis=0)
    return lat, ctx


def run_proj(x_shards, mod_l, w_in_l, parts=None, project=True):
    combine = parts is not None
    nc = build_proj(combine, project)
    ident = np.eye(128, dtype=np.float32)
    in_maps = []
    for i in range(NCORE):
        m = {"x": x_shards[i], "ident": ident}
        if project:
            sh1, sc1 = mod_l[0, 0:D], mod_l[0, D:2*D]
            csh1, csc1 = mod_l[1, 0:D], mod_l[1, D:2*D]
            m["modc"] = np.ascontiguousarray(np.stack([cols128(sh1), cols128(sc1), cols128(csh1), cols128(csc1)], axis=-1))
            m["w_in"] = w_in_l
        if combine:
            m["part"] = parts[i]
            g2 = np.stack([np.broadcast_to(mod_l_prev_g2[0], (128, D)), np.broadcast_to(mod_l_prev_g2[1], (128, D))])
            m["g2"] = np.ascontiguousarray(g2)
        in_maps.append(m)
    res = _run(nc, in_maps)
    z = [r["z"] for r in res] if project else None
    xo = [r["xo"] for r in res] if combine else None
    return z, xo


NCH = T_ALL // 128
SEG = 384
NSEG = T_ALL // SEG
NQ = 128 + SEQ // 2
NQB = NQ // 128
ATT_SCALE = 128 ** -0.5


def build_mix():
    nc = bass.Bass("TRN2", target_bir_lowering=False)
    di = lambda name, shape, dt=F32: nc.dram_tensor(name, list(shape), dt, kind="ExternalInput").ap()
    do = lambda name, shape, dt=F32: nc.dram_tensor(name, list(shape), dt, kind="ExternalOutput").ap()
    ident_d = di("ident", [128, 128])
    maskT_d = di("maskT", [128, 128])
    r_qkv = di("r_qkv", [T_ALL, 3, 128])
    r_tab = di("r_tab", [T_ALL, 4, 128])
    r_g = di("r_g", [128, 1])
    r_out = do("r_out", [T_ALL, 128])
    s_par = di("s_par", [128, 4, 3])
    s_B = di("s_B", [128, 2, 2, 32])
    s_C = di("s_C", [128, 2, 2, 64])
    s_uT = di("s_uT", [2, 2, 32, T_ALL])
    s_iota = di("s_iota", [128, SEG])
    s_out = do("s_out", [2, T_ALL, 64])
    a_q = di("a_q", [NQ, 128]); a_k = di("a_k", [T_ALL, 128]); a_v = di("a_v", [T_ALL, 128])
    a_qtab = di("a_qtab", [NQ, 2, 128]); a_ktab = di("a_ktab", [T_ALL, 2, 128])
    a_w = di("a_w", [128, 2, 128])
    a_out = do("a_out", [NQ, 128])

    P = Prog(nc)
    NF = 6
    psf = [P.ps(f"psf{i}", [128, 512], F32) for i in range(NF)]; b_psf = [P.buf() for _ in range(NF)]
    psb = [P.ps(f"psb{i}", [128, 512], BF16) for i in range(2)]; b_psb = [P.buf() for _ in range(2)]
    cnt = {"f": 0, "b": 0}

    def nf():
        i = cnt["f"] % NF; cnt["f"] += 1
        return psf[i], b_psf[i]

    def nb():
        i = cnt["b"] % 2; cnt["b"] += 1
        return psb[i], b_psb[i]

    ident = P.sb("identt", [128, 128], F32); b_id = P.buf()
    identb = P.sb("identb", [128, 128], BF16); b_idb = P.buf()
    maskT = P.sb("maskTt", [128, 128], F32); b_mask = P.buf()
    P.dma("sp", "ident", lambda e: e.dma_start(out=ident[:], in_=ident_d), writes=[b_id])
    P.dma("sp", "maskT", lambda e: e.dma_start(out=maskT[:], in_=maskT_d), writes=[b_mask])
    P.op("dve", lambda e: e.tensor_copy(out=identb[:], in_=ident[:]), reads=[b_id], writes=[b_idb])

    def s5_unit():
        par = P.sb("s_par_t", [128, 4, 3], F32); b_par = P.buf()
        Bt = P.sb("s_B_t", [128, 2, 2, 32], F32); b_B = P.buf()
        Ct = P.sb("s_C_t", [128, 2, 2, 64], F32); b_C = P.buf()
        io = P.sb("s_iota_t", [128, SEG], F32); b_io = P.buf()
        P.dma("sp", "s_par", lambda e: e.dma_start(out=par[:], in_=s_par), writes=[b_par])
        P.dma("sp", "s_B", lambda e: e.dma_start(out=Bt[:], in_=s_B), writes=[b_B])
        P.dma("sp", "s_C", lambda e: e.dma_start(out=Ct[:], in_=s_C), writes=[b_C])
        P.dma("sp", "s_iota", lambda e: e.dma_start(out=io[:], in_=s_iota), writes=[b_io])
        P.op("dve", lambda e: e.tensor_scalar(out=Ct[:, :, 1, :], in0=Ct[:, :, 1, :], scalar1=-1.0, scalar2=None, op0=ALU.mult), reads=[b_C], writes=[b_C])
        cs = P.sb("s_cs", [128, 4, SEG], F32); sn = P.sb("s_sn", [128, 4, SEG], F32); rb = P.sb("s_rb", [128, 4, SEG], F32)
        b_tab = [P.buf() for _ in range(4)]
        col = P.sb("s_col", [128, 4, 24], F32); b_col = [P.buf() for _ in range(4)]
        ph = P.sb("s_ph", [128, SEG], F32); b_ph = P.buf()
        ph2 = P.sb("s_ph2", [128, SEG], F32); b_ph2 = P.buf()
        phi = P.sb("s_phi", [128, SEG], I32); b_phi = P.buf()
        BpT = P.sb("s_BpT", [32, 4, 2, 128], F32); b_BpT = [P.buf() for _ in range(4)]
        Bp = P.sb("s_Bp", [128, 2, 32], F32); b_Bp = P.buf()
        tmpB = P.sb("s_tmpB", [128, 32], F32); b_tmpB = P.buf()
        st = P.sb("s_st", [128, 4, 2], F32); b_st = [P.buf() for _ in range(4)]

        def frac_sin(dst, src, bsrc, bdst_list):
            P.op("dve", lambda e: e.tensor_copy(out=phi[:], in_=src), reads=[bsrc], writes=[b_phi])
            P.op("dve", lambda e: e.tensor_tensor(out=ph2[:], in0=src, in1=phi[:], op=ALU.subtract), reads=[bsrc, b_phi], writes=[b_ph2])
            P.op("dve", lambda e: e.tensor_scalar(out=phi[:], in0=ph2[:], scalar1=0.5, scalar2=None, op0=ALU.is_gt), reads=[b_ph2], writes=[b_phi])
            P.op("dve", lambda e: e.tensor_tensor(out=ph2[:], in0=ph2[:], in1=phi[:], op=ALU.subtract), reads=[b_ph2, b_phi], writes=[b_ph2])
            P.op("dve", lambda e: e.tensor_scalar(out=phi[:], in0=ph2[:], scalar1=-0.5, scalar2=None, op0=ALU.is_lt), reads=[b_ph2], writes=[b_phi])
            P.op("dve", lambda e: e.tensor_tensor(out=ph2[:], in0=ph2[:], in1=phi[:], op=ALU.add), reads=[b_ph2, b_phi], writes=[b_ph2])
            P.op("act", lambda e: e.activation(out=dst, in_=ph2[:], func=AF.Sin, scale=2.0 * 3.14159265), reads=[b_ph2], writes=bdst_list)

        for cb in range(4):
            tl = cb % 2
            c_ = lambda j, cb=cb: col[:, cb, j:j+1]
            bc = b_col[cb]
            a_re = par[:, cb, 0:1]; a_im = par[:, cb, 1:2]; lst = par[:, cb, 2:3]
            P.op("act", lambda e, c_=c_, lst=lst: e.activation(out=c_(0), in_=lst, func=AF.Exp), reads=[b_par], writes=[bc])
            P.op("dve", lambda e, c_=c_, a_re=a_re: e.tensor_tensor(out=c_(1), in0=a_re, in1=c_(0), op=ALU.mult), reads=[b_par, bc], writes=[bc])
            P.op("act", lambda e, c_=c_: e.activation(out=c_(2), in_=c_(1), func=AF.Exp), reads=[bc], writes=[bc])
            P.op("dve", lambda e, c_=c_, a_im=a_im: e.tensor_tensor(out=c_(3), in0=a_im, in1=c_(0), op=ALU.mult), reads=[b_par, bc], writes=[bc])
            P.op("dve", lambda e, c_=c_: e.tensor_scalar(out=c_(3), in0=c_(3), scalar1=1.0 / (2.0 * np.pi), scalar2=None, op0=ALU.mult), reads=[bc], writes=[bc])
            P.op("dve", lambda e, c_=c_: e.tensor_scalar(out=ph[:], in0=io[:], scalar1=c_(3), scalar2=None, op0=ALU.mult), reads=[b_io, bc], writes=[b_ph])
            frac_sin(sn[:, cb, :], ph[:], b_ph, [b_tab[cb]])
            P.op("dve", lambda e: e.tensor_scalar(out=ph[:], in0=ph[:], scalar1=0.25, scalar2=None, op0=ALU.add), reads=[b_ph], writes=[b_ph])
            frac_sin(cs[:, cb, :], ph[:], b_ph, [b_tab[cb]])
            P.op("pool", lambda e, cb=cb: e.memset(rb[:, cb, :], 1.0), writes=[b_tab[cb]])
            P.op("dve", lambda e, cb=cb, c_=c_: e.tensor_scalar(out=rb[:, cb, :], in0=rb[:, cb, :], scalar1=c_(2), scalar2=None, op0=ALU.mult), reads=[bc, b_tab[cb]], writes=[b_tab[cb]])
            tt = lambda o, a, b, op, c_=c_: P.op("dve", lambda e: e.tensor_tensor(out=o, in0=a, in1=b, op=op), reads=[bc, b_par, b_tab[cb]], writes=[bc])
            tt(c_(4), c_(2), cs[:, cb, 0:1], ALU.mult)
            tt(c_(5), c_(2), sn[:, cb, 0:1], ALU.mult)
            P.op("dve", lambda e, c_=c_: e.tensor_scalar(out=c_(6), in0=c_(4), scalar1=-1.0, scalar2=None, op0=ALU.add), reads=[bc], writes=[bc])
            tt(c_(7), c_(6), a_re, ALU.mult)
            tt(c_(8), c_(5), a_im, ALU.mult)
            tt(c_(9), c_(7), c_(8), ALU.add)
            tt(c_(10), c_(5), a_re, ALU.mult)
            tt(c_(11), c_(6), a_im, ALU.mult)
            tt(c_(12), c_(10), c_(11), ALU.subtract)
            tt(c_(13), a_re, a_re, ALU.mult)
            tt(c_(14), a_im, a_im, ALU.mult)
            tt(c_(15), c_(13), c_(14), ALU.add)
            P.op("dve", lambda e, c_=c_: e.reciprocal(out=c_(16), in_=c_(15)), reads=[bc], writes=[bc])
            tt(c_(17), c_(9), c_(16), ALU.mult)
            tt(c_(18), c_(12), c_(16), ALU.mult)
            P.op("dve", lambda e, c_=c_, tl=tl: e.tensor_scalar(out=tmpB[:], in0=Bt[:, tl, 1, :], scalar1=c_(18), scalar2=None, op0=ALU.mult), reads=[bc, b_B], writes=[b_tmpB])
            P.op("dve", lambda e, c_=c_, tl=tl: e.scalar_tensor_tensor(out=Bp[:, 0, :], in0=Bt[:, tl, 0, :], scalar=c_(17), in1=tmpB[:], op0=ALU.mult, op1=ALU.subtract), reads=[bc, b_B, b_tmpB], writes=[b_Bp])
            P.op("dve", lambda e, c_=c_, tl=tl: e.tensor_scalar(out=tmpB[:], in0=Bt[:, tl, 0, :], scalar1=c_(18), scalar2=None, op0=ALU.mult), reads=[bc, b_B], writes=[b_tmpB])
            P.op("dve", lambda e, c_=c_, tl=tl: e.scalar_tensor_tensor(out=Bp[:, 1, :], in0=Bt[:, tl, 1, :], scalar=c_(17), in1=tmpB[:], op0=ALU.mult, op1=ALU.add), reads=[bc, b_B, b_tmpB], writes=[b_Bp])
            for ri in range(2):
                pt_, bpt_ = nf()
                P.op("pe", lambda e, pt_=pt_, ri=ri: e.transpose(pt_[0:32, 0:128], Bp[:, ri, :], ident[:]), reads=[b_Bp, b_id], writes=[bpt_])
                P.op("act", lambda e, pt_=pt_, ri=ri, cb=cb: e.copy(out=BpT[:, cb, ri, :], in_=pt_[0:32, 0:128]), reads=[bpt_], writes=[b_BpT[cb]])

        NU = 3
        ut = [P.sb(f"s_ut{i}", [32, SEG], F32) for i in range(NU)]; b_ut = [P.buf() for _ in range(NU)]
        m = [P.sb(f"s_m{i}", [128, SEG], F32) for i in range(4)]; b_m = [P.buf() for _ in range(4)]
        dr = [P.sb(f"s_dr{i}", [128, SEG], F32) for i in range(2)]; b_dr = [P.buf() for _ in range(2)]
        xs_ = [P.sb(f"s_xs{i}", [128, SEG], F32) for i in range(2)]; b_xs = [P.buf() for _ in range(2)]
        xr = [[P.sb(f"s_xr{tl}{ri}", [128, SEG], F32) for ri in range(2)] for tl in range(2)]
        b_xr = [[P.buf() for _ in range(2)] for _ in range(2)]
        ysb = [P.sb(f"s_y{i}", [128, 3, 64], F32) for i in range(2)]; b_ysb = [P.buf() for _ in range(2)]
        ui = 0; yi = 0
        for d in range(2):
            for sg in range(NSEG):
                for tl in range(2):
                    cb = d * 2 + tl
                    us = ui % NU; ui += 1
                    P.dma("sp", f"s_ut{us}", lambda e, us=us, d=d, tl=tl, sg=sg: e.dma_start(out=ut[us][:], in_=s_uT[d, tl, :, sg*SEG:(sg+1)*SEG]), writes=[b_ut[us]])
                    pre, bpre = nf(); pim, bpim = nf()
                    P.op("pe", lambda e, pre=pre, us=us, cb=cb: e.matmul(pre[:, 0:SEG], BpT[:, cb, 0, :], ut[us][:], start=True, stop=True), reads=[b_BpT[cb], b_ut[us]], writes=[bpre])
                    P.op("pe", lambda e, pim=pim, us=us, cb=cb: e.matmul(pim[:, 0:SEG], BpT[:, cb, 1, :], ut[us][:], start=True, stop=True), reads=[b_BpT[cb], b_ut[us]], writes=[bpim])
                    C_ = cs[:, cb, :]; S_ = sn[:, cb, :]
                    P.op("dve", lambda e, pre=pre, C_=C_: e.tensor_tensor(out=m[0][:], in0=pre[:, 0:SEG], in1=C_, op=ALU.mult), reads=[bpre, b_tab[cb]], writes=[b_m[0]])
                    P.op("dve", lambda e, pim=pim, S_=S_: e.tensor_tensor(out=m[1][:], in0=pim[:, 0:SEG], in1=S_, op=ALU.mult), reads=[bpim, b_tab[cb]], writes=[b_m[1]])
                    P.op("dve", lambda e, pim=pim, C_=C_: e.tensor_tensor(out=m[2][:], in0=pim[:, 0:SEG], in1=C_, op=ALU.mult), reads=[bpim, b_tab[cb]], writes=[b_m[2]])
                    P.op("dve", lambda e, pre=pre, S_=S_: e.tensor_tensor(out=m[3][:], in0=pre[:, 0:SEG], in1=S_, op=ALU.mult), reads=[bpre, b_tab[cb]], writes=[b_m[3]])
                    P.op("pool", lambda e: e.tensor_tensor(out=dr[0][:], in0=m[0][:], in1=m[1][:], op=ALU.add), reads=[b_m[0], b_m[1]], writes=[b_dr[0]])
                    P.op("pool", lambda e: e.tensor_tensor(out=dr[1][:], in0=m[2][:], in1=m[3][:], op=ALU.subtract), reads=[b_m[2], b_m[3]], writes=[b_dr[1]])
                    for ri in range(2):
                        init = 0.0 if sg == 0 else st[:, cb, ri:ri+1]
                        P.op("dve", lambda e, ri=ri, init=init, cb=cb: e.tensor_tensor_scan(out=xs_[ri][:], data0=rb[:, cb, :], data1=dr[ri][:], initial=init, op0=ALU.mult, op1=ALU.add),
                             reads=[b_tab[cb], b_dr[ri], b_st[cb]], writes=[b_xs[ri]])
                    P.op("pool", lambda e, C_=C_: e.tensor_tensor(out=m[0][:], in0=xs_[0][:], in1=C_, op=ALU.mult), reads=[b_xs[0], b_tab[cb]], writes=[b_m[0]])
                    P.op("dve", lambda e, S_=S_: e.tensor_tensor(out=m[1][:], in0=xs_[1][:], in1=S_, op=ALU.mult), reads=[b_xs[1], b_tab[cb]], writes=[b_m[1]])
                    P.op("pool", lambda e, S_=S_: e.tensor_tensor(out=m[2][:], in0=xs_[0][:], in1=S_, op=ALU.mult), reads=[b_xs[0], b_tab[cb]], writes=[b_m[2]])
                    P.op("dve", lambda e, C_=C_: e.tensor_tensor(out=m[3][:], in0=xs_[1][:], in1=C_, op=ALU.mult), reads=[b_xs[1], b_tab[cb]], writes=[b_m[3]])
                    P.op("pool", lambda e, tl=tl: e.tensor_tensor(out=xr[tl][0][:], in0=m[0][:], in1=m[1][:], op=ALU.subtract), reads=[b_m[0], b_m[1]], writes=[b_xr[tl][0]])
                    P.op("pool", lambda e, tl=tl: e.tensor_tensor(out=xr[tl][1][:], in0=m[2][:], in1=m[3][:], op=ALU.add), reads=[b_m[2], b_m[3]], writes=[b_xr[tl][1]])
                    for ri in range(2):
                        P.op("act", lambda e, tl=tl, ri=ri, cb=cb: e.copy(out=st[:, cb, ri:ri+1], in_=xr[tl][ri][:, SEG-1:SEG]), reads=[b_xr[tl][ri]], writes=[b_st[cb]])
                ys = yi % 2; yi += 1
                for blk in range(3):
                    py, bpy = nf()
                    j = 0
                    for tl in range(2):
                        for ri in range(2):
                            P.op("pe", lambda e, py=py, tl=tl, ri=ri, blk=blk, j=j: e.matmul(py[:, 0:64], xr[tl][ri][:, blk*128:(blk+1)*128], Ct[:, tl, ri, :], start=(j == 0), stop=(j == 3)),
                                 reads=[b_xr[tl][ri], b_C], writes=[bpy])
                            j += 1
                    P.op("act", lambda e, py=py, ys=ys, blk=blk: e.copy(out=ysb[ys][:, blk, :], in_=py[:, 0:64]), reads=[bpy], writes=[b_ysb[ys]])
                P.dma("sp", f"s_y{ys}", lambda e, ys=ys, d=d, sg=sg: e.dma_start(out=s_out[d, sg*SEG:(sg+1)*SEG, :].rearrange("(b p) c -> p b c", p=128), in_=ysb[ys][:]), reads=[b_ysb[ys]])
                yield

    def ret_unit():
        g = P.sb("r_g_t", [128, 1], F32); b_g = P.buf()
        P.dma("sp", "r_g", lambda e: e.dma_start(out=g[:], in_=r_g), writes=[b_g])
        NB = 2
        qkv = [P.sb(f"r_qkv{i}", [128, 3, 128], F32) for i in range(NB)]; b_qkv = [P.buf() for _ in range(NB)]
        tab = [P.sb(f"r_tab{i}", [128, 4, 128], F32) for i in range(NB)]; b_tb = [P.buf() for _ in range(NB)]
        NS = 2
        sw_l = [P.sb(f"r_sw{i}", [128, 2, 128], F32) for i in range(NS)]; b_sw_l = [P.buf() for _ in range(NS)]
        t1_l = [P.sb(f"r_t1{i}", [128, 2, 128], F32) for i in range(NS)]; b_t1_l = [P.buf() for _ in range(NS)]
        t2_l = [P.sb(f"r_t2{i}", [128, 2, 128], F32) for i in range(NS)]; b_t2_l = [P.buf() for _ in range(NS)]
        qk_l = [P.sb(f"r_qk{i}", [128, 2, 128], BF16) for i in range(NS)]; b_qk_l = [P.buf() for _ in range(NS)]
        qkT_l = [P.sb(f"r_qkT{i}", [128, 2, 128], BF16) for i in range(NS)]; b_qkT_l = [P.buf() for _ in range(NS)]
        vb_l = [P.sb(f"r_vb{i}", [128, 128], BF16) for i in range(NS)]; b_vb_l = [P.buf() for _ in range(NS)]
        sm_l = [P.sb(f"r_sm{i}", [128, 128], BF16) for i in range(NS)]; b_sm_l = [P.buf() for _ in range(NS)]
        S32 = P.sb("r_S32", [128, 128], F32); b_S32 = P.buf()
        Sb = P.sb("r_Sb", [128, 128], BF16); b_Sb = P.buf()
        stt_l = [P.sb(f"r_stt{i}", [128, 6], F32) for i in range(NS)]; b_stt_l = [P.buf() for _ in range(NS)]
        mv_l = [P.sb(f"r_mv{i}", [128, 4], F32) for i in range(NS)]; b_mv_l = [P.buf() for _ in range(NS)]
        yo = [P.sb(f"r_yo{i}", [128, 128], F32) for i in range(2)]; b_yo = [P.buf() for _ in range(2)]
        P.op("pool", lambda e: e.memset(S32[:], 0.0), writes=[b_S32])
        P.op("pool", lambda e: e.memset(Sb[:], 0.0), writes=[b_Sb])
        for n in range(NCH):
            s = n % NB
            Q = qkv[s]; TB = tab[s]
            z_ = n % NS
            sw, t1, t2, qk, qkT, vb, sm, stt, mv = sw_l[z_], t1_l[z_], t2_l[z_], qk_l[z_], qkT_l[z_], vb_l[z_], sm_l[z_], stt_l[z_], mv_l[z_]
            b_sw, b_t1, b_t2, b_qk, b_qkT, b_vb, b_sm, b_stt, b_mv = b_sw_l[z_], b_t1_l[z_], b_t2_l[z_], b_qk_l[z_], b_qkT_l[z_], b_vb_l[z_], b_sm_l[z_], b_stt_l[z_], b_mv_l[z_]
            P.dma("sp", f"r_qkv{s}", lambda e, sw=sw, t1=t1, t2=t2, qk=qk, qkT=qkT, vb=vb, sm=sm, stt=stt, mv=mv, Q=Q, n=n: e.dma_start(out=Q[:], in_=r_qkv[n*128:(n+1)*128]), writes=[b_qkv[s]])
            P.dma("sp", f"r_tab{s}", lambda e, sw=sw, t1=t1, t2=t2, qk=qk, qkT=qkT, vb=vb, sm=sm, stt=stt, mv=mv, TB=TB, n=n: e.dma_start(out=TB[:], in_=r_tab[n*128:(n+1)*128]), writes=[b_tb[s]])
            P.op("pool", lambda e, sw=sw, t1=t1, t2=t2, qk=qk, qkT=qkT, vb=vb, sm=sm, stt=stt, mv=mv, Q=Q: e.tensor_copy(out=sw[:, :, 0:64], in_=Q[:, 0:2, 64:128]), reads=[b_qkv[s]], writes=[b_sw])
            P.op("pool", lambda e, sw=sw, t1=t1, t2=t2, qk=qk, qkT=qkT, vb=vb, sm=sm, stt=stt, mv=mv, Q=Q: e.tensor_copy(out=sw[:, :, 64:128], in_=Q[:, 0:2, 0:64]), reads=[b_qkv[s]], writes=[b_sw])
            TBv = TB[:].rearrange("p (a b) d -> p a b d", b=2)
            P.op("dve", lambda e, sw=sw, t1=t1, t2=t2, qk=qk, qkT=qkT, vb=vb, sm=sm, stt=stt, mv=mv, Q=Q, TBv=TBv: e.tensor_tensor(out=t1[:], in0=Q[:, 0:2, :], in1=TBv[:, :, 0, :], op=ALU.mult), reads=[b_qkv[s], b_tb[s]], writes=[b_t1])
            P.op("pool", lambda e, sw=sw, t1=t1, t2=t2, qk=qk, qkT=qkT, vb=vb, sm=sm, stt=stt, mv=mv, TBv=TBv: e.tensor_tensor(out=t2[:], in0=sw[:], in1=TBv[:, :, 1, :], op=ALU.mult), reads=[b_sw, b_tb[s]], writes=[b_t2])
            P.op("dve", lambda e, sw=sw, t1=t1, t2=t2, qk=qk, qkT=qkT, vb=vb, sm=sm, stt=stt, mv=mv: e.tensor_tensor(out=qk[:], in0=t1[:], in1=t2[:], op=ALU.add), reads=[b_t1, b_t2], writes=[b_qk])
            P.op("act", lambda e, sw=sw, t1=t1, t2=t2, qk=qk, qkT=qkT, vb=vb, sm=sm, stt=stt, mv=mv, Q=Q: e.copy(out=vb[:], in_=Q[:, 2, :]), reads=[b_qkv[s]], writes=[b_vb])
            pt_, bpt_ = nb()
            P.op("pe", lambda e, sw=sw, t1=t1, t2=t2, qk=qk, qkT=qkT, vb=vb, sm=sm, stt=stt, mv=mv, pt_=pt_: e.transpose(pt_[:, 0:128], qk[:, 0, :], identb[:]), reads=[b_qk, b_idb], writes=[bpt_])
            P.op("pe", lambda e, sw=sw, t1=t1, t2=t2, qk=qk, qkT=qkT, vb=vb, sm=sm, stt=stt, mv=mv, pt_=pt_: e.transpose(pt_[:, 128:256], qk[:, 1, :], identb[:]), reads=[b_qk, b_idb], writes=[bpt_])
            P.op("act", lambda e, sw=sw, t1=t1, t2=t2, qk=qk, qkT=qkT, vb=vb, sm=sm, stt=stt, mv=mv, pt_=pt_: e.copy(out=qkT[:].rearrange("p a d -> p (a d)"), in_=pt_[:, 0:256]), reads=[bpt_], writes=[b_qkT])
            ps_s, bps_s = nf()
            P.op("pe", lambda e, sw=sw, t1=t1, t2=t2, qk=qk, qkT=qkT, vb=vb, sm=sm, stt=stt, mv=mv, ps_s=ps_s: e.matmul(ps_s[:, 0:128], qkT[:, 1, :], qkT[:, 0, :], start=True, stop=True), reads=[b_qkT], writes=[bps_s])
            P.op("dve", lambda e, sw=sw, t1=t1, t2=t2, qk=qk, qkT=qkT, vb=vb, sm=sm, stt=stt, mv=mv, ps_s=ps_s: e.tensor_tensor(out=sm[:], in0=ps_s[:, 0:128], in1=maskT[:], op=ALU.mult), reads=[bps_s, b_mask], writes=[b_sm])
            ps_y, bps_y = nf()
            P.op("pe", lambda e, sw=sw, t1=t1, t2=t2, qk=qk, qkT=qkT, vb=vb, sm=sm, stt=stt, mv=mv, ps_y=ps_y: e.matmul(ps_y[:, 0:128], sm[:], vb[:], start=True, stop=False), reads=[b_sm, b_vb], writes=[bps_y])
            P.op("pe", lambda e, sw=sw, t1=t1, t2=t2, qk=qk, qkT=qkT, vb=vb, sm=sm, stt=stt, mv=mv, ps_y=ps_y: e.matmul(ps_y[:, 0:128], qkT[:, 0, :], Sb[:], start=False, stop=True), reads=[b_qkT, b_Sb], writes=[bps_y])
            ps_kv, bps_kv = nf()
            P.op("pe", lambda e, sw=sw, t1=t1, t2=t2, qk=qk, qkT=qkT, vb=vb, sm=sm, stt=stt, mv=mv, ps_kv=ps_kv: e.matmul(ps_kv[:, 0:128], qk[:, 1, :], vb[:], start=True, stop=True), reads=[b_qk, b_vb], writes=[bps_kv])
            P.op("dve", lambda e, sw=sw, t1=t1, t2=t2, qk=qk, qkT=qkT, vb=vb, sm=sm, stt=stt, mv=mv, ps_kv=ps_kv: e.tensor_tensor(out=S32[:], in0=ps_kv[:, 0:128], in1=S32[:], op=ALU.add), reads=[bps_kv, b_S32], writes=[b_S32])
            P.op("dve", lambda e, sw=sw, t1=t1, t2=t2, qk=qk, qkT=qkT, vb=vb, sm=sm, stt=stt, mv=mv: e.tensor_scalar(out=S32[:], in0=S32[:], scalar1=g[:, 0:1], scalar2=None, op0=ALU.mult), reads=[b_S32, b_g], writes=[b_S32])
            P.op("act", lambda e, sw=sw, t1=t1, t2=t2, qk=qk, qkT=qkT, vb=vb, sm=sm, stt=stt, mv=mv: e.copy(out=Sb[:], in_=S32[:]), reads=[b_S32], writes=[b_Sb])
            P.op("dve", lambda e, sw=sw, t1=t1, t2=t2, qk=qk, qkT=qkT, vb=vb, sm=sm, stt=stt, mv=mv, ps_y=ps_y: e.bn_stats(out=stt[:], in_=ps_y[:, 0:128]), reads=[bps_y], writes=[b_stt])
            P.op("dve", lambda e, sw=sw, t1=t1, t2=t2, qk=qk, qkT=qkT, vb=vb, sm=sm, stt=stt, mv=mv: e.bn_aggr(out=mv[:, 0:2], in_=stt[:]), reads=[b_stt], writes=[b_mv])
            P.op("act", lambda e, sw=sw, t1=t1, t2=t2, qk=qk, qkT=qkT, vb=vb, sm=sm, stt=stt, mv=mv: e.activation(out=mv[:, 2:3], in_=mv[:, 1:2], func=AF.Sqrt, bias=EPS, scale=1.0), reads=[b_mv], writes=[b_mv])
            P.op("dve", lambda e, sw=sw, t1=t1, t2=t2, qk=qk, qkT=qkT, vb=vb, sm=sm, stt=stt, mv=mv: e.reciprocal(out=mv[:, 3:4], in_=mv[:, 2:3]), reads=[b_mv], writes=[b_mv])
            ys = n % 2
            P.op("dve", lambda e, sw=sw, t1=t1, t2=t2, qk=qk, qkT=qkT, vb=vb, sm=sm, stt=stt, mv=mv, ps_y=ps_y, ys=ys: e.tensor_scalar(out=yo[ys][:], in0=ps_y[:, 0:128], scalar1=mv[:, 0:1], scalar2=mv[:, 3:4], op0=ALU.subtract, op1=ALU.mult), reads=[bps_y, b_mv], writes=[b_yo[ys]])
            P.dma("sp", f"r_yo{ys}", lambda e, sw=sw, t1=t1, t2=t2, qk=qk, qkT=qkT, vb=vb, sm=sm, stt=stt, mv=mv, ys=ys, n=n: e.dma_start(out=r_out[n*128:(n+1)*128, :], in_=yo[ys][:]), reads=[b_yo[ys]])
            yield

    def att_unit():
        wt = P.sb("a_w_t", [128, 2, 128], F32); b_w = P.buf()
        P.dma("sp", "a_w", lambda e: e.dma_start(out=wt[:], in_=a_w), writes=[b_w])
        kT = P.sb("a_kT", [128, T_ALL], BF16); b_kT = P.buf()
        qT = P.sb("a_qT", [128, NQ], BF16); b_qT = P.buf()
        vb = P.sb("a_vb", [128, NCH, 128], BF16); b_vb = P.buf()
        xin = [P.sb(f"a_xin{i}", [128, 128], F32) for i in range(2)]; b_xin = [P.buf() for _ in range(2)]
        tb = [P.sb(f"a_tb{i}", [128, 2, 128], F32) for i in range(2)]; b_tb = [P.buf() for _ in range(2)]
        NS = 2
        mk = lambda nm, shp, dt: ([P.sb(f"{nm}{i}", shp, dt) for i in range(NS)], [P.buf() for _ in range(NS)])
        junk_l, b_junk_l = mk("a_junk", [128, 128], F32)
        col_l, b_col_l = mk("a_col", [128, 4], F32)
        xn_l, b_xn_l = mk("a_xn", [128, 128], F32)
        sw_l, b_sw_l = mk("a_sw", [128, 128], F32)
        t1_l, b_t1_l = mk("a_t1", [128, 128], F32)
        t2_l, b_t2_l = mk("a_t2", [128, 128], F32)
        xr_l, b_xr_l = mk("a_xr", [128, 128], BF16)
        vst = [P.sb(f"a_vst{i}", [128, 6, 128], F32) for i in range(2)]; b_vst = [P.buf() for _ in range(2)]
        a_v_v = a_v.rearrange("(b p) d -> p b d", p=128)
        for i in range(NCH // 6):
            s = i % 2
            P.dma("sp", f"a_vst{s}", lambda e, s=s, i=i: e.dma_start(out=vst[s][:], in_=a_v_v[:, i*6:(i+1)*6, :]), writes=[b_vst[s]])
            P.op("pool", lambda e, s=s, i=i: e.tensor_copy(out=vb[:, i*6:(i+1)*6, :], in_=vst[s][:]), reads=[b_vst[s]], writes=[b_vb])

        def prep(src, tabsrc, nblk, wi, dstT, b_dstT):
            for blk in range(nblk):
                s = blk % 2
                X = xin[s]; TB = tb[s]
                junk, col, xn, sw, t1, t2, xr = junk_l[s], col_l[s], xn_l[s], sw_l[s], t1_l[s], t2_l[s], xr_l[s]
                b_junk, b_col, b_xn, b_sw, b_t1, b_t2, b_xr = b_junk_l[s], b_col_l[s], b_xn_l[s], b_sw_l[s], b_t1_l[s], b_t2_l[s], b_xr_l[s]
                P.dma("sp", f"a_xin{s}", lambda e, junk=junk, col=col, xn=xn, sw=sw, t1=t1, t2=t2, xr=xr, X=X, blk=blk: e.dma_start(out=X[:], in_=src[blk*128:(blk+1)*128, :]), writes=[b_xin[s]])
                P.dma("sp", f"a_tb{s}", lambda e, junk=junk, col=col, xn=xn, sw=sw, t1=t1, t2=t2, xr=xr, TB=TB, blk=blk: e.dma_start(out=TB[:], in_=tabsrc[blk*128:(blk+1)*128]), writes=[b_tb[s]])
                P.op("act", lambda e, junk=junk, col=col, xn=xn, sw=sw, t1=t1, t2=t2, xr=xr, X=X: e.activation(out=junk[:], in_=X[:], func=AF.Square, accum_out=col[:, 0:1]), reads=[b_xin[s]], writes=[b_junk, b_col])
                P.op("act", lambda e, junk=junk, col=col, xn=xn, sw=sw, t1=t1, t2=t2, xr=xr: e.activation(out=col[:, 1:2], in_=col[:, 0:1], func=AF.Sqrt, scale=1.0 / 128, bias=EPS), reads=[b_col], writes=[b_col])
                P.op("dve", lambda e, junk=junk, col=col, xn=xn, sw=sw, t1=t1, t2=t2, xr=xr: e.reciprocal(out=col[:, 2:3], in_=col[:, 1:2]), reads=[b_col], writes=[b_col])
                P.op("dve", lambda e, junk=junk, col=col, xn=xn, sw=sw, t1=t1, t2=t2, xr=xr, X=X: e.scalar_tensor_tensor(out=xn[:], in0=X[:], scalar=col[:, 2:3], in1=wt[:, wi, :], op0=ALU.mult, op1=ALU.mult), reads=[b_xin[s], b_col, b_w], writes=[b_xn])
                xv = xn[:].rearrange("p (a b d) -> p a b d", a=2, b=2)
                sv = sw[:].rearrange("p (a b d) -> p a b d", a=2, b=2)
                P.op("pool", lambda e, junk=junk, col=col, xn=xn, sw=sw, t1=t1, t2=t2, xr=xr, xv=xv, sv=sv: e.tensor_copy(out=sv[:, :, 0, :], in_=xv[:, :, 1, :]), reads=[b_xn], writes=[b_sw])
                P.op("pool", lambda e, junk=junk, col=col, xn=xn, sw=sw, t1=t1, t2=t2, xr=xr, xv=xv, sv=sv: e.tensor_copy(out=sv[:, :, 1, :], in_=xv[:, :, 0, :]), reads=[b_xn], writes=[b_sw])
                P.op("dve", lambda e, junk=junk, col=col, xn=xn, sw=sw, t1=t1, t2=t2, xr=xr, TB=TB: e.tensor_tensor(out=t1[:], in0=xn[:], in1=TB[:, 0, :], op=ALU.mult), reads=[b_xn, b_tb[s]], writes=[b_t1])
                P.op("pool", lambda e, junk=junk, col=col, xn=xn, sw=sw, t1=t1, t2=t2, xr=xr, TB=TB: e.tensor_tensor(out=t2[:], in0=sw[:], in1=TB[:, 1, :], op=ALU.mult), reads=[b_sw, b_tb[s]], writes=[b_t2])
                P.op("dve", lambda e, junk=junk, col=col, xn=xn, sw=sw, t1=t1, t2=t2, xr=xr: e.tensor_tensor(out=xr[:], in0=t1[:], in1=t2[:], op=ALU.add), reads=[b_t1, b_t2], writes=[b_xr])
                pt_, bpt_ = nb()
                P.op("pe", lambda e, junk=junk, col=col, xn=xn, sw=sw, t1=t1, t2=t2, xr=xr, pt_=pt_: e.transpose(pt_[:, 0:128], xr[:], identb[:]), reads=[b_xr, b_idb], writes=[bpt_])
                P.op("act", lambda e, junk=junk, col=col, xn=xn, sw=sw, t1=t1, t2=t2, xr=xr, pt_=pt_, blk=blk: e.copy(out=dstT[:, blk*128:(blk+1)*128], in_=pt_[:, 0:128]), reads=[bpt_], writes=[b_dstT])
                yield

        yield from prep(a_k, a_ktab, NCH, 1, kT, b_kT)
        yield from prep(a_q, a_qtab, NQB, 0, qT, b_qT)

        Ssb = P.sb("a_S", [128, T_ALL], F32); b_S = P.buf()
        Pb = P.sb("a_P", [128, T_ALL], BF16); b_P = P.buf()
        PT = P.sb("a_PT", [128, NCH, 128], BF16); b_PT = P.buf()
        c2 = P.sb("a_c2", [128, 4], F32); b_c2 = P.buf()
        ob = [P.sb(f"a_o{i}", [128, 128], F32) for i in range(2)]; b_ob = [P.buf() for _ in range(2)]
        for qb in range(NQB):
            nk = CTX if qb == 0 else T_ALL
            nkt = (nk + 511) // 512
            for kt in range(nkt):
                w = min(512, nk - kt * 512)
                ps_, bps_ = nf()
                P.op("pe", lambda e, ps_=ps_, qb=qb, kt=kt, w=w: e.matmul(ps_[:, 0:w], qT[:, qb*128:(qb+1)*128], kT[:, kt*512:kt*512+w], start=True, stop=True), reads=[b_qT, b_kT], writes=[bps_])
                if kt % 2:
                    P.op("dve", lambda e, ps_=ps_, kt=kt, w=w: e.tensor_copy(out=Ssb[:, kt*512:kt*512+w], in_=ps_[:, 0:w]), reads=[bps_], writes=[b_S])
                else:
                    P.op("act", lambda e, ps_=ps_, kt=kt, w=w: e.copy(out=Ssb[:, kt*512:kt*512+w], in_=ps_[:, 0:w]), reads=[bps_], writes=[b_S])
            P.op("dve", lambda e, nk=nk: e.reduce_max(out=c2[:, 0:1], in_=Ssb[:, 0:nk], axis=AX.X), reads=[b_S], writes=[b_c2])
            P.op("dve", lambda e: e.tensor_scalar(out=c2[:, 1:2], in0=c2[:, 0:1], scalar1=-ATT_SCALE, scalar2=None, op0=ALU.mult), reads=[b_c2], writes=[b_c2])
            P.op("act", lambda e, nk=nk: e.activation(out=Pb[:, 0:nk], in_=Ssb[:, 0:nk], func=AF.Exp, scale=ATT_SCALE, bias=c2[:, 1:2], accum_out=c2[:, 2:3]), reads=[b_S, b_c2], writes=[b_P, b_c2])
            P.op("dve", lambda e: e.reciprocal(out=c2[:, 3:4], in_=c2[:, 2:3]), reads=[b_c2], writes=[b_c2])
            nkb = nk // 128
            for g0 in range(0, nkb, 4):
                gn = min(4, nkb - g0)
                pt_, bpt_ = nb()
                for j in range(gn):
                    P.op("pe", lambda e, pt_=pt_, j=j, g0=g0: e.transpose(pt_[:, j*128:(j+1)*128], Pb[:, (g0+j)*128:(g0+j+1)*128], identb[:]), reads=[b_P, b_idb], writes=[bpt_])
                if (g0 // 4) % 2:
                    P.op("dve", lambda e, pt_=pt_, g0=g0, gn=gn: e.tensor_copy(out=PT[:, g0:g0+gn, :].rearrange("p a d -> p (a d)"), in_=pt_[:, 0:gn*128]), reads=[bpt_], writes=[b_PT])
                else:
                    P.op("act", lambda e, pt_=pt_, g0=g0, gn=gn: e.copy(out=PT[:, g0:g0+gn, :].rearrange("p a d -> p (a d)"), in_=pt_[:, 0:gn*128]), reads=[bpt_], writes=[b_PT])
            po, bpo = nf()
            for kb in range(nkb):
                P.op("pe", lambda e, po=po, kb=kb, nkb=nkb: e.matmul(po[:, 0:128], PT[:, kb, :], vb[:, kb, :], start=(kb == 0), stop=(kb == nkb - 1)), reads=[b_PT, b_vb], writes=[bpo])
            os_ = qb % 2
            P.op("dve", lambda e, po=po, os_=os_: e.tensor_scalar(out=ob[os_][:], in0=po[:, 0:128], scalar1=c2[:, 3:4], scalar2=None, op0=ALU.mult), reads=[bpo, b_c2], writes=[b_ob[os_]])
            P.dma("sp", f"a_o{os_}", lambda e, os_=os_, qb=qb: e.dma_start(out=a_out[qb*128:(qb+1)*128, :], in_=ob[os_][:]), reads=[b_ob[os_]])
            yield

    units = globals().get("MIX_UNITS", "rsa")
    gens = []
    if "a" in units:
        gens.append(att_unit())
    if "r" in units:
        gens.append(ret_unit())
    if "s" in units:
        gens.append(s5_unit())
    while gens:
        for g_ in list(gens):
            try:
                next(g_)
            except StopIteration:
                gens.remove(g_)
    P.emit()
    return nc


def _order(d):
    if d == 0:
        return np.arange(T_ALL)
    return np.concatenate([np.arange(CTX)[::-1], CTX + np.arange(SEQ)[::-1]])


_CONST = {}


def mix_consts():
    if _CONST:
        return _CONST
    f64 = np.float64
    log_g = np.log(1.0 - 2.0 ** (-5.0 - np.arange(4, dtype=f64)))
    freqs = 10000.0 ** (-np.arange(0, 128, 2, dtype=f64) / 128)
    rt = {}
    for d in range(2):
        lg = log_g if d == 0 else log_g[::-1]
        order = _order(d)
        isl = order >= CTX
        pos = np.where(isl, order - CTX, 0).astype(f64)
        ang = pos[:, None] * freqs[None, :]
        cos = np.where(isl[:, None], np.cos(ang), 1.0)
        sin = np.where(isl[:, None], np.sin(ang), 0.0)
        cosf = np.concatenate([cos, cos], axis=1)
        sinf = np.concatenate([-sin, sin], axis=1)
        i = (np.arange(T_ALL) % 128).astype(f64)
        for h in range(4):
            gq = np.exp((i + 1.0) * lg[h])[:, None]
            gk = (128.0 ** -0.5) * np.exp(-(i + 1.0) * lg[h])[:, None]
            tab = np.stack([cosf * gq, sinf * gq, cosf * gk, sinf * gk], axis=1).astype(np.float32)
            rt[(h, d)] = (np.ascontiguousarray(tab), np.full((128, 1), np.exp(128.0 * lg[h]), np.float32))
    _CONST["ret"] = rt
    fr = 10000.0 ** (-np.arange(0, 64, 2, dtype=f64) / 64)
    pos = np.arange(SEQ)
    ar = (pos // 64).astype(f64)[:, None] * fr[None, :]
    ac = (pos % 64).astype(f64)[:, None] * fr[None, :]
    cosl = np.concatenate([np.cos(ar), np.cos(ar), np.cos(ac), np.cos(ac)], axis=1)
    sinl = np.concatenate([-np.sin(ar), np.sin(ar), -np.sin(ac), np.sin(ac)], axis=1)
    cosa = np.concatenate([np.ones((CTX, 128)), cosl], axis=0)
    sina = np.concatenate([np.zeros((CTX, 128)), sinl], axis=0)
    _CONST["att"] = np.ascontiguousarray(np.stack([cosa, sina], axis=1).astype(np.float32))
    _CONST["ident"] = np.eye(128, dtype=np.float32)
    _CONST["maskT"] = np.triu(np.ones((128, 128), np.float32))
    _CONST["iota"] = np.ascontiguousarray(np.broadcast_to(np.arange(1, SEG + 1, dtype=np.float32), (128, SEG)))
    return _CONST


def run_mix(z_all, prm):
    C = mix_consts()
    nc = build_mix()
    in_maps = []
    orders = [_order(0), _order(1)]
    for c in range(NCORE):
        m = {"ident": C["ident"], "maskT": C["maskT"]}
        h, d = c % 4, c // 4
        zo = z_all[orders[d]]
        m["r_qkv"] = np.ascontiguousarray(np.stack([zo[:, h*128:(h+1)*128], zo[:, 512+h*128:512+(h+1)*128], zo[:, 1024+h*128:1024+(h+1)*128]], axis=1))
        m["r_tab"], m["r_g"] = C["ret"][(h, d)]
        par = np.zeros((128, 4, 3), np.float32)
        sB = np.zeros((128, 2, 2, 32), np.float32)
        sC = np.zeros((128, 2, 2, 64), np.float32)
        uT = np.zeros((2, 2, 32, T_ALL), np.float32)
        for tl in range(2):
            for gi in range(2):
                g = 4 * c + 2 * tl + gi
                rows = slice(gi * 64, (gi + 1) * 64)
                for dd in range(2):
                    par[rows, dd*2+tl, 0] = prm["s5_a_re"][dd, g]
                    par[rows, dd*2+tl, 1] = prm["s5_a_im"][dd, g]
                    par[rows, dd*2+tl, 2] = prm["s5_log_step"][dd, g]
                sB[rows, tl, 0, gi*16:(gi+1)*16] = prm["s5_b_re"][g]
                sB[rows, tl, 1, gi*16:(gi+1)*16] = prm["s5_b_im"][g]
                sC[rows, tl, 0, tl*32+gi*16:tl*32+(gi+1)*16] = prm["s5_c_re"][g].T
                sC[rows, tl, 1, tl*32+gi*16:tl*32+(gi+1)*16] = prm["s5_c_im"][g].T
            for dd in range(2):
                ucols = z_all[orders[dd], 2560 + (4*c + 2*tl) * 16: 2560 + (4*c + 2*tl + 2) * 16]
                uT[dd, tl] = ucols.T
        m["s_par"], m["s_B"], m["s_C"], m["s_uT"], m["s_iota"] = par, sB, sC, uT, C["iota"]
        hq, half = c // 2, c % 2
        kvh = hq // 2
        A0 = 3072
        qsel = np.concatenate([np.arange(half*128, (half+1)*128), CTX + np.arange(half*4096, (half+1)*4096)])
        m["a_q"] = np.ascontiguousarray(z_all[qsel, A0 + hq*128: A0 + (hq+1)*128])
        m["a_k"] = np.ascontiguousarray(z_all[:, A0 + 512 + kvh*128: A0 + 512 + (kvh+1)*128])
        m["a_v"] = np.ascontiguousarray(z_all[:, A0 + 768 + kvh*128: A0 + 768 + (kvh+1)*128])
        m["a_qtab"] = np.ascontiguousarray(C["att"][qsel])
        m["a_ktab"] = C["att"]
        m["a_w"] = np.ascontiguousarray(np.stack([np.broadcast_to(prm["q_norm_w"], (128, 128)), np.broadcast_to(prm["k_norm_w"], (128, 128))], axis=1))
        in_maps.append(m)
    res = _run(nc, in_maps)
    ret = np.zeros((2, T_ALL, 512), np.float32)
    s5y = np.zeros((2, T_ALL, 512), np.float32)
    att = np.zeros((T_ALL, 512), np.float32)
    for c in range(NCORE):
        h, d = c % 4, c // 4
        if "r_out" in res[c]:
            ret[d][orders[d], h*128:(h+1)*128] = res[c]["r_out"]
        for dd in range(2):
            s5y[dd][orders[dd], c*64:(c+1)*64] = res[c]["s_out"][dd]
        hq, half = c // 2, c % 2
        qsel = np.concatenate([np.arange(half*128, (half+1)*128), CTX + np.arange(half*4096, (half+1)*4096)])
        att[qsel, hq*128:(hq+1)*128] = res[c]["a_out"]
    return ret, s5y, att


def build_out():
    nc = bass.Bass("TRN2", target_bir_lowering=False)
    di = lambda name, shape, dt=F32: nc.dram_tensor(name, list(shape), dt, kind="ExternalInput").ap()
    do = lambda name, shape, dt=F32: nc.dram_tensor(name, list(shape), dt, kind="ExternalOutput").ap()
    ident_d = di("ident", [128, 128])
    x = di("x", [TOK_PC, D])
    rg = di("rg", [TOK_PC, 4, 512])
    s5 = di("s5", [TOK_PC, 3, 512])
    att = di("att", [TOK_PC, 512])
    cv = di("cv", [TOK_PC, 7, 512])
    vecs = di("vecs", [128, 5, 512])
    w_glu = di("w_glu", [512, 512])
    w_out = di("w_out", [D, D])
    g1 = di("g1", [2, 128, D])
    modc = di("modc", [128, 16, 4])
    w_r = di("w_r", [128, 16, 32])
    b_r = di("b_r", [128, 32])
    xmid = do("xmid", [TOK_PC, D])
    fT = do("fT", [D, TOK_PC], BF16)
    gates = do("gates", [TOK_PC, 32])

    P = Prog(nc)
    NF = 5
    psf = [P.ps(f"psf{i}", [128, 512], F32) for i in range(NF)]; b_psf = [P.buf() for _ in range(NF)]
    psb = [P.ps(f"psb{i}", [128, 512], BF16) for i in range(2)]; b_psb = [P.buf() for _ in range(2)]
    cnt = {"f": 0, "b": 0}

    def nf():
        i = cnt["f"] % NF; cnt["f"] += 1
        return psf[i], b_psf[i]

    def nb():
        i = cnt["b"] % 2; cnt["b"] += 1
        return psb[i], b_psb[i]

    ident = P.sb("identt", [128, 128], F32); b_id = P.buf()
    identb = P.sb("identb", [128, 128], BF16); b_idb = P.buf()
    P.dma("sp", "ident", lambda e: e.dma_start(out=ident[:], in_=ident_d), writes=[b_id])
    P.op("dve", lambda e: e.tensor_copy(out=identb[:], in_=ident[:]), reads=[b_id], writes=[b_idb])
    vt = P.sb("vecs_t", [128, 5, 512], F32); b_vt = P.buf()
    P.dma("sp", "vecs", lambda e: e.dma_start(out=vt[:], in_=vecs), writes=[b_vt])
    g1t = P.sb("g1t", [128, 2, D], F32); b_g1 = P.buf()
    P.dma("sp", "g1", lambda e: e.dma_start(out=g1t[:], in_=g1.rearrange("r p d -> p r d")), writes=[b_g1])
    mc = P.sb("mc", [128, 16, 4], F32); b_mc = P.buf()
    P.dma("sp", "mc", lambda e: e.dma_start(out=mc[:], in_=modc), writes=[b_mc])
    P.op("dve", lambda e: e.tensor_scalar(out=mc[:, :, 1], in0=mc[:, :, 1], scalar1=1.0, scalar2=None, op0=ALU.add), reads=[b_mc], writes=[b_mc])
    P.op("dve", lambda e: e.tensor_scalar(out=mc[:, :, 3], in0=mc[:, :, 3], scalar1=1.0, scalar2=None, op0=ALU.add), reads=[b_mc], writes=[b_mc])
    wr = P.sb("wr", [128, 16, 32], F32); b_wr = P.buf()
    P.dma("sp", "wr", lambda e: e.dma_start(out=wr[:], in_=w_r), writes=[b_wr])
    brt = P.sb("brt", [128, 32], F32); b_br = P.buf()
    P.dma("sp", "brt", lambda e: e.dma_start(out=brt[:], in_=b_r), writes=[b_br])
    wst = [P.sb(f"wst{i}", [128, 4, 512], F32) for i in range(2)]; b_wst = [P.buf() for _ in range(2)]
    wg = P.sb("wg", [128, 4, 512], BF16); b_wg = P.buf()
    P.dma("sp", "wst0", lambda e: e.dma_start(out=wst[0][:], in_=w_glu.rearrange("(k p) n -> p k n", p=128)), writes=[b_wst[0]])
    P.op("pool", lambda e: e.tensor_copy(out=wg[:], in_=wst[0][:]), reads=[b_wst[0]], writes=[b_wg])

    mixT = P.sb("mixT", [128, 16, TOK_PC], BF16); b_mixT = [P.buf() for _ in TILES_PC]
    rgt = P.sb("rgt", [128, 4, 512], F32); b_rg = P.buf()
    s5t = P.sb("s5t", [128, 3, 512], F32); b_s5 = P.buf()
    att_t = P.sb("att_t", [128, 512], F32); b_att = P.buf()
    cvt = P.sb("cvt", [128, 7, 512], F32); b_cv = P.buf()
    tm = [P.sb(f"tm{i}", [128, 512], F32) for i in range(5)]; b_tm = [P.buf() for _ in range(5)]
    yb16 = P.sb("yb16", [128, 512], BF16); b_yb16 = P.buf()
    yT = P.sb("yT", [128, 4, 128], BF16); b_yT = P.buf()
    mix = P.sb("mix", [128, D], BF16); b_mix = [P.buf() for _ in range(4)]

    def tt(eng, o, a, b, op, rd, wr_):
        P.op(eng, lambda e: e.tensor_tensor(out=o, in0=a, in1=b, op=op), reads=rd, writes=wr_)

    for ti, (r0, n) in enumerate(TILES_PC):
        P.dma("sp", "rgt", lambda e, r0=r0, n=n: e.dma_start(out=rgt[0:n], in_=rg[r0:r0+n]), writes=[b_rg])
        P.dma("sp", "s5t", lambda e, r0=r0, n=n: e.dma_start(out=s5t[0:n], in_=s5[r0:r0+n]), writes=[b_s5])
        P.dma("sp", "att_t", lambda e, r0=r0, n=n: e.dma_start(out=att_t[0:n], in_=att[r0:r0+n]), writes=[b_att])
        P.dma("sp", "cvt", lambda e, r0=r0, n=n: e.dma_start(out=cvt[0:n], in_=cv[r0:r0+n]), writes=[b_cv])
        P.op("act", lambda e, n=n: e.activation(out=tm[0][0:n], in_=rgt[0:n, 2, :], func=AF.Silu), reads=[b_rg], writes=[b_tm[0]])
        P.op("act", lambda e, n=n: e.activation(out=tm[1][0:n], in_=rgt[0:n, 3, :], func=AF.Silu), reads=[b_rg], writes=[b_tm[1]])
        tt("dve", tm[0][0:n], tm[0][0:n], rgt[0:n, 0, :], ALU.mult, [b_tm[0], b_rg], [b_tm[0]])
        tt("pool", tm[1][0:n], tm[1][0:n], rgt[0:n, 1, :], ALU.mult, [b_tm[1], b_rg], [b_tm[1]])
        tt("dve", mix[0:n, 0:512], tm[0][0:n], tm[1][0:n], ALU.add, [b_tm[0], b_tm[1]], [b_mix[0]])
        tt("pool", tm[2][0:n], s5t[0:n, 0, :], s5t[0:n, 1, :], ALU.add, [b_s5], [b_tm[2]])
        tt("dve", tm[3][0:n], s5t[0:n, 2, :], vt[0:n, 0, :], ALU.mult, [b_s5, b_vt], [b_tm[3]])
        tt("pool", tm[2][0:n], tm[2][0:n], tm[3][0:n], ALU.add, [b_tm[2], b_tm[3]], [b_tm[2]])
        tt("pool", tm[3][0:n], tm[2][0:n], tm[2][0:n], ALU.mult, [b_tm[2]], [b_tm[3]])
        P.op("dve", lambda e, n=n: e.tensor_scalar(out=tm[3][0:n], in0=tm[3][0:n], scalar1=0.044715, scalar2=1.0, op0=ALU.mult, op1=ALU.add), reads=[b_tm[3]], writes=[b_tm[3]])
        tt("dve", tm[3][0:n], tm[3][0:n], tm[2][0:n], ALU.mult, [b_tm[3], b_tm[2]], [b_tm[3]])
        P.op("act", lambda e, n=n: e.activation(out=tm[3][0:n], in_=tm[3][0:n], func=AF.Sigmoid, scale=1.5957691216057308), reads=[b_tm[3]], writes=[b_tm[3]])
        tt("dve", tm[2][0:n], tm[2][0:n], tm[3][0:n], ALU.mult, [b_tm[2], b_tm[3]], [b_tm[2]])
        P.op("act", lambda e, n=n: e.copy(out=yb16[0:n], in_=tm[2][0:n]), reads=[b_tm[2]], writes=[b_yb16])
        pt_, bpt_ = nb()
        for k in range(4):
            P.op("pe", lambda e, pt_=pt_, k=k, n=n: e.transpose(pt_[:, k*128:k*128+n], yb16[0:n, k*128:(k+1)*128], identb[0:n, 0:n]), reads=[b_yb16, b_idb], writes=[bpt_])
        P.op("act", lambda e, pt_=pt_, n=n: e.copy(out=yT[:, :, 0:n], in_=pt_[:, 0:512].rearrange("p (a d) -> p a d", a=4)[:, :, 0:n]), reads=[bpt_], writes=[b_yT])
        pg, bpg = nf()
        for k in range(4):
            P.op("pe", lambda e, pg=pg, k=k, n=n: e.matmul(pg[0:n, :], yT[:, k, 0:n], wg[:, k, :], start=(k == 0), stop=(k == 3)), reads=[b_yT, b_wg], writes=[bpg])
        tt("dve", tm[3][0:n], pg[0:n, :], vt[0:n, 1, :], ALU.add, [bpg, b_vt], [b_tm[3]])
        P.op("act", lambda e, n=n: e.activation(out=tm[3][0:n], in_=tm[3][0:n], func=AF.Sigmoid), reads=[b_tm[3]], writes=[b_tm[3]])
        tt("dve", mix[0:n, 512:1024], tm[2][0:n], tm[3][0:n], ALU.mult, [b_tm[2], b_tm[3]], [b_mix[1]])
        P.op("act", lambda e, n=n: e.copy(out=mix[0:n, 1024:1536], in_=att_t[0:n]), reads=[b_att], writes=[b_mix[2]])
        tt("pool", tm[0][0:n], cvt[0:n, 1, :], cvt[0:n, 2, :], ALU.mult, [b_cv], [b_tm[0]])
        tt("dve", tm[1][0:n], cvt[0:n, 3, :], cvt[0:n, 4, :], ALU.mult, [b_cv], [b_tm[1]])
        tt("pool", tm[4][0:n], cvt[0:n, 5, :], cvt[0:n, 6, :], ALU.mult, [b_cv], [b_tm[4]])
        tt("dve", tm[0][0:n], tm[0][0:n], vt[0:n, 2, :], ALU.mult, [b_tm[0], b_vt], [b_tm[0]])
        tt("pool", tm[1][0:n], tm[1][0:n], vt[0:n, 3, :], ALU.mult, [b_tm[1], b_vt], [b_tm[1]])
        tt("dve", tm[4][0:n], tm[4][0:n], vt[0:n, 4, :], ALU.mult, [b_tm[4], b_vt], [b_tm[4]])
        tt("pool", tm[0][0:n], tm[0][0:n], tm[1][0:n], ALU.add, [b_tm[0], b_tm[1]], [b_tm[0]])
        tt("dve", tm[0][0:n], tm[0][0:n], tm[4][0:n], ALU.add, [b_tm[0], b_tm[4]], [b_tm[0]])
        tt("dve", mix[0:n, 1536:2048], tm[0][0:n], cvt[0:n, 0, :], ALU.mult, [b_tm[0], b_cv], [b_mix[3]])
        for kg in range(4):
            pt_, bpt_ = nb()
            for kk in range(4):
                k = kg * 4 + kk
                P.op("pe", lambda e, pt_=pt_, kk=kk, k=k, n=n: e.transpose(pt_[:, kk*128:kk*128+n], mix[0:n, k*128:(k+1)*128], identb[0:n, 0:n]), reads=[b_mix[kg], b_idb], writes=[bpt_])
            eng = "act" if kg % 2 else "dve"
            if eng == "act":
                P.op("act", lambda e, pt_=pt_, kg=kg, n=n, r0=r0: e.copy(out=mixT[:, kg*4:(kg+1)*4, r0:r0+n], in_=pt_[:, 0:512].rearrange("p (a d) -> p a d", a=4)[:, :, 0:n]), reads=[bpt_], writes=[b_mixT[ti]])
            else:
                P.op("dve", lambda e, pt_=pt_, kg=kg, n=n, r0=r0: e.tensor_copy(out=mixT[:, kg*4:(kg+1)*4, r0:r0+n], in_=pt_[:, 0:512].rearrange("p (a d) -> p a d", a=4)[:, :, 0:n]), reads=[bpt_], writes=[b_mixT[ti]])

    wb = [P.sb(f"wb{i}", [128, 16, 512], BF16) for i in range(2)]; b_wb = [P.buf() for _ in range(2)]
    xp = [P.sb(f"xp{i}", [128, 512], F32) for i in range(3)]; b_xp = [P.buf() for _ in range(3)]
    b_xm_dram = [P.buf() for _ in TILES_PC]
    w_v = w_out.rearrange("(k p) n -> p k n", p=128)
    wsi = 1; xi = 0
    def load_wo(cb_):
        nonlocal wsi
        s_ = cb_ % 2
        for kq in range(4):
            ws_ = wsi % 2; wsi += 1
            P.dma("sp", f"wst{ws_}", lambda e, ws_=ws_, cb_=cb_, kq=kq: e.dma_start(out=wst[ws_][:], in_=w_v[:, kq*4:(kq+1)*4, cb_*512:(cb_+1)*512]), writes=[b_wst[ws_]])
            P.op("pool", lambda e, ws_=ws_, s_=s_, kq=kq: e.tensor_copy(out=wb[s_][:, kq*4:(kq+1)*4, :], in_=wst[ws_][:]), reads=[b_wst[ws_]], writes=[b_wb[s_]])

    load_wo(0)
    for cb in range(4):
        s = cb % 2
        if cb + 1 < 4:
            load_wo(cb + 1)
        for ti, (r0, n) in enumerate(TILES_PC):
            isctx = 1 if r0 >= LAT_PC else 0
            xs_ = xi % 3; xi += 1
            P.dma("sp", f"xp{xs_}", lambda e, xs_=xs_, r0=r0, n=n, cb=cb: e.dma_start(out=xp[xs_][0:n], in_=x[r0:r0+n, cb*512:(cb+1)*512]), writes=[b_xp[xs_]])
            po, bpo = nf()
            for k in range(16):
                P.op("pe", lambda e, po=po, k=k, n=n, r0=r0, s=s: e.matmul(po[0:n, :], mixT[:, k, r0:r0+n], wb[s][:, k, :], start=(k == 0), stop=(k == 15)), reads=[b_mixT[ti], b_wb[s]], writes=[bpo])
            t_ = tm[xi % 2]; bt_ = b_tm[xi % 2]
            tt("dve", t_[0:n], po[0:n, :], g1t[0:n, isctx, cb*512:(cb+1)*512], ALU.mult, [bpo, b_g1], [bt_])
            tt("pool", xp[xs_][0:n], xp[xs_][0:n], t_[0:n], ALU.add, [b_xp[xs_], bt_], [b_xp[xs_]])
            P.dma("sp", f"xpo{xs_}", lambda e, xs_=xs_, r0=r0, n=n, cb=cb: e.dma_start(out=xmid[r0:r0+n, cb*512:(cb+1)*512], in_=xp[xs_][0:n]), reads=[b_xp[xs_]], writes=[b_xm_dram[ti]])

    xt = [P.sb(f"xt{i}", [128, D], F32) for i in range(1)]; b_xt = [P.buf() for _ in range(1)]
    xn = P.sb("xn", [128, D], F32); b_xn = P.buf()
    junk = mix
    ss = P.sb("ss", [128, 2], F32); b_ss = P.buf()
    f32t = P.sb("f32t", [128, 16, 128], F32); b_f32 = P.buf()
    lg = P.sb("lg", [128, 32], F32); b_lg = P.buf()
    rc = P.sb("rc", [128, 16], F32); b_rc = P.buf()
    ex = P.sb("ex", [128, 32], F32); b_ex = P.buf()
    gt = [P.sb(f"gt{i}", [128, 32], F32) for i in range(2)]; b_gt = [P.buf() for _ in range(2)]
    fT_v = fT.rearrange("(k p) t -> p k t", p=128)
    for ti, (r0, n) in enumerate(TILES_PC):
        s = ti % 2
        isctx = 1 if r0 >= LAT_PC else 0
        X = xt[0]; bX = b_xt[0]
        P.dma("sp", "xt0", lambda e, X=X, r0=r0, n=n: e.dma_start(out=X[0:n, :], in_=xmid[r0:r0+n, :]), reads=[b_xm_dram[ti]], writes=[bX])
        P.op("act", lambda e, X=X, n=n: e.activation(out=junk[0:n, :], in_=X[0:n, :], func=AF.Square, accum_out=ss[0:n, 0:1]), reads=[bX], writes=b_mix + [b_ss])
        P.op("act", lambda e, n=n: e.activation(out=ss[0:n, 1:2], in_=ss[0:n, 0:1], func=AF.Sqrt, scale=1.0 / D, bias=EPS), reads=[b_ss], writes=[b_ss])
        P.op("dve", lambda e, n=n: e.reciprocal(out=ss[0:n, 1:2], in_=ss[0:n, 1:2]), reads=[b_ss], writes=[b_ss])
        P.op("dve", lambda e, X=X, n=n: e.tensor_scalar(out=xn[0:n, :], in0=X[0:n, :], scalar1=ss[0:n, 1:2], scalar2=None, op0=ALU.mult), reads=[bX, b_ss], writes=[b_xn])
        for kg in range(4):
            pt_, bpt_ = nf()
            for kk in range(4):
                k = kg * 4 + kk
                P.op("pe", lambda e, pt_=pt_, kk=kk, k=k, n=n: e.transpose(pt_[:, kk*128:kk*128+n], xn[0:n, k*128:(k+1)*128], ident[0:n, 0:n]), reads=[b_xn, b_id], writes=[bpt_])
            for kk in range(4):
                k = kg * 4 + kk
                if kk % 2 == 0:
                    P.op("dve", lambda e, pt_=pt_, kk=kk, k=k, n=n, isctx=isctx: e.tensor_scalar(
                        out=f32t[:, k, 0:n], in0=pt_[:, kk*128:kk*128+n], scalar1=mc[:, k, 2*isctx+1:2*isctx+2], scalar2=mc[:, k, 2*isctx:2*isctx+1], op0=ALU.mult, op1=ALU.add),
                        reads=[bpt_, b_mc], writes=[b_f32])
                else:
                    P.op("act", lambda e, pt_=pt_, kk=kk, k=k, n=n, isctx=isctx: e.activation(
                        out=f32t[:, k, 0:n], in_=pt_[:, kk*128:kk*128+n], func=AF.Identity, scale=mc[:, k, 2*isctx+1:2*isctx+2], bias=mc[:, k, 2*isctx:2*isctx+1]),
                        reads=[bpt_, b_mc], writes=[b_f32])
        P.op("pool", lambda e, n=n, r0=r0: e.tensor_copy(out=mixT[:, :, r0:r0+n], in_=f32t[:, :, 0:n]), reads=[b_f32], writes=[b_mixT[ti]])
        pl, bpl = nf()
        for k in range(16):
            P.op("pe", lambda e, pl=pl, k=k, n=n: e.matmul(pl[0:n, 0:32], f32t[:, k, 0:n], wr[:, k, :], start=(k == 0), stop=(k == 15)), reads=[b_f32, b_wr], writes=[bpl])
        tt("dve", lg[0:n], pl[0:n, 0:32], brt[0:n], ALU.add, [bpl, b_br], [b_lg])
        P.op("dve", lambda e, n=n: e.max(out=rc[0:n, 0:8], in_=lg[0:n]), reads=[b_lg], writes=[b_rc])
        P.op("dve", lambda e, n=n: e.tensor_scalar(out=rc[0:n, 8:9], in0=rc[0:n, 0:1], scalar1=-1.0, scalar2=None, op0=ALU.mult), reads=[b_rc], writes=[b_rc])
        P.op("act", lambda e, n=n: e.activation(out=ex[0:n], in_=lg[0:n], func=AF.Exp, bias=rc[0:n, 8:9], scale=1.0), reads=[b_lg, b_rc], writes=[b_ex])
        P.op("dve", lambda e, n=n: e.tensor_scalar(out=lg[0:n], in0=lg[0:n], scalar1=rc[0:n, 3:4], scalar2=None, op0=ALU.is_ge), reads=[b_lg, b_rc], writes=[b_lg])
        tt("dve", ex[0:n], ex[0:n], lg[0:n], ALU.mult, [b_ex, b_lg], [b_ex])
        P.op("dve", lambda e, n=n: e.reduce_sum(out=rc[0:n, 9:10], in_=ex[0:n], axis=AX.X), reads=[b_ex], writes=[b_rc])
        P.op("dve", lambda e, n=n: e.reciprocal(out=rc[0:n, 10:11], in_=rc[0:n, 9:10]), reads=[b_rc], writes=[b_rc])
        P.op("dve", lambda e, n=n, s=s: e.tensor_scalar(out=gt[s][0:n], in0=ex[0:n], scalar1=rc[0:n, 10:11], scalar2=None, op0=ALU.mult), reads=[b_ex, b_rc], writes=[b_gt[s]])
        P.dma("sp", f"gto{s}", lambda e, s=s, n=n, r0=r0: e.dma_start(out=gates[r0:r0+n, :], in_=gt[s][0:n]), reads=[b_gt[s]])
    for k in range(16):
        P.dma("sp", "f16o", lambda e, k=k: e.dma_start(out=fT_v[:, k, :], in_=mixT[:, k, :]), reads=b_mixT)
    P.emit()
    return nc


def _shift(a, k):
    out = np.zeros_like(a)
    if k == -1:
        out[1:] = a[:-1]
    elif k == 1:
        out[:-1] = a[1:]
    return out


def run_out(x_shards, z_all, ret, s5y, att, mod_l, prm):
    C = mix_consts()
    nc = build_out()
    lat = lambda a: a[CTX:]
    ctx = lambda a: a[:CTX]
    sh = lambda a: shard_tokens(lat(a), ctx(a))
    gf = z_all[:, 1536:2048]; gb = z_all[:, 2048:2560]
    rg_s = sh(np.stack([ret[0], ret[1], gf, gb], axis=1))
    s5_s = sh(np.stack([s5y[0], s5y[1], z_all[:, 2560:3072]], axis=1))
    att_s = sh(att)
    zc = z_all[:, 4096:5632]
    bg, cg, hh = zc[:, 0:512], zc[:, 512:1024], zc[:, 1024:1536]

    def sh3(a):
        parts = []
        for k in (-1, 0, 1):
            parts.append(np.concatenate([_shift(ctx(a), k), _shift(lat(a), k)], axis=0) if k else a)
        return parts
    cs_, hs_ = sh3(cg), sh3(hh)
    cv_s = sh(np.stack([bg, cs_[0], hs_[0], cs_[1], hs_[1], cs_[2], hs_[2]], axis=1))
    rep = lambda v: np.broadcast_to(v, (128,) + v.shape)
    vecs = np.ascontiguousarray(np.stack([rep(prm["s5_d"]), rep(prm["s5_b_glu"]), rep(prm["conv_w"][0]), rep(prm["conv_w"][1]), rep(prm["conv_w"][2])], axis=1))
    g1 = np.ascontiguousarray(np.stack([rep(mod_l[0, 2*D:3*D]), rep(mod_l[1, 2*D:3*D])]))
    modc = np.ascontiguousarray(np.stack([cols128(mod_l[0, 3*D:4*D]), cols128(mod_l[0, 4*D:5*D]), cols128(mod_l[1, 3*D:4*D]), cols128(mod_l[1, 4*D:5*D])], axis=-1))
    b_r = np.ascontiguousarray(rep(prm["b_router"]))
    in_maps = []
    for c in range(NCORE):
        in_maps.append({"ident": C["ident"], "x": x_shards[c], "rg": rg_s[c], "s5": s5_s[c], "att": att_s[c], "cv": cv_s[c],
                        "vecs": vecs, "w_glu": prm["s5_w_glu"], "w_out": prm["w_out"], "g1": g1, "modc": modc,
                        "w_r": np.ascontiguousarray(prm["w_router"].reshape(16, 128, 32).transpose(1, 0, 2)), "b_r": b_r})
    res = _run(nc, in_maps)
    return [r["xmid"] for r in res], [r["fT"] for r in res], [r["gates"] for r in res]


E_PC = 4
NCORE_E = 32 // E_PC
DE = 1024
E_TILES = [(i * 512, 512) for i in range(16)] + [(8192, 256)]


def build_moe():
    nc = bass.Bass("TRN2", target_bir_lowering=False)
    di = lambda name, shape, dt=F32: nc.dram_tensor(name, list(shape), dt, kind="ExternalInput").ap()
    fT = di("fT", [D, T_ALL], BF16)
    gb = di("gb", [E_PC, 128, T_ALL])
    wg = di("wg", [E_PC, D, DE]); wu = di("wu", [E_PC, D, DE]); wd = di("wd", [E_PC, DE, D])
    bgu = di("bgu", [128, E_PC, 16]); bd = di("bd", [128, E_PC, 16])
    yT = nc.dram_tensor("yT", [D, T_ALL], F32, kind="ExternalOutput").ap()
    P = Prog(nc)
    NF = 8
    psf = [P.ps(f"psf{i}", [128, 512], F32) for i in range(NF)]; b_psf = [P.buf() for _ in range(NF)]
    cnt = {"f": 0}

    def nf():
        i = cnt["f"] % NF; cnt["f"] += 1
        return psf[i], b_psf[i]

    bgut = P.sb("bgut", [128, E_PC, 16], F32); b_bgu = P.buf()
    bdt = P.sb("bdt", [128, E_PC, 16], F32); b_bd = P.buf()
    P.dma("sp", "bgu", lambda e: e.dma_start(out=bgut[:], in_=bgu), writes=[b_bgu])
    P.dma("sp", "bd", lambda e: e.dma_start(out=bdt[:], in_=bd), writes=[b_bd])
    wgb = P.sb("wgb", [128, 16, DE], BF16); wub = P.sb("wub", [128, 16, DE], BF16); wdb = P.sb("wdb", [128, 8, D], BF16)
    b_wgb = [P.buf() for _ in range(2)]; b_wub = [P.buf() for _ in range(2)]; b_wdb = [P.buf() for _ in range(4)]
    wst = [P.sb(f"wst{i}", [128, 4, 512], F32) for i in range(3)]; b_wst = [P.buf() for _ in range(3)]
    ft = [P.sb(f"ft{i}", [128, 16, 512], BF16) for i in range(2)]; b_ft = [P.buf() for _ in range(2)]
    gtile = [P.sb(f"gtile{i}", [128, 512], F32) for i in range(2)]; b_gtile = [P.buf() for _ in range(2)]
    actT = P.sb("actT", [128, 8, 512], BF16); b_actT = P.buf()
    tg = [P.sb(f"tg{i}", [128, 512], F32) for i in range(2)]; b_tg = [P.buf() for _ in range(2)]
    tsg = [P.sb(f"tsg{i}", [128, 512], F32) for i in range(2)]; b_tsg = [P.buf() for _ in range(2)]
    tu = [P.sb(f"tu{i}", [128, 512], F32) for i in range(2)]; b_tu = [P.buf() for _ in range(2)]
    NYP = 8
    yp = [P.sb(f"yp{i}", [128, 512], F32) for i in range(NYP)]; b_yp = [P.buf() for _ in range(NYP)]
    yo = [P.sb(f"yo{i}", [128, 512], F32) for i in range(4)]; b_yo = [P.buf() for _ in range(4)]
    b_dram = [[P.buf() for _ in E_TILES] for _ in range(16)]
    fT_v = fT.rearrange("(k p) t -> p k t", p=128)
    yT_v = yT.rearrange("(m p) t -> p m t", p=128)
    wsi = 0; fi = 0; oi = 0; mi = 0
    ne = globals().get("MOE_NE", E_PC)
    def load_tile(i):
        ex_, tix_ = divmod(i, len(E_TILES))
        c0_, w_ = E_TILES[tix_]
        fs_ = i % 2
        for kq in range(4):
            P.dma("sp", f"ft{fs_}_{ex_ % 2}", lambda e, fs_=fs_, c0_=c0_, w_=w_, kq=kq: e.dma_start(out=ft[fs_][:, kq*4:(kq+1)*4, 0:w_], in_=fT_v[:, kq*4:(kq+1)*4, c0_:c0_+w_]), writes=[b_ft[fs_]])
        P.dma("sp", f"gtile{fs_}_{ex_ % 2}", lambda e, fs_=fs_, c0_=c0_, w_=w_, ex_=ex_: e.dma_start(out=gtile[fs_][:, 0:w_], in_=gb[ex_, :, c0_:c0_+w_]), writes=[b_gtile[fs_]])

    n_tiles_total = ne * len(E_TILES)
    load_tile(0)
    for ex in range(ne):
        for (src, dst, bdst_l, nk, cbk) in ((wg, wgb, b_wgb, 16, 0), (wu, wub, b_wub, 16, 0), (wg, wgb, b_wgb, 16, 1), (wu, wub, b_wub, 16, 1),
                                            (wd, wdb, b_wdb, 8, 0), (wd, wdb, b_wdb, 8, 1), (wd, wdb, b_wdb, 8, 2), (wd, wdb, b_wdb, 8, 3)):
            sv = src[ex].rearrange("(k p) n -> p k n", p=128)
            bdst = bdst_l[cbk]
            for kq in range(nk // 4):
                if True:
                    ws_ = wsi % 3; wsi += 1
                    P.dma("sp", f"wst{ws_}_{ex % 2}", lambda e, ws_=ws_, sv=sv, kq=kq, cbk=cbk: e.dma_start(out=wst[ws_][:], in_=sv[:, kq*4:(kq+1)*4, cbk*512:(cbk+1)*512]), writes=[b_wst[ws_]])
                    eng = "pool" if wsi % 2 else "act"
                    if eng == "pool":
                        P.op("pool", lambda e, ws_=ws_, dst=dst, kq=kq, cbk=cbk: e.tensor_copy(out=dst[:, kq*4:(kq+1)*4, cbk*512:(cbk+1)*512], in_=wst[ws_][:]), reads=[b_wst[ws_]], writes=[bdst])
                    else:
                        P.op("act", lambda e, ws_=ws_, dst=dst, kq=kq, cbk=cbk: e.copy(out=dst[:, kq*4:(kq+1)*4, cbk*512:(cbk+1)*512], in_=wst[ws_][:]), reads=[b_wst[ws_]], writes=[bdst])
        for tix, (c0, w) in enumerate(E_TILES):
            fs = fi % 2; fi += 1
            if fi < n_tiles_total:
                load_tile(fi)
            for m in range(8):
                psg, bpsg = nf(); psu, bpsu = nf()
                for k in range(16):
                    P.op("pe", lambda e, psg=psg, k=k, m=m, fs=fs, w=w: e.matmul(psg[:, 0:w], wgb[:, k, m*128:(m+1)*128], ft[fs][:, k, 0:w], start=(k == 0), stop=(k == 15)), reads=[b_wgb[m // 4], b_ft[fs]], writes=[bpsg])
                for k in range(16):
                    P.op("pe", lambda e, psu=psu, k=k, m=m, fs=fs, w=w: e.matmul(psu[:, 0:w], wub[:, k, m*128:(m+1)*128], ft[fs][:, k, 0:w], start=(k == 0), stop=(k == 15)), reads=[b_wub[m // 4], b_ft[fs]], writes=[bpsu])
                s = mi % 2; mi += 1
                P.op("dve", lambda e, psg=psg, s=s, m=m, w=w, ex=ex: e.tensor_scalar(out=tg[s][:, 0:w], in0=psg[:, 0:w], scalar1=bgut[:, ex, m:m+1], scalar2=7.0, op0=ALU.add, op1=ALU.min), reads=[bpsg, b_bgu], writes=[b_tg[s]])
                P.op("act", lambda e, s=s, w=w: e.activation(out=tsg[s][:, 0:w], in_=tg[s][:, 0:w], func=AF.Sigmoid, scale=1.702), reads=[b_tg[s]], writes=[b_tsg[s]])
                P.op("dve", lambda e, psu=psu, s=s, m=m, w=w, ex=ex: e.tensor_scalar(out=tu[s][:, 0:w], in0=psu[:, 0:w], scalar1=bgut[:, ex, 8+m:9+m], scalar2=7.0, op0=ALU.add, op1=ALU.min), reads=[bpsu, b_bgu], writes=[b_tu[s]])
                P.op("dve", lambda e, s=s, w=w: e.tensor_scalar(out=tu[s][:, 0:w], in0=tu[s][:, 0:w], scalar1=-7.0, scalar2=1.0, op0=ALU.max, op1=ALU.add), reads=[b_tu[s]], writes=[b_tu[s]])
                P.op("pool", lambda e, s=s, w=w: e.tensor_tensor(out=tg[s][:, 0:w], in0=tg[s][:, 0:w], in1=tsg[s][:, 0:w], op=ALU.mult), reads=[b_tg[s], b_tsg[s]], writes=[b_tg[s]])
                P.op("dve", lambda e, s=s, m=m, w=w: e.tensor_tensor(out=actT[:, m, 0:w], in0=tg[s][:, 0:w], in1=tu[s][:, 0:w], op=ALU.mult), reads=[b_tg[s], b_tu[s]], writes=[b_actT])
            def issue_yp(m2_, tix=tix, c0=c0, w=w, ex=ex):
                ys_ = m2_ % NYP
                P.dma("sp", f"yp{ys_}_{ex % 2}", lambda e, ys_=ys_, m2_=m2_, c0=c0, w=w: e.dma_start(out=yp[ys_][:, 0:w], in_=yT_v[:, m2_, c0:c0+w]), reads=[b_dram[m2_][tix]], writes=[b_yp[ys_]])

            if ex > 0:
                for m2 in range(NYP):
                    issue_yp(m2)
            for m2 in range(16):
                psy, bpsy = nf()
                for k in range(8):
                    P.op("pe", lambda e, psy=psy, k=k, m2=m2, w=w: e.matmul(psy[:, 0:w], wdb[:, k, m2*128:(m2+1)*128], actT[:, k, 0:w], start=(k == 0), stop=(k == 7)), reads=[b_wdb[m2 // 4], b_actT], writes=[bpsy])
                os_ = oi % 4; oi += 1
                ys_ = m2 % NYP
                bdr = b_dram[m2][tix]
                P.op("dve", lambda e, psy=psy, os_=os_, m2=m2, w=w, fs=fs, ex=ex: e.scalar_tensor_tensor(out=yo[os_][:, 0:w], in0=psy[:, 0:w], scalar=bdt[:, ex, m2:m2+1], in1=gtile[fs][:, 0:w], op0=ALU.add, op1=ALU.mult),
                     reads=[bpsy, b_bd, b_gtile[fs]], writes=[b_yo[os_]])
                if ex > 0:
                    P.op("dve", lambda e, os_=os_, ys_=ys_, w=w: e.tensor_tensor(out=yo[os_][:, 0:w], in0=yo[os_][:, 0:w], in1=yp[ys_][:, 0:w], op=ALU.add), reads=[b_yo[os_], b_yp[ys_]], writes=[b_yo[os_]])
                P.dma("sp", f"yo{os_}_{ex % 2}", lambda e, os_=os_, m2=m2, c0=c0, w=w: e.dma_start(out=yT_v[:, m2, c0:c0+w], in_=yo[os_][:, 0:w]), reads=[b_yo[os_]], writes=[bdr])
                if ex > 0 and m2 + NYP < 16:
                    issue_yp(m2 + NYP)
    P.emit()
    return nc


def run_moe(fT_shards, gate_shards, prm):
    nc = build_moe()
    fT = np.ascontiguousarray(np.concatenate(fT_shards, axis=1))
    gates = np.concatenate(gate_shards, axis=0)
    in_maps = []
    for c in range(NCORE_E):
        es = slice(E_PC * c, E_PC * (c + 1))
        wgu = prm["w_gate_up"][es]
        bgu = prm["b_gate_up"][es]
        bg = bgu[:, 0::2].reshape(E_PC, 8, 128); bu = bgu[:, 1::2].reshape(E_PC, 8, 128)
        bgu_l = np.ascontiguousarray(np.concatenate([bg, bu], axis=1).transpose(2, 0, 1))
        bd_l = np.ascontiguousarray(prm["b_down"][es].reshape(E_PC, 16, 128).transpose(2, 0, 1))
        gbc = np.ascontiguousarray(np.broadcast_to(gates[:, es].T[:, None, :], (E_PC, 128, T_ALL)))
        in_maps.append({"fT": fT, "gb": gbc, "wg": np.ascontiguousarray(wgu[:, :, 0::2]), "wu": np.ascontiguousarray(wgu[:, :, 1::2]),
                        "wd": prm["w_down"][es], "bgu": bgu_l, "bd": bd_l})
    res = _run(nc, in_maps)
    parts = []
    for cp in range(NCORE):
        parts.append(np.ascontiguousarray(np.stack([res[c]["yT"][:, cp*TOK_PC:(cp+1)*TOK_PC].T for c in range(NCORE_E)], axis=0)))
    return parts


_LAYER_KEYS = ["w_in", "w_out", "s5_a_re", "s5_a_im", "s5_log_step", "s5_b_re", "s5_b_im", "s5_c_re", "s5_c_im",
               "s5_d", "s5_w_glu", "s5_b_glu", "q_norm_w", "k_norm_w", "conv_w", "w_router", "b_router",
               "w_gate_up", "b_gate_up", "w_down", "b_down"]


def _combine_maps(x_shards, parts, g2pair):
    rep = lambda v: np.broadcast_to(v, (128, D))
    g2 = np.ascontiguousarray(np.stack([rep(g2pair[0]), rep(g2pair[1])]))
    return g2


def run_proj2(x_shards, mod_l, w_in_l, parts=None, g2pair=None, project=True):
    combine = parts is not None
    nc = build_proj(combine, project)
    ident = np.eye(128, dtype=np.float32)
    in_maps = []
    for i in range(NCORE):
        m = {"x": x_shards[i], "ident": ident}
        if project:
            m["modc"] = np.ascontiguousarray(np.stack([cols128(mod_l[0, 0:D]), cols128(mod_l[0, D:2*D]), cols128(mod_l[1, 0:D]), cols128(mod_l[1, D:2*D])], axis=-1))
            m["w_in"] = w_in_l
        if combine:
            m["part"] = parts[i]
            m["g2"] = _combine_maps(x_shards, parts, g2pair)
        in_maps.append(m)
    res = _run(nc, in_maps)
    z = [r["z"] for r in res] if project else None
    xo = [r["xo"] for r in res] if combine else None
    return z, xo


def kernel(**inputs):
    inp = {k: np.asarray(v) for k, v in inputs.items()}
    mod = run_mod(inp["c"], inp["c_ctx"], inp["w_mod"], inp["b_mod"])
    x_shards = shard_tokens(inp["x"][0], inp["ctx"][0])
    parts = None
    g2pair = None
    for l in range(2):
        prm = {k: inp[k][l] for k in _LAYER_KEYS}
        z, xo = run_proj2(x_shards, mod[l], prm["w_in"], parts, g2pair)
        if xo is not None:
            x_shards = xo
        zl, zc = unshard_tokens(z)
        z_all = np.concatenate([zc, zl], axis=0)
        ret, s5y, att = run_mix(z_all, prm)
        xmid, fT, gates = run_out(x_shards, z_all, ret, s5y, att, mod[l], prm)
        parts = run_moe(fT, gates, prm)
        x_shards = xmid
        g2pair = (mod[l][0, 5*D:6*D], mod[l][1, 5*D:6*D])
    _, xo = run_proj2(x_shards, None, None, parts, g2pair, project=False)
    lat, _ = unshard_tokens(xo)
    return lat[None].astype(np.float32)
```
